# Optimizing a Trainium2 kernel written in Bass

```python
import jax, jax.numpy as jnp
from jax import lax
import numpy as np

D_MODEL = 1024
BATCH = 32
SEQ = 2048
DEPTH = 1

D_MIX = D_MODEL
N_SB_HEADS = 8
SB_HEAD_DIM = 64
D_SB = N_SB_HEADS * SB_HEAD_DIM
D_CONV = D_MIX - D_SB
CONV_WIDTH = 31
Q_BLOCK = 128
D_IN_PROJ = 3 * D_SB + 2 * D_CONV

N_MEM = 256
N_XATTN_HEADS = 4
XATTN_HEAD_DIM = D_MODEL // N_XATTN_HEADS

N_GROUPS = 4
EXPERTS_PER_GROUP = 8
N_EXPERTS = N_GROUPS * EXPERTS_PER_GROUP
TOP_K_IN_GROUP = 2
D_EXPERT = 512
MOE_CHUNK = 256

EPS = 1e-6

kernel_name = "hymba_sb_conformer_hmoe_layer"


def rmsnorm(x, g):
    xf = x.astype(jnp.float32)
    y = xf * lax.rsqrt(jnp.mean(xf * xf, axis=-1, keepdims=True) + EPS)
    return (y * g.astype(jnp.float32)).astype(x.dtype)


def stick_breaking_attention(q, k, v):
    S = q.shape[2]
    scale = SB_HEAD_DIM ** -0.5
    outs = []
    for t0 in range(0, S, Q_BLOCK):
        L = t0 + Q_BLOCK
        z = jnp.einsum('bhqd,bhkd->bhqk', q[:, :, t0:L], k[:, :, :L]).astype(jnp.float32) * scale
        q_pos = t0 + jnp.arange(Q_BLOCK)
        k_pos = jnp.arange(L)
        strict = k_pos[None, :] < q_pos[:, None]
        log_keep = jnp.where(strict, jax.nn.log_sigmoid(-z), 0.0)
        later = lax.cumsum(log_keep, axis=3, reverse=True) - log_keep
        a = jnp.where(strict, jnp.exp(jax.nn.log_sigmoid(z) + later), 0.0)
        outs.append(jnp.einsum('bhqk,bhkd->bhqd', a.astype(v.dtype), v[:, :, :L]))
    return jnp.concatenate(outs, axis=2)


def conformer_conv(u, conv_w, conv_b, ln_g, ln_b):
    a, gate = jnp.split(u, 2, axis=-1)
    g = a * jax.nn.sigmoid(gate)
    y = lax.conv_general_dilated(
        g, conv_w[:, None, :], window_strides=(1,), padding=((CONV_WIDTH - 1, 0),),
        dimension_numbers=('NWC', 'WIO', 'NWC'), feature_group_count=D_CONV) + conv_b
    yf = y.astype(jnp.float32)
    mu = jnp.mean(yf, axis=-1, keepdims=True)
    var = jnp.mean(jnp.square(yf - mu), axis=-1, keepdims=True)
    yn = (yf - mu) * lax.rsqrt(var + EPS) * ln_g.astype(jnp.float32) + ln_b.astype(jnp.float32)
    return jax.nn.silu(yn).astype(u.dtype)


def memory_cross_attention(h, mem_n, w_q, w_kv, w_o):
    B, S, _ = h.shape
    M = mem_n.shape[1]
    q = (h @ w_q).reshape(B, S, N_XATTN_HEADS, XATTN_HEAD_DIM)
    kv = (mem_n @ w_kv).reshape(B, M, 2, N_XATTN_HEADS, XATTN_HEAD_DIM)
    k, v = kv[:, :, 0], kv[:, :, 1]
    s = jnp.einsum('bqhd,bkhd->bhqk', q, k).astype(jnp.float32) * (XATTN_HEAD_DIM ** -0.5)
    p = jax.nn.softmax(s, axis=-1).astype(v.dtype)
    o = jnp.einsum('bhqk,bkhd->bqhd', p, v).reshape(B, S, D_MODEL)
    return o @ w_o


def hierarchical_moe(h, w_group, b_group, w_er, b_er, w_gate, w_up, w_down):
    B, S, D = h.shape
    T = B * S
    hf = h.reshape(T, D)
    hr = hf.astype(jnp.float32)
    group_logits = hr @ w_group.astype(jnp.float32) + b_group.astype(jnp.float32)
    group_probs = jax.nn.softmax(group_logits, axis=-1)
    g_idx = jnp.argmax(group_logits, axis=-1).astype(jnp.int32)
    g_w = jnp.take_along_axis(group_probs, g_idx[:, None], axis=1)[:, 0]
    exp_logits = jnp.einsum('td,dge->tge', hr, w_er.astype(jnp.float32)) + b_er.astype(jnp.float32)
    in_group = jnp.take_along_axis(exp_logits, g_idx[:, None, None], axis=1)[:, 0]
    top_v, top_i = lax.top_k(in_group, TOP_K_IN_GROUP)
    weights = g_w[:, None] * jax.nn.softmax(top_v, axis=-1)
    expert_ids = g_idx[:, None] * EXPERTS_PER_GROUP + top_i.astype(jnp.int32)
    N = T * TOP_K_IN_GROUP
    e_flat = expert_ids.reshape(N)
    tok_flat = jnp.arange(N, dtype=jnp.int32) // TOP_K_IN_GROUP
    w_flat = weights.reshape(N)
    order = jnp.argsort(e_flat)
    e_sorted = e_flat[order]
    tok_sorted = tok_flat[order]
    w_sorted = w_flat[order]
    counts = jnp.zeros((N_EXPERTS,), jnp.int32).at[e_flat].add(1)
    padded = (counts + MOE_CHUNK - 1) // MOE_CHUNK * MOE_CHUNK
    start = jnp.cumsum(counts) - counts
    pend = jnp.cumsum(padded)
    pstart = pend - padded
    dest = pstart[e_sorted] + (jnp.arange(N, dtype=jnp.int32) - start[e_sorted])
    n_chunks = (N + MOE_CHUNK - 1) // MOE_CHUNK + N_EXPERTS
    P = n_chunks * MOE_CHUNK
    row_tok = jnp.full((P,), T, jnp.int32).at[dest].set(tok_sorted)
    h_pad = jnp.concatenate([hf, jnp.zeros((1, D), hf.dtype)], axis=0)
    xd = h_pad[row_tok].reshape(n_chunks, MOE_CHUNK, D)
    chunk_e = jnp.minimum(
        jnp.searchsorted(pend, jnp.arange(n_chunks, dtype=jnp.int32) * MOE_CHUNK, side='right'),
        N_EXPERTS - 1).astype(jnp.int32)

    def expert_block(args):
        xc, e = args
        return (jax.nn.silu(xc @ w_gate[e]) * (xc @ w_up[e])) @ w_down[e]

    yd = lax.map(expert_block, (xd, chunk_e)).reshape(P, D)
    y = jnp.zeros((T, D), jnp.float32).at[tok_sorted].add(
        w_sorted[:, None] * yd[dest].astype(jnp.float32))
    return y.astype(h.dtype).reshape(B, S, D)


def setup_inputs(seed: int = 0) -> dict:
    key = jax.random.key(seed)
    ks = jax.random.split(key, 24)
    f32 = jnp.float32
    nrm = lambda k, shape, scale: jax.random.normal(k, shape, f32) * scale
    gain = lambda k, shape: 1.0 + 0.02 * jax.random.normal(k, shape, f32)
    return {
        "x": jax.random.normal(ks[0], (BATCH, SEQ, D_MODEL), f32),
        "mem": jax.random.normal(ks[1], (BATCH, N_MEM, D_MODEL), f32),
        "ln_mix_g": gain(ks[2], (DEPTH, D_MODEL)),
        "w_in": nrm(ks[3], (DEPTH, D_MODEL, D_IN_PROJ), D_MODEL ** -0.5),
        "sb_out_g": gain(ks[4], (DEPTH, D_SB)),
        "conv_w": nrm(ks[5], (DEPTH, CONV_WIDTH, D_CONV), CONV_WIDTH ** -0.5),
        "conv_b": nrm(ks[6], (DEPTH, D_CONV), 0.02),
        "conv_ln_g": gain(ks[7], (DEPTH, D_CONV)),
        "conv_ln_b": nrm(ks[8], (DEPTH, D_CONV), 0.02),
        "w_out": nrm(ks[9], (DEPTH, D_MIX, D_MODEL), D_MIX ** -0.5),
        "ln_mem_x_g": gain(ks[10], (DEPTH, D_MODEL)),
        "ln_mem_g": gain(ks[11], (DEPTH, D_MODEL)),
        "w_xq": nrm(ks[12], (DEPTH, D_MODEL, D_MODEL), D_MODEL ** -0.5),
        "w_xkv": nrm(ks[13], (DEPTH, D_MODEL, 2 * D_MODEL), D_MODEL ** -0.5),
        "w_xo": nrm(ks[14], (DEPTH, D_MODEL, D_MODEL), D_MODEL ** -0.5),
        "ln_ffn_g": gain(ks[15], (DEPTH, D_MODEL)),
        "w_group": nrm(ks[16], (DEPTH, D_MODEL, N_GROUPS), D_MODEL ** -0.5),
        "b_group": nrm(ks[17], (DEPTH, N_GROUPS), 0.01),
        "w_er": nrm(ks[18], (DEPTH, D_MODEL, N_GROUPS, EXPERTS_PER_GROUP), D_MODEL ** -0.5),
        "b_er": nrm(ks[19], (DEPTH, N_GROUPS, EXPERTS_PER_GROUP), 0.01),
        "w_gate": nrm(ks[20], (DEPTH, N_EXPERTS, D_MODEL, D_EXPERT), D_MODEL ** -0.5),
        "w_up": nrm(ks[21], (DEPTH, N_EXPERTS, D_MODEL, D_EXPERT), D_MODEL ** -0.5),
        "w_down": nrm(ks[22], (DEPTH, N_EXPERTS, D_EXPERT, D_MODEL), D_EXPERT ** -0.5),
        "ln_final_g": gain(ks[23], (D_MODEL,)),
    }


def reference(x, mem, ln_mix_g, w_in, sb_out_g, conv_w, conv_b, conv_ln_g, conv_ln_b, w_out,
              ln_mem_x_g, ln_mem_g, w_xq, w_xkv, w_xo, ln_ffn_g, w_group, b_group, w_er, b_er,
              w_gate, w_up, w_down, ln_final_g):
    B, S, _ = x.shape
    for l in range(DEPTH):
        h = rmsnorm(x, ln_mix_g[l])
        proj = h @ w_in[l]
        qkv = proj[..., :3 * D_SB].reshape(B, S, 3, N_SB_HEADS, SB_HEAD_DIM).transpose(2, 0, 3, 1, 4)
        sb = stick_breaking_attention(qkv[0], qkv[1], qkv[2])
        sb = rmsnorm(sb.transpose(0, 2, 1, 3).reshape(B, S, D_SB), sb_out_g[l])
        cv = conformer_conv(proj[..., 3 * D_SB:], conv_w[l], conv_b[l], conv_ln_g[l], conv_ln_b[l])
        x = x + jnp.concatenate([sb, cv], axis=-1) @ w_out[l]
        x = x + memory_cross_attention(rmsnorm(x, ln_mem_x_g[l]), rmsnorm(mem, ln_mem_g[l]),
                                       w_xq[l], w_xkv[l], w_xo[l])
        x = x + hierarchical_moe(rmsnorm(x, ln_ffn_g[l]), w_group[l], b_group[l], w_er[l], b_er[l],
                                 w_gate[l], w_up[l], w_down[l])
    return rmsnorm(x, ln_final_g)
```

```python
import numpy as np
import concourse.bass as bass
import concourse.mybir as mybir
from concourse.bass_utils import run_bass_kernel_spmd
from contextlib import ExitStack

F32 = mybir.dt.float32
BF16 = mybir.dt.bfloat16
I32 = mybir.dt.int32
AF = mybir.ActivationFunctionType
ALU = mybir.AluOpType
AX = mybir.AxisListType

ENG = ['pe', 'act', 'dve', 'pool', 'sp']
SAME_ENGINE_SYNC = True

N_CORES = 8
NSEQ = 4
SEQ = 2048
DM = 1024
NT = SEQ // 128
NMEM = 256
NEXP = 32
CAP = 768
NROWS = NEXP * CAP
EPS = 1e-6


def I(name, *args, **kw):
    return (name, args, kw)


class Sched:
    def __init__(self, nc, es):
        self.nc = nc
        self.es = es
        self.ins = {e: [] for e in ENG}
        self.cnt = {e: 0 for e in ENG}
        self.sem = {e: es.enter_context(nc.semaphore('s_' + e)) for e in ENG}
        self.dsem = {}
        self.lastw = {}
        self.readers = {}
        self.waited = {e: {} for e in ENG}

    def _semof(self, key):
        return self.sem[key[1]] if key[0] == 'e' else self.dsem[key[1]][0]

    def op(self, eng, fn, r=(), w=(), dma=None, extra=()):
        deps = {}

        def need(ev):
            if ev is None:
                return
            key, val, src, is_dma = ev
            if (not is_dma) and src == eng and (eng == 'pe' or not SAME_ENGINE_SYNC):
                return
            if deps.get(key, 0) < val:
                deps[key] = val

        for k in r:
            need(self.lastw.get(k))
        for k in w:
            need(self.lastw.get(k))
            for ev in self.readers.get(k, {}).values():
                need(ev)
        for ev in extra:
            need(ev)
        waits = []
        wd = self.waited[eng]
        for key, val in deps.items():
            if wd.get(key, 0) >= val:
                continue
            wd[key] = val
            waits.append((key, val))
        if dma is not None:
            if dma not in self.dsem:
                self.dsem[dma] = [self.es.enter_context(self.nc.semaphore('d_' + dma)), 0]
            self.dsem[dma][1] += 16
            ev = (('d', dma), self.dsem[dma][1], eng, True)
        else:
            self.cnt[eng] += 1
            ev = (('e', eng), self.cnt[eng], eng, False)
        self.ins[eng].append((waits, fn, ev))
        for k in r:
            d = self.readers.setdefault(k, {})
            old = d.get(ev[0])
            if old is None or old[1] < ev[1]:
                d[ev[0]] = ev
        for k in w:
            self.lastw[k] = ev
            self.readers[k] = {}
        return ev

    def barrier(self):
        evs = []
        for e in ENG:
            if self.cnt[e] > 0:
                evs.append((('e', e), self.cnt[e], e, False))
        for slot, (s, c) in self.dsem.items():
            if c > 0:
                evs.append((('d', slot), c, 'sp', True))
        for e in ENG:
            self.op(e, I('nop'), extra=[ev for ev in evs if not (ev[2] == e and not ev[3])])
        self.lastw = {}
        self.readers = {}

    def emit(self):
        nc = self.nc
        with nc.Block() as block:
            def body(name):
                def f(e):
                    bc_reg = None
                    if name == 'pool':
                        bc_reg = e.alloc_register()
                        e.reg_mov(bc_reg, NROWS - 1)
                    for waits, fn, ev in self.ins[name]:
                        for key, val in waits:
                            e.wait_ge(self._semof(key), val)
                        try:
                            kw = fn[2]
                            if kw.get('bounds_check', None) == 'REG':
                                kw = dict(kw, bounds_check=bc_reg)
                            ins = getattr(e, fn[0])(*fn[1], **kw)
                        except Exception:
                            print("EMIT FAIL", name, fn[0], fn[1], fn[2])
                            raise
                        key, val, _, is_dma = ev
                        ins.then_inc(self._semof(key), 16 if is_dma else 1)
                return f
            block.tensor(body('pe'))
            block.scalar(body('act'))
            block.vector(body('dve'))
            block.gpsimd(body('pool'))
            block.sync(body('sp'))


def build(nseq=NSEQ, stage=99):
    nc = bass.Bass('TRN2', target_bir_lowering=False)
    ntok = nseq * SEQ

    def din(name, shape, dt=F32):
        return nc.dram_tensor(name, list(shape), dt, kind="ExternalInput").ap()

    x_d = din("x", [nseq, SEQ, DM])
    mem_d = din("mem", [nseq, NMEM, DM])
    ln_mix_g = din("ln_mix_g", [DM])
    w_in_d = din("w_in", [DM, 2560])
    sb_out_g = din("sb_out_g", [512])
    conv_w_d = din("conv_w", [31, 512])
    conv_b_d = din("conv_b", [512])
    conv_ln_g = din("conv_ln_g", [512])
    conv_ln_b = din("conv_ln_b", [512])
    w_out_d = din("w_out", [DM, DM])
    ln_mem_x_g = din("ln_mem_x_g", [DM])
    ln_mem_g = din("ln_mem_g", [DM])
    w_xq_d = din("w_xq", [DM, DM])
    w_xkv_d = din("w_xkv", [DM, 2 * DM])
    w_xo_d = din("w_xo", [DM, DM])
    ln_ffn_g = din("ln_ffn_g", [DM])
    w_rt_d = din("w_rt", [DM, 36])
    b_rt_d = din("b_rt", [36])
    w_gate_d = din("w_gate", [NEXP, DM, 512])
    w_up_d = din("w_up", [NEXP, DM, 512])
    w_down_d = din("w_down", [NEXP, 512, DM])
    ln_final_g = din("ln_final_g", [DM])
    out_d = nc.dram_tensor("out", [ntok, DM], F32, kind="ExternalOutput").ap()
    if stage == 1:
        dbg_mix = nc.dram_tensor("dbg_mix", [128, 8, SEQ], BF16, kind="ExternalOutput").ap()
        dbg_qk = nc.dram_tensor("dbg_qk", [128, 8, SEQ], BF16, kind="ExternalOutput").ap()
        dbg_v = nc.dram_tensor("dbg_v", [128, NT, 512], BF16, kind="ExternalOutput").ap()
        dbg_rsb = nc.dram_tensor("dbg_rsb", [128, 3, NT], F32, kind="ExternalOutput").ap()
    xd_d = nc.dram_tensor("xd_scr", [NROWS, DM], BF16).ap()
    yd_d = nc.dram_tensor("yd_scr", [NROWS, DM], F32).ap()
    xres_d = nc.dram_tensor("xres_scr", [ntok, DM], F32).ap()

    with ExitStack() as es:
        S = Sched(nc, es)
        op = S.op

        uid = [0]

        def T(scope, name, shape, dt):
            uid[0] += 1
            return scope.enter_context(nc.sbuf_tensor(f"{name}_u{uid[0]}", shape, dt))

        psA = es.enter_context(nc.psum_tensor("psA", [128, 2, 1024], BF16))
        psB = es.enter_context(nc.psum_tensor("psB", [128, 6, 512], F32))

        identf = T(es, "identf", [128, 128], F32)
        ident = T(es, "ident", [128, 128], BF16)
        negtri = T(es, "negtri", [128, 128], F32)
        negones = T(es, "negones", [128, 128], F32)
        meanmat = T(es, "meanmat", [128, 128], F32)
        ones2 = T(es, "ones2", [128, 2], F32)
        onesb = T(es, "onesb", [128, 128], BF16)
        Lmat = T(es, "Lmat", [128, 128], BF16)
        maskf = T(es, "maskf", [128, 4, 512], BF16)
        ecap = T(es, "ecap", [128, 32], F32)
        ecap_i = T(es, "ecap_i", [128, 32], I32)
        gcols = T(es, "gcols", [128, 3, 8], F32)
        sbg = T(es, "sbg", [128, 4], F32)
        cvb = T(es, "cvb", [128, 4], F32)
        cvg = T(es, "cvg", [128, 4], F32)
        cvbb = T(es, "cvbb", [128, 4], F32)
        cwT = T(es, "cwT", [128, 4, 31], F32)
        hT = T(es, "hT", [128, 8, SEQ], BF16)
        wr = [T(es, f"wr{i}", [128, 8, 512], BF16) for i in range(2)]
        stat = T(es, "stat", [128, 3, NT], F32)
        rsb = T(es, "rsb", [128, 3, NT], F32)
        slots = T(es, "slots", [128, nseq * NT, 2], I32)
        wts = T(es, "wts", [128, nseq * NT, 2], F32)
        base = T(es, "base", [128, 32], F32)
        wrt = T(es, "wrt", [128, 8, 36], F32)
        brt = T(es, "brt", [128, 36], F32)
        junk = T(es, "junk", [128, 1024], BF16)

        def HK(c, n):
            return f"H{c}_{n}"

        def small_col(dst_ap, src_ap, k, key):
            op('sp', I('dma_start', out=dst_ap, in_=src_ap.rearrange("(k p) -> p k", p=128),
                                           allow_slow_non_contiguous=True), w=[key], dma='c_' + key)

        small_col(gcols[:, 0, :], ln_mix_g, 8, 'gc0')
        small_col(gcols[:, 1, :], ln_mem_x_g, 8, 'gc1')
        small_col(gcols[:, 2, :], ln_mem_g, 8, 'gc2')
        small_col(sbg[:], sb_out_g, 4, 'sbg')
        small_col(cvb[:], conv_b_d, 4, 'cvb')
        small_col(cvg[:], conv_ln_g, 4, 'cvg')
        small_col(cvbb[:], conv_ln_b, 4, 'cvbb')
        op('sp', I('dma_start', out=wrt[:], in_=w_rt_d.rearrange("(k p) n -> p k n", p=128)),
           w=['wrt'], dma='c_wrt')
        op('sp', I('dma_start', out=brt[:], in_=b_rt_d.partition_broadcast(128)), w=['brt'], dma='c_brt')

        op('pool', I('memset', identf[:], 0.0), w=['identf'])
        op('pool', I('affine_select', out=identf[:], in_=identf[:], pattern=[[-1, 128]],
                                             compare_op=ALU.not_equal, fill=1.0, base=0, channel_multiplier=1),
           r=['identf'], w=['identf'])
        op('dve', I('tensor_copy', out=ident[:], in_=identf[:]), r=['identf'], w=['ident'])
        op('pool', I('memset', negtri[:], -1.0), w=['negtri'])
        op('pool', I('affine_select', out=negtri[:], in_=negtri[:], pattern=[[-1, 128]],
                                             compare_op=ALU.is_ge, fill=0.0, base=0, channel_multiplier=1),
           r=['negtri'], w=['negtri'])
        op('pool', I('memset', negones[:], -1.0), w=['negones'])
        op('pool', I('memset', meanmat[:], 1.0 / 512), w=['meanmat'])
        op('pool', I('memset', ones2[:], 1.0), w=['ones2'])
        op('pool', I('memset', onesb[:], 1.0), w=['onesb'])
        op('pool', I('memset', Lmat[:], 1.0), w=['Lmat'])
        op('pool', I('affine_select', out=Lmat[:], in_=Lmat[:], pattern=[[1, 128]],
                                             compare_op=ALU.is_gt, fill=0.0, base=0, channel_multiplier=-1),
           r=['Lmat'], w=['Lmat'])
        op('pool', I('memset', maskf[:], 1.0), w=['maskf'])
        for r_ in range(4):
            op('pool', I('affine_select', out=maskf[:, r_, :], in_=maskf[:, r_, :], pattern=[[1, 512]],
                                                        compare_op=ALU.is_gt, fill=0.0, base=-r_ * 128,
                                                        channel_multiplier=-1),
               r=['maskf'], w=['maskf'])
        op('pool', I('iota', ecap_i[:], pattern=[[CAP, 32]], base=0, channel_multiplier=0), w=['ecap_i'])
        op('dve', I('tensor_copy', out=ecap[:], in_=ecap_i[:]), r=['ecap_i'], w=['ecap'])
        op('pool', I('memset', base[:], 0.0), w=['base'])

        with ExitStack() as s0:
            cw_in = T(s0, "cw_in", [31, 512], F32)
            op('sp', I('dma_start', out=cw_in[:], in_=conv_w_d), w=['cw_in'], dma='c_cw')
            for c in range(4):
                op('pe', I('transpose', out=psB[:, 0, c * 32:c * 32 + 31], in_=cw_in[:, c * 128:(c + 1) * 128],
                                                    identity=identf[0:31, 0:31]),
                   r=['cw_in', 'identf'], w=['psB0'])
            op('act', I('activation', out=cwT[:], in_=psB[:, 0, 0:128].rearrange("p (c w) -> p c w", c=4)[:, :, 0:31],
                                             func=AF.Copy), r=['psB0'], w=['cwT'])
            S.barrier()

        wslot = [0]

        def load_w(src2d, nk=8):
            i = wslot[0]
            wslot[0] ^= 1
            op('pool', I('dma_start', out=wr[i][:, 0:nk, :], in_=src2d.rearrange("(k p) n -> p k n", p=128)),
               w=[f'wr{i}'], dma=f'wr{i}')
            return i

        evac_flip = [0]

        def evac_copy(out_ap, in_ap, r, w, scale=None, eng=None):
            if eng is None:
                eng = 'act' if (evac_flip[0] & 1) == 0 else 'dve'
                evac_flip[0] += 1
            if eng == 'act':
                if scale is None:
                    op('act', I('activation', out=out_ap, in_=in_ap, func=AF.Copy), r=r, w=w)
                else:
                    op('act', I('activation', out=out_ap, in_=in_ap, func=AF.Copy, scale=scale), r=r, w=w)
            else:
                if scale is None:
                    op('dve', I('tensor_copy', out=out_ap, in_=in_ap), r=r, w=w)
                else:
                    op('dve', I('tensor_scalar', out=out_ap, in0=in_ap, scalar1=scale, scalar2=None,
                                                        op0=ALU.mult), r=r, w=w)

        def rstd_from_ss(st, col, inv_n):
            op('act', I('activation', out=st[:, 1, col:col + 1], in_=st[:, 0, col:col + 1], func=AF.Sqrt,
                                             scale=inv_n, bias=EPS), r=['stat'], w=['stat'])
            op('dve', I('reciprocal', out=st[:, 2, col:col + 1], in_=st[:, 1, col:col + 1]),
               r=['stat'], w=['stat'])

        def norm_T(src_ap, src_key, hn_t, hn_key, gi, dst3, dst_keys, t, pa):
            op('act', I('activation', out=junk[:], in_=src_ap, func=AF.Square, accum_out=stat[:, 0, t:t + 1]),
               r=[src_key], w=['junk', 'stat'])
            rstd_from_ss(stat, t, 1.0 / DM)
            op('act', I('activation', out=hn_t[:], in_=src_ap, func=AF.Copy, scale=stat[:, 2, t:t + 1]),
               r=[src_key, 'stat'], w=[hn_key])
            for kc in range(8):
                op('pe', I('transpose', out=psA[:, pa, kc * 128:(kc + 1) * 128],
                                                      in_=hn_t[:, kc * 128:(kc + 1) * 128], identity=ident[:]),
                   r=[hn_key, 'ident'], w=[f'psA{pa}'])
            op('dve', I('tensor_tensor', out=dst3, in0=psA[:, pa, :].rearrange("p (k c) -> p k c", k=8),
                                                in1=gcols[:, gi, :, None].to_broadcast([128, 8, 128]), op=ALU.mult),
               r=[f'psA{pa}', f'gc{gi}'], w=dst_keys)

        def fm_proj(slot, ncc, rhs_fn, rhs_keys_fn, nN, N, evac_fn, banks):
            k = 0
            for cc in range(ncc):
                for n in range(nN):
                    bk = banks[k % len(banks)]
                    k += 1
                    for kc in range(8):
                        op('pe', I('matmul',
                            psB[:, bk, 0:N], wr[slot][:, kc, cc * 128:(cc + 1) * 128], rhs_fn(kc, n),
                            start=(kc == 0), stop=(kc == 7)),
                           r=[f'wr{slot}'] + rhs_keys_fn(kc, n), w=[f'psB{bk}'])
                    evac_fn(cc, n, bk)

        dbg = {}

        for b in range(nseq):
            with ExitStack() as s1:
                qk = T(s1, "qk", [128, 8, SEQ], BF16)
                v_sb = T(s1, "v_sb", [128, NT, 512], BF16)
                with ExitStack() as s1a:
                    xin = [T(s1a, f"xin{i}", [128, DM], F32) for i in range(3)]
                    hn = [T(s1a, f"hn{i}", [128, DM], BF16) for i in range(2)]
                    gT = T(s1a, "gT", [128, 4, 30 + SEQ], BF16)
                    ycv = T(s1a, "ycv", [128, 4, SEQ], F32)
                    dg = T(s1a, "dg", [128, 31, 128], BF16)
                    ysq = T(s1a, "ysq", [128, 4, 512], F32)
                    lnw = T(s1a, "lnw", [128, 4, 512], F32)

                    for t in range(NT):
                        xi = xin[t % 3]
                        op('sp', I('dma_start', out=xi[:], in_=x_d[b, t * 128:(t + 1) * 128, :]),
                           w=[f'xin{t % 3}'], dma=f'xin{t % 3}')
                        norm_T(xi[:], f'xin{t % 3}', hn[t % 2], f'hn{t % 2}', 0,
                               hT[:, :, t * 128:(t + 1) * 128], [HK(c, t // 4) for c in range(8)], t, t % 2)

                    hrhs = lambda kc, n: hT[:, kc, n * 512:(n + 1) * 512]
                    hkeys = lambda kc, n: [HK(kc, n)]
                    sl = load_w(w_in_d[:, 0:512])
                    nxt = load_w(w_in_d[:, 512:1024])
                    fm_proj(sl, 4, hrhs, hkeys, 4, 512,
                            lambda cc, n, bk: evac_copy(qk[:, cc, n * 512:(n + 1) * 512], psB[:, bk, :], [f'psB{bk}'],
                                                        [f'q{cc}_{n}'], scale=0.125), [0, 1, 2, 3])
                    sl = nxt
                    nxt = load_w(w_in_d[:, 1024:1536])
                    fm_proj(sl, 4, hrhs, hkeys, 4, 512,
                            lambda cc, n, bk: evac_copy(qk[:, 4 + cc, n * 512:(n + 1) * 512], psB[:, bk, :],
                                                        [f'psB{bk}'], [f'k{cc}_{n}']), [0, 1, 2, 3])
                    sl = nxt
                    nxt = load_w(w_in_d[:, 2048:2560])
                    for t in range(NT):
                        bk = t % 4
                        for kc in range(8):
                            op('pe', I('matmul',
                                psB[:, bk, :], hT[:, kc, t * 128:(t + 1) * 128], wr[sl][:, kc, :],
                                start=(kc == 0), stop=(kc == 7)),
                               r=[f'wr{sl}', HK(kc, t // 4)], w=[f'psB{bk}'])
                        evac_copy(v_sb[:, t, :], psB[:, bk, :], [f'psB{bk}'], [f'v{t}'])
                    op('pool', I('memset', gT[:, :, 0:30], 0.0), w=['gTpad'])
                    sl = nxt
                    nxt = load_w(w_in_d[:, 1536:2048])
                    fm_proj(sl, 4, hrhs, hkeys, 4, 512,
                            lambda cc, n, bk: op('act', I('activation',
                                out=gT[:, cc, 30 + n * 512:30 + (n + 1) * 512], in_=psB[:, bk, :], func=AF.Sigmoid),
                                r=[f'psB{bk}'], w=[f'g{cc}_{n}']), [0, 1, 2, 3])
                    sl = nxt
                    fm_proj(sl, 4, hrhs, hkeys, 4, 512,
                            lambda cc, n, bk: op('dve', I('tensor_tensor',
                                out=gT[:, cc, 30 + n * 512:30 + (n + 1) * 512], in0=psB[:, bk, :],
                                in1=gT[:, cc, 30 + n * 512:30 + (n + 1) * 512], op=ALU.mult),
                                r=[f'psB{bk}', f'g{cc}_{n}'], w=[f'g{cc}_{n}']), [0, 1, 2, 3])

                    for c in range(4):
                        for w_ in range(31):
                            op('pool', I('tensor_scalar',
                                out=dg[:, w_, :], in0=ident[:], scalar1=cwT[:, c, w_:w_ + 1], scalar2=None,
                                op0=ALU.mult), r=['ident', 'cwT'], w=['dg'])
                        for n in range(4):
                            bk = n % 4
                            gkeys = [f'g{c}_{n}'] + ([f'g{c}_{n - 1}'] if n > 0 else ['gTpad'])
                            for w_ in range(31):
                                op('pe', I('matmul',
                                    psB[:, bk, :], dg[:, w_, :], gT[:, c, n * 512 + w_:n * 512 + w_ + 512],
                                    start=(w_ == 0), stop=(w_ == 30)),
                                   r=['dg'] + gkeys, w=[f'psB{bk}'])
                            op('act', I('activation',
                                out=ycv[:, c, n * 512:(n + 1) * 512], in_=psB[:, bk, :], func=AF.Identity,
                                bias=cvb[:, c:c + 1]), r=[f'psB{bk}', 'cvb'], w=[f'y{c}_{n}'])
                    for n in range(4):
                        ns = slice(n * 512, (n + 1) * 512)
                        for c in range(4):
                            op('pe', I('matmul', psB[:, 4, :], meanmat[:], ycv[:, c, ns],
                                                                    start=(c == 0), stop=(c == 3)),
                               r=['meanmat', f'y{c}_{n}'], w=['psB4'])
                        for c in range(4):
                            op('act', I('activation', out=ysq[:, c, :], in_=ycv[:, c, ns],
                                                                         func=AF.Square),
                               r=[f'y{c}_{n}'], w=[f'ysq{c}'])
                        for c in range(4):
                            op('pe', I('matmul', psB[:, 5, :], meanmat[:], ysq[:, c, :],
                                                             start=(c == 0), stop=(c == 3)),
                               r=['meanmat', f'ysq{c}'], w=['psB5'])
                        op('act', I('activation', out=lnw[:, 0, :], in_=psB[:, 4, :], func=AF.Copy),
                           r=['psB4'], w=['lnw0'])
                        op('pool', I('tensor_tensor', out=lnw[:, 1, :], in0=lnw[:, 0, :], in1=lnw[:, 0, :],
                                                             op=ALU.mult), r=['lnw0'], w=['lnw1'])
                        op('dve', I('tensor_tensor', out=lnw[:, 1, :], in0=psB[:, 5, :], in1=lnw[:, 1, :],
                                                            op=ALU.subtract), r=['psB5', 'lnw1'], w=['lnw1'])
                        op('dve', I('tensor_scalar', out=lnw[:, 1, :], in0=lnw[:, 1, :], scalar1=0.0,
                                                            scalar2=None, op0=ALU.max), r=['lnw1'], w=['lnw1'])
                        op('act', I('activation', out=lnw[:, 1, :], in_=lnw[:, 1, :], func=AF.Sqrt, bias=EPS),
                           r=['lnw1'], w=['lnw1'])
                        op('dve', I('reciprocal', out=lnw[:, 2, :], in_=lnw[:, 1, :]), r=['lnw1'], w=['lnw2'])
                        for c in range(4):
                            op('pool', I('tensor_tensor', out=lnw[:, 3, :], in0=ycv[:, c, ns],
                                                                             in1=lnw[:, 0, :], op=ALU.subtract),
                               r=[f'y{c}_{n}', 'lnw0'], w=['lnw3'])
                            op('pool', I('tensor_tensor', out=lnw[:, 3, :], in0=lnw[:, 3, :], in1=lnw[:, 2, :],
                                                                 op=ALU.mult), r=['lnw3', 'lnw2'], w=['lnw3'])
                            op('act', I('activation',
                                out=hT[:, 4 + c, ns], in_=lnw[:, 3, :], func=AF.Silu, scale=cvg[:, c:c + 1],
                                bias=cvbb[:, c:c + 1]), r=['lnw3', 'cvg', 'cvbb'], w=[HK(4 + c, n)])
                    S.barrier()

                with ExitStack() as s1b:
                    spb = [T(s1b, f"spb{i}", [128, 512], F32) for i in range(4)]
                    ab = [T(s1b, f"ab{i}", [128, 512], BF16) for i in range(4)]
                    Rb = [T(s1b, f"Rb{i}", [128, 512], F32) for i in range(2)]
                    osq = [T(s1b, f"osq{i}", [128, 512], F32) for i in range(2)]
                    blk = 0
                    oq = 0
                    for j in range(4):
                        for qn in range(4):
                            kcs = list(range(4 * qn + 3, -1, -1))
                            qs = slice(qn * 512, (qn + 1) * 512)
                            for idx, kc in enumerate(kcs):
                                band = kc >= 4 * qn
                                r_ = kc - 4 * qn
                                for hp in range(2):
                                    h = 2 * j + hp
                                    P = slice(hp * 64, hp * 64 + 64)
                                    sb_i = hp * 2 + (blk & 1)
                                    sp_t, ab_t = spb[sb_i], ab[sb_i]
                                    spk, abk = f'spb{sb_i}', f'ab{sb_i}'
                                    zk, ek = f'psB{hp}', f'psB{2 + hp}'
                                    kk = [f'k{j}_{kc // 4}']
                                    qq = [f'q{j}_{qn}']
                                    ks = slice(kc * 128, (kc + 1) * 128)
                                    op('pe', I('matmul',
                                        psB[:, hp, :], qk[P, 4 + j, ks], qk[P, j, qs], start=True, stop=True),
                                       r=kk + qq, w=[zk])
                                    op('act', I('activation', out=sp_t[:], in_=psB[:, hp, :],
                                                                                       func=AF.Exp),
                                       r=[zk], w=[spk])
                                    op('act', I('activation', out=sp_t[:], in_=sp_t[:], func=AF.Ln,
                                                                                bias=1.0), r=[spk], w=[spk])
                                    if band:
                                        op('pool', I('tensor_tensor',
                                            out=sp_t[:], in0=sp_t[:], in1=maskf[:, r_, :], op=ALU.mult),
                                           r=[spk, 'maskf'], w=[spk])
                                    last_e = (idx == 0)
                                    op('pe', I('matmul',
                                        psB[:, 2 + hp, :], qk[P, 4 + j, ks], qk[P, j, qs], start=True, stop=False),
                                       r=kk + qq, w=[ek])
                                    op('pe', I('matmul',
                                        psB[:, 2 + hp, :], negtri[:], sp_t[:], start=False, stop=last_e),
                                       r=['negtri', spk], w=[ek])
                                    if idx > 0:
                                        op('pe', I('matmul',
                                            psB[:, 2 + hp, :], negones[:], Rb[hp][:], start=False, stop=True),
                                           r=['negones', f'Rb{hp}'], w=[ek])
                                    op('act', I('activation', out=ab_t[:], in_=psB[:, 2 + hp, :],
                                                                                       func=AF.Exp),
                                       r=[ek], w=[abk])
                                    if band:
                                        op('dve', I('tensor_tensor',
                                            out=ab_t[:], in0=ab_t[:], in1=maskf[:, r_, :], op=ALU.mult),
                                           r=[abk, 'maskf'], w=[abk])
                                    op('pe', I('matmul',
                                        psB[P, 4, :], v_sb[:, kc, h * 64:(h + 1) * 64], ab_t[:],
                                        start=(idx == 0), stop=(idx == len(kcs) - 1)),
                                       r=[f'v{kc}', abk], w=[f'psB4_{hp}'])
                                    if idx < len(kcs) - 1:
                                        if idx == 0:
                                            op('pool', I('tensor_copy', out=Rb[hp][:], in_=sp_t[:]),
                                               r=[spk], w=[f'Rb{hp}'])
                                        else:
                                            op('pool', I('tensor_tensor',
                                                out=Rb[hp][:], in0=Rb[hp][:], in1=sp_t[:], op=ALU.add),
                                               r=[spk, f'Rb{hp}'], w=[f'Rb{hp}'])
                                blk += 1
                            oq_t = osq[oq & 1]
                            oqk = f'osq{oq & 1}'
                            oq += 1
                            op('act', I('activation', out=hT[:, j, qs], in_=psB[:, 4, :], func=AF.Copy,
                                                                         scale=sbg[:, j:j + 1]),
                               r=['psB4_0', 'psB4_1', 'sbg'], w=[HK(j, qn)])
                            op('act', I('activation', out=oq_t[:], in_=psB[:, 4, :], func=AF.Square),
                               r=['psB4_0', 'psB4_1'], w=[oqk])
                            for tt in range(4):
                                col = (j * 16 + qn * 4 + tt) * 2
                                op('pe', I('matmul',
                                    psB[:, 5, col:col + 2], oq_t[:, tt * 128:(tt + 1) * 128], ones2[:],
                                    start=True, stop=True), r=[oqk, 'ones2'], w=['psB5'])
                    ssv = lambda j: psB[:, 5, j * 32:(j + 1) * 32].rearrange("p (t two) -> p t two", two=2)[:, :, 0]
                    op('act', I('activation', out=rsb[:, 0, :], in_=ssv(0), func=AF.Copy), r=['psB5'], w=['rsb'])
                    for j in range(1, 4):
                        op('dve', I('tensor_tensor', out=rsb[:, 0, :], in0=ssv(j), in1=rsb[:, 0, :],
                                                                 op=ALU.add), r=['psB5', 'rsb'], w=['rsb'])
                    op('act', I('activation', out=rsb[:, 1, :], in_=rsb[:, 0, :], func=AF.Sqrt, scale=1.0 / 512,
                                                     bias=EPS), r=['rsb'], w=['rsb'])
                    op('dve', I('reciprocal', out=rsb[:, 2, :], in_=rsb[:, 1, :]), r=['rsb'], w=['rsb'])
                    S.barrier()
                    if stage == 1 and b == 0:
                        op('sp', I('dma_start', out=dbg_mix, in_=hT[:]), dma='dbg0')
                        op('sp', I('dma_start', out=dbg_qk, in_=qk[:]), dma='dbg1')
                        op('sp', I('dma_start', out=dbg_v, in_=v_sb[:]), dma='dbg2')
                        op('sp', I('dma_start', out=dbg_rsb, in_=rsb[:]), dma='dbg3')
                        S.barrier()

            with ExitStack() as s2:
                x_sb = T(s2, "x_sb", [128, NT, DM], F32)
                for t in range(NT):
                    op('sp', I('dma_start', out=x_sb[:, t, :], in_=x_d[b, t * 128:(t + 1) * 128, :]),
                       w=[f'x{t}'], dma=f'x{t}')
                nxt = load_w(w_out_d[:, 0:512])
                for n in range(2):
                    sl = nxt
                    nxt = load_w(w_out_d[:, 512:1024]) if n == 0 else load_w(w_xkv_d[:, 0:512])
                    ns = slice(n * 512, (n + 1) * 512)
                    for t in range(NT):
                        b0, b1 = (t % 2) * 2, (t % 2) * 2 + 1
                        ts_ = slice(t * 128, (t + 1) * 128)
                        for jj in range(4):
                            op('pe', I('matmul',
                                psB[:, b0, :], hT[:, jj, ts_], wr[sl][:, jj, :], start=(jj == 0), stop=(jj == 3)),
                               r=[HK(jj, t // 4), f'wr{sl}'], w=[f'psB{b0}'])
                        for jj in range(4):
                            op('pe', I('matmul',
                                psB[:, b1, :], hT[:, 4 + jj, ts_], wr[sl][:, 4 + jj, :], start=(jj == 0), stop=(jj == 3)),
                               r=[HK(4 + jj, t // 4), f'wr{sl}'], w=[f'psB{b1}'])
                        op('dve', I('tensor_tensor',
                            out=x_sb[:, t, ns], in0=psB[:, b1, :], in1=x_sb[:, t, ns], op=ALU.add),
                           r=[f'psB{b1}', f'x{t}'], w=[f'x{t}'])
                        op('dve', I('scalar_tensor_tensor',
                            out=x_sb[:, t, ns], in0=psB[:, b0, :], scalar=rsb[:, 2, t:t + 1], in1=x_sb[:, t, ns],
                            op0=ALU.mult, op1=ALU.add), r=[f'psB{b0}', 'rsb', f'x{t}'], w=[f'x{t}'])
                if stage == 1:
                    for t in range(NT):
                        ev = op('sp', I('dma_start',
                            out=out_d[b * SEQ + t * 128:b * SEQ + (t + 1) * 128, :], in_=x_sb[:, t, :]),
                            r=[f'x{t}'], dma=f'o{t % 4}')
                    S.barrier()
                    continue

                with ExitStack() as s2f:
                    memin = [T(s2f, f"memin{i}", [128, DM], F32) for i in range(2)]
                    hn2 = [T(s2f, f"hnb{i}", [128, DM], BF16) for i in range(2)]
                    memT = T(s2f, "memT", [128, 8, NMEM], BF16)
                    kxT = T(s2f, "kxT", [128, 8, NMEM], BF16)
                    vx = T(s2f, "vx", [128, 2, DM], BF16)
                    qxT = T(s2f, "qxT", [128, 8, SEQ], BF16)
                    pf = [T(s2f, "pf0", [128, 4, NMEM], F32)] * 2
                    pn = [T(s2f, f"pn{i}", [128, 4, NMEM], BF16) for i in range(2)]
                    pT = [T(s2f, "pT0", [128, 4, 2, 512], BF16)] * 2
                    sm = T(s2f, "sm", [128, 4, 4], F32)
                    for mt in range(2):
                        op('sp', I('dma_start', out=memin[mt][:], in_=mem_d[b, mt * 128:(mt + 1) * 128, :]),
                           w=[f'memin{mt}'], dma=f'memin{mt}')
                        norm_T(memin[mt][:], f'memin{mt}', hn2[mt], f'hnb{mt}', 2,
                               memT[:, :, mt * 128:(mt + 1) * 128], ['memT'], mt, mt)
                    mrhs = lambda kc, n: memT[:, kc, :]
                    mkeys = lambda kc, n: ['memT']
                    for g in range(2):
                        sl = nxt
                        nxt = load_w(w_xkv_d[:, (g + 1) * 512:(g + 2) * 512])
                        fm_proj(sl, 4, mrhs, mkeys, 1, NMEM,
                                lambda cc, n, bk, g=g: evac_copy(kxT[:, g * 4 + cc, :], psB[:, bk, 0:NMEM], [f'psB{bk}'],
                                                                 ['kxT']), [0, 1, 2, 3])
                    for g in range(2):
                        sl = nxt
                        nxt = load_w(w_xkv_d[:, 1536:2048]) if g == 0 else load_w(w_xq_d[:, 0:512])
                        for mt in range(2):
                            bk = mt
                            for kc in range(8):
                                op('pe', I('matmul',
                                    psB[:, bk, :], memT[:, kc, mt * 128:(mt + 1) * 128], wr[sl][:, kc, :],
                                    start=(kc == 0), stop=(kc == 7)), r=['memT', f'wr{sl}'], w=[f'psB{bk}'])
                            evac_copy(vx[:, mt, g * 512:(g + 1) * 512], psB[:, bk, :], [f'psB{bk}'], ['vx'])
                    for t in range(NT):
                        norm_T(x_sb[:, t, :], f'x{t}', hn2[t % 2], f'hnb{t % 2}', 1,
                               hT[:, :, t * 128:(t + 1) * 128], [HK(c, t // 4) for c in range(8)], t, t % 2)
                    for g in range(2):
                        sl = nxt
                        nxt = load_w(w_xq_d[:, 512:1024]) if g == 0 else load_w(w_xo_d[:, 0:512])
                        fm_proj(sl, 4, hrhs, hkeys, 4, 512,
                                lambda cc, n, bk, g=g: evac_copy(qxT[:, g * 4 + cc, n * 512:(n + 1) * 512], psB[:, bk, :],
                                                                 [f'psB{bk}'], [f'qx{g * 4 + cc}_{n}'], scale=1.0 / 16),
                                [0, 1, 2, 3])
                    psS = psB[:, 0:2, :].rearrange("p a (h m) -> p (a h) m", h=2)
                    for n in range(4):
                        pT_t = pT[n % 2]
                        pTk = 'pT0'
                        for tt in range(4):
                            t = n * 4 + tt
                            ts_ = slice(t * 128, (t + 1) * 128)
                            pi = t % 2
                            for hx in range(4):
                                for dc in range(2):
                                    op('pe', I('matmul',
                                        psS[:, hx, :], qxT[:, 2 * hx + dc, ts_], kxT[:, 2 * hx + dc, :],
                                        start=(dc == 0), stop=(dc == 1)),
                                       r=[f'qx{2 * hx + dc}_{n}', 'kxT'], w=[f'psB{hx // 2}'])
                            op('dve', I('tensor_reduce', out=sm[:, 0, :], in_=psS, axis=AX.X, op=ALU.max),
                               r=['psB0', 'psB1'], w=['sm'])
                            op('dve', I('tensor_scalar', out=sm[:, 1, :], in0=sm[:, 0, :], scalar1=-1.0,
                                                                scalar2=None, op0=ALU.mult), r=['sm'], w=['sm'])
                            for hx in range(4):
                                op('act', I('activation',
                                    out=pf[pi][:, hx, :], in_=psS[:, hx, :], func=AF.Exp, bias=sm[:, 1, hx:hx + 1],
                                    accum_out=sm[:, 2, hx:hx + 1]),
                                   r=[f'psB{hx // 2}', 'sm'], w=['pf0', 'sm'])
                            op('dve', I('reciprocal', out=sm[:, 3, :], in_=sm[:, 2, :]), r=['sm'], w=['sm'])
                            for hx in range(4):
                                op('dve', I('tensor_scalar',
                                    out=pn[pi][:, hx, :], in0=pf[pi][:, hx, :], scalar1=sm[:, 3, hx:hx + 1],
                                    scalar2=None, op0=ALU.mult), r=['pf0', 'sm'], w=[f'pn{pi}'])
                            for hx in range(4):
                                for mc in range(2):
                                    op('pe', I('transpose',
                                        out=psA[:, pi, (hx * 2 + mc) * 128:(hx * 2 + mc + 1) * 128],
                                        in_=pn[pi][:, hx, mc * 128:(mc + 1) * 128], identity=ident[:]),
                                       r=[f'pn{pi}', 'ident'], w=[f'psA{pi}'])
                            op('act', I('activation',
                                out=pT_t[:, :, :, tt * 128:(tt + 1) * 128],
                                in_=psA[:, pi, :].rearrange("p (h m c) -> p h m c", h=4, m=2), func=AF.Copy),
                               r=[f'psA{pi}'], w=[pTk])
                        k = 0
                        for hx in range(4):
                            for dc in range(2):
                                bk = 2 + (k % 4)
                                k += 1
                                for mc in range(2):
                                    op('pe', I('matmul',
                                        psB[:, bk, :], vx[:, mc, hx * 256 + dc * 128:hx * 256 + (dc + 1) * 128],
                                        pT_t[:, hx, mc, :], start=(mc == 0), stop=(mc == 1)),
                                       r=['vx', pTk], w=[f'psB{bk}'])
                                evac_copy(hT[:, 2 * hx + dc, n * 512:(n + 1) * 512], psB[:, bk, :], [f'psB{bk}'],
                                          [HK(2 * hx + dc, n)])
                    for n in range(2):
                        sl = nxt
                        nxt = load_w(w_xo_d[:, 512:1024]) if n == 0 else None
                        ns = slice(n * 512, (n + 1) * 512)
                        for t in range(NT):
                            bk = t % 4
                            for cc in range(8):
                                op('pe', I('matmul',
                                    psB[:, bk, :], hT[:, cc, t * 128:(t + 1) * 128], wr[sl][:, cc, :],
                                    start=(cc == 0), stop=(cc == 7)), r=[HK(cc, t // 4), f'wr{sl}'], w=[f'psB{bk}'])
                            op('dve', I('tensor_tensor',
                                out=x_sb[:, t, ns], in0=psB[:, bk, :], in1=x_sb[:, t, ns], op=ALU.add),
                               r=[f'psB{bk}', f'x{t}'], w=[f'x{t}'])
                    S.barrier()
                if stage == 2:
                    for t in range(NT):
                        ev = op('sp', I('dma_start',
                            out=out_d[b * SEQ + t * 128:b * SEQ + (t + 1) * 128, :], in_=x_sb[:, t, :]),
                            r=[f'x{t}'], dma=f'o{t % 4}')
                    S.barrier()
                    continue

                with ExitStack() as s2g:
                    gbc = T(s2g, "gbc", [128, DM], F32)
                    h3 = [T(s2g, f"h3_{i}", [128, DM], F32) for i in range(2)]
                    h3b = [T(s2g, f"h3b_{i}", [128, DM], BF16) for i in range(2)]
                    h3T = T(s2g, "h3T", [128, 8, 128], F32)
                    lg = T(s2g, "lg", [128, 36], F32)
                    rw = T(s2g, "rw", [128, 16], F32)
                    gm = T(s2g, "gm", [128, 4], F32)
                    elm = T(s2g, "elm", [128, 4, 8], F32)
                    ig = T(s2g, "ig", [128, 4, 8], F32)
                    S32 = T(s2g, "S32", [128, 4, 8], F32)
                    S32b = T(s2g, "S32b", [128, 32], BF16)
                    M1 = T(s2g, "M1", [128, 4, 8], F32)
                    M2 = T(s2g, "M2", [128, 4, 8], F32)
                    pos = T(s2g, "pos", [128, 32], F32)
                    slf = T(s2g, "slf", [128, 2], F32)
                    op('sp', I('dma_start', out=gbc[:], in_=ln_ffn_g.partition_broadcast(128)), w=['gbc'],
                       dma='gbc')
                    for t in range(NT):
                        tg = b * NT + t
                        hi = t % 2
                        op('act', I('activation', out=junk[:], in_=x_sb[:, t, :], func=AF.Square,
                                                              accum_out=stat[:, 0, t:t + 1]),
                           r=[f'x{t}'], w=['junk', 'stat'])
                        rstd_from_ss(stat, t, 1.0 / DM)
                        op('dve', I('scalar_tensor_tensor',
                            out=h3[hi][:], in0=x_sb[:, t, :], scalar=stat[:, 2, t:t + 1], in1=gbc[:], op0=ALU.mult,
                            op1=ALU.mult), r=[f'x{t}', 'stat', 'gbc'], w=[f'h3_{hi}'])
                        op('pool', I('tensor_copy', out=h3b[hi][:], in_=h3[hi][:]), r=[f'h3_{hi}'],
                           w=[f'h3b_{hi}'])
                        for kc in range(8):
                            op('pe', I('transpose',
                                out=psB[:, 4 + kc // 4, (kc % 4) * 128:(kc % 4 + 1) * 128],
                                in_=h3[hi][:, kc * 128:(kc + 1) * 128], identity=identf[:]),
                               r=[f'h3_{hi}', 'identf'], w=[f'psB{4 + kc // 4}'])
                        op('act', I('activation', out=h3T[:, 0:4, :], in_=psB[:, 4, :].rearrange("p (k c) -> p k c", k=4),
                                                         func=AF.Copy), r=['psB4'], w=['h3T'])
                        op('dve', I('tensor_copy', out=h3T[:, 4:8, :], in_=psB[:, 5, :].rearrange("p (k c) -> p k c", k=4)),
                           r=['psB5'], w=['h3T'])
                        for kc in range(8):
                            op('pe', I('matmul', psB[:, 0, 0:36], h3T[:, kc, :], wrt[:, kc, :],
                                                               start=(kc == 0), stop=(kc == 7)),
                               r=['h3T', 'wrt'], w=['psB0'])
                        V = lambda f: op('dve', f, r=['rt'], w=['rt'])
                        op('dve', I('tensor_tensor', out=lg[:], in0=psB[:, 0, 0:36], in1=brt[:], op=ALU.add),
                           r=['psB0', 'brt', 'rt'], w=['rt'])
                        el = lg[:, 4:36].rearrange("p (g e) -> p g e", g=4)
                        V(I('tensor_reduce', out=rw[:, 0:1], in_=lg[:, 0:4], axis=AX.X, op=ALU.max))
                        V(I('tensor_scalar', out=rw[:, 1:2], in0=rw[:, 0:1], scalar1=-1.0, scalar2=None,
                                                    op0=ALU.mult))
                        op('act', I('activation', out=gm[:], in_=lg[:, 0:4], func=AF.Exp, bias=rw[:, 1:2],
                                                         accum_out=rw[:, 2:3]), r=['rt'], w=['rt'])
                        V(I('reciprocal', out=rw[:, 3:4], in_=rw[:, 2:3]))
                        V(I('tensor_scalar', out=gm[:], in0=lg[:, 0:4], scalar1=rw[:, 0:1], scalar2=None,
                                                    op0=ALU.is_equal))
                        V(I('tensor_tensor', out=elm[:], in0=el, in1=gm[:, :, None].to_broadcast([128, 4, 8]),
                                                    op=ALU.mult))
                        V(I('tensor_reduce', out=ig[:, 0, :], in_=elm[:].rearrange("p g e -> p e g"),
                                                    axis=AX.X, op=ALU.add))
                        V(I('tensor_reduce', out=rw[:, 4:5], in_=ig[:, 0, :], axis=AX.X, op=ALU.max))
                        V(I('tensor_scalar', out=ig[:, 1, :], in0=ig[:, 0, :], scalar1=rw[:, 4:5], scalar2=None,
                                                    op0=ALU.is_equal))
                        V(I('scalar_tensor_tensor', out=ig[:, 2, :], in0=ig[:, 1, :], scalar=-1e30,
                                                           in1=ig[:, 0, :], op0=ALU.mult, op1=ALU.add))
                        V(I('tensor_reduce', out=rw[:, 5:6], in_=ig[:, 2, :], axis=AX.X, op=ALU.max))
                        V(I('tensor_scalar', out=ig[:, 3, :], in0=ig[:, 2, :], scalar1=rw[:, 5:6], scalar2=None,
                                                    op0=ALU.is_equal))
                        V(I('tensor_tensor', out=rw[:, 6:7], in0=rw[:, 5:6], in1=rw[:, 4:5], op=ALU.subtract))
                        op('act', I('activation', out=rw[:, 7:8], in_=rw[:, 6:7], func=AF.Exp), r=['rt'], w=['rt'])
                        V(I('tensor_scalar', out=rw[:, 8:9], in0=rw[:, 7:8], scalar1=1.0, scalar2=None,
                                                    op0=ALU.add))
                        V(I('reciprocal', out=rw[:, 9:10], in_=rw[:, 8:9]))
                        op('dve', I('tensor_tensor', out=wts[:, tg, 0:1], in0=rw[:, 9:10], in1=rw[:, 3:4],
                                                                   op=ALU.mult), r=['rt'], w=['rt', 'wts'])
                        op('dve', I('tensor_tensor', out=wts[:, tg, 1:2], in0=rw[:, 7:8],
                                                                   in1=wts[:, tg, 0:1], op=ALU.mult),
                           r=['rt', 'wts'], w=['rt', 'wts'])
                        V(I('tensor_tensor', out=M1[:], in0=gm[:, :, None].to_broadcast([128, 4, 8]),
                                                    in1=ig[:, 1:2, :].to_broadcast([128, 4, 8]), op=ALU.mult))
                        V(I('tensor_tensor', out=M2[:], in0=gm[:, :, None].to_broadcast([128, 4, 8]),
                                                    in1=ig[:, 3:4, :].to_broadcast([128, 4, 8]), op=ALU.mult))
                        V(I('tensor_tensor', out=S32[:], in0=M1[:], in1=M2[:], op=ALU.add))
                        V(I('tensor_copy', out=S32b[:], in_=S32[:].rearrange("p g e -> p (g e)")))
                        op('pe', I('matmul', psB[:, 1, 0:32], Lmat[:], S32b[:], start=True, stop=True),
                           r=['Lmat', 'rt'], w=['psB1'])
                        op('pe', I('matmul', psB[:, 1, 32:64], onesb[:], S32b[:], start=True, stop=True),
                           r=['onesb', 'rt'], w=['psB1'])
                        op('dve', I('tensor_tensor', out=pos[:], in0=psB[:, 1, 0:32], in1=base[:], op=ALU.add),
                           r=['psB1', 'base', 'rt'], w=['rt'])
                        op('dve', I('tensor_tensor', out=base[:], in0=psB[:, 1, 32:64], in1=base[:], op=ALU.add),
                           r=['psB1', 'base'], w=['base'])
                        V(I('tensor_scalar', out=elm[:].rearrange("p g e -> p (g e)"), in0=pos[:],
                                                    scalar1=float(CAP), scalar2=1e7, op0=ALU.is_ge, op1=ALU.mult))
                        V(I('tensor_tensor', out=pos[:], in0=pos[:], in1=elm[:].rearrange("p g e -> p (g e)"),
                                                    op=ALU.add))
                        V(I('tensor_tensor', out=pos[:], in0=pos[:], in1=ecap[:], op=ALU.add))
                        V(I('tensor_tensor', out=elm[:].rearrange("p g e -> p (g e)"), in0=pos[:],
                                                    in1=M1[:].rearrange("p g e -> p (g e)"), op=ALU.mult))
                        V(I('tensor_reduce', out=slf[:, 0:1], in_=elm[:].rearrange("p g e -> p (g e)"), axis=AX.X,
                                                    op=ALU.add))
                        V(I('tensor_tensor', out=elm[:].rearrange("p g e -> p (g e)"), in0=pos[:],
                                                    in1=M2[:].rearrange("p g e -> p (g e)"), op=ALU.mult))
                        V(I('tensor_reduce', out=slf[:, 1:2], in_=elm[:].rearrange("p g e -> p (g e)"), axis=AX.X,
                                                    op=ALU.add))
                        op('dve', I('tensor_copy', out=slots[:, tg, :], in_=slf[:]), r=['rt'],
                           w=['rt', f'slots{tg}'])
                        for k2 in range(2):
                            op('pool', I('indirect_dma_start',
                                out=xd_d[:, :], out_offset=bass.IndirectOffsetOnAxis(ap=slots[:, tg, k2:k2 + 1], axis=0),
                                in_=h3b[hi][:], in_offset=None, bounds_check='REG', oob_is_err=False),
                               r=[f'slots{tg}', f'h3b_{hi}'], w=[f'xd{tg}_{k2}'], dma=f'sc{hi}')
                        op('sp', I('dma_start',
                            out=xres_d[b * SEQ + t * 128:b * SEQ + (t + 1) * 128, :], in_=x_sb[:, t, :]),
                            r=[f'x{t}'], w=[f'xres{t}'], dma=f'xw{t % 4}')
                    S.barrier()

        if stage >= 3:
            with ExitStack() as s3:
                wg = [T(s3, f"wg{i}", [128, 8, 512], BF16) for i in range(2)]
                wu = [T(s3, f"wu{i}", [128, 8, 512], BF16) for i in range(2)]
                wd = [T(s3, f"wd{i}", [128, 4, DM], BF16) for i in range(2)]
                xr = [T(s3, f"xr{i}", [128, DM], BF16) for i in range(3)]
                xT = [T(s3, f"xT{i}", [128, 8, 384], BF16) for i in range(2)]
                sg = [T(s3, f"sg{i}", [128, 384], F32) for i in range(2)]
                h1T = [T(s3, f"h1T{i}", [128, 4, 384], BF16) for i in range(2)]
                ydt = [T(s3, f"ydt{i}", [128, DM], F32) for i in range(2)]

                def load_expert(e_):
                    i = e_ % 2
                    op('pool', I('dma_start', out=wg[i][:], in_=w_gate_d[e_].rearrange("(k p) n -> p k n", p=128)),
                       w=[f'wg{i}'], dma=f'wg{i}')
                    op('pool', I('dma_start', out=wu[i][:], in_=w_up_d[e_].rearrange("(k p) n -> p k n", p=128)),
                       w=[f'wu{i}'], dma=f'wu{i}')
                    op('pool', I('dma_start', out=wd[i][:], in_=w_down_d[e_].rearrange("(k p) n -> p k n", p=128)),
                       w=[f'wd{i}'], dma=f'wd{i}')

                load_expert(0)
                cnt = 0
                yc = 0
                for e_ in range(NEXP):
                    wi = e_ % 2
                    if e_ + 1 < NEXP:
                        load_expert(e_ + 1)
                    for half in range(2):
                        r0 = e_ * CAP + half * 384
                        hb = cnt % 2
                        cnt += 1
                        for ci in range(3):
                            xi = (cnt * 3 + ci) % 3
                            op('sp', I('dma_start',
                                out=xr[xi][:], in_=xd_d[r0 + ci * 128:r0 + (ci + 1) * 128, :]),
                                r=['xd'], w=[f'xr{xi}'], dma=f'xr{xi}')
                            pa = ci % 2
                            for kc in range(8):
                                op('pe', I('transpose',
                                    out=psA[:, pa, kc * 128:(kc + 1) * 128], in_=xr[xi][:, kc * 128:(kc + 1) * 128],
                                    identity=ident[:]), r=[f'xr{xi}', 'ident'], w=[f'psA{pa}'])
                            evac_copy(xT[hb][:, :, ci * 128:(ci + 1) * 128],
                                      psA[:, pa, :].rearrange("p (k c) -> p k c", k=8), [f'psA{pa}'], [f'xT{hb}'])
                        for dc in range(4):
                            bg, bu = (dc % 2) * 2, (dc % 2) * 2 + 1
                            for kc in range(8):
                                op('pe', I('matmul',
                                    psB[:, bg, 0:384], wg[wi][:, kc, dc * 128:(dc + 1) * 128], xT[hb][:, kc, :],
                                    start=(kc == 0), stop=(kc == 7)), r=[f'wg{wi}', f'xT{hb}'], w=[f'psB{bg}'])
                            for kc in range(8):
                                op('pe', I('matmul',
                                    psB[:, bu, 0:384], wu[wi][:, kc, dc * 128:(dc + 1) * 128], xT[hb][:, kc, :],
                                    start=(kc == 0), stop=(kc == 7)), r=[f'wu{wi}', f'xT{hb}'], w=[f'psB{bu}'])
                            si = dc % 2
                            op('act', I('activation', out=sg[si][:], in_=psB[:, bg, 0:384],
                                                                           func=AF.Silu),
                               r=[f'psB{bg}'], w=[f'sg{si}'])
                            op('dve', I('tensor_tensor',
                                out=h1T[hb][:, dc, :], in0=psB[:, bu, 0:384], in1=sg[si][:], op=ALU.mult),
                               r=[f'psB{bu}', f'sg{si}'], w=[f'h1T{hb}_{dc}'])
                        for ci in range(3):
                            yi = yc % 2
                            yc += 1
                            for nn in range(2):
                                for dc in range(4):
                                    op('pe', I('matmul',
                                        psB[:, 4 + nn, :], h1T[hb][:, dc, ci * 128:(ci + 1) * 128],
                                        wd[wi][:, dc, nn * 512:(nn + 1) * 512], start=(dc == 0), stop=(dc == 3)),
                                       r=[f'h1T{hb}_{dc}', f'wd{wi}'], w=[f'psB{4 + nn}'])
                            op('act', I('activation', out=ydt[yi][:, 0:512], in_=psB[:, 4, :], func=AF.Copy),
                               r=['psB4'], w=[f'ydt{yi}a'])
                            op('dve', I('tensor_copy', out=ydt[yi][:, 512:1024], in_=psB[:, 5, :]),
                               r=['psB5'], w=[f'ydt{yi}b'])
                            op('sp', I('dma_start',
                                out=yd_d[r0 + ci * 128:r0 + (ci + 1) * 128, :], in_=ydt[yi][:]),
                                r=[f'ydt{yi}a', f'ydt{yi}b'], w=[f'yd{yc}'], dma=f'yo{yi}')
                S.barrier()

            with ExitStack() as s4:
                gbf = T(s4, "gbf", [128, DM], F32)
                y1 = [T(s4, f"y1_{i}", [128, DM], F32) for i in range(2)]
                y2 = [T(s4, f"y2_{i}", [128, DM], F32) for i in range(2)]
                xf = [T(s4, f"xf{i}", [128, DM], F32) for i in range(2)]
                of = [T(s4, f"of{i}", [128, DM], F32) for i in range(2)]
                fst = T(s4, "fst", [128, 3, nseq * NT], F32)
                op('sp', I('dma_start', out=gbf[:], in_=ln_final_g.partition_broadcast(128)), w=['gbf'], dma='gbf')
                for tg in range(nseq * NT):
                    i = tg % 2
                    for ybuf, k2, nm in ((y1, 0, 'y1'), (y2, 1, 'y2')):
                        op('pool', I('memset', ybuf[i][:], 0.0), w=[f'{nm}_{i}'])
                        op('pool', I('indirect_dma_start',
                            out=ybuf[i][:], out_offset=None, in_=yd_d[:, :],
                            in_offset=bass.IndirectOffsetOnAxis(ap=slots[:, tg, k2:k2 + 1], axis=0),
                            bounds_check='REG', oob_is_err=False),
                           r=['yd'], w=[f'{nm}_{i}'], dma=f'{nm}_{i}')
                    op('sp', I('dma_start', out=xf[i][:], in_=xres_d[tg * 128:(tg + 1) * 128, :]),
                       r=['xres'], w=[f'xf{i}'], dma=f'xf{i}')
                    op('dve', I('scalar_tensor_tensor',
                        out=xf[i][:], in0=y1[i][:], scalar=wts[:, tg, 0:1], in1=xf[i][:], op0=ALU.mult, op1=ALU.add),
                       r=[f'y1_{i}', f'xf{i}'], w=[f'xf{i}'])
                    op('dve', I('scalar_tensor_tensor',
                        out=xf[i][:], in0=y2[i][:], scalar=wts[:, tg, 1:2], in1=xf[i][:], op0=ALU.mult, op1=ALU.add),
                       r=[f'y2_{i}', f'xf{i}'], w=[f'xf{i}'])
                    op('act', I('activation', out=junk[:], in_=xf[i][:], func=AF.Square,
                                                                 accum_out=fst[:, 0, tg:tg + 1]),
                       r=[f'xf{i}'], w=['junk', 'stat'])
                    rstd_from_ss(fst, tg, 1.0 / DM)
                    op('act', I('activation', out=xf[i][:], in_=xf[i][:], func=AF.Copy,
                                                                 scale=fst[:, 2, tg:tg + 1]),
                       r=[f'xf{i}', 'stat'], w=[f'xf{i}'])
                    op('pool', I('tensor_tensor', out=of[i][:], in0=xf[i][:], in1=gbf[:], op=ALU.mult),
                       r=[f'xf{i}', 'gbf'], w=[f'of{i}'])
                    op('sp', I('dma_start', out=out_d[tg * 128:(tg + 1) * 128, :], in_=of[i][:]),
                       r=[f'of{i}'], dma=f'of{i}')
                S.barrier()
        S.barrier()
        S.emit()
    return nc


_NC_CACHE = {}


def _prep(inputs, c, nseq=NSEQ):
    f = lambda a: np.ascontiguousarray(np.asarray(a, dtype=np.float32))
    d = {
        "x": f(inputs["x"][c * nseq:(c + 1) * nseq]),
        "mem": f(inputs["mem"][c * nseq:(c + 1) * nseq]),
        "ln_mix_g": f(inputs["ln_mix_g"][0]),
        "w_in": f(inputs["w_in"][0]),
        "sb_out_g": f(inputs["sb_out_g"][0]),
        "conv_w": f(inputs["conv_w"][0]),
        "conv_b": f(inputs["conv_b"][0]),
        "conv_ln_g": f(inputs["conv_ln_g"][0]),
        "conv_ln_b": f(inputs["conv_ln_b"][0]),
        "w_out": f(inputs["w_out"][0]),
        "ln_mem_x_g": f(inputs["ln_mem_x_g"][0]),
        "ln_mem_g": f(inputs["ln_mem_g"][0]),
        "w_xq": f(inputs["w_xq"][0]),
        "w_xkv": f(inputs["w_xkv"][0]),
        "w_xo": f(inputs["w_xo"][0]),
        "ln_ffn_g": f(inputs["ln_ffn_g"][0]),
        "w_rt": f(np.concatenate([np.asarray(inputs["w_group"][0]), np.asarray(inputs["w_er"][0]).reshape(DM, 32)], axis=1)),
        "b_rt": f(np.concatenate([np.asarray(inputs["b_group"][0]), np.asarray(inputs["b_er"][0]).reshape(32)])),
        "w_gate": f(inputs["w_gate"][0]),
        "w_up": f(inputs["w_up"][0]),
        "w_down": f(inputs["w_down"][0]),
        "ln_final_g": f(inputs["ln_final_g"]),
    }
    return d


def kernel(**inputs):
    if 'nc' not in _NC_CACHE:
        _NC_CACHE['nc'] = build()
    nc = _NC_CACHE['nc']
    in_maps = [_prep(inputs, c) for c in range(N_CORES)]
    res = run_bass_kernel_spmd(nc, in_maps, core_ids=list(range(N_CORES)))
    out = np.concatenate([np.asarray(r["out"]).reshape(NSEQ, SEQ, DM) for r in res.results], axis=0)
    return out.astype(np.float32)
```

```python
import numpy as np
import concourse.bass as bass
import concourse.mybir as mybir
from concourse.bass_utils import run_bass_kernel_spmd
from contextlib import ExitStack

F32 = mybir.dt.float32
F32R = mybir.dt.float32r
BF16 = mybir.dt.bfloat16
I32 = mybir.dt.int32
AF = mybir.ActivationFunctionType
ALU = mybir.AluOpType
AX = mybir.AxisListType

ENG = ['pe', 'act', 'dve', 'pool', 'sp']
SAME_ENGINE_SYNC = True

N_CORES = 8
NSEQ = 4
SEQ = 2048
DM = 1024
NT = SEQ // 128
NMEM = 256
NEXP = 32
CAP = 768
NROWS = NEXP * CAP
EPS = 1e-6


def I(name, *args, **kw):
    return (name, args, kw)


class Sched:
    def __init__(self, nc, es):
        self.nc = nc
        self.es = es
        self.ins = {e: [] for e in ENG}
        self.cnt = {e: 0 for e in ENG}
        self.sem = {e: es.enter_context(nc.semaphore('s_' + e)) for e in ENG}
        self.dsem = {}
        self.lastw = {}
        self.readers = {}
        self.waited = {e: {} for e in ENG}

    def _semof(self, key):
        return self.sem[key[1]] if key[0] == 'e' else self.dsem[key[1]][0]

    def op(self, eng, fn, r=(), w=(), dma=None, extra=()):
        deps = {}

        def need(ev):
            if ev is None:
                return
            key, val, src, is_dma = ev
            if (not is_dma) and src == eng and (eng == 'pe' or not SAME_ENGINE_SYNC):
                return
            if deps.get(key, 0) < val:
                deps[key] = val

        for k in r:
            need(self.lastw.get(k))
        for k in w:
            need(self.lastw.get(k))
            for ev in self.readers.get(k, {}).values():
                need(ev)
        for ev in extra:
            need(ev)
        waits = []
        wd = self.waited[eng]
        for key, val in deps.items():
            if wd.get(key, 0) >= val:
                continue
            wd[key] = val
            waits.append((key, val))
        if dma is not None:
            if dma not in self.dsem:
                self.dsem[dma] = [self.es.enter_context(self.nc.semaphore('d_' + dma)), 0]
            self.dsem[dma][1] += 16
            ev = (('d', dma), self.dsem[dma][1], eng, True)
        else:
            self.cnt[eng] += 1
            ev = (('e', eng), self.cnt[eng], eng, False)
        self.ins[eng].append((waits, fn, ev))
        for k in r:
            d = self.readers.setdefault(k, {})
            old = d.get(ev[0])
            if old is None or old[1] < ev[1]:
                d[ev[0]] = ev
        for k in w:
            self.lastw[k] = ev
            self.readers[k] = {}
        return ev

    def barrier(self):
        evs = []
        for e in ENG:
            if self.cnt[e] > 0:
                evs.append((('e', e), self.cnt[e], e, False))
        for slot, (s, c) in self.dsem.items():
            if c > 0:
                evs.append((('d', slot), c, 'sp', True))
        for e in ENG:
            self.op(e, I('nop'), extra=[ev for ev in evs if not (ev[2] == e and not ev[3])])
        self.lastw = {}
        self.readers = {}

    def emit(self):
        nc = self.nc
        with nc.Block() as block:
            def body(name):
                def f(e):
                    bc_reg = None
                    if name == 'pool':
                        bc_reg = e.alloc_register()
                        e.reg_mov(bc_reg, NROWS - 1)
                    for waits, fn, ev in self.ins[name]:
                        for key, val in waits:
                            e.wait_ge(self._semof(key), val)
                        try:
                            kw = fn[2]
                            if kw.get('bounds_check', None) == 'REG':
                                kw = dict(kw, bounds_check=bc_reg)
                            ins = getattr(e, fn[0])(*fn[1], **kw)
                        except Exception:
                            print("EMIT FAIL", name, fn[0], fn[1], fn[2])
                            raise
                        key, val, _, is_dma = ev
                        ins.then_inc(self._semof(key), 16 if is_dma else 1)
                return f
            block.tensor(body('pe'))
            block.scalar(body('act'))
            block.vector(body('dve'))
            block.gpsimd(body('pool'))
            block.sync(body('sp'))


def build(nseq=NSEQ, stage=99):
    nc = bass.Bass('TRN2', target_bir_lowering=False)
    ntok = nseq * SEQ

    def din(name, shape, dt=F32):
        return nc.dram_tensor(name, list(shape), dt, kind="ExternalInput").ap()

    x_d = din("x", [nseq, SEQ, DM])
    mem_d = din("mem", [nseq, NMEM, DM])
    ln_mix_g = din("ln_mix_g", [DM])
    w_in_d = din("w_in", [DM, 2560])
    sb_out_g = din("sb_out_g", [512])
    conv_w_d = din("conv_w", [31, 512])
    conv_b_d = din("conv_b", [512])
    conv_ln_g = din("conv_ln_g", [512])
    conv_ln_b = din("conv_ln_b", [512])
    w_out_d = din("w_out", [DM, DM])
    ln_mem_x_g = din("ln_mem_x_g", [DM])
    ln_mem_g = din("ln_mem_g", [DM])
    w_xq_d = din("w_xq", [DM, DM])
    w_xkv_d = din("w_xkv", [DM, 2 * DM])
    w_xo_d = din("w_xo", [DM, DM])
    ln_ffn_g = din("ln_ffn_g", [DM])
    w_rt_d = din("w_rt", [DM, 36])
    b_rt_d = din("b_rt", [36])
    w_gate_d = din("w_gate", [NEXP, DM, 512])
    w_up_d = din("w_up", [NEXP, DM, 512])
    w_down_d = din("w_down", [NEXP, 512, DM])
    ln_final_g = din("ln_final_g", [DM])
    out_d = nc.dram_tensor("out", [ntok, DM], F32, kind="ExternalOutput").ap()
    if stage == 1:
        dbg_mix = nc.dram_tensor("dbg_mix", [128, 8, SEQ], BF16, kind="ExternalOutput").ap()
        dbg_qk = nc.dram_tensor("dbg_qk", [128, 8, SEQ], BF16, kind="ExternalOutput").ap()
        dbg_v = nc.dram_tensor("dbg_v", [128, NT, 512], BF16, kind="ExternalOutput").ap()
        dbg_rsb = nc.dram_tensor("dbg_rsb", [128, 3, NT], F32, kind="ExternalOutput").ap()
    xd_d = nc.dram_tensor("xd_scr", [NROWS, DM], BF16).ap()
    yd_d = nc.dram_tensor("yd_scr", [NROWS, DM], F32).ap()
    xres_d = nc.dram_tensor("xres_scr", [ntok, DM], F32).ap()

    with ExitStack() as es:
        S = Sched(nc, es)
        op = S.op

        uid = [0]

        def T(scope, name, shape, dt):
            uid[0] += 1
            return scope.enter_context(nc.sbuf_tensor(f"{name}_u{uid[0]}", shape, dt))

        psA = es.enter_context(nc.psum_tensor("psA", [128, 2, 1024], BF16))
        psB = es.enter_context(nc.psum_tensor("psB", [128, 6, 512], F32))

        identf = T(es, "identf", [128, 128], F32)
        ident = T(es, "ident", [128, 128], BF16)
        negtri = T(es, "negtri", [128, 128], F32)
        negones = T(es, "negones", [128, 128], F32)
        negtriR = T(es, "negtriR", [128, 128], F32)
        negonesR = T(es, "negonesR", [128, 128], F32)
        meanmat = T(es, "meanmat", [128, 128], F32)
        ones2 = T(es, "ones2", [128, 2], F32)
        onesb = T(es, "onesb", [128, 128], BF16)
        Lmat = T(es, "Lmat", [128, 128], BF16)
        maskf = T(es, "maskf", [128, 4, 512], BF16)
        ecap = T(es, "ecap", [128, 32], F32)
        ecap_i = T(es, "ecap_i", [128, 32], I32)
        gcols = T(es, "gcols", [128, 3, 8], F32)
        sbg = T(es, "sbg", [128, 4], F32)
        cvb = T(es, "cvb", [128, 4], F32)
        cvg = T(es, "cvg", [128, 4], F32)
        cvbb = T(es, "cvbb", [128, 4], F32)
        cwT = T(es, "cwT", [128, 4, 31], F32)
        hT = T(es, "hT", [128, 8, SEQ], BF16)
        wr = [T(es, f"wr{i}", [128, 8, 512], BF16) for i in range(2)]
        stat = T(es, "stat", [128, 3, NT], F32)
        rsb = T(es, "rsb", [128, 3, NT], F32)
        slots = T(es, "slots", [128, nseq * NT, 2], I32)
        wts = T(es, "wts", [128, nseq * NT, 2], F32)
        base = T(es, "base", [128, 32], F32)
        wrt = T(es, "wrt", [128, 8, 36], F32)
        brt = T(es, "brt", [128, 36], F32)
        junk = T(es, "junk", [128, 1024], BF16)

        def HK(c, n):
            return f"H{c}_{n}"

        def small_col(dst_ap, src_ap, k, key):
            op('sp', I('dma_start', out=dst_ap, in_=src_ap.rearrange("(k p) -> p k", p=128),
                                           allow_slow_non_contiguous=True), w=[key], dma='c_' + key)

        small_col(gcols[:, 0, :], ln_mix_g, 8, 'gc0')
        small_col(gcols[:, 1, :], ln_mem_x_g, 8, 'gc1')
        small_col(gcols[:, 2, :], ln_mem_g, 8, 'gc2')
        small_col(sbg[:], sb_out_g, 4, 'sbg')
        small_col(cvb[:], conv_b_d, 4, 'cvb')
        small_col(cvg[:], conv_ln_g, 4, 'cvg')
        small_col(cvbb[:], conv_ln_b, 4, 'cvbb')
        op('sp', I('dma_start', out=wrt[:], in_=w_rt_d.rearrange("(k p) n -> p k n", p=128)),
           w=['wrt'], dma='c_wrt')
        op('sp', I('dma_start', out=brt[:], in_=b_rt_d.partition_broadcast(128)), w=['brt'], dma='c_brt')

        op('pool', I('memset', identf[:], 0.0), w=['identf'])
        op('pool', I('affine_select', out=identf[:], in_=identf[:], pattern=[[-1, 128]],
                                             compare_op=ALU.not_equal, fill=1.0, base=0, channel_multiplier=1),
           r=['identf'], w=['identf'])
        op('dve', I('tensor_copy', out=ident[:], in_=identf[:]), r=['identf'], w=['ident'])
        op('pool', I('memset', negtri[:], -1.0), w=['negtri'])
        op('pool', I('affine_select', out=negtri[:], in_=negtri[:], pattern=[[-1, 128]],
                                             compare_op=ALU.is_ge, fill=0.0, base=0, channel_multiplier=1),
           r=['negtri'], w=['negtri'])
        op('pool', I('memset', negones[:], -1.0), w=['negones'])
        op('dve', I('tensor_copy', out=negtriR[:].bitcast(F32R), in_=negtri[:]), r=['negtri'], w=['negtriR'])
        op('dve', I('tensor_copy', out=negonesR[:].bitcast(F32R), in_=negones[:]), r=['negones'], w=['negonesR'])
        op('pool', I('memset', meanmat[:], 1.0 / 512), w=['meanmat'])
        op('pool', I('memset', ones2[:], 1.0), w=['ones2'])
        op('pool', I('memset', onesb[:], 1.0), w=['onesb'])
        op('pool', I('memset', Lmat[:], 1.0), w=['Lmat'])
        op('pool', I('affine_select', out=Lmat[:], in_=Lmat[:], pattern=[[1, 128]],
                                             compare_op=ALU.is_gt, fill=0.0, base=0, channel_multiplier=-1),
           r=['Lmat'], w=['Lmat'])
        op('pool', I('memset', maskf[:], 1.0), w=['maskf'])
        for r_ in range(4):
            op('pool', I('affine_select', out=maskf[:, r_, :], in_=maskf[:, r_, :], pattern=[[1, 512]],
                                                        compare_op=ALU.is_gt, fill=0.0, base=-r_ * 128,
                                                        channel_multiplier=-1),
               r=['maskf'], w=['maskf'])
        op('pool', I('iota', ecap_i[:], pattern=[[CAP, 32]], base=0, channel_multiplier=0), w=['ecap_i'])
        op('dve', I('tensor_copy', out=ecap[:], in_=ecap_i[:]), r=['ecap_i'], w=['ecap'])
        op('pool', I('memset', base[:], 0.0), w=['base'])

        with ExitStack() as s0:
            cw_in = T(s0, "cw_in", [31, 512], F32)
            op('sp', I('dma_start', out=cw_in[:], in_=conv_w_d), w=['cw_in'], dma='c_cw')
            for c in range(4):
                op('pe', I('transpose', out=psB[:, 0, c * 32:c * 32 + 31], in_=cw_in[:, c * 128:(c + 1) * 128],
                                                    identity=identf[0:31, 0:31]),
                   r=['cw_in', 'identf'], w=['psB0'])
            op('act', I('activation', out=cwT[:], in_=psB[:, 0, 0:128].rearrange("p (c w) -> p c w", c=4)[:, :, 0:31],
                                             func=AF.Copy), r=['psB0'], w=['cwT'])
            S.barrier()

        wslot = [0]

        def load_w(src2d, nk=8):
            i = wslot[0]
            wslot[0] ^= 1
            op('pool', I('dma_start', out=wr[i][:, 0:nk, :], in_=src2d.rearrange("(k p) n -> p k n", p=128)),
               w=[f'wr{i}'], dma=f'wr{i}')
            return i

        evac_flip = [0]

        def evac_copy(out_ap, in_ap, r, w, scale=None, eng=None):
            if eng is None:
                eng = 'act' if (evac_flip[0] & 1) == 0 else 'dve'
                evac_flip[0] += 1
            if eng == 'act':
                if scale is None:
                    op('act', I('activation', out=out_ap, in_=in_ap, func=AF.Copy), r=r, w=w)
                else:
                    op('act', I('activation', out=out_ap, in_=in_ap, func=AF.Copy, scale=scale), r=r, w=w)
            else:
                if scale is None:
                    op('dve', I('tensor_copy', out=out_ap, in_=in_ap), r=r, w=w)
                else:
                    op('dve', I('tensor_scalar', out=out_ap, in0=in_ap, scalar1=scale, scalar2=None,
                                                        op0=ALU.mult), r=r, w=w)

        def rstd_from_ss(st, col, inv_n):
            op('act', I('activation', out=st[:, 1, col:col + 1], in_=st[:, 0, col:col + 1], func=AF.Sqrt,
                                             scale=inv_n, bias=EPS), r=['stat'], w=['stat'])
            op('dve', I('reciprocal', out=st[:, 2, col:col + 1], in_=st[:, 1, col:col + 1]),
               r=['stat'], w=['stat'])

        def norm_T(src_ap, src_key, hn_t, hn_key, gi, dst3, dst_keys, t, pa):
            op('act', I('activation', out=junk[:], in_=src_ap, func=AF.Square, accum_out=stat[:, 0, t:t + 1]),
               r=[src_key], w=['junk', 'stat'])
            rstd_from_ss(stat, t, 1.0 / DM)
            op('act', I('activation', out=hn_t[:], in_=src_ap, func=AF.Copy, scale=stat[:, 2, t:t + 1]),
               r=[src_key, 'stat'], w=[hn_key])
            for kc in range(8):
                op('pe', I('transpose', out=psA[:, pa, kc * 128:(kc + 1) * 128],
                                                      in_=hn_t[:, kc * 128:(kc + 1) * 128], identity=ident[:]),
                   r=[hn_key, 'ident'], w=[f'psA{pa}'])
            op('dve', I('tensor_tensor', out=dst3, in0=psA[:, pa, :].rearrange("p (k c) -> p k c", k=8),
                                                in1=gcols[:, gi, :, None].to_broadcast([128, 8, 128]), op=ALU.mult),
               r=[f'psA{pa}', f'gc{gi}'], w=dst_keys)

        def fm_proj(slot, ncc, rhs_fn, rhs_keys_fn, nN, N, evac_fn, banks):
            k = 0
            for cc in range(ncc):
                for n in range(nN):
                    bk = banks[k % len(banks)]
                    k += 1
                    for kc in range(8):
                        op('pe', I('matmul',
                            psB[:, bk, 0:N], wr[slot][:, kc, cc * 128:(cc + 1) * 128], rhs_fn(kc, n),
                            start=(kc == 0), stop=(kc == 7)),
                           r=[f'wr{slot}'] + rhs_keys_fn(kc, n), w=[f'psB{bk}'])
                    evac_fn(cc, n, bk)

        dbg = {}

        for b in range(nseq):
            with ExitStack() as s1:
                qk = T(s1, "qk", [128, 8, SEQ], BF16)
                v_sb = T(s1, "v_sb", [128, NT, 512], BF16)
                with ExitStack() as s1a:
                    xin = [T(s1a, f"xin{i}", [128, DM], F32) for i in range(3)]
                    hn = [T(s1a, f"hn{i}", [128, DM], BF16) for i in range(2)]
                    gT = T(s1a, "gT", [128, 4, 30 + SEQ], BF16)
                    ycv = T(s1a, "ycv", [128, 4, SEQ], F32)
                    dg = T(s1a, "dg", [128, 31, 128], BF16)
                    ysq = T(s1a, "ysq", [128, 4, 512], F32)
                    lnw = T(s1a, "lnw", [128, 4, 512], F32)

                    for t in range(NT):
                        xi = xin[t % 3]
                        op('sp', I('dma_start', out=xi[:], in_=x_d[b, t * 128:(t + 1) * 128, :]),
                           w=[f'xin{t % 3}'], dma=f'xin{t % 3}')
                        norm_T(xi[:], f'xin{t % 3}', hn[t % 2], f'hn{t % 2}', 0,
                               hT[:, :, t * 128:(t + 1) * 128], [HK(c, t // 4) for c in range(8)], t, t % 2)

                    hrhs = lambda kc, n: hT[:, kc, n * 512:(n + 1) * 512]
                    hkeys = lambda kc, n: [HK(kc, n)]
                    sl = load_w(w_in_d[:, 0:512])
                    nxt = load_w(w_in_d[:, 512:1024])
                    fm_proj(sl, 4, hrhs, hkeys, 4, 512,
                            lambda cc, n, bk: evac_copy(qk[:, cc, n * 512:(n + 1) * 512], psB[:, bk, :], [f'psB{bk}'],
                                                        [f'q{cc}_{n}'], scale=0.125), [0, 1, 2, 3])
                    sl = nxt
                    nxt = load_w(w_in_d[:, 1024:1536])
                    fm_proj(sl, 4, hrhs, hkeys, 4, 512,
                            lambda cc, n, bk: evac_copy(qk[:, 4 + cc, n * 512:(n + 1) * 512], psB[:, bk, :],
                                                        [f'psB{bk}'], [f'k{cc}_{n}']), [0, 1, 2, 3])
                    sl = nxt
                    nxt = load_w(w_in_d[:, 2048:2560])
                    for t in range(NT):
                        bk = t % 4
                        for kc in range(8):
                            op('pe', I('matmul',
                                psB[:, bk, :], hT[:, kc, t * 128:(t + 1) * 128], wr[sl][:, kc, :],
                                start=(kc == 0), stop=(kc == 7)),
                               r=[f'wr{sl}', HK(kc, t // 4)], w=[f'psB{bk}'])
                        evac_copy(v_sb[:, t, :], psB[:, bk, :], [f'psB{bk}'], [f'v{t}'])
                    op('pool', I('memset', gT[:, :, 0:30], 0.0), w=['gTpad'])
                    sl = nxt
                    nxt = load_w(w_in_d[:, 1536:2048])
                    fm_proj(sl, 4, hrhs, hkeys, 4, 512,
                            lambda cc, n, bk: op('act', I('activation',
                                out=gT[:, cc, 30 + n * 512:30 + (n + 1) * 512], in_=psB[:, bk, :], func=AF.Sigmoid),
                                r=[f'psB{bk}'], w=[f'g{cc}_{n}']), [0, 1, 2, 3])
                    sl = nxt
                    fm_proj(sl, 4, hrhs, hkeys, 4, 512,
                            lambda cc, n, bk: op('dve', I('tensor_tensor',
                                out=gT[:, cc, 30 + n * 512:30 + (n + 1) * 512], in0=psB[:, bk, :],
                                in1=gT[:, cc, 30 + n * 512:30 + (n + 1) * 512], op=ALU.mult),
                                r=[f'psB{bk}', f'g{cc}_{n}'], w=[f'g{cc}_{n}']), [0, 1, 2, 3])

                    for c in range(4):
                        for w_ in range(31):
                            op('dve', I('tensor_scalar',
                                out=dg[:, w_, :], in0=ident[:], scalar1=cwT[:, c, w_:w_ + 1], scalar2=None,
                                op0=ALU.mult), r=['ident', 'cwT'], w=[f'dg{w_}'])
                        for n in range(4):
                            bk = n % 4
                            gkeys = [f'g{c}_{n}'] + ([f'g{c}_{n - 1}'] if n > 0 else ['gTpad'])
                            for w_ in range(31):
                                op('pe', I('matmul',
                                    psB[:, bk, :], dg[:, w_, :], gT[:, c, n * 512 + w_:n * 512 + w_ + 512],
                                    start=(w_ == 0), stop=(w_ == 30)),
                                   r=[f'dg{w_}'] + gkeys, w=[f'psB{bk}'])
                            op('act', I('activation',
                                out=ycv[:, c, n * 512:(n + 1) * 512], in_=psB[:, bk, :], func=AF.Identity,
                                bias=cvb[:, c:c + 1]), r=[f'psB{bk}', 'cvb'], w=[f'y{c}_{n}'])
                    for n in range(4):
                        ns = slice(n * 512, (n + 1) * 512)
                        for c in range(4):
                            op('pe', I('matmul', psB[:, 4, :], meanmat[:], ycv[:, c, ns],
                                                                    start=(c == 0), stop=(c == 3)),
                               r=['meanmat', f'y{c}_{n}'], w=['psB4'])
                        for c in range(4):
                            op('act', I('activation', out=ysq[:, c, :], in_=ycv[:, c, ns],
                                                                         func=AF.Square),
                               r=[f'y{c}_{n}'], w=[f'ysq{c}'])
                        for c in range(4):
                            op('pe', I('matmul', psB[:, 5, :], meanmat[:], ysq[:, c, :],
                                                             start=(c == 0), stop=(c == 3)),
                               r=['meanmat', f'ysq{c}'], w=['psB5'])
                        op('act', I('activation', out=lnw[:, 0, :], in_=psB[:, 4, :], func=AF.Copy),
                           r=['psB4'], w=['lnw0'])
                        op('pool', I('tensor_tensor', out=lnw[:, 1, :], in0=lnw[:, 0, :], in1=lnw[:, 0, :],
                                                             op=ALU.mult), r=['lnw0'], w=['lnw1'])
                        op('dve', I('tensor_tensor', out=lnw[:, 1, :], in0=psB[:, 5, :], in1=lnw[:, 1, :],
                                                            op=ALU.subtract), r=['psB5', 'lnw1'], w=['lnw1'])
                        op('dve', I('tensor_scalar', out=lnw[:, 1, :], in0=lnw[:, 1, :], scalar1=0.0,
                                                            scalar2=None, op0=ALU.max), r=['lnw1'], w=['lnw1'])
                        op('act', I('activation', out=lnw[:, 1, :], in_=lnw[:, 1, :], func=AF.Sqrt, bias=EPS),
                           r=['lnw1'], w=['lnw1'])
                        op('dve', I('reciprocal', out=lnw[:, 2, :], in_=lnw[:, 1, :]), r=['lnw1'], w=['lnw2'])
                        for c in range(4):
                            op('pool', I('tensor_tensor', out=lnw[:, 3, :], in0=ycv[:, c, ns],
                                                                             in1=lnw[:, 0, :], op=ALU.subtract),
                               r=[f'y{c}_{n}', 'lnw0'], w=['lnw3'])
                            op('pool', I('tensor_tensor', out=lnw[:, 3, :], in0=lnw[:, 3, :], in1=lnw[:, 2, :],
                                                                 op=ALU.mult), r=['lnw3', 'lnw2'], w=['lnw3'])
                            op('act', I('activation',
                                out=hT[:, 4 + c, ns], in_=lnw[:, 3, :], func=AF.Silu, scale=cvg[:, c:c + 1],
                                bias=cvbb[:, c:c + 1]), r=['lnw3', 'cvg', 'cvbb'], w=[HK(4 + c, n)])
                    S.barrier()

                with ExitStack() as s1b:
                    spb = [T(s1b, f"spb{i}", [128, 512], F32) for i in range(4)]
                    ab = [T(s1b, f"ab{i}", [128, 512], BF16) for i in range(4)]
                    Rb = [T(s1b, f"Rb{i}", [128, 512], F32) for i in range(2)]
                    osq = [T(s1b, f"osq{i}", [128, 512], F32) for i in range(2)]
                    units = []
                    for j in range(4):
                        for qn in range(4):
                            kcs = list(range(4 * qn + 3, -1, -1))
                            for idx, kc in enumerate(kcs):
                                for hp in range(2):
                                    units.append((j, qn, idx, kc, hp, len(kcs)))
                    nU = len(units)
                    negtri_r = negtriR[:].bitcast(F32R)
                    negones_r = negonesR[:].bitcast(F32R)
                    oqc = [0]

                    def S1(i):
                        j, qn, idx, kc, hp, nk = units[i]
                        P = slice(hp * 64, hp * 64 + 64)
                        zb = i % 2
                        sp_t, spk = spb[i % 4], f'spb{i % 4}'
                        qs = slice(qn * 512, (qn + 1) * 512)
                        ks = slice(kc * 128, (kc + 1) * 128)
                        op('pe', I('matmul', psB[:, zb, :], qk[P, 4 + j, ks], qk[P, j, qs], start=True, stop=True),
                           r=[f'k{j}_{kc // 4}', f'q{j}_{qn}'], w=[f'psB{zb}'])
                        op('act', I('activation', out=sp_t[:].bitcast(F32R), in_=psB[:, zb, :], func=AF.Exp), r=[f'psB{zb}'], w=[spk])
                        op('act', I('activation', out=sp_t[:].bitcast(F32R), in_=sp_t[:], func=AF.Ln, bias=1.0), r=[spk], w=[spk])
                        if kc >= 4 * qn:
                            op('pool', I('tensor_tensor', out=sp_t[:].bitcast(F32R), in0=sp_t[:], in1=maskf[:, kc - 4 * qn, :],
                                         op=ALU.mult), r=[spk, 'maskf'], w=[spk])

                    def S2(i):
                        j, qn, idx, kc, hp, nk = units[i]
                        P = slice(hp * 64, hp * 64 + 64)
                        eb = 2 + i % 2
                        ek = f'psB{eb}'
                        sp_t, spk = spb[i % 4], f'spb{i % 4}'
                        ab_t, abk = ab[i % 4], f'ab{i % 4}'
                        qs = slice(qn * 512, (qn + 1) * 512)
                        ks = slice(kc * 128, (kc + 1) * 128)
                        op('pe', I('matmul', psB[:, eb, :], qk[P, 4 + j, ks], qk[P, j, qs], start=True, stop=False),
                           r=[f'k{j}_{kc // 4}', f'q{j}_{qn}'], w=[ek])
                        op('pe', I('matmul', psB[:, eb, :], negtri_r, sp_t[:].bitcast(F32R), start=False,
                                   stop=(idx == 0)), r=['negtriR', spk], w=[ek])
                        if idx > 0:
                            op('pe', I('matmul', psB[:, eb, :], negones_r, Rb[hp][:].bitcast(F32R), start=False,
                                       stop=True), r=['negonesR', f'Rb{hp}'], w=[ek])
                        op('act', I('activation', out=ab_t[:], in_=psB[:, eb, :], func=AF.Exp), r=[ek], w=[abk])
                        if kc >= 4 * qn:
                            op('dve', I('tensor_tensor', out=ab_t[:], in0=ab_t[:], in1=maskf[:, kc - 4 * qn, :],
                                        op=ALU.mult), r=[abk, 'maskf'], w=[abk])
                        if idx < nk - 1:
                            if idx == 0:
                                op('pool', I('tensor_copy', out=Rb[hp][:].bitcast(F32R), in_=sp_t[:]), r=[spk], w=[f'Rb{hp}'])
                            else:
                                op('pool', I('tensor_tensor', out=Rb[hp][:].bitcast(F32R), in0=Rb[hp][:], in1=sp_t[:], op=ALU.add),
                                   r=[spk, f'Rb{hp}'], w=[f'Rb{hp}'])

                    def S3(i):
                        j, qn, idx, kc, hp, nk = units[i]
                        h = 2 * j + hp
                        P = slice(hp * 64, hp * 64 + 64)
                        ab_t, abk = ab[i % 4], f'ab{i % 4}'
                        qs = slice(qn * 512, (qn + 1) * 512)
                        op('pe', I('matmul', psB[P, 4, :], v_sb[:, kc, h * 64:(h + 1) * 64], ab_t[:],
                                   start=(idx == 0), stop=(idx == nk - 1)), r=[f'v{kc}', abk], w=[f'psB4_{hp}'])
                        if idx == nk - 1 and hp == 1:
                            oq_t = osq[oqc[0] & 1]
                            oqk = f'osq{oqc[0] & 1}'
                            oqc[0] += 1
                            op('act', I('activation', out=hT[:, j, qs], in_=psB[:, 4, :], func=AF.Copy,
                                        scale=sbg[:, j:j + 1]), r=['psB4_0', 'psB4_1', 'sbg'], w=[HK(j, qn)])
                            op('act', I('activation', out=oq_t[:], in_=psB[:, 4, :], func=AF.Square),
                               r=['psB4_0', 'psB4_1'], w=[oqk])
                            for tt in range(4):
                                col = (j * 16 + qn * 4 + tt) * 2
                                op('pe', I('matmul', psB[:, 5, col:col + 2], oq_t[:, tt * 128:(tt + 1) * 128], ones2[:],
                                           start=True, stop=True), r=[oqk, 'ones2'], w=['psB5'])

                    for i in range(nU + 3):
                        if i < nU:
                            S1(i)
                        if 0 <= i - 2 < nU:
                            S2(i - 2)
                        if 0 <= i - 3 < nU:
                            S3(i - 3)
                    ssv = lambda j: psB[:, 5, j * 32:(j + 1) * 32].rearrange("p (t two) -> p t two", two=2)[:, :, 0]
                    op('act', I('activation', out=rsb[:, 0, :], in_=ssv(0), func=AF.Copy), r=['psB5'], w=['rsb'])
                    for j in range(1, 4):
                        op('dve', I('tensor_tensor', out=rsb[:, 0, :], in0=ssv(j), in1=rsb[:, 0, :],
                                                                 op=ALU.add), r=['psB5', 'rsb'], w=['rsb'])
                    op('act', I('activation', out=rsb[:, 1, :], in_=rsb[:, 0, :], func=AF.Sqrt, scale=1.0 / 512,
                                                     bias=EPS), r=['rsb'], w=['rsb'])
                    op('dve', I('reciprocal', out=rsb[:, 2, :], in_=rsb[:, 1, :]), r=['rsb'], w=['rsb'])
                    S.barrier()
                    if stage == 1 and b == 0:
                        op('sp', I('dma_start', out=dbg_mix, in_=hT[:]), dma='dbg0')
                        op('sp', I('dma_start', out=dbg_qk, in_=qk[:]), dma='dbg1')
                        op('sp', I('dma_start', out=dbg_v, in_=v_sb[:]), dma='dbg2')
                        op('sp', I('dma_start', out=dbg_rsb, in_=rsb[:]), dma='dbg3')
                        S.barrier()

            with ExitStack() as s2:
                x_sb = T(s2, "x_sb", [128, NT, DM], F32)
                for t in range(NT):
                    op('sp', I('dma_start', out=x_sb[:, t, :], in_=x_d[b, t * 128:(t + 1) * 128, :]),
                       w=[f'x{t}'], dma=f'x{t}')
                nxt = load_w(w_out_d[:, 0:512])
                for n in range(2):
                    sl = nxt
                    nxt = load_w(w_out_d[:, 512:1024]) if n == 0 else load_w(w_xkv_d[:, 0:512])
                    ns = slice(n * 512, (n + 1) * 512)
                    for t in range(NT):
                        b0, b1 = (t % 2) * 2, (t % 2) * 2 + 1
                        ts_ = slice(t * 128, (t + 1) * 128)
                        for jj in range(4):
                            op('pe', I('matmul',
                                psB[:, b0, :], hT[:, jj, ts_], wr[sl][:, jj, :], start=(jj == 0), stop=(jj == 3)),
                               r=[HK(jj, t // 4), f'wr{sl}'], w=[f'psB{b0}'])
                        for jj in range(4):
                            op('pe', I('matmul',
                                psB[:, b1, :], hT[:, 4 + jj, ts_], wr[sl][:, 4 + jj, :], start=(jj == 0), stop=(jj == 3)),
                               r=[HK(4 + jj, t // 4), f'wr{sl}'], w=[f'psB{b1}'])
                        op('dve', I('tensor_tensor',
                            out=x_sb[:, t, ns], in0=psB[:, b1, :], in1=x_sb[:, t, ns], op=ALU.add),
                           r=[f'psB{b1}', f'x{t}'], w=[f'x{t}'])
                        op('dve', I('scalar_tensor_tensor',
                            out=x_sb[:, t, ns], in0=psB[:, b0, :], scalar=rsb[:, 2, t:t + 1], in1=x_sb[:, t, ns],
                            op0=ALU.mult, op1=ALU.add), r=[f'psB{b0}', 'rsb', f'x{t}'], w=[f'x{t}'])
                if stage == 1:
                    for t in range(NT):
                        ev = op('sp', I('dma_start',
                            out=out_d[b * SEQ + t * 128:b * SEQ + (t + 1) * 128, :], in_=x_sb[:, t, :]),
                            r=[f'x{t}'], dma=f'o{t % 4}')
                    S.barrier()
                    continue

                with ExitStack() as s2f:
                    memin = [T(s2f, f"memin{i}", [128, DM], F32) for i in range(2)]
                    hn2 = [T(s2f, f"hnb{i}", [128, DM], BF16) for i in range(2)]
                    memT = T(s2f, "memT", [128, 8, NMEM], BF16)
                    kxT = T(s2f, "kxT", [128, 8, NMEM], BF16)
                    vx = T(s2f, "vx", [128, 2, DM], BF16)
                    qxT = T(s2f, "qxT", [128, 8, SEQ], BF16)
                    pf = [T(s2f, "pf0", [128, 4, NMEM], F32)] * 2
                    pn = [T(s2f, f"pn{i}", [128, 4, NMEM], BF16) for i in range(2)]
                    pT = [T(s2f, "pT0", [128, 4, 2, 512], BF16)] * 2
                    sm = T(s2f, "sm", [128, 4, 4], F32)
                    for mt in range(2):
                        op('sp', I('dma_start', out=memin[mt][:], in_=mem_d[b, mt * 128:(mt + 1) * 128, :]),
                           w=[f'memin{mt}'], dma=f'memin{mt}')
                        norm_T(memin[mt][:], f'memin{mt}', hn2[mt], f'hnb{mt}', 2,
                               memT[:, :, mt * 128:(mt + 1) * 128], ['memT'], mt, mt)
                    mrhs = lambda kc, n: memT[:, kc, :]
                    mkeys = lambda kc, n: ['memT']
                    for g in range(2):
                        sl = nxt
                        nxt = load_w(w_xkv_d[:, (g + 1) * 512:(g + 2) * 512])
                        fm_proj(sl, 4, mrhs, mkeys, 1, NMEM,
                                lambda cc, n, bk, g=g: evac_copy(kxT[:, g * 4 + cc, :], psB[:, bk, 0:NMEM], [f'psB{bk}'],
                                                                 ['kxT']), [0, 1, 2, 3])
                    for g in range(2):
                        sl = nxt
                        nxt = load_w(w_xkv_d[:, 1536:2048]) if g == 0 else load_w(w_xq_d[:, 0:512])
                        for mt in range(2):
                            bk = mt
                            for kc in range(8):
                                op('pe', I('matmul',
                                    psB[:, bk, :], memT[:, kc, mt * 128:(mt + 1) * 128], wr[sl][:, kc, :],
                                    start=(kc == 0), stop=(kc == 7)), r=['memT', f'wr{sl}'], w=[f'psB{bk}'])
                            evac_copy(vx[:, mt, g * 512:(g + 1) * 512], psB[:, bk, :], [f'psB{bk}'], ['vx'])
                    for t in range(NT):
                        norm_T(x_sb[:, t, :], f'x{t}', hn2[t % 2], f'hnb{t % 2}', 1,
                               hT[:, :, t * 128:(t + 1) * 128], [HK(c, t // 4) for c in range(8)], t, t % 2)
                    for g in range(2):
                        sl = nxt
                        nxt = load_w(w_xq_d[:, 512:1024]) if g == 0 else load_w(w_xo_d[:, 0:512])
                        fm_proj(sl, 4, hrhs, hkeys, 4, 512,
                                lambda cc, n, bk, g=g: evac_copy(qxT[:, g * 4 + cc, n * 512:(n + 1) * 512], psB[:, bk, :],
                                                                 [f'psB{bk}'], [f'qx{g * 4 + cc}_{n}'], scale=1.0 / 16),
                                [0, 1, 2, 3])
                    psS = psB[:, 0:2, :].rearrange("p a (h m) -> p (a h) m", h=2)
                    for n in range(4):
                        pT_t = pT[n % 2]
                        pTk = 'pT0'
                        for tt in range(4):
                            t = n * 4 + tt
                            ts_ = slice(t * 128, (t + 1) * 128)
                            pi = t % 2
                            for hx in range(4):
                                for dc in range(2):
                                    op('pe', I('matmul',
                                        psS[:, hx, :], qxT[:, 2 * hx + dc, ts_], kxT[:, 2 * hx + dc, :],
                                        start=(dc == 0), stop=(dc == 1)),
                                       r=[f'qx{2 * hx + dc}_{n}', 'kxT'], w=[f'psB{hx // 2}'])
                            op('dve', I('tensor_reduce', out=sm[:, 0, :], in_=psS, axis=AX.X, op=ALU.max),
                               r=['psB0', 'psB1'], w=['sm'])
                            op('dve', I('tensor_scalar', out=sm[:, 1, :], in0=sm[:, 0, :], scalar1=-1.0,
                                                                scalar2=None, op0=ALU.mult), r=['sm'], w=['sm'])
                            for hx in range(4):
                                op('act', I('activation',
                                    out=pf[pi][:, hx, :], in_=psS[:, hx, :], func=AF.Exp, bias=sm[:, 1, hx:hx + 1],
                                    accum_out=sm[:, 2, hx:hx + 1]),
                                   r=[f'psB{hx // 2}', 'sm'], w=['pf0', 'sm'])
                            op('dve', I('reciprocal', out=sm[:, 3, :], in_=sm[:, 2, :]), r=['sm'], w=['sm'])
                            for hx in range(4):
                                op('dve', I('tensor_scalar',
                                    out=pn[pi][:, hx, :], in0=pf[pi][:, hx, :], scalar1=sm[:, 3, hx:hx + 1],
                                    scalar2=None, op0=ALU.mult), r=['pf0', 'sm'], w=[f'pn{pi}'])
                            for hx in range(4):
                                for mc in range(2):
                                    op('pe', I('transpose',
                                        out=psA[:, pi, (hx * 2 + mc) * 128:(hx * 2 + mc + 1) * 128],
                                        in_=pn[pi][:, hx, mc * 128:(mc + 1) * 128], identity=ident[:]),
                                       r=[f'pn{pi}', 'ident'], w=[f'psA{pi}'])
                            op('act', I('activation',
                                out=pT_t[:, :, :, tt * 128:(tt + 1) * 128],
                                in_=psA[:, pi, :].rearrange("p (h m c) -> p h m c", h=4, m=2), func=AF.Copy),
                               r=[f'psA{pi}'], w=[pTk])
                        k = 0
                        for hx in range(4):
                            for dc in range(2):
                                bk = 2 + (k % 4)
                                k += 1
                                for mc in range(2):
                                    op('pe', I('matmul',
                                        psB[:, bk, :], vx[:, mc, hx * 256 + dc * 128:hx * 256 + (dc + 1) * 128],
                                        pT_t[:, hx, mc, :], start=(mc == 0), stop=(mc == 1)),
                                       r=['vx', pTk], w=[f'psB{bk}'])
                                evac_copy(hT[:, 2 * hx + dc, n * 512:(n + 1) * 512], psB[:, bk, :], [f'psB{bk}'],
                                          [HK(2 * hx + dc, n)])
                    for n in range(2):
                        sl = nxt
                        nxt = load_w(w_xo_d[:, 512:1024]) if n == 0 else None
                        ns = slice(n * 512, (n + 1) * 512)
                        for t in range(NT):
                            bk = t % 4
                            for cc in range(8):
                                op('pe', I('matmul',
                                    psB[:, bk, :], hT[:, cc, t * 128:(t + 1) * 128], wr[sl][:, cc, :],
                                    start=(cc == 0), stop=(cc == 7)), r=[HK(cc, t // 4), f'wr{sl}'], w=[f'psB{bk}'])
                            op('dve', I('tensor_tensor',
                                out=x_sb[:, t, ns], in0=psB[:, bk, :], in1=x_sb[:, t, ns], op=ALU.add),
                               r=[f'psB{bk}', f'x{t}'], w=[f'x{t}'])
                    S.barrier()
                if stage == 2:
                    for t in range(NT):
                        ev = op('sp', I('dma_start',
                            out=out_d[b * SEQ + t * 128:b * SEQ + (t + 1) * 128, :], in_=x_sb[:, t, :]),
                            r=[f'x{t}'], dma=f'o{t % 4}')
                    S.barrier()
                    continue

                with ExitStack() as s2g:
                    gbc = T(s2g, "gbc", [128, DM], F32)
                    h3 = [T(s2g, f"h3_{i}", [128, DM], F32) for i in range(2)]
                    h3b = [T(s2g, f"h3b_{i}", [128, DM], BF16) for i in range(2)]
                    h3T = T(s2g, "h3T", [128, 8, 128], F32)
                    lg = T(s2g, "lg", [128, 36], F32)
                    rw = T(s2g, "rw", [128, 16], F32)
                    gm = T(s2g, "gm", [128, 4], F32)
                    elm = T(s2g, "elm", [128, 4, 8], F32)
                    ig = T(s2g, "ig", [128, 4, 8], F32)
                    S32 = T(s2g, "S32", [128, 4, 8], F32)
                    S32b = T(s2g, "S32b", [128, 32], BF16)
                    M1 = T(s2g, "M1", [128, 4, 8], F32)
                    M2 = T(s2g, "M2", [128, 4, 8], F32)
                    pos = T(s2g, "pos", [128, 32], F32)
                    slf = T(s2g, "slf", [128, 2], F32)
                    op('sp', I('dma_start', out=gbc[:], in_=ln_ffn_g.partition_broadcast(128)), w=['gbc'],
                       dma='gbc')
                    for t in range(NT):
                        tg = b * NT + t
                        hi = t % 2
                        op('act', I('activation', out=junk[:], in_=x_sb[:, t, :], func=AF.Square,
                                                              accum_out=stat[:, 0, t:t + 1]),
                           r=[f'x{t}'], w=['junk', 'stat'])
                        rstd_from_ss(stat, t, 1.0 / DM)
                        op('dve', I('scalar_tensor_tensor',
                            out=h3[hi][:], in0=x_sb[:, t, :], scalar=stat[:, 2, t:t + 1], in1=gbc[:], op0=ALU.mult,
                            op1=ALU.mult), r=[f'x{t}', 'stat', 'gbc'], w=[f'h3_{hi}'])
                        op('pool', I('tensor_copy', out=h3b[hi][:], in_=h3[hi][:]), r=[f'h3_{hi}'],
                           w=[f'h3b_{hi}'])
                        for kc in range(8):
                            op('pe', I('transpose',
                                out=psB[:, 4 + kc // 4, (kc % 4) * 128:(kc % 4 + 1) * 128],
                                in_=h3[hi][:, kc * 128:(kc + 1) * 128], identity=identf[:]),
                               r=[f'h3_{hi}', 'identf'], w=[f'psB{4 + kc // 4}'])
                        op('act', I('activation', out=h3T[:, 0:4, :], in_=psB[:, 4, :].rearrange("p (k c) -> p k c", k=4),
                                                         func=AF.Copy), r=['psB4'], w=['h3T'])
                        op('dve', I('tensor_copy', out=h3T[:, 4:8, :], in_=psB[:, 5, :].rearrange("p (k c) -> p k c", k=4)),
                           r=['psB5'], w=['h3T'])
                        for kc in range(8):
                            op('pe', I('matmul', psB[:, 0, 0:36], h3T[:, kc, :], wrt[:, kc, :],
                                                               start=(kc == 0), stop=(kc == 7)),
                               r=['h3T', 'wrt'], w=['psB0'])
                        V = lambda f: op('dve', f, r=['rt'], w=['rt'])
                        op('dve', I('tensor_tensor', out=lg[:], in0=psB[:, 0, 0:36], in1=brt[:], op=ALU.add),
                           r=['psB0', 'brt', 'rt'], w=['rt'])
                        el = lg[:, 4:36].rearrange("p (g e) -> p g e", g=4)
                        V(I('tensor_reduce', out=rw[:, 0:1], in_=lg[:, 0:4], axis=AX.X, op=ALU.max))
                        V(I('tensor_scalar', out=rw[:, 1:2], in0=rw[:, 0:1], scalar1=-1.0, scalar2=None,
                                                    op0=ALU.mult))
                        op('act', I('activation', out=gm[:], in_=lg[:, 0:4], func=AF.Exp, bias=rw[:, 1:2],
                                                         accum_out=rw[:, 2:3]), r=['rt'], w=['rt'])
                        V(I('reciprocal', out=rw[:, 3:4], in_=rw[:, 2:3]))
                        V(I('tensor_scalar', out=gm[:], in0=lg[:, 0:4], scalar1=rw[:, 0:1], scalar2=None,
                                                    op0=ALU.is_equal))
                        V(I('tensor_tensor', out=elm[:], in0=el, in1=gm[:, :, None].to_broadcast([128, 4, 8]),
                                                    op=ALU.mult))
                        V(I('tensor_reduce', out=ig[:, 0, :], in_=elm[:].rearrange("p g e -> p e g"),
                                                    axis=AX.X, op=ALU.add))
                        V(I('tensor_reduce', out=rw[:, 4:5], in_=ig[:, 0, :], axis=AX.X, op=ALU.max))
                        V(I('tensor_scalar', out=ig[:, 1, :], in0=ig[:, 0, :], scalar1=rw[:, 4:5], scalar2=None,
                                                    op0=ALU.is_equal))
                        V(I('scalar_tensor_tensor', out=ig[:, 2, :], in0=ig[:, 1, :], scalar=-1e30,
                                                           in1=ig[:, 0, :], op0=ALU.mult, op1=ALU.add))
                        V(I('tensor_reduce', out=rw[:, 5:6], in_=ig[:, 2, :], axis=AX.X, op=ALU.max))
                        V(I('tensor_scalar', out=ig[:, 3, :], in0=ig[:, 2, :], scalar1=rw[:, 5:6], scalar2=None,
                                                    op0=ALU.is_equal))
                        V(I('tensor_tensor', out=rw[:, 6:7], in0=rw[:, 5:6], in1=rw[:, 4:5], op=ALU.subtract))
                        op('act', I('activation', out=rw[:, 7:8], in_=rw[:, 6:7], func=AF.Exp), r=['rt'], w=['rt'])
                        V(I('tensor_scalar', out=rw[:, 8:9], in0=rw[:, 7:8], scalar1=1.0, scalar2=None,
                                                    op0=ALU.add))
                        V(I('reciprocal', out=rw[:, 9:10], in_=rw[:, 8:9]))
                        op('dve', I('tensor_tensor', out=wts[:, tg, 0:1], in0=rw[:, 9:10], in1=rw[:, 3:4],
                                                                   op=ALU.mult), r=['rt'], w=['rt', 'wts'])
                        op('dve', I('tensor_tensor', out=wts[:, tg, 1:2], in0=rw[:, 7:8],
                                                                   in1=wts[:, tg, 0:1], op=ALU.mult),
                           r=['rt', 'wts'], w=['rt', 'wts'])
                        V(I('tensor_tensor', out=M1[:], in0=gm[:, :, None].to_broadcast([128, 4, 8]),
                                                    in1=ig[:, 1:2, :].to_broadcast([128, 4, 8]), op=ALU.mult))
                        V(I('tensor_tensor', out=M2[:], in0=gm[:, :, None].to_broadcast([128, 4, 8]),
                                                    in1=ig[:, 3:4, :].to_broadcast([128, 4, 8]), op=ALU.mult))
                        V(I('tensor_tensor', out=S32[:], in0=M1[:], in1=M2[:], op=ALU.add))
                        V(I('tensor_copy', out=S32b[:], in_=S32[:].rearrange("p g e -> p (g e)")))
                        op('pe', I('matmul', psB[:, 1, 0:32], Lmat[:], S32b[:], start=True, stop=True),
                           r=['Lmat', 'rt'], w=['psB1'])
                        op('pe', I('matmul', psB[:, 1, 32:64], onesb[:], S32b[:], start=True, stop=True),
                           r=['onesb', 'rt'], w=['psB1'])
                        op('dve', I('tensor_tensor', out=pos[:], in0=psB[:, 1, 0:32], in1=base[:], op=ALU.add),
                           r=['psB1', 'base', 'rt'], w=['rt'])
                        op('dve', I('tensor_tensor', out=base[:], in0=psB[:, 1, 32:64], in1=base[:], op=ALU.add),
                           r=['psB1', 'base'], w=['base'])
                        V(I('tensor_scalar', out=elm[:].rearrange("p g e -> p (g e)"), in0=pos[:],
                                                    scalar1=float(CAP), scalar2=1e7, op0=ALU.is_ge, op1=ALU.mult))
                        V(I('tensor_tensor', out=pos[:], in0=pos[:], in1=elm[:].rearrange("p g e -> p (g e)"),
                                                    op=ALU.add))
                        V(I('tensor_tensor', out=pos[:], in0=pos[:], in1=ecap[:], op=ALU.add))
                        V(I('tensor_tensor', out=elm[:].rearrange("p g e -> p (g e)"), in0=pos[:],
                                                    in1=M1[:].rearrange("p g e -> p (g e)"), op=ALU.mult))
                        V(I('tensor_reduce', out=slf[:, 0:1], in_=elm[:].rearrange("p g e -> p (g e)"), axis=AX.X,
                                                    op=ALU.add))
                        V(I('tensor_tensor', out=elm[:].rearrange("p g e -> p (g e)"), in0=pos[:],
                                                    in1=M2[:].rearrange("p g e -> p (g e)"), op=ALU.mult))
                        V(I('tensor_reduce', out=slf[:, 1:2], in_=elm[:].rearrange("p g e -> p (g e)"), axis=AX.X,
                                                    op=ALU.add))
                        op('dve', I('tensor_copy', out=slots[:, tg, :], in_=slf[:]), r=['rt'],
                           w=['rt', f'slots{tg}'])
                        for k2 in range(2):
                            op('pool', I('indirect_dma_start',
                                out=xd_d[:, :], out_offset=bass.IndirectOffsetOnAxis(ap=slots[:, tg, k2:k2 + 1], axis=0),
                                in_=h3b[hi][:], in_offset=None, bounds_check='REG', oob_is_err=False),
                               r=[f'slots{tg}', f'h3b_{hi}'], w=[f'xd{tg}_{k2}'], dma=f'sc{hi}')
                        op('sp', I('dma_start',
                            out=xres_d[b * SEQ + t * 128:b * SEQ + (t + 1) * 128, :], in_=x_sb[:, t, :]),
                            r=[f'x{t}'], w=[f'xres{t}'], dma=f'xw{t % 4}')
                    S.barrier()

        if stage >= 3:
            with ExitStack() as s3:
                wg = [T(s3, f"wg{i}", [128, 8, 512], BF16) for i in range(2)]
                wu = [T(s3, f"wu{i}", [128, 8, 512], BF16) for i in range(2)]
                wd = [T(s3, f"wd{i}", [128, 4, DM], BF16) for i in range(2)]
                xr = [T(s3, f"xr{i}", [128, DM], BF16) for i in range(3)]
                xT = [T(s3, f"xT{i}", [128, 8, 384], BF16) for i in range(2)]
                sg = [T(s3, f"sg{i}", [128, 384], F32) for i in range(2)]
                h1T = [T(s3, f"h1T{i}", [128, 4, 384], BF16) for i in range(2)]
                ydt = [T(s3, f"ydt{i}", [128, DM], F32) for i in range(2)]

                wst = [T(s3, f"wst{i}", [128, 8, 512], F32) for i in range(3)]

                def load_dma(e_):
                    op('sp', I('dma_start', out=wst[0][:], in_=w_gate_d[e_].rearrange("(k p) n -> p k n", p=128)),
                       w=['wst0'], dma='wst0')
                    op('sp', I('dma_start', out=wst[1][:], in_=w_up_d[e_].rearrange("(k p) n -> p k n", p=128)),
                       w=['wst1'], dma='wst1')
                    op('sp', I('dma_start', out=wst[2][:].rearrange("p (a b) n -> p a (b n)", a=4),
                               in_=w_down_d[e_].rearrange("(k p) n -> p k n", p=128)), w=['wst2'], dma='wst2')

                def cast_gu(e_):
                    i = e_ % 2
                    op('pool', I('tensor_copy', out=wg[i][:], in_=wst[0][:]), r=['wst0'], w=[f'wg{i}'])
                    op('pool', I('tensor_copy', out=wu[i][:], in_=wst[1][:]), r=['wst1'], w=[f'wu{i}'])

                def cast_d(e_):
                    i = e_ % 2
                    op('act', I('activation', out=wd[i][:], in_=wst[2][:].rearrange("p (a b) n -> p a (b n)", a=4),
                                func=AF.Copy), r=['wst2'], w=[f'wd{i}'])

                def load_expert(e_):
                    load_dma(e_)
                    cast_gu(e_)
                    cast_d(e_)

                load_expert(0)
                cnt = 0
                yc = 0
                for e_ in range(NEXP):
                    wi = e_ % 2
                    if e_ + 1 < NEXP:
                        load_dma(e_ + 1)
                    for half in range(2):
                        if half == 1 and e_ + 1 < NEXP:
                            cast_gu(e_ + 1)
                        r0 = e_ * CAP + half * 384
                        hb = cnt % 2
                        cnt += 1
                        for ci in range(3):
                            xi = (cnt * 3 + ci) % 3
                            op('sp', I('dma_start',
                                out=xr[xi][:], in_=xd_d[r0 + ci * 128:r0 + (ci + 1) * 128, :]),
                                r=['xd'], w=[f'xr{xi}'], dma=f'xr{xi}')
                            pa = ci % 2
                            for kc in range(8):
                                op('pe', I('transpose',
                                    out=psA[:, pa, kc * 128:(kc + 1) * 128], in_=xr[xi][:, kc * 128:(kc + 1) * 128],
                                    identity=ident[:]), r=[f'xr{xi}', 'ident'], w=[f'psA{pa}'])
                            evac_copy(xT[hb][:, :, ci * 128:(ci + 1) * 128],
                                      psA[:, pa, :].rearrange("p (k c) -> p k c", k=8), [f'psA{pa}'], [f'xT{hb}'])
                        for dc in range(4):
                            bg, bu = (dc % 2) * 2, (dc % 2) * 2 + 1
                            for kc in range(8):
                                op('pe', I('matmul',
                                    psB[:, bg, 0:384], wg[wi][:, kc, dc * 128:(dc + 1) * 128], xT[hb][:, kc, :],
                                    start=(kc == 0), stop=(kc == 7)), r=[f'wg{wi}', f'xT{hb}'], w=[f'psB{bg}'])
                            for kc in range(8):
                                op('pe', I('matmul',
                                    psB[:, bu, 0:384], wu[wi][:, kc, dc * 128:(dc + 1) * 128], xT[hb][:, kc, :],
                                    start=(kc == 0), stop=(kc == 7)), r=[f'wu{wi}', f'xT{hb}'], w=[f'psB{bu}'])
                            si = dc % 2
                            op('act', I('activation', out=sg[si][:], in_=psB[:, bg, 0:384],
                                                                           func=AF.Silu),
                               r=[f'psB{bg}'], w=[f'sg{si}'])
                            op('dve', I('tensor_tensor',
                                out=h1T[hb][:, dc, :], in0=psB[:, bu, 0:384], in1=sg[si][:], op=ALU.mult),
                               r=[f'psB{bu}', f'sg{si}'], w=[f'h1T{hb}_{dc}'])
                        for ci in range(3):
                            yi = yc % 2
                            yc += 1
                            for nn in range(2):
                                for dc in range(4):
                                    op('pe', I('matmul',
                                        psB[:, 4 + nn, :], h1T[hb][:, dc, ci * 128:(ci + 1) * 128],
                                        wd[wi][:, dc, nn * 512:(nn + 1) * 512], start=(dc == 0), stop=(dc == 3)),
                                       r=[f'h1T{hb}_{dc}', f'wd{wi}'], w=[f'psB{4 + nn}'])
                            op('act', I('activation', out=ydt[yi][:, 0:512], in_=psB[:, 4, :], func=AF.Copy),
                               r=['psB4'], w=[f'ydt{yi}a'])
                            op('dve', I('tensor_copy', out=ydt[yi][:, 512:1024], in_=psB[:, 5, :]),
                               r=['psB5'], w=[f'ydt{yi}b'])
                            op('sp', I('dma_start',
                                out=yd_d[r0 + ci * 128:r0 + (ci + 1) * 128, :], in_=ydt[yi][:]),
                                r=[f'ydt{yi}a', f'ydt{yi}b'], w=[f'yd{yc}'], dma=f'yo{yi}')
                    if e_ + 1 < NEXP:
                        cast_d(e_ + 1)
                S.barrier()

            with ExitStack() as s4:
                gbf = T(s4, "gbf", [128, DM], F32)
                y1 = [T(s4, f"y1_{i}", [128, DM], F32) for i in range(2)]
                y2 = [T(s4, f"y2_{i}", [128, DM], F32) for i in range(2)]
                xf = [T(s4, f"xf{i}", [128, DM], F32) for i in range(2)]
                of = [T(s4, f"of{i}", [128, DM], F32) for i in range(2)]
                fst = T(s4, "fst", [128, 3, nseq * NT], F32)
                zt = T(s4, "zt", [128, DM], F32)
                op('pool', I('memset', zt[:], 0.0), w=['zt'])
                op('sp', I('dma_start', out=gbf[:], in_=ln_final_g.partition_broadcast(128)), w=['gbf'], dma='gbf')
                for tg in range(nseq * NT):
                    i = tg % 2
                    for ybuf, k2, nm in ((y1, 0, 'y1'), (y2, 1, 'y2')):
                        op('act', I('activation', out=ybuf[i][:], in_=zt[:], func=AF.Copy), r=['zt'], w=[f'{nm}_{i}'])
                        op('pool', I('indirect_dma_start',
                            out=ybuf[i][:], out_offset=None, in_=yd_d[:, :],
                            in_offset=bass.IndirectOffsetOnAxis(ap=slots[:, tg, k2:k2 + 1], axis=0),
                            bounds_check='REG', oob_is_err=False),
                           r=['yd'], w=[f'{nm}_{i}'], dma=f'{nm}_{i}')
                    op('sp', I('dma_start', out=xf[i][:], in_=xres_d[tg * 128:(tg + 1) * 128, :]),
                       r=['xres'], w=[f'xf{i}'], dma=f'xf{i}')
                    op('dve', I('scalar_tensor_tensor',
                        out=xf[i][:], in0=y1[i][:], scalar=wts[:, tg, 0:1], in1=xf[i][:], op0=ALU.mult, op1=ALU.add),
                       r=[f'y1_{i}', f'xf{i}'], w=[f'xf{i}'])
                    op('dve', I('scalar_tensor_tensor',
                        out=xf[i][:], in0=y2[i][:], scalar=wts[:, tg, 1:2], in1=xf[i][:], op0=ALU.mult, op1=ALU.add),
                       r=[f'y2_{i}', f'xf{i}'], w=[f'xf{i}'])
                    op('act', I('activation', out=junk[:], in_=xf[i][:], func=AF.Square,
                                                                 accum_out=fst[:, 0, tg:tg + 1]),
                       r=[f'xf{i}'], w=['junk', 'stat'])
                    rstd_from_ss(fst, tg, 1.0 / DM)
                    op('dve', I('scalar_tensor_tensor',
                        out=of[i][:], in0=xf[i][:], scalar=fst[:, 2, tg:tg + 1], in1=gbf[:], op0=ALU.mult, op1=ALU.mult),
                       r=[f'xf{i}', 'stat', 'gbf'], w=[f'of{i}'])
                    op('sp', I('dma_start', out=out_d[tg * 128:(tg + 1) * 128, :], in_=of[i][:]),
                       r=[f'of{i}'], dma=f'of{i}')
                S.barrier()
        S.barrier()
        S.emit()
    return nc


_NC_CACHE = {}


def _prep(inputs, c, nseq=NSEQ):
    f = lambda a: np.ascontiguousarray(np.asarray(a, dtype=np.float32))
    d = {
        "x": f(inputs["x"][c * nseq:(c + 1) * nseq]),
        "mem": f(inputs["mem"][c * nseq:(c + 1) * nseq]),
        "ln_mix_g": f(inputs["ln_mix_g"][0]),
        "w_in": f(inputs["w_in"][0]),
        "sb_out_g": f(inputs["sb_out_g"][0]),
        "conv_w": f(inputs["conv_w"][0]),
        "conv_b": f(inputs["conv_b"][0]),
        "conv_ln_g": f(inputs["conv_ln_g"][0]),
        "conv_ln_b": f(inputs["conv_ln_b"][0]),
        "w_out": f(inputs["w_out"][0]),
        "ln_mem_x_g": f(inputs["ln_mem_x_g"][0]),
        "ln_mem_g": f(inputs["ln_mem_g"][0]),
        "w_xq": f(inputs["w_xq"][0]),
        "w_xkv": f(inputs["w_xkv"][0]),
        "w_xo": f(inputs["w_xo"][0]),
        "ln_ffn_g": f(inputs["ln_ffn_g"][0]),
        "w_rt": f(np.concatenate([np.asarray(inputs["w_group"][0]), np.asarray(inputs["w_er"][0]).reshape(DM, 32)], axis=1)),
        "b_rt": f(np.concatenate([np.asarray(inputs["b_group"][0]), np.asarray(inputs["b_er"][0]).reshape(32)])),
        "w_gate": f(inputs["w_gate"][0]),
        "w_up": f(inputs["w_up"][0]),
        "w_down": f(inputs["w_down"][0]),
        "ln_final_g": f(inputs["ln_final_g"]),
    }
    return d


def kernel(**inputs):
    if 'nc' not in _NC_CACHE:
        _NC_CACHE['nc'] = build()
    nc = _NC_CACHE['nc']
    in_maps = [_prep(inputs, c) for c in range(N_CORES)]
    res = run_bass_kernel_spmd(nc, in_maps, core_ids=list(range(N_CORES)))
    out = np.concatenate([np.asarray(r["out"]).reshape(NSEQ, SEQ, DM) for r in res.results], axis=0)
    return out.astype(np.float32)
```

```python
import numpy as np
import concourse.bass as bass
import concourse.mybir as mybir
from concourse.bass_utils import run_bass_kernel_spmd
from contextlib import ExitStack

F32 = mybir.dt.float32
F32R = mybir.dt.float32r
BF16 = mybir.dt.bfloat16
I32 = mybir.dt.int32
AF = mybir.ActivationFunctionType
ALU = mybir.AluOpType
AX = mybir.AxisListType

ENG = ['pe', 'act', 'dve', 'pool', 'sp']
SAME_ENGINE_SYNC = True

N_CORES = 8
NSEQ = 4
SEQ = 2048
DM = 1024
NT = SEQ // 128
NMEM = 256
NEXP = 32
CAP = 768
NROWS = NEXP * CAP
EPS = 1e-6


def I(name, *args, **kw):
    return (name, args, kw)


class Sched:
    def __init__(self, nc, es):
        self.nc = nc
        self.es = es
        self.ins = {e: [] for e in ENG}
        self.cnt = {e: 0 for e in ENG}
        self.sem = {e: es.enter_context(nc.semaphore('s_' + e)) for e in ENG}
        self.dsem = {}
        self.lastw = {}
        self.readers = {}
        self.waited = {e: {} for e in ENG}

    def _semof(self, key):
        return self.sem[key[1]] if key[0] == 'e' else self.dsem[key[1]][0]

    def op(self, eng, fn, r=(), w=(), dma=None, extra=()):
        deps = {}

        def need(ev):
            if ev is None:
                return
            key, val, src, is_dma = ev
            if (not is_dma) and src == eng and (eng == 'pe' or not SAME_ENGINE_SYNC):
                return
            if deps.get(key, 0) < val:
                deps[key] = val

        for k in r:
            need(self.lastw.get(k))
        for k in w:
            need(self.lastw.get(k))
            for ev in self.readers.get(k, {}).values():
                need(ev)
        for ev in extra:
            need(ev)
        waits = []
        wd = self.waited[eng]
        for key, val in deps.items():
            if wd.get(key, 0) >= val:
                continue
            wd[key] = val
            waits.append((key, val))
        if dma is not None:
            if dma not in self.dsem:
                self.dsem[dma] = [self.es.enter_context(self.nc.semaphore('d_' + dma)), 0]
            self.dsem[dma][1] += 16
            ev = (('d', dma), self.dsem[dma][1], eng, True)
        else:
            self.cnt[eng] += 1
            ev = (('e', eng), self.cnt[eng], eng, False)
        self.ins[eng].append((waits, fn, ev))
        for k in r:
            d = self.readers.setdefault(k, {})
            old = d.get(ev[0])
            if old is None or old[1] < ev[1]:
                d[ev[0]] = ev
        for k in w:
            self.lastw[k] = ev
            self.readers[k] = {}
        return ev

    def barrier(self):
        evs = []
        for e in ENG:
            if self.cnt[e] > 0:
                evs.append((('e', e), self.cnt[e], e, False))
        for slot, (s, c) in self.dsem.items():
            if c > 0:
                evs.append((('d', slot), c, 'sp', True))
        for e in ENG:
            self.op(e, I('nop'), extra=[ev for ev in evs if not (ev[2] == e and not ev[3])])
        self.lastw = {}
        self.readers = {}

    def emit(self):
        nc = self.nc
        with nc.Block() as block:
            def body(name):
                def f(e):
                    bc_reg = None
                    if name == 'pool':
                        bc_reg = e.alloc_register()
                        e.reg_mov(bc_reg, NROWS - 1)
                    for waits, fn, ev in self.ins[name]:
                        for key, val in waits:
                            e.wait_ge(self._semof(key), val)
                        try:
                            kw = fn[2]
                            if kw.get('bounds_check', None) == 'REG':
                                kw = dict(kw, bounds_check=bc_reg)
                            ins = getattr(e, fn[0])(*fn[1], **kw)
                        except Exception:
                            print("EMIT FAIL", name, fn[0], fn[1], fn[2])
                            raise
                        key, val, _, is_dma = ev
                        ins.then_inc(self._semof(key), 16 if is_dma else 1)
                return f
            block.tensor(body('pe'))
            block.scalar(body('act'))
            block.vector(body('dve'))
            block.gpsimd(body('pool'))
            block.sync(body('sp'))


def build(nseq=NSEQ, stage=99):
    nc = bass.Bass('TRN2', target_bir_lowering=False)
    ntok = nseq * SEQ

    def din(name, shape, dt=F32):
        return nc.dram_tensor(name, list(shape), dt, kind="ExternalInput").ap()

    x_d = din("x", [nseq, SEQ, DM])
    mem_d = din("mem", [nseq, NMEM, DM])
    ln_mix_g = din("ln_mix_g", [DM])
    w_in_d = din("w_in", [DM, 2560])
    sb_out_g = din("sb_out_g", [512])
    conv_w_d = din("conv_w", [31, 512])
    conv_b_d = din("conv_b", [512])
    conv_ln_g = din("conv_ln_g", [512])
    conv_ln_b = din("conv_ln_b", [512])
    w_out_d = din("w_out", [DM, DM])
    ln_mem_x_g = din("ln_mem_x_g", [DM])
    ln_mem_g = din("ln_mem_g", [DM])
    w_xq_d = din("w_xq", [DM, DM])
    w_xkv_d = din("w_xkv", [DM, 2 * DM])
    w_xo_d = din("w_xo", [DM, DM])
    ln_ffn_g = din("ln_ffn_g", [DM])
    w_rt_d = din("w_rt", [DM, 36])
    b_rt_d = din("b_rt", [36])
    w_gate_d = din("w_gate", [NEXP, DM, 512])
    w_up_d = din("w_up", [NEXP, DM, 512])
    w_down_d = din("w_down", [NEXP, 512, DM])
    ln_final_g = din("ln_final_g", [DM])
    out_d = nc.dram_tensor("out", [ntok, DM], F32, kind="ExternalOutput").ap()
    if stage == 1:
        dbg_mix = nc.dram_tensor("dbg_mix", [128, 8, SEQ], BF16, kind="ExternalOutput").ap()
        dbg_qk = nc.dram_tensor("dbg_qk", [128, 8, SEQ], BF16, kind="ExternalOutput").ap()
        dbg_v = nc.dram_tensor("dbg_v", [128, NT, 512], BF16, kind="ExternalOutput").ap()
        dbg_rsb = nc.dram_tensor("dbg_rsb", [128, 3, NT], F32, kind="ExternalOutput").ap()
    xd_d = nc.dram_tensor("xd_scr", [NROWS, DM], BF16).ap()
    yd_d = nc.dram_tensor("yd_scr", [NROWS, DM], F32).ap()
    xres_d = nc.dram_tensor("xres_scr", [ntok, DM], F32).ap()

    with ExitStack() as es:
        S = Sched(nc, es)
        op = S.op

        uid = [0]

        def T(scope, name, shape, dt):
            uid[0] += 1
            return scope.enter_context(nc.sbuf_tensor(f"{name}_u{uid[0]}", shape, dt))

        psA = es.enter_context(nc.psum_tensor("psA", [128, 2, 1024], BF16))
        psB = es.enter_context(nc.psum_tensor("psB", [128, 6, 512], F32))

        identf = T(es, "identf", [128, 128], F32)
        ident = T(es, "ident", [128, 128], BF16)
        negtri = T(es, "negtri", [128, 128], F32)
        negones = T(es, "negones", [128, 128], F32)
        negtriR = T(es, "negtriR", [128, 128], F32)
        negonesR = T(es, "negonesR", [128, 128], F32)
        meanmat = T(es, "meanmat", [128, 128], F32)
        ones2 = T(es, "ones2", [128, 2], F32)
        onesb = T(es, "onesb", [128, 128], BF16)
        Lmat = T(es, "Lmat", [128, 128], BF16)
        maskf = T(es, "maskf", [128, 4, 512], BF16)
        ecap = T(es, "ecap", [128, 32], F32)
        ecap_i = T(es, "ecap_i", [128, 32], I32)
        gcols = T(es, "gcols", [128, 3, 8], F32)
        sbg = T(es, "sbg", [128, 4], F32)
        cvb = T(es, "cvb", [128, 4], F32)
        cvg = T(es, "cvg", [128, 4], F32)
        cvbb = T(es, "cvbb", [128, 4], F32)
        cwT = T(es, "cwT", [128, 4, 31], F32)
        hT = T(es, "hT", [128, 8, SEQ], BF16)
        wr = [T(es, f"wr{i}", [128, 8, 512], BF16) for i in range(2)]
        stat = T(es, "stat", [128, 3, NT], F32)
        rsb = T(es, "rsb", [128, 3, NT], F32)
        slots = T(es, "slots", [128, nseq * NT, 2], I32)
        wts = T(es, "wts", [128, nseq * NT, 2], F32)
        base = T(es, "base", [128, 32], F32)
        wrt = T(es, "wrt", [128, 8, 36], F32)
        brt = T(es, "brt", [128, 36], F32)
        junk = T(es, "junk", [128, 1024], BF16)

        def HK(c, n):
            return f"H{c}_{n}"

        def small_col(dst_ap, src_ap, k, key):
            op('sp', I('dma_start', out=dst_ap, in_=src_ap.rearrange("(k p) -> p k", p=128),
                                           allow_slow_non_contiguous=True), w=[key], dma='c_' + key)

        small_col(gcols[:, 0, :], ln_mix_g, 8, 'gc0')
        small_col(gcols[:, 1, :], ln_mem_x_g, 8, 'gc1')
        small_col(gcols[:, 2, :], ln_mem_g, 8, 'gc2')
        small_col(sbg[:], sb_out_g, 4, 'sbg')
        small_col(cvb[:], conv_b_d, 4, 'cvb')
        small_col(cvg[:], conv_ln_g, 4, 'cvg')
        small_col(cvbb[:], conv_ln_b, 4, 'cvbb')
        op('sp', I('dma_start', out=wrt[:], in_=w_rt_d.rearrange("(k p) n -> p k n", p=128)),
           w=['wrt'], dma='c_wrt')
        op('sp', I('dma_start', out=brt[:], in_=b_rt_d.partition_broadcast(128)), w=['brt'], dma='c_brt')

        op('pool', I('memset', identf[:], 0.0), w=['identf'])
        op('pool', I('affine_select', out=identf[:], in_=identf[:], pattern=[[-1, 128]],
                                             compare_op=ALU.not_equal, fill=1.0, base=0, channel_multiplier=1),
           r=['identf'], w=['identf'])
        op('dve', I('tensor_copy', out=ident[:], in_=identf[:]), r=['identf'], w=['ident'])
        op('pool', I('memset', negtri[:], -1.0), w=['negtri'])
        op('pool', I('affine_select', out=negtri[:], in_=negtri[:], pattern=[[-1, 128]],
                                             compare_op=ALU.is_ge, fill=0.0, base=0, channel_multiplier=1),
           r=['negtri'], w=['negtri'])
        op('pool', I('memset', negones[:], -1.0), w=['negones'])
        op('dve', I('tensor_copy', out=negtriR[:].bitcast(F32R), in_=negtri[:]), r=['negtri'], w=['negtriR'])
        op('dve', I('tensor_copy', out=negonesR[:].bitcast(F32R), in_=negones[:]), r=['negones'], w=['negonesR'])
        op('pool', I('memset', meanmat[:], 1.0 / 512), w=['meanmat'])
        op('pool', I('memset', ones2[:], 1.0), w=['ones2'])
        op('pool', I('memset', onesb[:], 1.0), w=['onesb'])
        op('pool', I('memset', Lmat[:], 1.0), w=['Lmat'])
        op('pool', I('affine_select', out=Lmat[:], in_=Lmat[:], pattern=[[1, 128]],
                                             compare_op=ALU.is_gt, fill=0.0, base=0, channel_multiplier=-1),
           r=['Lmat'], w=['Lmat'])
        op('pool', I('memset', maskf[:], 1.0), w=['maskf'])
        for r_ in range(4):
            op('pool', I('affine_select', out=maskf[:, r_, :], in_=maskf[:, r_, :], pattern=[[1, 512]],
                                                        compare_op=ALU.is_gt, fill=0.0, base=-r_ * 128,
                                                        channel_multiplier=-1),
               r=['maskf'], w=['maskf'])
        op('pool', I('iota', ecap_i[:], pattern=[[CAP, 32]], base=0, channel_multiplier=0), w=['ecap_i'])
        op('dve', I('tensor_copy', out=ecap[:], in_=ecap_i[:]), r=['ecap_i'], w=['ecap'])
        op('pool', I('memset', base[:], 0.0), w=['base'])

        with ExitStack() as s0:
            cw_in = T(s0, "cw_in", [31, 512], F32)
            op('sp', I('dma_start', out=cw_in[:], in_=conv_w_d), w=['cw_in'], dma='c_cw')
            for c in range(4):
                op('pe', I('transpose', out=psB[:, 0, c * 32:c * 32 + 31], in_=cw_in[:, c * 128:(c + 1) * 128],
                                                    identity=identf[0:31, 0:31]),
                   r=['cw_in', 'identf'], w=['psB0'])
            op('act', I('activation', out=cwT[:], in_=psB[:, 0, 0:128].rearrange("p (c w) -> p c w", c=4)[:, :, 0:31],
                                             func=AF.Copy), r=['psB0'], w=['cwT'])
            S.barrier()

        wslot = [0]

        def load_w(src2d, nk=8):
            i = wslot[0]
            wslot[0] ^= 1
            op('pool', I('dma_start', out=wr[i][:, 0:nk, :], in_=src2d.rearrange("(k p) n -> p k n", p=128)),
               w=[f'wr{i}'], dma=f'wr{i}')
            return i

        evac_flip = [0]

        def evac_copy(out_ap, in_ap, r, w, scale=None, eng=None):
            if eng is None:
                eng = 'act' if (evac_flip[0] & 1) == 0 else 'dve'
                evac_flip[0] += 1
            if eng == 'act':
                if scale is None:
                    op('act', I('activation', out=out_ap, in_=in_ap, func=AF.Copy), r=r, w=w)
                else:
                    op('act', I('activation', out=out_ap, in_=in_ap, func=AF.Copy, scale=scale), r=r, w=w)
            else:
                if scale is None:
                    op('dve', I('tensor_copy', out=out_ap, in_=in_ap), r=r, w=w)
                else:
                    op('dve', I('tensor_scalar', out=out_ap, in0=in_ap, scalar1=scale, scalar2=None,
                                                        op0=ALU.mult), r=r, w=w)

        def rstd_from_ss(st, col, inv_n):
            op('act', I('activation', out=st[:, 1, col:col + 1], in_=st[:, 0, col:col + 1], func=AF.Sqrt,
                                             scale=inv_n, bias=EPS), r=['stat'], w=['stat'])
            op('dve', I('reciprocal', out=st[:, 2, col:col + 1], in_=st[:, 1, col:col + 1]),
               r=['stat'], w=['stat'])

        def norm_T(src_ap, src_key, hn_t, hn_key, gi, dst3, dst_keys, t, pa):
            op('act', I('activation', out=junk[:], in_=src_ap, func=AF.Square, accum_out=stat[:, 0, t:t + 1]),
               r=[src_key], w=['junk', 'stat'])
            rstd_from_ss(stat, t, 1.0 / DM)
            op('act', I('activation', out=hn_t[:], in_=src_ap, func=AF.Copy, scale=stat[:, 2, t:t + 1]),
               r=[src_key, 'stat'], w=[hn_key])
            for kc in range(8):
                op('pe', I('transpose', out=psA[:, pa, kc * 128:(kc + 1) * 128],
                                                      in_=hn_t[:, kc * 128:(kc + 1) * 128], identity=ident[:]),
                   r=[hn_key, 'ident'], w=[f'psA{pa}'])
            op('dve', I('tensor_tensor', out=dst3, in0=psA[:, pa, :].rearrange("p (k c) -> p k c", k=8),
                                                in1=gcols[:, gi, :, None].to_broadcast([128, 8, 128]), op=ALU.mult),
               r=[f'psA{pa}', f'gc{gi}'], w=dst_keys)

        def fm_proj(slot, ncc, rhs_fn, rhs_keys_fn, nN, N, evac_fn, banks):
            k = 0
            for cc in range(ncc):
                for n in range(nN):
                    bk = banks[k % len(banks)]
                    k += 1
                    for kc in range(8):
                        op('pe', I('matmul',
                            psB[:, bk, 0:N], wr[slot][:, kc, cc * 128:(cc + 1) * 128], rhs_fn(kc, n),
                            start=(kc == 0), stop=(kc == 7)),
                           r=[f'wr{slot}'] + rhs_keys_fn(kc, n), w=[f'psB{bk}'])
                    evac_fn(cc, n, bk)

        dbg = {}

        for b in range(nseq):
            with ExitStack() as s1:
                qk = T(s1, "qk", [128, 8, SEQ], BF16)
                v_sb = T(s1, "v_sb", [128, NT, 512], BF16)
                with ExitStack() as s1a:
                    xin = [T(s1a, f"xin{i}", [128, DM], F32) for i in range(3)]
                    hn = [T(s1a, f"hn{i}", [128, DM], BF16) for i in range(2)]
                    gT = T(s1a, "gT", [128, 4, 30 + SEQ], BF16)
                    ycv = T(s1a, "ycv", [128, 4, SEQ], F32)
                    dg = T(s1a, "dg", [128, 31, 128], BF16)
                    ysq = T(s1a, "ysq", [128, 4, 512], F32)
                    lnw = T(s1a, "lnw", [128, 4, 512], F32)

                    for t in range(NT):
                        xi = xin[t % 3]
                        op('sp', I('dma_start', out=xi[:], in_=x_d[b, t * 128:(t + 1) * 128, :]),
                           w=[f'xin{t % 3}'], dma=f'xin{t % 3}')
                        norm_T(xi[:], f'xin{t % 3}', hn[t % 2], f'hn{t % 2}', 0,
                               hT[:, :, t * 128:(t + 1) * 128], [HK(c, t // 4) for c in range(8)], t, t % 2)

                    hrhs = lambda kc, n: hT[:, kc, n * 512:(n + 1) * 512]
                    hkeys = lambda kc, n: [HK(kc, n)]
                    sl = load_w(w_in_d[:, 0:512])
                    nxt = load_w(w_in_d[:, 512:1024])
                    fm_proj(sl, 4, hrhs, hkeys, 4, 512,
                            lambda cc, n, bk: evac_copy(qk[:, cc, n * 512:(n + 1) * 512], psB[:, bk, :], [f'psB{bk}'],
                                                        [f'q{cc}_{n}'], scale=0.125), [0, 1, 2, 3])
                    sl = nxt
                    nxt = load_w(w_in_d[:, 1024:1536])
                    fm_proj(sl, 4, hrhs, hkeys, 4, 512,
                            lambda cc, n, bk: evac_copy(qk[:, 4 + cc, n * 512:(n + 1) * 512], psB[:, bk, :],
                                                        [f'psB{bk}'], [f'k{cc}_{n}']), [0, 1, 2, 3])
                    sl = nxt
                    nxt = load_w(w_in_d[:, 2048:2560])
                    for t in range(NT):
                        bk = t % 4
                        for kc in range(8):
                            op('pe', I('matmul',
                                psB[:, bk, :], hT[:, kc, t * 128:(t + 1) * 128], wr[sl][:, kc, :],
                                start=(kc == 0), stop=(kc == 7)),
                               r=[f'wr{sl}', HK(kc, t // 4)], w=[f'psB{bk}'])
                        evac_copy(v_sb[:, t, :], psB[:, bk, :], [f'psB{bk}'], [f'v{t}'])
                    op('pool', I('memset', gT[:, :, 0:30], 0.0), w=['gTpad'])
                    sl = nxt
                    nxt = load_w(w_in_d[:, 1536:2048])
                    fm_proj(sl, 4, hrhs, hkeys, 4, 512,
                            lambda cc, n, bk: op('act', I('activation',
                                out=gT[:, cc, 30 + n * 512:30 + (n + 1) * 512], in_=psB[:, bk, :], func=AF.Sigmoid),
                                r=[f'psB{bk}'], w=[f'g{cc}_{n}']), [0, 1, 2, 3])
                    sl = nxt
                    fm_proj(sl, 4, hrhs, hkeys, 4, 512,
                            lambda cc, n, bk: op('dve', I('tensor_tensor',
                                out=gT[:, cc, 30 + n * 512:30 + (n + 1) * 512], in0=psB[:, bk, :],
                                in1=gT[:, cc, 30 + n * 512:30 + (n + 1) * 512], op=ALU.mult),
                                r=[f'psB{bk}', f'g{cc}_{n}'], w=[f'g{cc}_{n}']), [0, 1, 2, 3])

                    for c in range(4):
                        for w_ in range(31):
                            op('dve', I('tensor_scalar',
                                out=dg[:, w_, :], in0=ident[:], scalar1=cwT[:, c, w_:w_ + 1], scalar2=None,
                                op0=ALU.mult), r=['ident', 'cwT'], w=[f'dg{w_}'])
                        for n in range(4):
                            bk = n % 4
                            gkeys = [f'g{c}_{n}'] + ([f'g{c}_{n - 1}'] if n > 0 else ['gTpad'])
                            for w_ in range(31):
                                op('pe', I('matmul',
                                    psB[:, bk, :], dg[:, w_, :], gT[:, c, n * 512 + w_:n * 512 + w_ + 512],
                                    start=(w_ == 0), stop=(w_ == 30)),
                                   r=[f'dg{w_}'] + gkeys, w=[f'psB{bk}'])
                            op('act', I('activation',
                                out=ycv[:, c, n * 512:(n + 1) * 512], in_=psB[:, bk, :], func=AF.Identity,
                                bias=cvb[:, c:c + 1]), r=[f'psB{bk}', 'cvb'], w=[f'y{c}_{n}'])
                    for n in range(4):
                        ns = slice(n * 512, (n + 1) * 512)
                        for c in range(4):
                            op('pe', I('matmul', psB[:, 4, :], meanmat[:], ycv[:, c, ns],
                                                                    start=(c == 0), stop=(c == 3)),
                               r=['meanmat', f'y{c}_{n}'], w=['psB4'])
                        for c in range(4):
                            op('act', I('activation', out=ysq[:, c, :], in_=ycv[:, c, ns],
                                                                         func=AF.Square),
                               r=[f'y{c}_{n}'], w=[f'ysq{c}'])
                        for c in range(4):
                            op('pe', I('matmul', psB[:, 5, :], meanmat[:], ysq[:, c, :],
                                                             start=(c == 0), stop=(c == 3)),
                               r=['meanmat', f'ysq{c}'], w=['psB5'])
                        op('act', I('activation', out=lnw[:, 0, :], in_=psB[:, 4, :], func=AF.Copy),
                           r=['psB4'], w=['lnw0'])
                        op('pool', I('tensor_tensor', out=lnw[:, 1, :], in0=lnw[:, 0, :], in1=lnw[:, 0, :],
                                                             op=ALU.mult), r=['lnw0'], w=['lnw1'])
                        op('dve', I('tensor_tensor', out=lnw[:, 1, :], in0=psB[:, 5, :], in1=lnw[:, 1, :],
                                                            op=ALU.subtract), r=['psB5', 'lnw1'], w=['lnw1'])
                        op('dve', I('tensor_scalar', out=lnw[:, 1, :], in0=lnw[:, 1, :], scalar1=0.0,
                                                            scalar2=None, op0=ALU.max), r=['lnw1'], w=['lnw1'])
                        op('act', I('activation', out=lnw[:, 1, :], in_=lnw[:, 1, :], func=AF.Sqrt, bias=EPS),
                           r=['lnw1'], w=['lnw1'])
                        op('dve', I('reciprocal', out=lnw[:, 2, :], in_=lnw[:, 1, :]), r=['lnw1'], w=['lnw2'])
                        for c in range(4):
                            op('pool', I('tensor_tensor', out=lnw[:, 3, :], in0=ycv[:, c, ns],
                                                                             in1=lnw[:, 0, :], op=ALU.subtract),
                               r=[f'y{c}_{n}', 'lnw0'], w=['lnw3'])
                            op('pool', I('tensor_tensor', out=lnw[:, 3, :], in0=lnw[:, 3, :], in1=lnw[:, 2, :],
                                                                 op=ALU.mult), r=['lnw3', 'lnw2'], w=['lnw3'])
                            op('act', I('activation',
                                out=hT[:, 4 + c, ns], in_=lnw[:, 3, :], func=AF.Silu, scale=cvg[:, c:c + 1],
                                bias=cvbb[:, c:c + 1]), r=['lnw3', 'cvg', 'cvbb'], w=[HK(4 + c, n)])
                    S.barrier()

                with ExitStack() as s1b:
                    spb = [T(s1b, f"spb{i}", [128, 512], F32) for i in range(4)]
                    ab = [T(s1b, f"ab{i}", [128, 512], BF16) for i in range(4)]
                    Rb = [T(s1b, f"Rb{i}", [128, 512], F32) for i in range(2)]
                    osq = [T(s1b, f"osq{i}", [128, 512], F32) for i in range(2)]
                    units = []
                    for j in range(4):
                        for qn in range(4):
                            kcs = list(range(4 * qn + 3, -1, -1))
                            for idx, kc in enumerate(kcs):
                                for hp in range(2):
                                    units.append((j, qn, idx, kc, hp, len(kcs)))
                    nU = len(units)
                    negtri_r = negtriR[:].bitcast(F32R)
                    negones_r = negonesR[:].bitcast(F32R)
                    oqc = [0]

                    def S1(i):
                        j, qn, idx, kc, hp, nk = units[i]
                        P = slice(hp * 64, hp * 64 + 64)
                        zb = i % 2
                        sp_t, spk = spb[i % 4], f'spb{i % 4}'
                        qs = slice(qn * 512, (qn + 1) * 512)
                        ks = slice(kc * 128, (kc + 1) * 128)
                        op('pe', I('matmul', psB[:, zb, :], qk[P, 4 + j, ks], qk[P, j, qs], start=True, stop=True),
                           r=[f'k{j}_{kc // 4}', f'q{j}_{qn}'], w=[f'psB{zb}'])
                        op('act', I('activation', out=sp_t[:].bitcast(F32R), in_=psB[:, zb, :], func=AF.Exp), r=[f'psB{zb}'], w=[spk])
                        op('act', I('activation', out=sp_t[:].bitcast(F32R), in_=sp_t[:], func=AF.Ln, bias=1.0), r=[spk], w=[spk])
                        if kc >= 4 * qn:
                            op('pool', I('tensor_tensor', out=sp_t[:].bitcast(F32R), in0=sp_t[:], in1=maskf[:, kc - 4 * qn, :],
                                         op=ALU.mult), r=[spk, 'maskf'], w=[spk])

                    def S2(i):
                        j, qn, idx, kc, hp, nk = units[i]
                        P = slice(hp * 64, hp * 64 + 64)
                        eb = 2 + i % 2
                        ek = f'psB{eb}'
                        sp_t, spk = spb[i % 4], f'spb{i % 4}'
                        ab_t, abk = ab[i % 4], f'ab{i % 4}'
                        qs = slice(qn * 512, (qn + 1) * 512)
                        ks = slice(kc * 128, (kc + 1) * 128)
                        op('pe', I('matmul', psB[:, eb, :], qk[P, 4 + j, ks], qk[P, j, qs], start=True, stop=False),
                           r=[f'k{j}_{kc // 4}', f'q{j}_{qn}'], w=[ek])
                        op('pe', I('matmul', psB[:, eb, :], negtri_r, sp_t[:].bitcast(F32R), start=False,
                                   stop=(idx == 0)), r=['negtriR', spk], w=[ek])
                        if idx > 0:
                            op('pe', I('matmul', psB[:, eb, :], negones_r, Rb[hp][:].bitcast(F32R), start=False,
                                       stop=True), r=['negonesR', f'Rb{hp}'], w=[ek])
                        op('act', I('activation', out=ab_t[:], in_=psB[:, eb, :], func=AF.Exp), r=[ek], w=[abk])
                        if kc >= 4 * qn:
                            op('dve', I('tensor_tensor', out=ab_t[:], in0=ab_t[:], in1=maskf[:, kc - 4 * qn, :],
                                        op=ALU.mult), r=[abk, 'maskf'], w=[abk])
                        if idx < nk - 1:
                            if idx == 0:
                                op('pool', I('tensor_copy', out=Rb[hp][:].bitcast(F32R), in_=sp_t[:]), r=[spk], w=[f'Rb{hp}'])
                            else:
                                op('pool', I('tensor_tensor', out=Rb[hp][:].bitcast(F32R), in0=Rb[hp][:], in1=sp_t[:], op=ALU.add),
                                   r=[spk, f'Rb{hp}'], w=[f'Rb{hp}'])

                    def S3(i):
                        j, qn, idx, kc, hp, nk = units[i]
                        h = 2 * j + hp
                        P = slice(hp * 64, hp * 64 + 64)
                        ab_t, abk = ab[i % 4], f'ab{i % 4}'
                        qs = slice(qn * 512, (qn + 1) * 512)
                        op('pe', I('matmul', psB[P, 4, :], v_sb[:, kc, h * 64:(h + 1) * 64], ab_t[:],
                                   start=(idx == 0), stop=(idx == nk - 1)), r=[f'v{kc}', abk], w=[f'psB4_{hp}'])
                        if idx == nk - 1 and hp == 1:
                            oq_t = osq[oqc[0] & 1]
                            oqk = f'osq{oqc[0] & 1}'
                            oqc[0] += 1
                            op('act', I('activation', out=hT[:, j, qs], in_=psB[:, 4, :], func=AF.Copy,
                                        scale=sbg[:, j:j + 1]), r=['psB4_0', 'psB4_1', 'sbg'], w=[HK(j, qn)])
                            op('act', I('activation', out=oq_t[:], in_=psB[:, 4, :], func=AF.Square),
                               r=['psB4_0', 'psB4_1'], w=[oqk])
                            for tt in range(4):
                                col = (j * 16 + qn * 4 + tt) * 2
                                op('pe', I('matmul', psB[:, 5, col:col + 2], oq_t[:, tt * 128:(tt + 1) * 128], ones2[:],
                                           start=True, stop=True), r=[oqk, 'ones2'], w=['psB5'])

                    for i in range(nU + 3):
                        if i < nU:
                            S1(i)
                        if 0 <= i - 2 < nU:
                            S2(i - 2)
                        if 0 <= i - 3 < nU:
                            S3(i - 3)
                    ssv = lambda j: psB[:, 5, j * 32:(j + 1) * 32].rearrange("p (t two) -> p t two", two=2)[:, :, 0]
                    op('act', I('activation', out=rsb[:, 0, :], in_=ssv(0), func=AF.Copy), r=['psB5'], w=['rsb'])
                    for j in range(1, 4):
                        op('dve', I('tensor_tensor', out=rsb[:, 0, :], in0=ssv(j), in1=rsb[:, 0, :],
                                                                 op=ALU.add), r=['psB5', 'rsb'], w=['rsb'])
                    op('act', I('activation', out=rsb[:, 1, :], in_=rsb[:, 0, :], func=AF.Sqrt, scale=1.0 / 512,
                                                     bias=EPS), r=['rsb'], w=['rsb'])
                    op('dve', I('reciprocal', out=rsb[:, 2, :], in_=rsb[:, 1, :]), r=['rsb'], w=['rsb'])
                    S.barrier()
                    if stage == 1 and b == 0:
                        op('sp', I('dma_start', out=dbg_mix, in_=hT[:]), dma='dbg0')
                        op('sp', I('dma_start', out=dbg_qk, in_=qk[:]), dma='dbg1')
                        op('sp', I('dma_start', out=dbg_v, in_=v_sb[:]), dma='dbg2')
                        op('sp', I('dma_start', out=dbg_rsb, in_=rsb[:]), dma='dbg3')
                        S.barrier()

            with ExitStack() as s2:
                x_sb = T(s2, "x_sb", [128, NT, DM], F32)
                for t in range(NT):
                    op('sp', I('dma_start', out=x_sb[:, t, :], in_=x_d[b, t * 128:(t + 1) * 128, :]),
                       w=[f'x{t}'], dma=f'x{t}')
                nxt = load_w(w_out_d[:, 0:512])
                for n in range(2):
                    sl = nxt
                    nxt = load_w(w_out_d[:, 512:1024]) if n == 0 else load_w(w_xkv_d[:, 0:512])
                    ns = slice(n * 512, (n + 1) * 512)
                    for t in range(NT):
                        b0, b1 = (t % 2) * 2, (t % 2) * 2 + 1
                        ts_ = slice(t * 128, (t + 1) * 128)
                        for jj in range(4):
                            op('pe', I('matmul',
                                psB[:, b0, :], hT[:, jj, ts_], wr[sl][:, jj, :], start=(jj == 0), stop=(jj == 3)),
                               r=[HK(jj, t // 4), f'wr{sl}'], w=[f'psB{b0}'])
                        for jj in range(4):
                            op('pe', I('matmul',
                                psB[:, b1, :], hT[:, 4 + jj, ts_], wr[sl][:, 4 + jj, :], start=(jj == 0), stop=(jj == 3)),
                               r=[HK(4 + jj, t // 4), f'wr{sl}'], w=[f'psB{b1}'])
                        op('dve', I('tensor_tensor',
                            out=x_sb[:, t, ns], in0=psB[:, b1, :], in1=x_sb[:, t, ns], op=ALU.add),
                           r=[f'psB{b1}', f'x{t}'], w=[f'x{t}'])
                        op('dve', I('scalar_tensor_tensor',
                            out=x_sb[:, t, ns], in0=psB[:, b0, :], scalar=rsb[:, 2, t:t + 1], in1=x_sb[:, t, ns],
                            op0=ALU.mult, op1=ALU.add), r=[f'psB{b0}', 'rsb', f'x{t}'], w=[f'x{t}'])
                if stage == 1:
                    for t in range(NT):
                        ev = op('sp', I('dma_start',
                            out=out_d[b * SEQ + t * 128:b * SEQ + (t + 1) * 128, :], in_=x_sb[:, t, :]),
                            r=[f'x{t}'], dma=f'o{t % 4}')
                    S.barrier()
                    continue

                with ExitStack() as s2f:
                    memin = [T(s2f, f"memin{i}", [128, DM], F32) for i in range(2)]
                    hn2 = [T(s2f, f"hnb{i}", [128, DM], BF16) for i in range(2)]
                    memT = T(s2f, "memT", [128, 8, NMEM], BF16)
                    kxT = T(s2f, "kxT", [128, 8, NMEM], BF16)
                    vx = T(s2f, "vx", [128, 2, DM], BF16)
                    qxT = T(s2f, "qxT", [128, 8, SEQ], BF16)
                    pf = [T(s2f, "pf0", [128, 4, NMEM], F32)] * 2
                    pn = [T(s2f, f"pn{i}", [128, 4, NMEM], BF16) for i in range(2)]
                    pT = [T(s2f, "pT0", [128, 4, 2, 512], BF16)] * 2
                    sm = T(s2f, "sm", [128, 4, 4], F32)
                    for mt in range(2):
                        op('sp', I('dma_start', out=memin[mt][:], in_=mem_d[b, mt * 128:(mt + 1) * 128, :]),
                           w=[f'memin{mt}'], dma=f'memin{mt}')
                        norm_T(memin[mt][:], f'memin{mt}', hn2[mt], f'hnb{mt}', 2,
                               memT[:, :, mt * 128:(mt + 1) * 128], ['memT'], mt, mt)
                    mrhs = lambda kc, n: memT[:, kc, :]
                    mkeys = lambda kc, n: ['memT']
                    for g in range(2):
                        sl = nxt
                        nxt = load_w(w_xkv_d[:, (g + 1) * 512:(g + 2) * 512])
                        fm_proj(sl, 4, mrhs, mkeys, 1, NMEM,
                                lambda cc, n, bk, g=g: evac_copy(kxT[:, g * 4 + cc, :], psB[:, bk, 0:NMEM], [f'psB{bk}'],
                                                                 ['kxT']), [0, 1, 2, 3])
                    for g in range(2):
                        sl = nxt
                        nxt = load_w(w_xkv_d[:, 1536:2048]) if g == 0 else load_w(w_xq_d[:, 0:512])
                        for mt in range(2):
                            bk = mt
                            for kc in range(8):
                                op('pe', I('matmul',
                                    psB[:, bk, :], memT[:, kc, mt * 128:(mt + 1) * 128], wr[sl][:, kc, :],
                                    start=(kc == 0), stop=(kc == 7)), r=['memT', f'wr{sl}'], w=[f'psB{bk}'])
                            evac_copy(vx[:, mt, g * 512:(g + 1) * 512], psB[:, bk, :], [f'psB{bk}'], ['vx'])
                    for t in range(NT):
                        norm_T(x_sb[:, t, :], f'x{t}', hn2[t % 2], f'hnb{t % 2}', 1,
                               hT[:, :, t * 128:(t + 1) * 128], [HK(c, t // 4) for c in range(8)], t, t % 2)
                    for g in range(2):
                        sl = nxt
                        nxt = load_w(w_xq_d[:, 512:1024]) if g == 0 else load_w(w_xo_d[:, 0:512])
                        fm_proj(sl, 4, hrhs, hkeys, 4, 512,
                                lambda cc, n, bk, g=g: evac_copy(qxT[:, g * 4 + cc, n * 512:(n + 1) * 512], psB[:, bk, :],
                                                                 [f'psB{bk}'], [f'qx{g * 4 + cc}_{n}'], scale=1.0 / 16),
                                [0, 1, 2, 3])
                    psS = psB[:, 0:2, :].rearrange("p a (h m) -> p (a h) m", h=2)
                    for n in range(4):
                        pT_t = pT[n % 2]
                        pTk = 'pT0'
                        for tt in range(4):
                            t = n * 4 + tt
                            ts_ = slice(t * 128, (t + 1) * 128)
                            pi = t % 2
                            for hx in range(4):
                                for dc in range(2):
                                    op('pe', I('matmul',
                                        psS[:, hx, :], qxT[:, 2 * hx + dc, ts_], kxT[:, 2 * hx + dc, :],
                                        start=(dc == 0), stop=(dc == 1)),
                                       r=[f'qx{2 * hx + dc}_{n}', 'kxT'], w=[f'psB{hx // 2}'])
                            op('dve', I('tensor_reduce', out=sm[:, 0, :], in_=psS, axis=AX.X, op=ALU.max),
                               r=['psB0', 'psB1'], w=['sm'])
                            op('dve', I('tensor_scalar', out=sm[:, 1, :], in0=sm[:, 0, :], scalar1=-1.0,
                                                                scalar2=None, op0=ALU.mult), r=['sm'], w=['sm'])
                            for hx in range(4):
                                op('act', I('activation',
                                    out=pf[pi][:, hx, :], in_=psS[:, hx, :], func=AF.Exp, bias=sm[:, 1, hx:hx + 1],
                                    accum_out=sm[:, 2, hx:hx + 1]),
                                   r=[f'psB{hx // 2}', 'sm'], w=['pf0', 'sm'])
                            op('dve', I('reciprocal', out=sm[:, 3, :], in_=sm[:, 2, :]), r=['sm'], w=['sm'])
                            for hx in range(4):
                                op('dve', I('tensor_scalar',
                                    out=pn[pi][:, hx, :], in0=pf[pi][:, hx, :], scalar1=sm[:, 3, hx:hx + 1],
                                    scalar2=None, op0=ALU.mult), r=['pf0', 'sm'], w=[f'pn{pi}'])
                            for hx in range(4):
                                for mc in range(2):
                                    op('pe', I('transpose',
                                        out=psA[:, pi, (hx * 2 + mc) * 128:(hx * 2 + mc + 1) * 128],
                                        in_=pn[pi][:, hx, mc * 128:(mc + 1) * 128], identity=ident[:]),
                                       r=[f'pn{pi}', 'ident'], w=[f'psA{pi}'])
                            op('act', I('activation',
                                out=pT_t[:, :, :, tt * 128:(tt + 1) * 128],
                                in_=psA[:, pi, :].rearrange("p (h m c) -> p h m c", h=4, m=2), func=AF.Copy),
                               r=[f'psA{pi}'], w=[pTk])
                        k = 0
                        for hx in range(4):
                            for dc in range(2):
                                bk = 2 + (k % 4)
                                k += 1
                                for mc in range(2):
                                    op('pe', I('matmul',
                                        psB[:, bk, :], vx[:, mc, hx * 256 + dc * 128:hx * 256 + (dc + 1) * 128],
                                        pT_t[:, hx, mc, :], start=(mc == 0), stop=(mc == 1)),
                                       r=['vx', pTk], w=[f'psB{bk}'])
                                evac_copy(hT[:, 2 * hx + dc, n * 512:(n + 1) * 512], psB[:, bk, :], [f'psB{bk}'],
                                          [HK(2 * hx + dc, n)])
                    for n in range(2):
                        sl = nxt
                        nxt = load_w(w_xo_d[:, 512:1024]) if n == 0 else None
                        ns = slice(n * 512, (n + 1) * 512)
                        for t in range(NT):
                            bk = t % 4
                            for cc in range(8):
                                op('pe', I('matmul',
                                    psB[:, bk, :], hT[:, cc, t * 128:(t + 1) * 128], wr[sl][:, cc, :],
                                    start=(cc == 0), stop=(cc == 7)), r=[HK(cc, t // 4), f'wr{sl}'], w=[f'psB{bk}'])
                            op('dve', I('tensor_tensor',
                                out=x_sb[:, t, ns], in0=psB[:, bk, :], in1=x_sb[:, t, ns], op=ALU.add),
                               r=[f'psB{bk}', f'x{t}'], w=[f'x{t}'])
                    S.barrier()
                if stage == 2:
                    for t in range(NT):
                        ev = op('sp', I('dma_start',
                            out=out_d[b * SEQ + t * 128:b * SEQ + (t + 1) * 128, :], in_=x_sb[:, t, :]),
                            r=[f'x{t}'], dma=f'o{t % 4}')
                    S.barrier()
                    continue

                with ExitStack() as s2g:
                    gbc = T(s2g, "gbc", [128, DM], F32)
                    h3 = [T(s2g, f"h3_{i}", [128, DM], F32) for i in range(2)]
                    h3b = [T(s2g, f"h3b_{i}", [128, DM], BF16) for i in range(2)]
                    h3T = [T(s2g, f"h3T{i}", [128, 8, 128], F32) for i in range(2)]
                    lgs = [T(s2g, f"lg{i}", [128, 36], F32) for i in range(2)]
                    rw = T(s2g, "rw", [128, 16], F32)
                    gm = T(s2g, "gm", [128, 4], F32)
                    elm = T(s2g, "elm", [128, 4, 8], F32)
                    elm3 = T(s2g, "elm3", [128, 32], F32)
                    ig = T(s2g, "ig", [128, 4, 8], F32)
                    S32 = T(s2g, "S32", [128, 4, 8], F32)
                    S32b = T(s2g, "S32b", [128, 32], BF16)
                    M1 = T(s2g, "M1", [128, 4, 8], F32)
                    M2 = T(s2g, "M2", [128, 4, 8], F32)
                    pos = T(s2g, "pos", [128, 32], F32)
                    slf = T(s2g, "slf", [128, 2], F32)
                    op('sp', I('dma_start', out=gbc[:], in_=ln_ffn_g.partition_broadcast(128)), w=['gbc'],
                       dma='gbc')

                    def G1(t):
                        hi = t % 2
                        op('act', I('activation', out=junk[:], in_=x_sb[:, t, :], func=AF.Square,
                                    accum_out=stat[:, 0, t:t + 1]), r=[f'x{t}'], w=['junk', 'stat'])
                        rstd_from_ss(stat, t, 1.0 / DM)
                        op('dve', I('scalar_tensor_tensor', out=h3[hi][:], in0=x_sb[:, t, :],
                                    scalar=stat[:, 2, t:t + 1], in1=gbc[:], op0=ALU.mult, op1=ALU.mult),
                           r=[f'x{t}', 'stat', 'gbc'], w=[f'h3_{hi}'])
                        op('pool', I('tensor_copy', out=h3b[hi][:], in_=h3[hi][:]), r=[f'h3_{hi}'], w=[f'h3b_{hi}'])
                        b4, b5 = (4, 5) if hi == 0 else (2, 3)
                        for kc in range(8):
                            bb = b4 if kc < 4 else b5
                            op('pe', I('transpose', out=psB[:, bb, (kc % 4) * 128:(kc % 4 + 1) * 128],
                                       in_=h3[hi][:, kc * 128:(kc + 1) * 128], identity=identf[:]),
                               r=[f'h3_{hi}', 'identf'], w=[f'psB{bb}'])
                        op('act', I('activation', out=h3T[hi][:, 0:4, :],
                                    in_=psB[:, b4, :].rearrange("p (k c) -> p k c", k=4), func=AF.Copy),
                           r=[f'psB{b4}'], w=[f'h3T{hi}a'])
                        op('dve', I('tensor_copy', out=h3T[hi][:, 4:8, :],
                                    in_=psB[:, b5, :].rearrange("p (k c) -> p k c", k=4)),
                           r=[f'psB{b5}'], w=[f'h3T{hi}b'])
                        for kc in range(8):
                            op('pe', I('matmul', psB[:, hi, 0:36], h3T[hi][:, kc, :], wrt[:, kc, :],
                                       start=(kc == 0), stop=(kc == 7)),
                               r=[f'h3T{hi}a', f'h3T{hi}b', 'wrt'], w=[f'psB{hi}'])
                        op('dve', I('tensor_tensor', out=lgs[hi][:], in0=psB[:, hi, 0:36], in1=brt[:], op=ALU.add),
                           r=[f'psB{hi}', 'brt'], w=[f'lg{hi}'])
                        op('sp', I('dma_start', out=xres_d[b * SEQ + t * 128:b * SEQ + (t + 1) * 128, :],
                                   in_=x_sb[:, t, :]), r=[f'x{t}'], w=[f'xres{t}'], dma=f'xw{t % 4}')

                    def G2(t):
                        tg = b * NT + t
                        lg = lgs[t % 2]
                        lk = f'lg{t % 2}'
                        V = lambda f, r=(), w=(): op('dve', f, r=['rt2'] + list(r), w=['rt2'] + list(w))
                        el = lg[:, 4:36].rearrange("p (g e) -> p g e", g=4)
                        V(I('tensor_reduce', out=rw[:, 0:1], in_=lg[:, 0:4], axis=AX.X, op=ALU.max), r=[lk])
                        V(I('tensor_scalar', out=rw[:, 1:2], in0=rw[:, 0:1], scalar1=-1.0, scalar2=None, op0=ALU.mult))
                        op('act', I('activation', out=gm[:], in_=lg[:, 0:4], func=AF.Exp, bias=rw[:, 1:2],
                                    accum_out=rw[:, 2:3]), r=['rt2', lk], w=['rt2'])
                        V(I('reciprocal', out=rw[:, 3:4], in_=rw[:, 2:3]))
                        V(I('tensor_scalar', out=gm[:], in0=lg[:, 0:4], scalar1=rw[:, 0:1], scalar2=None,
                            op0=ALU.is_equal), r=[lk])
                        V(I('tensor_tensor', out=elm[:], in0=el, in1=gm[:, :, None].to_broadcast([128, 4, 8]),
                            op=ALU.mult), r=[lk])
                        V(I('tensor_reduce', out=ig[:, 0, :], in_=elm[:].rearrange("p g e -> p e g"), axis=AX.X,
                            op=ALU.add))
                        V(I('tensor_reduce', out=rw[:, 4:5], in_=ig[:, 0, :], axis=AX.X, op=ALU.max))
                        V(I('tensor_scalar', out=ig[:, 1, :], in0=ig[:, 0, :], scalar1=rw[:, 4:5], scalar2=None,
                            op0=ALU.is_equal))
                        V(I('scalar_tensor_tensor', out=ig[:, 2, :], in0=ig[:, 1, :], scalar=-1e30, in1=ig[:, 0, :],
                            op0=ALU.mult, op1=ALU.add))
                        V(I('tensor_reduce', out=rw[:, 5:6], in_=ig[:, 2, :], axis=AX.X, op=ALU.max))
                        V(I('tensor_scalar', out=ig[:, 3, :], in0=ig[:, 2, :], scalar1=rw[:, 5:6], scalar2=None,
                            op0=ALU.is_equal))
                        V(I('tensor_tensor', out=rw[:, 6:7], in0=rw[:, 5:6], in1=rw[:, 4:5], op=ALU.subtract))
                        op('act', I('activation', out=rw[:, 7:8], in_=rw[:, 6:7], func=AF.Exp), r=['rt2'], w=['rt2'])
                        V(I('tensor_scalar', out=rw[:, 8:9], in0=rw[:, 7:8], scalar1=1.0, scalar2=None, op0=ALU.add))
                        V(I('reciprocal', out=rw[:, 9:10], in_=rw[:, 8:9]))
                        V(I('tensor_tensor', out=wts[:, tg, 0:1], in0=rw[:, 9:10], in1=rw[:, 3:4], op=ALU.mult))
                        V(I('tensor_tensor', out=wts[:, tg, 1:2], in0=rw[:, 7:8], in1=wts[:, tg, 0:1], op=ALU.mult))
                        V(I('tensor_tensor', out=M1[:], in0=gm[:, :, None].to_broadcast([128, 4, 8]),
                            in1=ig[:, 1:2, :].to_broadcast([128, 4, 8]), op=ALU.mult), w=['M12'])
                        V(I('tensor_tensor', out=M2[:], in0=gm[:, :, None].to_broadcast([128, 4, 8]),
                            in1=ig[:, 3:4, :].to_broadcast([128, 4, 8]), op=ALU.mult), w=['M12'])
                        V(I('tensor_tensor', out=S32[:], in0=M1[:], in1=M2[:], op=ALU.add))
                        V(I('tensor_copy', out=S32b[:], in_=S32[:].rearrange("p g e -> p (g e)")), w=['S32b'])

                    def G3(t):
                        tg = b * NT + t
                        hi = t % 2
                        W = lambda f, r=(), w=(): op('dve', f, r=['rt3'] + list(r), w=['rt3'] + list(w))
                        op('pe', I('matmul', psB[:, 1 - hi, 64:96], Lmat[:], S32b[:], start=True, stop=True),
                           r=['Lmat', 'S32b'], w=[f'psB{1 - hi}'])
                        op('pe', I('matmul', psB[:, 1 - hi, 96:128], onesb[:], S32b[:], start=True, stop=True),
                           r=['onesb', 'S32b'], w=[f'psB{1 - hi}'])
                        W(I('tensor_tensor', out=pos[:], in0=psB[:, 1 - hi, 64:96], in1=base[:], op=ALU.add),
                          r=[f'psB{1 - hi}', 'base'])
                        W(I('tensor_tensor', out=base[:], in0=psB[:, 1 - hi, 96:128], in1=base[:], op=ALU.add),
                          r=[f'psB{1 - hi}', 'base'], w=['base'])
                        W(I('tensor_scalar', out=elm3[:], in0=pos[:], scalar1=float(CAP), scalar2=1e7, op0=ALU.is_ge,
                            op1=ALU.mult))
                        W(I('tensor_tensor', out=pos[:], in0=pos[:], in1=elm3[:], op=ALU.add))
                        W(I('tensor_tensor', out=pos[:], in0=pos[:], in1=ecap[:], op=ALU.add))
                        W(I('tensor_tensor', out=elm3[:], in0=pos[:], in1=M1[:].rearrange("p g e -> p (g e)"),
                            op=ALU.mult), r=['M12'])
                        W(I('tensor_reduce', out=slf[:, 0:1], in_=elm3[:], axis=AX.X, op=ALU.add))
                        W(I('tensor_tensor', out=elm3[:], in0=pos[:], in1=M2[:].rearrange("p g e -> p (g e)"),
                            op=ALU.mult), r=['M12'])
                        W(I('tensor_reduce', out=slf[:, 1:2], in_=elm3[:], axis=AX.X, op=ALU.add))
                        W(I('tensor_copy', out=slots[:, tg, :], in_=slf[:]), w=[f'slots{tg}'])
                        for k2 in range(2):
                            op('pool', I('indirect_dma_start', out=xd_d[:, :],
                                         out_offset=bass.IndirectOffsetOnAxis(ap=slots[:, tg, k2:k2 + 1], axis=0),
                                         in_=h3b[hi][:], in_offset=None, bounds_check='REG', oob_is_err=False),
                               r=[f'slots{tg}', f'h3b_{hi}'], w=[f'xd{tg}_{k2}'], dma=f'sc{hi}')

                    for i in range(NT + 2):
                        if 0 <= i - 2 < NT:
                            G3(i - 2)
                        if 0 <= i - 1 < NT:
                            G2(i - 1)
                        if i < NT:
                            G1(i)
                    S.barrier()

        if stage >= 3:
            with ExitStack() as s3:
                wg = [T(s3, f"wg{i}", [128, 8, 512], BF16) for i in range(2)]
                wu = [T(s3, f"wu{i}", [128, 8, 512], BF16) for i in range(2)]
                wd = [T(s3, f"wd{i}", [128, 4, DM], BF16) for i in range(2)]
                xr = [T(s3, f"xr{i}", [128, DM], BF16) for i in range(3)]
                xT = [T(s3, f"xT{i}", [128, 8, 384], BF16) for i in range(2)]
                sg = [T(s3, f"sg{i}", [128, 384], F32) for i in range(2)]
                h1T = [T(s3, f"h1T{i}", [128, 4, 384], BF16) for i in range(2)]
                ydt = [T(s3, f"ydt{i}", [128, DM], F32) for i in range(2)]

                wst = [T(s3, f"wst{i}", [128, 8, 512], F32) for i in range(3)]

                def load_dma(e_):
                    op('sp', I('dma_start', out=wst[0][:], in_=w_gate_d[e_].rearrange("(k p) n -> p k n", p=128)),
                       w=['wst0'], dma='wst0')
                    op('sp', I('dma_start', out=wst[1][:], in_=w_up_d[e_].rearrange("(k p) n -> p k n", p=128)),
                       w=['wst1'], dma='wst1')
                    op('sp', I('dma_start', out=wst[2][:].rearrange("p (a b) n -> p a (b n)", a=4),
                               in_=w_down_d[e_].rearrange("(k p) n -> p k n", p=128)), w=['wst2'], dma='wst2')

                def cast_gu(e_):
                    i = e_ % 2
                    op('dve', I('tensor_copy', out=wg[i][:], in_=wst[0][:]), r=['wst0'], w=[f'wg{i}'])
                    op('act', I('activation', out=wu[i][:], in_=wst[1][:], func=AF.Copy), r=['wst1'], w=[f'wu{i}'])

                def cast_d(e_):
                    i = e_ % 2
                    wsv = wst[2][:].rearrange("p (a b) n -> p a (b n)", a=4)
                    op('act', I('activation', out=wd[i][:, 0:2, :], in_=wsv[:, 0:2, :], func=AF.Copy),
                       r=['wst2'], w=[f'wd{i}a'])
                    op('dve', I('tensor_copy', out=wd[i][:, 2:4, :], in_=wsv[:, 2:4, :]), r=['wst2'], w=[f'wd{i}b'])

                def load_expert(e_):
                    load_dma(e_)
                    cast_gu(e_)
                    cast_d(e_)

                load_expert(0)
                cnt = 0
                yc = 0
                for e_ in range(NEXP):
                    wi = e_ % 2
                    if e_ + 1 < NEXP:
                        load_dma(e_ + 1)
                    for half in range(2):
                        if half == 1 and e_ + 1 < NEXP:
                            cast_gu(e_ + 1)
                        r0 = e_ * CAP + half * 384
                        hb = cnt % 2
                        cnt += 1
                        for ci in range(3):
                            xi = (cnt * 3 + ci) % 3
                            op('sp', I('dma_start',
                                out=xr[xi][:], in_=xd_d[r0 + ci * 128:r0 + (ci + 1) * 128, :]),
                                r=['xd'], w=[f'xr{xi}'], dma=f'xr{xi}')
                            pa = ci % 2
                            for kc in range(8):
                                op('pe', I('transpose',
                                    out=psA[:, pa, kc * 128:(kc + 1) * 128], in_=xr[xi][:, kc * 128:(kc + 1) * 128],
                                    identity=ident[:]), r=[f'xr{xi}', 'ident'], w=[f'psA{pa}'])
                            evac_copy(xT[hb][:, :, ci * 128:(ci + 1) * 128],
                                      psA[:, pa, :].rearrange("p (k c) -> p k c", k=8), [f'psA{pa}'], [f'xT{hb}'])
                        for dc in range(4):
                            bg, bu = (dc % 2) * 2, (dc % 2) * 2 + 1
                            for kc in range(8):
                                op('pe', I('matmul',
                                    psB[:, bg, 0:384], wg[wi][:, kc, dc * 128:(dc + 1) * 128], xT[hb][:, kc, :],
                                    start=(kc == 0), stop=(kc == 7)), r=[f'wg{wi}', f'xT{hb}'], w=[f'psB{bg}'])
                            for kc in range(8):
                                op('pe', I('matmul',
                                    psB[:, bu, 0:384], wu[wi][:, kc, dc * 128:(dc + 1) * 128], xT[hb][:, kc, :],
                                    start=(kc == 0), stop=(kc == 7)), r=[f'wu{wi}', f'xT{hb}'], w=[f'psB{bu}'])
                            si = dc % 2
                            op('act', I('activation', out=sg[si][:], in_=psB[:, bg, 0:384],
                                                                           func=AF.Silu),
                               r=[f'psB{bg}'], w=[f'sg{si}'])
                            op('dve', I('tensor_tensor',
                                out=h1T[hb][:, dc, :], in0=psB[:, bu, 0:384], in1=sg[si][:], op=ALU.mult),
                               r=[f'psB{bu}', f'sg{si}'], w=[f'h1T{hb}_{dc}'])
                        for ci in range(3):
                            yi = yc % 2
                            yc += 1
                            for nn in range(2):
                                for dc in range(4):
                                    op('pe', I('matmul',
                                        psB[:, 4 + nn, :], h1T[hb][:, dc, ci * 128:(ci + 1) * 128],
                                        wd[wi][:, dc, nn * 512:(nn + 1) * 512], start=(dc == 0), stop=(dc == 3)),
                                       r=[f'h1T{hb}_{dc}', f'wd{wi}a', f'wd{wi}b'], w=[f'psB{4 + nn}'])
                            op('act', I('activation', out=ydt[yi][:, 0:512], in_=psB[:, 4, :], func=AF.Copy),
                               r=['psB4'], w=[f'ydt{yi}a'])
                            op('dve', I('tensor_copy', out=ydt[yi][:, 512:1024], in_=psB[:, 5, :]),
                               r=['psB5'], w=[f'ydt{yi}b'])
                            op('sp', I('dma_start',
                                out=yd_d[r0 + ci * 128:r0 + (ci + 1) * 128, :], in_=ydt[yi][:]),
                                r=[f'ydt{yi}a', f'ydt{yi}b'], w=[f'yd{yc}'], dma=f'yo{yi}')
                    if e_ + 1 < NEXP:
                        cast_d(e_ + 1)
                S.barrier()

            with ExitStack() as s4:
                gbf = T(s4, "gbf", [128, DM], F32)
                y1 = [T(s4, f"y1_{i}", [128, DM], F32) for i in range(2)]
                y2 = [T(s4, f"y2_{i}", [128, DM], F32) for i in range(2)]
                xf = [T(s4, f"xf{i}", [128, DM], F32) for i in range(2)]
                of = [T(s4, f"of{i}", [128, DM], F32) for i in range(2)]
                fst = T(s4, "fst", [128, 3, nseq * NT], F32)
                zt = T(s4, "zt", [128, DM], F32)
                op('pool', I('memset', zt[:], 0.0), w=['zt'])
                op('sp', I('dma_start', out=gbf[:], in_=ln_final_g.partition_broadcast(128)), w=['gbf'], dma='gbf')
                def fetch(tg):
                    i = tg % 2
                    for ybuf, k2, nm in ((y1, 0, 'y1'), (y2, 1, 'y2')):
                        op('act', I('activation', out=ybuf[i][:], in_=zt[:], func=AF.Copy), r=['zt'], w=[f'{nm}_{i}'])
                        op('pool', I('indirect_dma_start', out=ybuf[i][:], out_offset=None, in_=yd_d[:, :],
                                     in_offset=bass.IndirectOffsetOnAxis(ap=slots[:, tg, k2:k2 + 1], axis=0),
                                     bounds_check='REG', oob_is_err=False),
                           r=['yd'], w=[f'{nm}_{i}'], dma=f'{nm}_{i}')
                    op('sp', I('dma_start', out=xf[i][:], in_=xres_d[tg * 128:(tg + 1) * 128, :]),
                       r=['xres'], w=[f'xf{i}'], dma=f'xf{i}')

                def compute(tg):
                    i = tg % 2
                    op('dve', I('scalar_tensor_tensor', out=xf[i][:], in0=y1[i][:], scalar=wts[:, tg, 0:1], in1=xf[i][:],
                                op0=ALU.mult, op1=ALU.add), r=[f'y1_{i}', f'xf{i}'], w=[f'xf{i}'])
                    op('dve', I('scalar_tensor_tensor', out=xf[i][:], in0=y2[i][:], scalar=wts[:, tg, 1:2], in1=xf[i][:],
                                op0=ALU.mult, op1=ALU.add), r=[f'y2_{i}', f'xf{i}'], w=[f'xf{i}'])
                    op('act', I('activation', out=junk[:], in_=xf[i][:], func=AF.Square, accum_out=fst[:, 0, tg:tg + 1]),
                       r=[f'xf{i}'], w=['junk', 'stat'])
                    rstd_from_ss(fst, tg, 1.0 / DM)
                    op('dve', I('scalar_tensor_tensor', out=of[i][:], in0=xf[i][:], scalar=fst[:, 2, tg:tg + 1],
                                in1=gbf[:], op0=ALU.mult, op1=ALU.mult), r=[f'xf{i}', 'stat', 'gbf'], w=[f'of{i}'])
                    op('sp', I('dma_start', out=out_d[tg * 128:(tg + 1) * 128, :], in_=of[i][:]),
                       r=[f'of{i}'], dma=f'of{i}')

                ntile = nseq * NT
                fetch(0)
                for tg in range(ntile):
                    if tg + 1 < ntile:
                        fetch(tg + 1)
                    compute(tg)
                S.barrier()
        S.barrier()
        S.emit()
    return nc


_NC_CACHE = {}


def _prep(inputs, c, nseq=NSEQ):
    f = lambda a: np.ascontiguousarray(np.asarray(a, dtype=np.float32))
    d = {
        "x": f(inputs["x"][c * nseq:(c + 1) * nseq]),
        "mem": f(inputs["mem"][c * nseq:(c + 1) * nseq]),
        "ln_mix_g": f(inputs["ln_mix_g"][0]),
        "w_in": f(inputs["w_in"][0]),
        "sb_out_g": f(inputs["sb_out_g"][0]),
        "conv_w": f(inputs["conv_w"][0]),
        "conv_b": f(inputs["conv_b"][0]),
        "conv_ln_g": f(inputs["conv_ln_g"][0]),
        "conv_ln_b": f(inputs["conv_ln_b"][0]),
        "w_out": f(inputs["w_out"][0]),
        "ln_mem_x_g": f(inputs["ln_mem_x_g"][0]),
        "ln_mem_g": f(inputs["ln_mem_g"][0]),
        "w_xq": f(inputs["w_xq"][0]),
        "w_xkv": f(inputs["w_xkv"][0]),
        "w_xo": f(inputs["w_xo"][0]),
        "ln_ffn_g": f(inputs["ln_ffn_g"][0]),
        "w_rt": f(np.concatenate([np.asarray(inputs["w_group"][0]), np.asarray(inputs["w_er"][0]).reshape(DM, 32)], axis=1)),
        "b_rt": f(np.concatenate([np.asarray(inputs["b_group"][0]), np.asarray(inputs["b_er"][0]).reshape(32)])),
        "w_gate": f(inputs["w_gate"][0]),
        "w_up": f(inputs["w_up"][0]),
        "w_down": f(inputs["w_down"][0]),
        "ln_final_g": f(inputs["ln_final_g"]),
    }
    return d


def kernel(**inputs):
    if 'nc' not in _NC_CACHE:
        _NC_CACHE['nc'] = build()
    nc = _NC_CACHE['nc']
    in_maps = [_prep(inputs, c) for c in range(N_CORES)]
    res = run_bass_kernel_spmd(nc, in_maps, core_ids=list(range(N_CORES)))
    out = np.concatenate([np.asarray(r["out"]).reshape(NSEQ, SEQ, DM) for r in res.results], axis=0)
    return out.astype(np.float32)
```

```python
import numpy as np
import concourse.bass as bass
import concourse.mybir as mybir
from concourse.bass_utils import run_bass_kernel_spmd
from contextlib import ExitStack

F32 = mybir.dt.float32
F32R = mybir.dt.float32r
BF16 = mybir.dt.bfloat16
I32 = mybir.dt.int32
AF = mybir.ActivationFunctionType
ALU = mybir.AluOpType
AX = mybir.AxisListType

ENG = ['pe', 'act', 'dve', 'pool', 'sp']
SAME_ENGINE_SYNC = True

N_CORES = 8
NSEQ = 4
SEQ = 2048
DM = 1024
NT = SEQ // 128
NMEM = 256
NEXP = 32
CAP = 768
NROWS = NEXP * CAP
EPS = 1e-6


def I(name, *args, **kw):
    return (name, args, kw)


class Sched:
    def __init__(self, nc, es):
        self.nc = nc
        self.es = es
        self.ins = {e: [] for e in ENG}
        self.cnt = {e: 0 for e in ENG}
        self.sem = {e: es.enter_context(nc.semaphore('s_' + e)) for e in ENG}
        self.dsem = {}
        self.lastw = {}
        self.readers = {}
        self.waited = {e: {} for e in ENG}

    def _semof(self, key):
        return self.sem[key[1]] if key[0] == 'e' else self.dsem[key[1]][0]

    def op(self, eng, fn, r=(), w=(), dma=None, extra=()):
        deps = {}

        def need(ev):
            if ev is None:
                return
            key, val, src, is_dma = ev
            if (not is_dma) and src == eng and (eng == 'pe' or not SAME_ENGINE_SYNC):
                return
            if deps.get(key, 0) < val:
                deps[key] = val

        for k in r:
            need(self.lastw.get(k))
        for k in w:
            need(self.lastw.get(k))
            for ev in self.readers.get(k, {}).values():
                need(ev)
        for ev in extra:
            need(ev)
        waits = []
        wd = self.waited[eng]
        for key, val in deps.items():
            if wd.get(key, 0) >= val:
                continue
            wd[key] = val
            waits.append((key, val))
        if dma is not None:
            if dma not in self.dsem:
                self.dsem[dma] = [self.es.enter_context(self.nc.semaphore('d_' + dma)), 0]
            self.dsem[dma][1] += 16
            ev = (('d', dma), self.dsem[dma][1], eng, True)
        else:
            self.cnt[eng] += 1
            ev = (('e', eng), self.cnt[eng], eng, False)
        self.ins[eng].append((waits, fn, ev))
        for k in r:
            d = self.readers.setdefault(k, {})
            old = d.get(ev[0])
            if old is None or old[1] < ev[1]:
                d[ev[0]] = ev
        for k in w:
            self.lastw[k] = ev
            self.readers[k] = {}
        return ev

    def barrier(self):
        evs = []
        for e in ENG:
            if self.cnt[e] > 0:
                evs.append((('e', e), self.cnt[e], e, False))
        for slot, (s, c) in self.dsem.items():
            if c > 0:
                evs.append((('d', slot), c, 'sp', True))
        for e in ENG:
            self.op(e, I('nop'), extra=[ev for ev in evs if not (ev[2] == e and not ev[3])])
        self.lastw = {}
        self.readers = {}

    def emit(self):
        nc = self.nc
        with nc.Block() as block:
            def body(name):
                def f(e):
                    bc_reg = None
                    if name == 'pool':
                        bc_reg = e.alloc_register()
                        e.reg_mov(bc_reg, NROWS - 1)
                    for waits, fn, ev in self.ins[name]:
                        for key, val in waits:
                            e.wait_ge(self._semof(key), val)
                        try:
                            kw = fn[2]
                            if kw.get('bounds_check', None) == 'REG':
                                kw = dict(kw, bounds_check=bc_reg)
                            ins = getattr(e, fn[0])(*fn[1], **kw)
                        except Exception:
                            print("EMIT FAIL", name, fn[0], fn[1], fn[2])
                            raise
                        key, val, _, is_dma = ev
                        ins.then_inc(self._semof(key), 16 if is_dma else 1)
                return f
            block.tensor(body('pe'))
            block.scalar(body('act'))
            block.vector(body('dve'))
            block.gpsimd(body('pool'))
            block.sync(body('sp'))


def build(nseq=NSEQ, stage=99):
    nc = bass.Bass('TRN2', target_bir_lowering=False)
    ntok = nseq * SEQ

    def din(name, shape, dt=F32):
        return nc.dram_tensor(name, list(shape), dt, kind="ExternalInput").ap()

    x_d = din("x", [nseq, SEQ, DM])
    mem_d = din("mem", [nseq, NMEM, DM])
    ln_mix_g = din("ln_mix_g", [DM])
    w_in_d = din("w_in", [DM, 2560])
    sb_out_g = din("sb_out_g", [512])
    conv_w_d = din("conv_w", [31, 512])
    conv_b_d = din("conv_b", [512])
    conv_ln_g = din("conv_ln_g", [512])
    conv_ln_b = din("conv_ln_b", [512])
    w_out_d = din("w_out", [DM, DM])
    ln_mem_x_g = din("ln_mem_x_g", [DM])
    ln_mem_g = din("ln_mem_g", [DM])
    w_xq_d = din("w_xq", [DM, DM])
    w_xkv_d = din("w_xkv", [DM, 2 * DM])
    w_xo_d = din("w_xo", [DM, DM])
    ln_ffn_g = din("ln_ffn_g", [DM])
    w_rt_d = din("w_rt", [DM, 36])
    b_rt_d = din("b_rt", [36])
    w_gate_d = din("w_gate", [NEXP, DM, 512])
    w_up_d = din("w_up", [NEXP, DM, 512])
    w_down_d = din("w_down", [NEXP, 512, DM])
    ln_final_g = din("ln_final_g", [DM])
    out_d = nc.dram_tensor("out", [ntok, DM], F32, kind="ExternalOutput").ap()
    if stage == 1:
        dbg_mix = nc.dram_tensor("dbg_mix", [128, 8, SEQ], BF16, kind="ExternalOutput").ap()
        dbg_qk = nc.dram_tensor("dbg_qk", [128, 8, SEQ], BF16, kind="ExternalOutput").ap()
        dbg_v = nc.dram_tensor("dbg_v", [128, NT, 512], BF16, kind="ExternalOutput").ap()
        dbg_rsb = nc.dram_tensor("dbg_rsb", [128, 3, NT], F32, kind="ExternalOutput").ap()
    xd_d = nc.dram_tensor("xd_scr", [NROWS, DM], BF16).ap()
    yd_d = nc.dram_tensor("yd_scr", [NROWS, DM], F32).ap()
    xres_d = nc.dram_tensor("xres_scr", [ntok, DM], F32).ap()

    with ExitStack() as es:
        S = Sched(nc, es)
        op = S.op

        uid = [0]

        def T(scope, name, shape, dt):
            uid[0] += 1
            return scope.enter_context(nc.sbuf_tensor(f"{name}_u{uid[0]}", shape, dt))

        psA = es.enter_context(nc.psum_tensor("psA", [128, 2, 1024], BF16))
        psB = es.enter_context(nc.psum_tensor("psB", [128, 6, 512], F32))

        identf = T(es, "identf", [128, 128], F32)
        ident = T(es, "ident", [128, 128], BF16)
        negtri = T(es, "negtri", [128, 128], F32)
        negones = T(es, "negones", [128, 128], F32)
        negtriR = T(es, "negtriR", [128, 128], F32)
        negonesR = T(es, "negonesR", [128, 128], F32)
        meanmat = T(es, "meanmat", [128, 128], F32)
        ones2 = T(es, "ones2", [128, 2], F32)
        onesb = T(es, "onesb", [128, 128], BF16)
        Lmat = T(es, "Lmat", [128, 128], BF16)
        maskf = T(es, "maskf", [128, 4, 512], BF16)
        ecap = T(es, "ecap", [128, 32], F32)
        ecap_i = T(es, "ecap_i", [128, 32], I32)
        gcols = T(es, "gcols", [128, 3, 8], F32)
        sbg = T(es, "sbg", [128, 4], F32)
        cvb = T(es, "cvb", [128, 4], F32)
        cvg = T(es, "cvg", [128, 4], F32)
        cvbb = T(es, "cvbb", [128, 4], F32)
        cwT = T(es, "cwT", [128, 4, 31], F32)
        hT = T(es, "hT", [128, 8, SEQ], BF16)
        wr = [T(es, f"wr{i}", [128, 8, 512], BF16) for i in range(2)]
        stat = T(es, "stat", [128, 3, NT], F32)
        rsb = T(es, "rsb", [128, 3, NT], F32)
        slots = T(es, "slots", [128, nseq * NT, 2], I32)
        wts = T(es, "wts", [128, nseq * NT, 2], F32)
        base = T(es, "base", [128, 32], F32)
        wrt = T(es, "wrt", [128, 8, 36], F32)
        brt = T(es, "brt", [128, 36], F32)
        junk = T(es, "junk", [128, 1024], BF16)

        def HK(c, n):
            return f"H{c}_{n}"

        def small_col(dst_ap, src_ap, k, key):
            op('sp', I('dma_start', out=dst_ap, in_=src_ap.rearrange("(k p) -> p k", p=128),
                                           allow_slow_non_contiguous=True), w=[key], dma='c_' + key)

        small_col(gcols[:, 0, :], ln_mix_g, 8, 'gc0')
        small_col(gcols[:, 1, :], ln_mem_x_g, 8, 'gc1')
        small_col(gcols[:, 2, :], ln_mem_g, 8, 'gc2')
        small_col(sbg[:], sb_out_g, 4, 'sbg')
        small_col(cvb[:], conv_b_d, 4, 'cvb')
        small_col(cvg[:], conv_ln_g, 4, 'cvg')
        small_col(cvbb[:], conv_ln_b, 4, 'cvbb')
        op('sp', I('dma_start', out=wrt[:], in_=w_rt_d.rearrange("(k p) n -> p k n", p=128)),
           w=['wrt'], dma='c_wrt')
        op('sp', I('dma_start', out=brt[:], in_=b_rt_d.partition_broadcast(128)), w=['brt'], dma='c_brt')

        op('pool', I('memset', identf[:], 0.0), w=['identf'])
        op('pool', I('affine_select', out=identf[:], in_=identf[:], pattern=[[-1, 128]],
                                             compare_op=ALU.not_equal, fill=1.0, base=0, channel_multiplier=1),
           r=['identf'], w=['identf'])
        op('dve', I('tensor_copy', out=ident[:], in_=identf[:]), r=['identf'], w=['ident'])
        op('pool', I('memset', negtri[:], -1.0), w=['negtri'])
        op('pool', I('affine_select', out=negtri[:], in_=negtri[:], pattern=[[-1, 128]],
                                             compare_op=ALU.is_ge, fill=0.0, base=0, channel_multiplier=1),
           r=['negtri'], w=['negtri'])
        op('pool', I('memset', negones[:], -1.0), w=['negones'])
        op('dve', I('tensor_copy', out=negtriR[:].bitcast(F32R), in_=negtri[:]), r=['negtri'], w=['negtriR'])
        op('dve', I('tensor_copy', out=negonesR[:].bitcast(F32R), in_=negones[:]), r=['negones'], w=['negonesR'])
        op('pool', I('memset', meanmat[:], 1.0 / 512), w=['meanmat'])
        op('pool', I('memset', ones2[:], 1.0), w=['ones2'])
        op('pool', I('memset', onesb[:], 1.0), w=['onesb'])
        op('pool', I('memset', Lmat[:], 1.0), w=['Lmat'])
        op('pool', I('affine_select', out=Lmat[:], in_=Lmat[:], pattern=[[1, 128]],
                                             compare_op=ALU.is_gt, fill=0.0, base=0, channel_multiplier=-1),
           r=['Lmat'], w=['Lmat'])
        op('pool', I('memset', maskf[:], 1.0), w=['maskf'])
        for r_ in range(4):
            op('pool', I('affine_select', out=maskf[:, r_, :], in_=maskf[:, r_, :], pattern=[[1, 512]],
                                                        compare_op=ALU.is_gt, fill=0.0, base=-r_ * 128,
                                                        channel_multiplier=-1),
               r=['maskf'], w=['maskf'])
        op('pool', I('iota', ecap_i[:], pattern=[[CAP, 32]], base=0, channel_multiplier=0), w=['ecap_i'])
        op('dve', I('tensor_copy', out=ecap[:], in_=ecap_i[:]), r=['ecap_i'], w=['ecap'])
        op('pool', I('memset', base[:], 0.0), w=['base'])

        with ExitStack() as s0:
            cw_in = T(s0, "cw_in", [31, 512], F32)
            op('sp', I('dma_start', out=cw_in[:], in_=conv_w_d), w=['cw_in'], dma='c_cw')
            for c in range(4):
                op('pe', I('transpose', out=psB[:, 0, c * 32:c * 32 + 31], in_=cw_in[:, c * 128:(c + 1) * 128],
                                                    identity=identf[0:31, 0:31]),
                   r=['cw_in', 'identf'], w=['psB0'])
            op('act', I('activation', out=cwT[:], in_=psB[:, 0, 0:128].rearrange("p (c w) -> p c w", c=4)[:, :, 0:31],
                                             func=AF.Copy), r=['psB0'], w=['cwT'])
            S.barrier()

        wslot = [0]

        def load_w(src2d, nk=8):
            i = wslot[0]
            wslot[0] ^= 1
            op('pool', I('dma_start', out=wr[i][:, 0:nk, :], in_=src2d.rearrange("(k p) n -> p k n", p=128)),
               w=[f'wr{i}'], dma=f'wr{i}')
            return i

        evac_flip = [0]

        def evac_copy(out_ap, in_ap, r, w, scale=None, eng=None):
            if eng is None:
                eng = 'act' if (evac_flip[0] & 1) == 0 else 'dve'
                evac_flip[0] += 1
            if eng == 'act':
                if scale is None:
                    op('act', I('activation', out=out_ap, in_=in_ap, func=AF.Copy), r=r, w=w)
                else:
                    op('act', I('activation', out=out_ap, in_=in_ap, func=AF.Copy, scale=scale), r=r, w=w)
            else:
                if scale is None:
                    op('dve', I('tensor_copy', out=out_ap, in_=in_ap), r=r, w=w)
                else:
                    op('dve', I('tensor_scalar', out=out_ap, in0=in_ap, scalar1=scale, scalar2=None,
                                                        op0=ALU.mult), r=r, w=w)

        def rstd_from_ss(st, col, inv_n):
            op('act', I('activation', out=st[:, 1, col:col + 1], in_=st[:, 0, col:col + 1], func=AF.Sqrt,
                                             scale=inv_n, bias=EPS), r=['stat'], w=['stat'])
            op('dve', I('reciprocal', out=st[:, 2, col:col + 1], in_=st[:, 1, col:col + 1]),
               r=['stat'], w=['stat'])

        def norm_T(src_ap, src_key, hn_t, hn_key, gi, dst3, dst_keys, t, pa):
            op('act', I('activation', out=junk[:], in_=src_ap, func=AF.Square, accum_out=stat[:, 0, t:t + 1]),
               r=[src_key], w=['junk', 'stat'])
            rstd_from_ss(stat, t, 1.0 / DM)
            op('act', I('activation', out=hn_t[:], in_=src_ap, func=AF.Copy, scale=stat[:, 2, t:t + 1]),
               r=[src_key, 'stat'], w=[hn_key])
            for kc in range(8):
                op('pe', I('transpose', out=psA[:, pa, kc * 128:(kc + 1) * 128],
                                                      in_=hn_t[:, kc * 128:(kc + 1) * 128], identity=ident[:]),
                   r=[hn_key, 'ident'], w=[f'psA{pa}'])
            op('dve', I('tensor_tensor', out=dst3, in0=psA[:, pa, :].rearrange("p (k c) -> p k c", k=8),
                                                in1=gcols[:, gi, :, None].to_broadcast([128, 8, 128]), op=ALU.mult),
               r=[f'psA{pa}', f'gc{gi}'], w=dst_keys)

        def fm_proj(slot, ncc, rhs_fn, rhs_keys_fn, nN, N, evac_fn, banks):
            k = 0
            for cc in range(ncc):
                for n in range(nN):
                    bk = banks[k % len(banks)]
                    k += 1
                    for kc in range(8):
                        op('pe', I('matmul',
                            psB[:, bk, 0:N], wr[slot][:, kc, cc * 128:(cc + 1) * 128], rhs_fn(kc, n),
                            start=(kc == 0), stop=(kc == 7)),
                           r=[f'wr{slot}'] + rhs_keys_fn(kc, n), w=[f'psB{bk}'])
                    evac_fn(cc, n, bk)

        dbg = {}

        for b in range(nseq):
            with ExitStack() as s1:
                qk = T(s1, "qk", [128, 8, SEQ], BF16)
                v_sb = T(s1, "v_sb", [128, NT, 512], BF16)
                with ExitStack() as s1a:
                    xin = [T(s1a, f"xin{i}", [128, DM], F32) for i in range(3)]
                    hn = [T(s1a, f"hn{i}", [128, DM], BF16) for i in range(2)]
                    gT = T(s1a, "gT", [128, 4, 30 + SEQ], BF16)
                    ycv = T(s1a, "ycv", [128, 4, SEQ], F32)
                    dg = T(s1a, "dg", [128, 31, 128], BF16)
                    ysq = T(s1a, "ysq", [128, 4, 512], F32)
                    lnw = T(s1a, "lnw", [128, 4, 512], F32)

                    for t in range(NT):
                        xi = xin[t % 3]
                        op('sp', I('dma_start', out=xi[:], in_=x_d[b, t * 128:(t + 1) * 128, :]),
                           w=[f'xin{t % 3}'], dma=f'xin{t % 3}')
                        norm_T(xi[:], f'xin{t % 3}', hn[t % 2], f'hn{t % 2}', 0,
                               hT[:, :, t * 128:(t + 1) * 128], [HK(c, t // 4) for c in range(8)], t, t % 2)

                    hrhs = lambda kc, n: hT[:, kc, n * 512:(n + 1) * 512]
                    hkeys = lambda kc, n: [HK(kc, n)]
                    sl = load_w(w_in_d[:, 0:512])
                    nxt = load_w(w_in_d[:, 512:1024])
                    fm_proj(sl, 4, hrhs, hkeys, 4, 512,
                            lambda cc, n, bk: evac_copy(qk[:, cc, n * 512:(n + 1) * 512], psB[:, bk, :], [f'psB{bk}'],
                                                        [f'q{cc}_{n}'], scale=0.125), [0, 1, 2, 3])
                    sl = nxt
                    nxt = load_w(w_in_d[:, 1024:1536])
                    fm_proj(sl, 4, hrhs, hkeys, 4, 512,
                            lambda cc, n, bk: evac_copy(qk[:, 4 + cc, n * 512:(n + 1) * 512], psB[:, bk, :],
                                                        [f'psB{bk}'], [f'k{cc}_{n}']), [0, 1, 2, 3])
                    sl = nxt
                    nxt = load_w(w_in_d[:, 2048:2560])
                    for t in range(NT):
                        bk = t % 4
                        for kc in range(8):
                            op('pe', I('matmul',
                                psB[:, bk, :], hT[:, kc, t * 128:(t + 1) * 128], wr[sl][:, kc, :],
                                start=(kc == 0), stop=(kc == 7)),
                               r=[f'wr{sl}', HK(kc, t // 4)], w=[f'psB{bk}'])
                        evac_copy(v_sb[:, t, :], psB[:, bk, :], [f'psB{bk}'], [f'v{t}'])
                    op('pool', I('memset', gT[:, :, 0:30], 0.0), w=['gTpad'])
                    sl = nxt
                    nxt = load_w(w_in_d[:, 1536:2048])
                    fm_proj(sl, 4, hrhs, hkeys, 4, 512,
                            lambda cc, n, bk: op('act', I('activation',
                                out=gT[:, cc, 30 + n * 512:30 + (n + 1) * 512], in_=psB[:, bk, :], func=AF.Sigmoid),
                                r=[f'psB{bk}'], w=[f'g{cc}_{n}']), [0, 1, 2, 3])
                    sl = nxt
                    fm_proj(sl, 4, hrhs, hkeys, 4, 512,
                            lambda cc, n, bk: op('dve', I('tensor_tensor',
                                out=gT[:, cc, 30 + n * 512:30 + (n + 1) * 512], in0=psB[:, bk, :],
                                in1=gT[:, cc, 30 + n * 512:30 + (n + 1) * 512], op=ALU.mult),
                                r=[f'psB{bk}', f'g{cc}_{n}'], w=[f'g{cc}_{n}']), [0, 1, 2, 3])

                    for c in range(4):
                        for w_ in range(31):
                            op('dve', I('tensor_scalar',
                                out=dg[:, w_, :], in0=ident[:], scalar1=cwT[:, c, w_:w_ + 1], scalar2=None,
                                op0=ALU.mult), r=['ident', 'cwT'], w=[f'dg{w_}'])
                        for n in range(4):
                            bk = n % 4
                            gkeys = [f'g{c}_{n}'] + ([f'g{c}_{n - 1}'] if n > 0 else ['gTpad'])
                            for w_ in range(31):
                                op('pe', I('matmul',
                                    psB[:, bk, :], dg[:, w_, :], gT[:, c, n * 512 + w_:n * 512 + w_ + 512],
                                    start=(w_ == 0), stop=(w_ == 30)),
                                   r=[f'dg{w_}'] + gkeys, w=[f'psB{bk}'])
                            op('act', I('activation',
                                out=ycv[:, c, n * 512:(n + 1) * 512], in_=psB[:, bk, :], func=AF.Identity,
                                bias=cvb[:, c:c + 1]), r=[f'psB{bk}', 'cvb'], w=[f'y{c}_{n}'])
                    for n in range(4):
                        ns = slice(n * 512, (n + 1) * 512)
                        for c in range(4):
                            op('pe', I('matmul', psB[:, 4, :], meanmat[:], ycv[:, c, ns],
                                                                    start=(c == 0), stop=(c == 3)),
                               r=['meanmat', f'y{c}_{n}'], w=['psB4'])
                        for c in range(4):
                            op('act', I('activation', out=ysq[:, c, :], in_=ycv[:, c, ns],
                                                                         func=AF.Square),
                               r=[f'y{c}_{n}'], w=[f'ysq{c}'])
                        for c in range(4):
                            op('pe', I('matmul', psB[:, 5, :], meanmat[:], ysq[:, c, :],
                                                             start=(c == 0), stop=(c == 3)),
                               r=['meanmat', f'ysq{c}'], w=['psB5'])
                        op('act', I('activation', out=lnw[:, 0, :], in_=psB[:, 4, :], func=AF.Copy),
                           r=['psB4'], w=['lnw0'])
                        op('pool', I('tensor_tensor', out=lnw[:, 1, :], in0=lnw[:, 0, :], in1=lnw[:, 0, :],
                                                             op=ALU.mult), r=['lnw0'], w=['lnw1'])
                        op('dve', I('tensor_tensor', out=lnw[:, 1, :], in0=psB[:, 5, :], in1=lnw[:, 1, :],
                                                            op=ALU.subtract), r=['psB5', 'lnw1'], w=['lnw1'])
                        op('dve', I('tensor_scalar', out=lnw[:, 1, :], in0=lnw[:, 1, :], scalar1=0.0,
                                                            scalar2=None, op0=ALU.max), r=['lnw1'], w=['lnw1'])
                        op('act', I('activation', out=lnw[:, 1, :], in_=lnw[:, 1, :], func=AF.Sqrt, bias=EPS),
                           r=['lnw1'], w=['lnw1'])
                        op('dve', I('reciprocal', out=lnw[:, 2, :], in_=lnw[:, 1, :]), r=['lnw1'], w=['lnw2'])
                        for c in range(4):
                            op('pool', I('tensor_tensor', out=lnw[:, 3, :], in0=ycv[:, c, ns],
                                                                             in1=lnw[:, 0, :], op=ALU.subtract),
                               r=[f'y{c}_{n}', 'lnw0'], w=['lnw3'])
                            op('pool', I('tensor_tensor', out=lnw[:, 3, :], in0=lnw[:, 3, :], in1=lnw[:, 2, :],
                                                                 op=ALU.mult), r=['lnw3', 'lnw2'], w=['lnw3'])
                            op('act', I('activation',
                                out=hT[:, 4 + c, ns], in_=lnw[:, 3, :], func=AF.Silu, scale=cvg[:, c:c + 1],
                                bias=cvbb[:, c:c + 1]), r=['lnw3', 'cvg', 'cvbb'], w=[HK(4 + c, n)])
                    S.barrier()

                with ExitStack() as s1b:
                    spb = [T(s1b, f"spb{i}", [128, 512], F32) for i in range(4)]
                    ab = [T(s1b, f"ab{i}", [128, 512], BF16) for i in range(4)]
                    Rb = [T(s1b, f"Rb{i}", [128, 512], F32) for i in range(2)]
                    osq = [T(s1b, f"osq{i}", [128, 512], F32) for i in range(2)]
                    units = []
                    for j in range(4):
                        for qn in range(4):
                            kcs = list(range(4 * qn + 3, -1, -1))
                            for idx, kc in enumerate(kcs):
                                for hp in range(2):
                                    units.append((j, qn, idx, kc, hp, len(kcs)))
                    nU = len(units)
                    negtri_r = negtriR[:].bitcast(F32R)
                    negones_r = negonesR[:].bitcast(F32R)
                    oqc = [0]

                    def S1(i):
                        j, qn, idx, kc, hp, nk = units[i]
                        P = slice(hp * 64, hp * 64 + 64)
                        zb = i % 2
                        sp_t, spk = spb[i % 4], f'spb{i % 4}'
                        qs = slice(qn * 512, (qn + 1) * 512)
                        ks = slice(kc * 128, (kc + 1) * 128)
                        op('pe', I('matmul', psB[:, zb, :], qk[P, 4 + j, ks], qk[P, j, qs], start=True, stop=True),
                           r=[f'k{j}_{kc // 4}', f'q{j}_{qn}'], w=[f'psB{zb}'])
                        op('act', I('activation', out=sp_t[:].bitcast(F32R), in_=psB[:, zb, :], func=AF.Exp), r=[f'psB{zb}'], w=[spk])
                        op('act', I('activation', out=sp_t[:].bitcast(F32R), in_=sp_t[:], func=AF.Ln, bias=1.0), r=[spk], w=[spk])
                        if kc >= 4 * qn:
                            op('pool', I('tensor_tensor', out=sp_t[:].bitcast(F32R), in0=sp_t[:], in1=maskf[:, kc - 4 * qn, :],
                                         op=ALU.mult), r=[spk, 'maskf'], w=[spk])

                    def S2(i):
                        j, qn, idx, kc, hp, nk = units[i]
                        P = slice(hp * 64, hp * 64 + 64)
                        eb = 2 + i % 2
                        ek = f'psB{eb}'
                        sp_t, spk = spb[i % 4], f'spb{i % 4}'
                        ab_t, abk = ab[i % 4], f'ab{i % 4}'
                        qs = slice(qn * 512, (qn + 1) * 512)
                        ks = slice(kc * 128, (kc + 1) * 128)
                        op('pe', I('matmul', psB[:, eb, :], qk[P, 4 + j, ks], qk[P, j, qs], start=True, stop=False),
                           r=[f'k{j}_{kc // 4}', f'q{j}_{qn}'], w=[ek])
                        op('pe', I('matmul', psB[:, eb, :], negtri_r, sp_t[:].bitcast(F32R), start=False,
                                   stop=(idx == 0)), r=['negtriR', spk], w=[ek])
                        if idx > 0:
                            op('pe', I('matmul', psB[:, eb, :], negones_r, Rb[hp][:].bitcast(F32R), start=False,
                                       stop=True), r=['negonesR', f'Rb{hp}'], w=[ek])
                        op('act', I('activation', out=ab_t[:], in_=psB[:, eb, :], func=AF.Exp), r=[ek], w=[abk])
                        if kc >= 4 * qn:
                            op('dve', I('tensor_tensor', out=ab_t[:], in0=ab_t[:], in1=maskf[:, kc - 4 * qn, :],
                                        op=ALU.mult), r=[abk, 'maskf'], w=[abk])
                        if idx < nk - 1:
                            if idx == 0:
                                op('pool', I('tensor_copy', out=Rb[hp][:].bitcast(F32R), in_=sp_t[:]), r=[spk], w=[f'Rb{hp}'])
                            else:
                                op('pool', I('tensor_tensor', out=Rb[hp][:].bitcast(F32R), in0=Rb[hp][:], in1=sp_t[:], op=ALU.add),
                                   r=[spk, f'Rb{hp}'], w=[f'Rb{hp}'])

                    def S3(i):
                        j, qn, idx, kc, hp, nk = units[i]
                        h = 2 * j + hp
                        P = slice(hp * 64, hp * 64 + 64)
                        ab_t, abk = ab[i % 4], f'ab{i % 4}'
                        qs = slice(qn * 512, (qn + 1) * 512)
                        op('pe', I('matmul', psB[P, 4, :], v_sb[:, kc, h * 64:(h + 1) * 64], ab_t[:],
                                   start=(idx == 0), stop=(idx == nk - 1)), r=[f'v{kc}', abk], w=[f'psB4_{hp}'])
                        if idx == nk - 1 and hp == 1:
                            oq_t = osq[oqc[0] & 1]
                            oqk = f'osq{oqc[0] & 1}'
                            oqc[0] += 1
                            op('act', I('activation', out=hT[:, j, qs], in_=psB[:, 4, :], func=AF.Copy,
                                        scale=sbg[:, j:j + 1]), r=['psB4_0', 'psB4_1', 'sbg'], w=[HK(j, qn)])
                            op('act', I('activation', out=oq_t[:], in_=psB[:, 4, :], func=AF.Square),
                               r=['psB4_0', 'psB4_1'], w=[oqk])
                            for tt in range(4):
                                col = (j * 16 + qn * 4 + tt) * 2
                                op('pe', I('matmul', psB[:, 5, col:col + 2], oq_t[:, tt * 128:(tt + 1) * 128], ones2[:],
                                           start=True, stop=True), r=[oqk, 'ones2'], w=['psB5'])

                    for i in range(nU + 3):
                        if i < nU:
                            S1(i)
                        if 0 <= i - 2 < nU:
                            S2(i - 2)
                        if 0 <= i - 3 < nU:
                            S3(i - 3)
                    ssv = lambda j: psB[:, 5, j * 32:(j + 1) * 32].rearrange("p (t two) -> p t two", two=2)[:, :, 0]
                    op('act', I('activation', out=rsb[:, 0, :], in_=ssv(0), func=AF.Copy), r=['psB5'], w=['rsb'])
                    for j in range(1, 4):
                        op('dve', I('tensor_tensor', out=rsb[:, 0, :], in0=ssv(j), in1=rsb[:, 0, :],
                                                                 op=ALU.add), r=['psB5', 'rsb'], w=['rsb'])
                    op('act', I('activation', out=rsb[:, 1, :], in_=rsb[:, 0, :], func=AF.Sqrt, scale=1.0 / 512,
                                                     bias=EPS), r=['rsb'], w=['rsb'])
                    op('dve', I('reciprocal', out=rsb[:, 2, :], in_=rsb[:, 1, :]), r=['rsb'], w=['rsb'])
                    S.barrier()
                    if stage == 1 and b == 0:
                        op('sp', I('dma_start', out=dbg_mix, in_=hT[:]), dma='dbg0')
                        op('sp', I('dma_start', out=dbg_qk, in_=qk[:]), dma='dbg1')
                        op('sp', I('dma_start', out=dbg_v, in_=v_sb[:]), dma='dbg2')
                        op('sp', I('dma_start', out=dbg_rsb, in_=rsb[:]), dma='dbg3')
                        S.barrier()

            with ExitStack() as s2:
                x_sb = T(s2, "x_sb", [128, NT, DM], F32)
                for t in range(NT):
                    op('sp', I('dma_start', out=x_sb[:, t, :], in_=x_d[b, t * 128:(t + 1) * 128, :]),
                       w=[f'x{t}'], dma=f'x{t}')
                nxt = load_w(w_out_d[:, 0:512])
                for n in range(2):
                    sl = nxt
                    nxt = load_w(w_out_d[:, 512:1024]) if n == 0 else load_w(w_xkv_d[:, 0:512])
                    ns = slice(n * 512, (n + 1) * 512)
                    for t in range(NT):
                        b0, b1 = (t % 2) * 2, (t % 2) * 2 + 1
                        ts_ = slice(t * 128, (t + 1) * 128)
                        for jj in range(4):
                            op('pe', I('matmul',
                                psB[:, b0, :], hT[:, jj, ts_], wr[sl][:, jj, :], start=(jj == 0), stop=(jj == 3)),
                               r=[HK(jj, t // 4), f'wr{sl}'], w=[f'psB{b0}'])
                        for jj in range(4):
                            op('pe', I('matmul',
                                psB[:, b1, :], hT[:, 4 + jj, ts_], wr[sl][:, 4 + jj, :], start=(jj == 0), stop=(jj == 3)),
                               r=[HK(4 + jj, t // 4), f'wr{sl}'], w=[f'psB{b1}'])
                        op('dve', I('tensor_tensor',
                            out=x_sb[:, t, ns], in0=psB[:, b1, :], in1=x_sb[:, t, ns], op=ALU.add),
                           r=[f'psB{b1}', f'x{t}'], w=[f'x{t}'])
                        op('dve', I('scalar_tensor_tensor',
                            out=x_sb[:, t, ns], in0=psB[:, b0, :], scalar=rsb[:, 2, t:t + 1], in1=x_sb[:, t, ns],
                            op0=ALU.mult, op1=ALU.add), r=[f'psB{b0}', 'rsb', f'x{t}'], w=[f'x{t}'])
                if stage == 1:
                    for t in range(NT):
                        ev = op('sp', I('dma_start',
                            out=out_d[b * SEQ + t * 128:b * SEQ + (t + 1) * 128, :], in_=x_sb[:, t, :]),
                            r=[f'x{t}'], dma=f'o{t % 4}')
                    S.barrier()
                    continue

                with ExitStack() as s2f:
                    memin = [T(s2f, f"memin{i}", [128, DM], F32) for i in range(2)]
                    hn2 = [T(s2f, f"hnb{i}", [128, DM], BF16) for i in range(2)]
                    memT = T(s2f, "memT", [128, 8, NMEM], BF16)
                    kxT = T(s2f, "kxT", [128, 8, NMEM], BF16)
                    vx = T(s2f, "vx", [128, 2, DM], BF16)
                    qxT = T(s2f, "qxT", [128, 8, SEQ], BF16)
                    pf = [T(s2f, "pf0", [128, 4, NMEM], F32)] * 2
                    pn = [T(s2f, f"pn{i}", [128, 4, NMEM], BF16) for i in range(2)]
                    pT = [T(s2f, "pT0", [128, 4, 2, 512], BF16)] * 2
                    sm = T(s2f, "sm", [128, 4, 4], F32)
                    for mt in range(2):
                        op('sp', I('dma_start', out=memin[mt][:], in_=mem_d[b, mt * 128:(mt + 1) * 128, :]),
                           w=[f'memin{mt}'], dma=f'memin{mt}')
                        norm_T(memin[mt][:], f'memin{mt}', hn2[mt], f'hnb{mt}', 2,
                               memT[:, :, mt * 128:(mt + 1) * 128], ['memT'], mt, mt)
                    mrhs = lambda kc, n: memT[:, kc, :]
                    mkeys = lambda kc, n: ['memT']
                    for g in range(2):
                        sl = nxt
                        nxt = load_w(w_xkv_d[:, (g + 1) * 512:(g + 2) * 512])
                        fm_proj(sl, 4, mrhs, mkeys, 1, NMEM,
                                lambda cc, n, bk, g=g: evac_copy(kxT[:, g * 4 + cc, :], psB[:, bk, 0:NMEM], [f'psB{bk}'],
                                                                 ['kxT']), [0, 1, 2, 3])
                    for g in range(2):
                        sl = nxt
                        nxt = load_w(w_xkv_d[:, 1536:2048]) if g == 0 else load_w(w_xq_d[:, 0:512])
                        for mt in range(2):
                            bk = mt
                            for kc in range(8):
                                op('pe', I('matmul',
                                    psB[:, bk, :], memT[:, kc, mt * 128:(mt + 1) * 128], wr[sl][:, kc, :],
                                    start=(kc == 0), stop=(kc == 7)), r=['memT', f'wr{sl}'], w=[f'psB{bk}'])
                            evac_copy(vx[:, mt, g * 512:(g + 1) * 512], psB[:, bk, :], [f'psB{bk}'], ['vx'])
                    for t in range(NT):
                        norm_T(x_sb[:, t, :], f'x{t}', hn2[t % 2], f'hnb{t % 2}', 1,
                               hT[:, :, t * 128:(t + 1) * 128], [HK(c, t // 4) for c in range(8)], t, t % 2)
                    for g in range(2):
                        sl = nxt
                        nxt = load_w(w_xq_d[:, 512:1024]) if g == 0 else load_w(w_xo_d[:, 0:512])
                        fm_proj(sl, 4, hrhs, hkeys, 4, 512,
                                lambda cc, n, bk, g=g: evac_copy(qxT[:, g * 4 + cc, n * 512:(n + 1) * 512], psB[:, bk, :],
                                                                 [f'psB{bk}'], [f'qx{g * 4 + cc}_{n}'], scale=1.0 / 16),
                                [0, 1, 2, 3])
                    psS = psB[:, 0:2, :].rearrange("p a (h m) -> p (a h) m", h=2)
                    for n in range(4):
                        pT_t = pT[n % 2]
                        pTk = 'pT0'
                        for tt in range(4):
                            t = n * 4 + tt
                            ts_ = slice(t * 128, (t + 1) * 128)
                            pi = t % 2
                            for hx in range(4):
                                for dc in range(2):
                                    op('pe', I('matmul',
                                        psS[:, hx, :], qxT[:, 2 * hx + dc, ts_], kxT[:, 2 * hx + dc, :],
                                        start=(dc == 0), stop=(dc == 1)),
                                       r=[f'qx{2 * hx + dc}_{n}', 'kxT'], w=[f'psB{hx // 2}'])
                            op('dve', I('tensor_reduce', out=sm[:, 0, :], in_=psS, axis=AX.X, op=ALU.max),
                               r=['psB0', 'psB1'], w=['sm'])
                            op('dve', I('tensor_scalar', out=sm[:, 1, :], in0=sm[:, 0, :], scalar1=-1.0,
                                                                scalar2=None, op0=ALU.mult), r=['sm'], w=['sm'])
                            for hx in range(4):
                                op('act', I('activation',
                                    out=pf[pi][:, hx, :], in_=psS[:, hx, :], func=AF.Exp, bias=sm[:, 1, hx:hx + 1],
                                    accum_out=sm[:, 2, hx:hx + 1]),
                                   r=[f'psB{hx // 2}', 'sm'], w=['pf0', 'sm'])
                            op('dve', I('reciprocal', out=sm[:, 3, :], in_=sm[:, 2, :]), r=['sm'], w=['sm'])
                            for hx in range(4):
                                op('dve', I('tensor_scalar',
                                    out=pn[pi][:, hx, :], in0=pf[pi][:, hx, :], scalar1=sm[:, 3, hx:hx + 1],
                                    scalar2=None, op0=ALU.mult), r=['pf0', 'sm'], w=[f'pn{pi}'])
                            for hx in range(4):
                                for mc in range(2):
                                    op('pe', I('transpose',
                                        out=psA[:, pi, (hx * 2 + mc) * 128:(hx * 2 + mc + 1) * 128],
                                        in_=pn[pi][:, hx, mc * 128:(mc + 1) * 128], identity=ident[:]),
                                       r=[f'pn{pi}', 'ident'], w=[f'psA{pi}'])
                            op('act', I('activation',
                                out=pT_t[:, :, :, tt * 128:(tt + 1) * 128],
                                in_=psA[:, pi, :].rearrange("p (h m c) -> p h m c", h=4, m=2), func=AF.Copy),
                               r=[f'psA{pi}'], w=[pTk])
                        k = 0
                        for hx in range(4):
                            for dc in range(2):
                                bk = 2 + (k % 4)
                                k += 1
                                for mc in range(2):
                                    op('pe', I('matmul',
                                        psB[:, bk, :], vx[:, mc, hx * 256 + dc * 128:hx * 256 + (dc + 1) * 128],
                                        pT_t[:, hx, mc, :], start=(mc == 0), stop=(mc == 1)),
                                       r=['vx', pTk], w=[f'psB{bk}'])
                                evac_copy(hT[:, 2 * hx + dc, n * 512:(n + 1) * 512], psB[:, bk, :], [f'psB{bk}'],
                                          [HK(2 * hx + dc, n)])
                    for n in range(2):
                        sl = nxt
                        nxt = load_w(w_xo_d[:, 512:1024]) if n == 0 else None
                        ns = slice(n * 512, (n + 1) * 512)
                        for t in range(NT):
                            bk = t % 4
                            for cc in range(8):
                                op('pe', I('matmul',
                                    psB[:, bk, :], hT[:, cc, t * 128:(t + 1) * 128], wr[sl][:, cc, :],
                                    start=(cc == 0), stop=(cc == 7)), r=[HK(cc, t // 4), f'wr{sl}'], w=[f'psB{bk}'])
                            op('dve', I('tensor_tensor',
                                out=x_sb[:, t, ns], in0=psB[:, bk, :], in1=x_sb[:, t, ns], op=ALU.add),
                               r=[f'psB{bk}', f'x{t}'], w=[f'x{t}'])
                    S.barrier()
                if stage == 2:
                    for t in range(NT):
                        ev = op('sp', I('dma_start',
                            out=out_d[b * SEQ + t * 128:b * SEQ + (t + 1) * 128, :], in_=x_sb[:, t, :]),
                            r=[f'x{t}'], dma=f'o{t % 4}')
                    S.barrier()
                    continue

                with ExitStack() as s2g:
                    GB = 8
                    gbc = T(s2g, "gbc", [128, DM], F32)
                    h3 = [T(s2g, f"h3_{i}", [128, DM], F32) for i in range(2)]
                    h3b = T(s2g, "h3b", [128, GB, DM], BF16)
                    h3T = [T(s2g, f"h3T{i}", [128, 8, 128], F32) for i in range(2)]
                    LG = T(s2g, "LG", [128, GB, 36], F32)
                    gmax = T(s2g, "gmax", [128, GB], F32)
                    gsh = T(s2g, "gsh", [128, GB, 4], F32)
                    gex = T(s2g, "gex", [128, GB, 4], F32)
                    gsum = T(s2g, "gsum", [128, GB], F32)
                    gw = T(s2g, "gw", [128, GB], F32)
                    gm = T(s2g, "gm", [128, GB, 4], F32)
                    elm = T(s2g, "elm", [128, GB, 4, 8], F32)
                    igr = T(s2g, "igr", [128, GB, 8], F32)
                    ig2 = T(s2g, "ig2", [128, GB, 8], F32)
                    mk1 = T(s2g, "mk1", [128, GB, 8], F32)
                    mk2 = T(s2g, "mk2", [128, GB, 8], F32)
                    m12 = T(s2g, "m12", [128, 4, GB], F32)
                    M1 = T(s2g, "M1", [128, GB, 4, 8], F32)
                    M2 = T(s2g, "M2", [128, GB, 4, 8], F32)
                    S32b = T(s2g, "S32b", [128, GB, 32], BF16)
                    posa = T(s2g, "posa", [128, GB, 32], F32)
                    ova = T(s2g, "ova", [128, GB, 32], F32)
                    slf = T(s2g, "slf", [128, 2, GB], F32)
                    op('sp', I('dma_start', out=gbc[:], in_=ln_ffn_g.partition_broadcast(128)), w=['gbc'],
                       dma='gbc')

                    def G1(t):
                        hi = t % 2
                        tl = t % GB
                        op('act', I('activation', out=junk[:], in_=x_sb[:, t, :], func=AF.Square,
                                    accum_out=stat[:, 0, t:t + 1]), r=[f'x{t}'], w=['junk', 'stat'])
                        rstd_from_ss(stat, t, 1.0 / DM)
                        op('dve', I('scalar_tensor_tensor', out=h3[hi][:], in0=x_sb[:, t, :],
                                    scalar=stat[:, 2, t:t + 1], in1=gbc[:], op0=ALU.mult, op1=ALU.mult),
                           r=[f'x{t}', 'stat', 'gbc'], w=[f'h3_{hi}'])
                        op('pool', I('tensor_copy', out=h3b[:, tl, :], in_=h3[hi][:]), r=[f'h3_{hi}'], w=[f'h3b{tl}'])
                        b4, b5 = (4, 5) if hi == 0 else (2, 3)
                        for kc in range(8):
                            bb = b4 if kc < 4 else b5
                            op('pe', I('transpose', out=psB[:, bb, (kc % 4) * 128:(kc % 4 + 1) * 128],
                                       in_=h3[hi][:, kc * 128:(kc + 1) * 128], identity=identf[:]),
                               r=[f'h3_{hi}', 'identf'], w=[f'psB{bb}'])
                        op('act', I('activation', out=h3T[hi][:, 0:4, :],
                                    in_=psB[:, b4, :].rearrange("p (k c) -> p k c", k=4), func=AF.Copy),
                           r=[f'psB{b4}'], w=[f'h3T{hi}a'])
                        op('dve', I('tensor_copy', out=h3T[hi][:, 4:8, :],
                                    in_=psB[:, b5, :].rearrange("p (k c) -> p k c", k=4)),
                           r=[f'psB{b5}'], w=[f'h3T{hi}b'])
                        for kc in range(8):
                            op('pe', I('matmul', psB[:, hi, 0:36], h3T[hi][:, kc, :], wrt[:, kc, :],
                                       start=(kc == 0), stop=(kc == 7)),
                               r=[f'h3T{hi}a', f'h3T{hi}b', 'wrt'], w=[f'psB{hi}'])
                        op('dve', I('tensor_tensor', out=LG[:, tl, :], in0=psB[:, hi, 0:36], in1=brt[:], op=ALU.add),
                           r=[f'psB{hi}', 'brt'], w=['LG'])
                        op('sp', I('dma_start', out=xres_d[b * SEQ + t * 128:b * SEQ + (t + 1) * 128, :],
                                   in_=x_sb[:, t, :]), r=[f'x{t}'], w=[f'xres{t}'], dma=f'xw{t % 4}')

                    def G23(g):
                        tg0 = b * NT + g * GB
                        V = lambda f, r=(), w=(): op('dve', f, r=['rt2'] + list(r), w=['rt2'] + list(w))
                        GL = LG[:, :, 0:4]
                        EL = LG[:, :, 4:36].rearrange("p t (g e) -> p t g e", g=4)
                        bc3 = lambda ap2, n: ap2[:, :, None].to_broadcast([128, GB, n])
                        V(I('tensor_reduce', out=gmax[:], in_=GL, axis=AX.X, op=ALU.max), r=['LG'])
                        V(I('tensor_tensor', out=gsh[:], in0=GL, in1=bc3(gmax, 4), op=ALU.subtract), r=['LG'])
                        op('act', I('activation', out=gex[:], in_=gsh[:], func=AF.Exp), r=['rt2'], w=['rt2'])
                        V(I('tensor_reduce', out=gsum[:], in_=gex[:], axis=AX.X, op=ALU.add))
                        V(I('reciprocal', out=gw[:], in_=gsum[:]))
                        V(I('tensor_scalar', out=gm[:], in0=gsh[:], scalar1=0.0, scalar2=None, op0=ALU.is_equal))
                        V(I('tensor_tensor', out=elm[:], in0=EL, in1=gm[:, :, :, None].to_broadcast([128, GB, 4, 8]),
                            op=ALU.mult), r=['LG'])
                        V(I('tensor_reduce', out=igr[:], in_=elm[:].rearrange("p t g e -> p t e g"), axis=AX.X,
                            op=ALU.add))
                        V(I('tensor_reduce', out=m12[:, 0, :], in_=igr[:], axis=AX.X, op=ALU.max))
                        V(I('tensor_tensor', out=mk1[:], in0=igr[:], in1=bc3(m12[:, 0, :], 8), op=ALU.is_equal))
                        V(I('scalar_tensor_tensor', out=ig2[:], in0=mk1[:], scalar=-1e30, in1=igr[:], op0=ALU.mult,
                            op1=ALU.add))
                        V(I('tensor_reduce', out=m12[:, 1, :], in_=ig2[:], axis=AX.X, op=ALU.max))
                        V(I('tensor_tensor', out=mk2[:], in0=ig2[:], in1=bc3(m12[:, 1, :], 8), op=ALU.is_equal))
                        V(I('tensor_tensor', out=m12[:, 2, :], in0=m12[:, 1, :], in1=m12[:, 0, :], op=ALU.subtract))
                        op('act', I('activation', out=m12[:, 2, :], in_=m12[:, 2, :], func=AF.Exp), r=['rt2'], w=['rt2'])
                        V(I('tensor_scalar', out=m12[:, 3, :], in0=m12[:, 2, :], scalar1=1.0, scalar2=None, op0=ALU.add))
                        V(I('reciprocal', out=m12[:, 3, :], in_=m12[:, 3, :]))
                        V(I('tensor_tensor', out=wts[:, tg0:tg0 + GB, 0], in0=m12[:, 3, :], in1=gw[:], op=ALU.mult))
                        V(I('tensor_tensor', out=wts[:, tg0:tg0 + GB, 1], in0=m12[:, 2, :], in1=wts[:, tg0:tg0 + GB, 0],
                            op=ALU.mult))
                        gm4 = gm[:, :, :, None].to_broadcast([128, GB, 4, 8])
                        V(I('tensor_tensor', out=M1[:], in0=gm4, in1=mk1[:, :, None, :].to_broadcast([128, GB, 4, 8]),
                            op=ALU.mult))
                        V(I('tensor_tensor', out=M2[:], in0=gm4, in1=mk2[:, :, None, :].to_broadcast([128, GB, 4, 8]),
                            op=ALU.mult))
                        V(I('tensor_tensor', out=elm[:], in0=M1[:], in1=M2[:], op=ALU.add))
                        V(I('tensor_copy', out=S32b[:], in_=elm[:].rearrange("p t g e -> p t (g e)")))
                        for tl in range(GB):
                            pb = tl % 2
                            op('pe', I('matmul', psB[:, pb, 64:96], Lmat[:], S32b[:, tl, :], start=True, stop=True),
                               r=['Lmat', 'rt2'], w=[f'psB{pb}'])
                            op('pe', I('matmul', psB[:, pb, 96:128], onesb[:], S32b[:, tl, :], start=True, stop=True),
                               r=['onesb', 'rt2'], w=[f'psB{pb}'])
                            V(I('tensor_tensor', out=posa[:, tl, :], in0=psB[:, pb, 64:96], in1=base[:], op=ALU.add),
                              r=[f'psB{pb}', 'base'])
                            V(I('tensor_tensor', out=base[:], in0=psB[:, pb, 96:128], in1=base[:], op=ALU.add),
                              r=[f'psB{pb}', 'base'], w=['base'])
                        V(I('tensor_scalar', out=ova[:], in0=posa[:], scalar1=float(CAP), scalar2=1e7, op0=ALU.is_ge,
                            op1=ALU.mult))
                        V(I('tensor_tensor', out=posa[:], in0=posa[:], in1=ova[:], op=ALU.add))
                        V(I('tensor_tensor', out=posa[:], in0=posa[:], in1=ecap[:, None, :].to_broadcast([128, GB, 32]),
                            op=ALU.add))
                        V(I('tensor_tensor', out=ova[:], in0=posa[:], in1=M1[:].rearrange("p t g e -> p t (g e)"),
                            op=ALU.mult))
                        V(I('tensor_reduce', out=slf[:, 0, :], in_=ova[:], axis=AX.X, op=ALU.add))
                        V(I('tensor_tensor', out=ova[:], in0=posa[:], in1=M2[:].rearrange("p t g e -> p t (g e)"),
                            op=ALU.mult))
                        V(I('tensor_reduce', out=slf[:, 1, :], in_=ova[:], axis=AX.X, op=ALU.add))
                        V(I('tensor_copy', out=slots[:, tg0:tg0 + GB, :], in_=slf[:].rearrange("p k t -> p t k")),
                          w=['slotsg'])
                        for tl in range(GB):
                            for k2 in range(2):
                                op('pool', I('indirect_dma_start', out=xd_d[:, :],
                                             out_offset=bass.IndirectOffsetOnAxis(ap=slots[:, tg0 + tl, k2:k2 + 1], axis=0),
                                             in_=h3b[:, tl, :], in_offset=None, bounds_check='REG', oob_is_err=False),
                                   r=['slotsg', f'h3b{tl}'], w=[f'xd{tg0 + tl}_{k2}'], dma=f'sc{tl}_{k2}')

                    for g in range(NT // GB):
                        for tl in range(GB):
                            G1(g * GB + tl)
                        G23(g)
                    S.barrier()

        if stage >= 3:
            with ExitStack() as s3:
                wg = [T(s3, f"wg{i}", [128, 8, 512], BF16) for i in range(2)]
                wu = [T(s3, f"wu{i}", [128, 8, 512], BF16) for i in range(2)]
                wd = [T(s3, f"wd{i}", [128, 4, DM], BF16) for i in range(2)]
                xr = [T(s3, f"xr{i}", [128, DM], BF16) for i in range(3)]
                xT = [T(s3, f"xT{i}", [128, 8, 384], BF16) for i in range(2)]
                sg = [T(s3, f"sg{i}", [128, 384], F32) for i in range(2)]
                h1T = [T(s3, f"h1T{i}", [128, 4, 384], BF16) for i in range(2)]
                ydt = [T(s3, f"ydt{i}", [128, DM], F32) for i in range(2)]

                wst = [T(s3, f"wst{i}", [128, 8, 512], F32) for i in range(3)]

                def load_dma(e_):
                    op('act', I('dma_start', out=wst[0][:], in_=w_gate_d[e_].rearrange("(k p) n -> p k n", p=128)),
                       w=['wst0'], dma='wst0')
                    op('act', I('dma_start', out=wst[1][:], in_=w_up_d[e_].rearrange("(k p) n -> p k n", p=128)),
                       w=['wst1'], dma='wst1')
                    op('act', I('dma_start', out=wst[2][:].rearrange("p (a b) n -> p a (b n)", a=4),
                               in_=w_down_d[e_].rearrange("(k p) n -> p k n", p=128)), w=['wst2'], dma='wst2')

                def cast_gu(e_):
                    i = e_ % 2
                    op('dve', I('tensor_copy', out=wg[i][:], in_=wst[0][:]), r=['wst0'], w=[f'wg{i}'])
                    op('act', I('activation', out=wu[i][:], in_=wst[1][:], func=AF.Copy), r=['wst1'], w=[f'wu{i}'])

                def cast_d(e_):
                    i = e_ % 2
                    wsv = wst[2][:].rearrange("p (a b) n -> p a (b n)", a=4)
                    op('act', I('activation', out=wd[i][:, 0:2, :], in_=wsv[:, 0:2, :], func=AF.Copy),
                       r=['wst2'], w=[f'wd{i}a'])
                    op('dve', I('tensor_copy', out=wd[i][:, 2:4, :], in_=wsv[:, 2:4, :]), r=['wst2'], w=[f'wd{i}b'])

                def load_expert(e_):
                    load_dma(e_)
                    cast_gu(e_)
                    cast_d(e_)

                load_expert(0)
                cnt = 0
                yc = 0
                for e_ in range(NEXP):
                    wi = e_ % 2
                    if e_ + 1 < NEXP:
                        load_dma(e_ + 1)
                    for half in range(2):
                        if half == 1 and e_ + 1 < NEXP:
                            cast_gu(e_ + 1)
                        r0 = e_ * CAP + half * 384
                        hb = cnt % 2
                        cnt += 1
                        for ci in range(3):
                            xi = (cnt * 3 + ci) % 3
                            op('sp', I('dma_start',
                                out=xr[xi][:], in_=xd_d[r0 + ci * 128:r0 + (ci + 1) * 128, :]),
                                r=['xd'], w=[f'xr{xi}'], dma=f'xr{xi}')
                            pa = ci % 2
                            for kc in range(8):
                                op('pe', I('transpose',
                                    out=psA[:, pa, kc * 128:(kc + 1) * 128], in_=xr[xi][:, kc * 128:(kc + 1) * 128],
                                    identity=ident[:]), r=[f'xr{xi}', 'ident'], w=[f'psA{pa}'])
                            evac_copy(xT[hb][:, :, ci * 128:(ci + 1) * 128],
                                      psA[:, pa, :].rearrange("p (k c) -> p k c", k=8), [f'psA{pa}'], [f'xT{hb}'])
                        for dc in range(4):
                            bg, bu = (dc % 2) * 2, (dc % 2) * 2 + 1
                            for kc in range(8):
                                op('pe', I('matmul',
                                    psB[:, bg, 0:384], wg[wi][:, kc, dc * 128:(dc + 1) * 128], xT[hb][:, kc, :],
                                    start=(kc == 0), stop=(kc == 7)), r=[f'wg{wi}', f'xT{hb}'], w=[f'psB{bg}'])
                            for kc in range(8):
                                op('pe', I('matmul',
                                    psB[:, bu, 0:384], wu[wi][:, kc, dc * 128:(dc + 1) * 128], xT[hb][:, kc, :],
                                    start=(kc == 0), stop=(kc == 7)), r=[f'wu{wi}', f'xT{hb}'], w=[f'psB{bu}'])
                            si = dc % 2
                            op('act', I('activation', out=sg[si][:], in_=psB[:, bg, 0:384],
                                                                           func=AF.Silu),
                               r=[f'psB{bg}'], w=[f'sg{si}'])
                            op('dve', I('tensor_tensor',
                                out=h1T[hb][:, dc, :], in0=psB[:, bu, 0:384], in1=sg[si][:], op=ALU.mult),
                               r=[f'psB{bu}', f'sg{si}'], w=[f'h1T{hb}_{dc}'])
                        for ci in range(3):
                            yi = yc % 2
                            yc += 1
                            for nn in range(2):
                                for dc in range(4):
                                    op('pe', I('matmul',
                                        psB[:, 4 + nn, :], h1T[hb][:, dc, ci * 128:(ci + 1) * 128],
                                        wd[wi][:, dc, nn * 512:(nn + 1) * 512], start=(dc == 0), stop=(dc == 3)),
                                       r=[f'h1T{hb}_{dc}', f'wd{wi}a', f'wd{wi}b'], w=[f'psB{4 + nn}'])
                            op('act', I('activation', out=ydt[yi][:, 0:512], in_=psB[:, 4, :], func=AF.Copy),
                               r=['psB4'], w=[f'ydt{yi}a'])
                            op('dve', I('tensor_copy', out=ydt[yi][:, 512:1024], in_=psB[:, 5, :]),
                               r=['psB5'], w=[f'ydt{yi}b'])
                            op('sp', I('dma_start',
                                out=yd_d[r0 + ci * 128:r0 + (ci + 1) * 128, :], in_=ydt[yi][:]),
                                r=[f'ydt{yi}a', f'ydt{yi}b'], w=[f'yd{yc}'], dma=f'yo{yi}')
                    if e_ + 1 < NEXP:
                        cast_d(e_ + 1)
                S.barrier()

            with ExitStack() as s4:
                gbf = T(s4, "gbf", [128, DM], F32)
                y1 = [T(s4, f"y1_{i}", [128, DM], F32) for i in range(2)]
                y2 = [T(s4, f"y2_{i}", [128, DM], F32) for i in range(2)]
                xf = [T(s4, f"xf{i}", [128, DM], F32) for i in range(2)]
                of = [T(s4, f"of{i}", [128, DM], F32) for i in range(2)]
                fst = T(s4, "fst", [128, 3, nseq * NT], F32)
                zt = T(s4, "zt", [128, DM], F32)
                op('pool', I('memset', zt[:], 0.0), w=['zt'])
                op('sp', I('dma_start', out=gbf[:], in_=ln_final_g.partition_broadcast(128)), w=['gbf'], dma='gbf')
                def fetch(tg):
                    i = tg % 2
                    for ybuf, k2, nm in ((y1, 0, 'y1'), (y2, 1, 'y2')):
                        op('act', I('activation', out=ybuf[i][:], in_=zt[:], func=AF.Copy), r=['zt'], w=[f'{nm}_{i}'])
                        op('pool', I('indirect_dma_start', out=ybuf[i][:], out_offset=None, in_=yd_d[:, :],
                                     in_offset=bass.IndirectOffsetOnAxis(ap=slots[:, tg, k2:k2 + 1], axis=0),
                                     bounds_check='REG', oob_is_err=False),
                           r=['yd'], w=[f'{nm}_{i}'], dma=f'{nm}_{i}')
                    op('sp', I('dma_start', out=xf[i][:], in_=xres_d[tg * 128:(tg + 1) * 128, :]),
                       r=['xres'], w=[f'xf{i}'], dma=f'xf{i}')

                def compute(tg):
                    i = tg % 2
                    op('dve', I('scalar_tensor_tensor', out=xf[i][:], in0=y1[i][:], scalar=wts[:, tg, 0:1], in1=xf[i][:],
                                op0=ALU.mult, op1=ALU.add), r=[f'y1_{i}', f'xf{i}'], w=[f'xf{i}'])
                    op('dve', I('scalar_tensor_tensor', out=xf[i][:], in0=y2[i][:], scalar=wts[:, tg, 1:2], in1=xf[i][:],
                                op0=ALU.mult, op1=ALU.add), r=[f'y2_{i}', f'xf{i}'], w=[f'xf{i}'])
                    op('act', I('activation', out=junk[:], in_=xf[i][:], func=AF.Square, accum_out=fst[:, 0, tg:tg + 1]),
                       r=[f'xf{i}'], w=['junk', 'stat'])
                    rstd_from_ss(fst, tg, 1.0 / DM)
                    op('dve', I('scalar_tensor_tensor', out=of[i][:], in0=xf[i][:], scalar=fst[:, 2, tg:tg + 1],
                                in1=gbf[:], op0=ALU.mult, op1=ALU.mult), r=[f'xf{i}', 'stat', 'gbf'], w=[f'of{i}'])
                    op('sp', I('dma_start', out=out_d[tg * 128:(tg + 1) * 128, :], in_=of[i][:]),
                       r=[f'of{i}'], dma=f'of{i}')

                ntile = nseq * NT
                fetch(0)
                for tg in range(ntile):
                    if tg + 1 < ntile:
                        fetch(tg + 1)
                    compute(tg)
                S.barrier()
        S.barrier()
        S.emit()
    return nc


_NC_CACHE = {}


def _prep(inputs, c, nseq=NSEQ):
    f = lambda a: np.ascontiguousarray(np.asarray(a, dtype=np.float32))
    d = {
        "x": f(inputs["x"][c * nseq:(c + 1) * nseq]),
        "mem": f(inputs["mem"][c * nseq:(c + 1) * nseq]),
        "ln_mix_g": f(inputs["ln_mix_g"][0]),
        "w_in": f(inputs["w_in"][0]),
        "sb_out_g": f(inputs["sb_out_g"][0]),
        "conv_w": f(inputs["conv_w"][0]),
        "conv_b": f(inputs["conv_b"][0]),
        "conv_ln_g": f(inputs["conv_ln_g"][0]),
        "conv_ln_b": f(inputs["conv_ln_b"][0]),
        "w_out": f(inputs["w_out"][0]),
        "ln_mem_x_g": f(inputs["ln_mem_x_g"][0]),
        "ln_mem_g": f(inputs["ln_mem_g"][0]),
        "w_xq": f(inputs["w_xq"][0]),
        "w_xkv": f(inputs["w_xkv"][0]),
        "w_xo": f(inputs["w_xo"][0]),
        "ln_ffn_g": f(inputs["ln_ffn_g"][0]),
        "w_rt": f(np.concatenate([np.asarray(inputs["w_group"][0]), np.asarray(inputs["w_er"][0]).reshape(DM, 32)], axis=1)),
        "b_rt": f(np.concatenate([np.asarray(inputs["b_group"][0]), np.asarray(inputs["b_er"][0]).reshape(32)])),
        "w_gate": f(inputs["w_gate"][0]),
        "w_up": f(inputs["w_up"][0]),
        "w_down": f(inputs["w_down"][0]),
        "ln_final_g": f(inputs["ln_final_g"]),
    }
    return d


def kernel(**inputs):
    if 'nc' not in _NC_CACHE:
        _NC_CACHE['nc'] = build()
    nc = _NC_CACHE['nc']
    in_maps = [_prep(inputs, c) for c in range(N_CORES)]
    res = run_bass_kernel_spmd(nc, in_maps, core_ids=list(range(N_CORES)))
    out = np.concatenate([np.asarray(r["out"]).reshape(NSEQ, SEQ, DM) for r in res.results], axis=0)
    return out.astype(np.float32)
```

```python
import numpy as np
import concourse.bass as bass
import concourse.mybir as mybir
from concourse.bass_utils import run_bass_kernel_spmd
from contextlib import ExitStack

F32 = mybir.dt.float32
F32R = mybir.dt.float32r
BF16 = mybir.dt.bfloat16
I32 = mybir.dt.int32
AF = mybir.ActivationFunctionType
ALU = mybir.AluOpType
AX = mybir.AxisListType

ENG = ['pe', 'act', 'dve', 'pool', 'sp']
SAME_ENGINE_SYNC = True

N_CORES = 8
NSEQ = 4
SEQ = 2048
DM = 1024
NT = SEQ // 128
NMEM = 256
NEXP = 32
CAP = 768
NROWS = NEXP * CAP
EPS = 1e-6


def I(name, *args, **kw):
    return (name, args, kw)


class Sched:
    def __init__(self, nc, es):
        self.nc = nc
        self.es = es
        self.ins = {e: [] for e in ENG}
        self.cnt = {e: 0 for e in ENG}
        self.sem = {e: es.enter_context(nc.semaphore('s_' + e)) for e in ENG}
        self.dsem = {}
        self.lastw = {}
        self.readers = {}
        self.waited = {e: {} for e in ENG}

    def _semof(self, key):
        return self.sem[key[1]] if key[0] == 'e' else self.dsem[key[1]][0]

    def op(self, eng, fn, r=(), w=(), dma=None, extra=()):
        deps = {}

        def need(ev):
            if ev is None:
                return
            key, val, src, is_dma = ev
            if (not is_dma) and src == eng and (eng == 'pe' or not SAME_ENGINE_SYNC):
                return
            if deps.get(key, 0) < val:
                deps[key] = val

        for k in r:
            need(self.lastw.get(k))
        for k in w:
            need(self.lastw.get(k))
            for ev in self.readers.get(k, {}).values():
                need(ev)
        for ev in extra:
            need(ev)
        waits = []
        wd = self.waited[eng]
        for key, val in deps.items():
            if wd.get(key, 0) >= val:
                continue
            wd[key] = val
            waits.append((key, val))
        if dma is not None:
            if dma not in self.dsem:
                self.dsem[dma] = [self.es.enter_context(self.nc.semaphore('d_' + dma)), 0]
            self.dsem[dma][1] += 16
            ev = (('d', dma), self.dsem[dma][1], eng, True)
        else:
            self.cnt[eng] += 1
            ev = (('e', eng), self.cnt[eng], eng, False)
        self.ins[eng].append((waits, fn, ev))
        for k in r:
            d = self.readers.setdefault(k, {})
            old = d.get(ev[0])
            if old is None or old[1] < ev[1]:
                d[ev[0]] = ev
        for k in w:
            self.lastw[k] = ev
            self.readers[k] = {}
        return ev

    def barrier(self):
        evs = []
        for e in ENG:
            if self.cnt[e] > 0:
                evs.append((('e', e), self.cnt[e], e, False))
        for slot, (s, c) in self.dsem.items():
            if c > 0:
                evs.append((('d', slot), c, 'sp', True))
        for e in ENG:
            self.op(e, I('nop'), extra=[ev for ev in evs if not (ev[2] == e and not ev[3])])
        self.lastw = {}
        self.readers = {}

    def emit(self):
        nc = self.nc
        with nc.Block() as block:
            def body(name):
                def f(e):
                    bc_reg = None
                    if name == 'pool':
                        bc_reg = e.alloc_register()
                        e.reg_mov(bc_reg, NROWS - 1)
                    for waits, fn, ev in self.ins[name]:
                        for key, val in waits:
                            e.wait_ge(self._semof(key), val)
                        try:
                            kw = fn[2]
                            if kw.get('bounds_check', None) == 'REG':
                                kw = dict(kw, bounds_check=bc_reg)
                            ins = getattr(e, fn[0])(*fn[1], **kw)
                        except Exception:
                            print("EMIT FAIL", name, fn[0], fn[1], fn[2])
                            raise
                        key, val, _, is_dma = ev
                        ins.then_inc(self._semof(key), 16 if is_dma else 1)
                return f
            block.tensor(body('pe'))
            block.scalar(body('act'))
            block.vector(body('dve'))
            block.gpsimd(body('pool'))
            block.sync(body('sp'))


def build(nseq=NSEQ, stage=99):
    nc = bass.Bass('TRN2', target_bir_lowering=False)
    ntok = nseq * SEQ

    def din(name, shape, dt=F32):
        return nc.dram_tensor(name, list(shape), dt, kind="ExternalInput").ap()

    x_d = din("x", [nseq, SEQ, DM])
    mem_d = din("mem", [nseq, NMEM, DM])
    ln_mix_g = din("ln_mix_g", [DM])
    w_in_d = din("w_in", [DM, 2560])
    sb_out_g = din("sb_out_g", [512])
    conv_w_d = din("conv_w", [31, 512])
    conv_b_d = din("conv_b", [512])
    conv_ln_g = din("conv_ln_g", [512])
    conv_ln_b = din("conv_ln_b", [512])
    w_out_d = din("w_out", [DM, DM])
    ln_mem_x_g = din("ln_mem_x_g", [DM])
    ln_mem_g = din("ln_mem_g", [DM])
    w_xq_d = din("w_xq", [DM, DM])
    w_xkv_d = din("w_xkv", [DM, 2 * DM])
    w_xo_d = din("w_xo", [DM, DM])
    ln_ffn_g = din("ln_ffn_g", [DM])
    w_rt_d = din("w_rt", [DM, 36])
    b_rt_d = din("b_rt", [36])
    w_gate_d = din("w_gate", [NEXP, DM, 512])
    w_up_d = din("w_up", [NEXP, DM, 512])
    w_down_d = din("w_down", [NEXP, 512, DM])
    ln_final_g = din("ln_final_g", [DM])
    out_d = nc.dram_tensor("out", [ntok, DM], F32, kind="ExternalOutput").ap()
    if stage == 1:
        dbg_mix = nc.dram_tensor("dbg_mix", [128, 8, SEQ], BF16, kind="ExternalOutput").ap()
        dbg_qk = nc.dram_tensor("dbg_qk", [128, 8, SEQ], BF16, kind="ExternalOutput").ap()
        dbg_v = nc.dram_tensor("dbg_v", [128, NT, 512], BF16, kind="ExternalOutput").ap()
        dbg_rsb = nc.dram_tensor("dbg_rsb", [128, 3, NT], F32, kind="ExternalOutput").ap()
    xd_d = nc.dram_tensor("xd_scr", [NROWS, DM], BF16).ap()
    yd_d = nc.dram_tensor("yd_scr", [NROWS, DM], F32).ap()
    xres_d = nc.dram_tensor("xres_scr", [ntok, DM], F32).ap()

    with ExitStack() as es:
        S = Sched(nc, es)
        op = S.op

        uid = [0]

        def T(scope, name, shape, dt):
            uid[0] += 1
            return scope.enter_context(nc.sbuf_tensor(f"{name}_u{uid[0]}", shape, dt))

        psA = es.enter_context(nc.psum_tensor("psA", [128, 2, 1024], BF16))
        psB = es.enter_context(nc.psum_tensor("psB", [128, 6, 512], F32))

        identf = T(es, "identf", [128, 128], F32)
        ident = T(es, "ident", [128, 128], BF16)
        negtri = T(es, "negtri", [128, 128], F32)
        negones = T(es, "negones", [128, 128], F32)
        negtriR = T(es, "negtriR", [128, 128], F32)
        negonesR = T(es, "negonesR", [128, 128], F32)
        meanmat = T(es, "meanmat", [128, 128], F32)
        ones2 = T(es, "ones2", [128, 2], F32)
        onesb = T(es, "onesb", [128, 128], BF16)
        Lmat = T(es, "Lmat", [128, 128], BF16)
        maskf = T(es, "maskf", [128, 4, 512], BF16)
        ecap = T(es, "ecap", [128, 32], F32)
        ecap_i = T(es, "ecap_i", [128, 32], I32)
        gcols = T(es, "gcols", [128, 3, 8], F32)
        sbg = T(es, "sbg", [128, 4], F32)
        cvb = T(es, "cvb", [128, 4], F32)
        cvg = T(es, "cvg", [128, 4], F32)
        cvbb = T(es, "cvbb", [128, 4], F32)
        cwT = T(es, "cwT", [128, 4, 31], F32)
        hT = T(es, "hT", [128, 8, SEQ], BF16)
        wr = [T(es, f"wr{i}", [128, 8, 512], BF16) for i in range(2)]
        stat = T(es, "stat", [128, 3, NT], F32)
        rsb = T(es, "rsb", [128, 3, NT], F32)
        slots = T(es, "slots", [128, nseq * NT, 2], I32)
        wts = T(es, "wts", [128, nseq * NT, 2], F32)
        base = T(es, "base", [128, 32], F32)
        wrt = T(es, "wrt", [128, 8, 36], F32)
        brt = T(es, "brt", [128, 36], F32)
        junk = T(es, "junk", [128, 1024], BF16)

        def HK(c, n):
            return f"H{c}_{n}"

        def small_col(dst_ap, src_ap, k, key):
            op('sp', I('dma_start', out=dst_ap, in_=src_ap.rearrange("(k p) -> p k", p=128),
                                           allow_slow_non_contiguous=True), w=[key], dma='c_' + key)

        small_col(gcols[:, 0, :], ln_mix_g, 8, 'gc0')
        small_col(gcols[:, 1, :], ln_mem_x_g, 8, 'gc1')
        small_col(gcols[:, 2, :], ln_mem_g, 8, 'gc2')
        small_col(sbg[:], sb_out_g, 4, 'sbg')
        small_col(cvb[:], conv_b_d, 4, 'cvb')
        small_col(cvg[:], conv_ln_g, 4, 'cvg')
        small_col(cvbb[:], conv_ln_b, 4, 'cvbb')
        op('sp', I('dma_start', out=wrt[:], in_=w_rt_d.rearrange("(k p) n -> p k n", p=128)),
           w=['wrt'], dma='c_wrt')
        op('sp', I('dma_start', out=brt[:], in_=b_rt_d.partition_broadcast(128)), w=['brt'], dma='c_brt')

        op('pool', I('memset', identf[:], 0.0), w=['identf'])
        op('pool', I('affine_select', out=identf[:], in_=identf[:], pattern=[[-1, 128]],
                                             compare_op=ALU.not_equal, fill=1.0, base=0, channel_multiplier=1),
           r=['identf'], w=['identf'])
        op('dve', I('tensor_copy', out=ident[:], in_=identf[:]), r=['identf'], w=['ident'])
        op('pool', I('memset', negtri[:], -1.0), w=['negtri'])
        op('pool', I('affine_select', out=negtri[:], in_=negtri[:], pattern=[[-1, 128]],
                                             compare_op=ALU.is_ge, fill=0.0, base=0, channel_multiplier=1),
           r=['negtri'], w=['negtri'])
        op('pool', I('memset', negones[:], -1.0), w=['negones'])
        op('dve', I('tensor_copy', out=negtriR[:].bitcast(F32R), in_=negtri[:]), r=['negtri'], w=['negtriR'])
        op('dve', I('tensor_copy', out=negonesR[:].bitcast(F32R), in_=negones[:]), r=['negones'], w=['negonesR'])
        op('pool', I('memset', meanmat[:], 1.0 / 512), w=['meanmat'])
        op('pool', I('memset', ones2[:], 1.0), w=['ones2'])
        op('pool', I('memset', onesb[:], 1.0), w=['onesb'])
        op('pool', I('memset', Lmat[:], 1.0), w=['Lmat'])
        op('pool', I('affine_select', out=Lmat[:], in_=Lmat[:], pattern=[[1, 128]],
                                             compare_op=ALU.is_gt, fill=0.0, base=0, channel_multiplier=-1),
           r=['Lmat'], w=['Lmat'])
        op('pool', I('memset', maskf[:], 1.0), w=['maskf'])
        for r_ in range(4):
            op('pool', I('affine_select', out=maskf[:, r_, :], in_=maskf[:, r_, :], pattern=[[1, 512]],
                                                        compare_op=ALU.is_gt, fill=0.0, base=-r_ * 128,
                                                        channel_multiplier=-1),
               r=['maskf'], w=['maskf'])
        op('pool', I('iota', ecap_i[:], pattern=[[CAP, 32]], base=0, channel_multiplier=0), w=['ecap_i'])
        op('dve', I('tensor_copy', out=ecap[:], in_=ecap_i[:]), r=['ecap_i'], w=['ecap'])
        op('pool', I('memset', base[:], 0.0), w=['base'])

        with ExitStack() as s0:
            cw_in = T(s0, "cw_in", [31, 512], F32)
            op('sp', I('dma_start', out=cw_in[:], in_=conv_w_d), w=['cw_in'], dma='c_cw')
            for c in range(4):
                op('pe', I('transpose', out=psB[:, 0, c * 32:c * 32 + 31], in_=cw_in[:, c * 128:(c + 1) * 128],
                                                    identity=identf[0:31, 0:31]),
                   r=['cw_in', 'identf'], w=['psB0'])
            op('act', I('activation', out=cwT[:], in_=psB[:, 0, 0:128].rearrange("p (c w) -> p c w", c=4)[:, :, 0:31],
                                             func=AF.Copy), r=['psB0'], w=['cwT'])
            S.barrier()

        wslot = [0]

        def load_w(src2d, nk=8):
            i = wslot[0]
            wslot[0] ^= 1
            op('pool', I('dma_start', out=wr[i][:, 0:nk, :], in_=src2d.rearrange("(k p) n -> p k n", p=128)),
               w=[f'wr{i}'], dma=f'wr{i}')
            return i

        evac_flip = [0]

        def evac_copy(out_ap, in_ap, r, w, scale=None, eng=None):
            if eng is None:
                eng = 'act' if (evac_flip[0] & 1) == 0 else 'dve'
                evac_flip[0] += 1
            if eng == 'act':
                if scale is None:
                    op('act', I('activation', out=out_ap, in_=in_ap, func=AF.Copy), r=r, w=w)
                else:
                    op('act', I('activation', out=out_ap, in_=in_ap, func=AF.Copy, scale=scale), r=r, w=w)
            else:
                if scale is None:
                    op('dve', I('tensor_copy', out=out_ap, in_=in_ap), r=r, w=w)
                else:
                    op('dve', I('tensor_scalar', out=out_ap, in0=in_ap, scalar1=scale, scalar2=None,
                                                        op0=ALU.mult), r=r, w=w)

        def rstd_from_ss(st, col, inv_n):
            op('act', I('activation', out=st[:, 1, col:col + 1], in_=st[:, 0, col:col + 1], func=AF.Sqrt,
                                             scale=inv_n, bias=EPS), r=['stat'], w=['stat'])
            op('dve', I('reciprocal', out=st[:, 2, col:col + 1], in_=st[:, 1, col:col + 1]),
               r=['stat'], w=['stat'])

        def norm_T(src_ap, src_key, hn_t, hn_key, gi, dst3, dst_keys, t, pa):
            op('act', I('activation', out=junk[:], in_=src_ap, func=AF.Square, accum_out=stat[:, 0, t:t + 1]),
               r=[src_key], w=['junk', 'stat'])
            rstd_from_ss(stat, t, 1.0 / DM)
            op('act', I('activation', out=hn_t[:], in_=src_ap, func=AF.Copy, scale=stat[:, 2, t:t + 1]),
               r=[src_key, 'stat'], w=[hn_key])
            for kc in range(8):
                op('pe', I('transpose', out=psA[:, pa, kc * 128:(kc + 1) * 128],
                                                      in_=hn_t[:, kc * 128:(kc + 1) * 128], identity=ident[:]),
                   r=[hn_key, 'ident'], w=[f'psA{pa}'])
            op('dve', I('tensor_tensor', out=dst3, in0=psA[:, pa, :].rearrange("p (k c) -> p k c", k=8),
                                                in1=gcols[:, gi, :, None].to_broadcast([128, 8, 128]), op=ALU.mult),
               r=[f'psA{pa}', f'gc{gi}'], w=dst_keys)

        def fm_proj(slot, ncc, rhs_fn, rhs_keys_fn, nN, N, evac_fn, banks):
            k = 0
            for cc in range(ncc):
                for n in range(nN):
                    bk = banks[k % len(banks)]
                    k += 1
                    for kc in range(8):
                        op('pe', I('matmul',
                            psB[:, bk, 0:N], wr[slot][:, kc, cc * 128:(cc + 1) * 128], rhs_fn(kc, n),
                            start=(kc == 0), stop=(kc == 7)),
                           r=[f'wr{slot}'] + rhs_keys_fn(kc, n), w=[f'psB{bk}'])
                    evac_fn(cc, n, bk)

        dbg = {}

        for b in range(nseq):
            with ExitStack() as s1:
                qk = T(s1, "qk", [128, 8, SEQ], BF16)
                v_sb = T(s1, "v_sb", [128, NT, 512], BF16)
                with ExitStack() as s1a:
                    xin = [T(s1a, f"xin{i}", [128, DM], F32) for i in range(3)]
                    hn = [T(s1a, f"hn{i}", [128, DM], BF16) for i in range(2)]
                    gT = T(s1a, "gT", [128, 4, 30 + SEQ], BF16)
                    ycv = T(s1a, "ycv", [128, 4, SEQ], F32)
                    dg = T(s1a, "dg", [128, 31, 128], BF16)
                    ysq = T(s1a, "ysq", [128, 4, 512], F32)
                    lnw = T(s1a, "lnw", [128, 4, 512], F32)

                    for t in range(NT):
                        xi = xin[t % 3]
                        op('sp', I('dma_start', out=xi[:], in_=x_d[b, t * 128:(t + 1) * 128, :]),
                           w=[f'xin{t % 3}'], dma=f'xin{t % 3}')
                        norm_T(xi[:], f'xin{t % 3}', hn[t % 2], f'hn{t % 2}', 0,
                               hT[:, :, t * 128:(t + 1) * 128], [HK(c, t // 4) for c in range(8)], t, t % 2)

                    hrhs = lambda kc, n: hT[:, kc, n * 512:(n + 1) * 512]
                    hkeys = lambda kc, n: [HK(kc, n)]
                    sl = load_w(w_in_d[:, 0:512])
                    nxt = load_w(w_in_d[:, 512:1024])
                    fm_proj(sl, 4, hrhs, hkeys, 4, 512,
                            lambda cc, n, bk: evac_copy(qk[:, cc, n * 512:(n + 1) * 512], psB[:, bk, :], [f'psB{bk}'],
                                                        [f'q{cc}_{n}'], scale=0.125), [0, 1, 2, 3])
                    sl = nxt
                    nxt = load_w(w_in_d[:, 1024:1536])
                    fm_proj(sl, 4, hrhs, hkeys, 4, 512,
                            lambda cc, n, bk: evac_copy(qk[:, 4 + cc, n * 512:(n + 1) * 512], psB[:, bk, :],
                                                        [f'psB{bk}'], [f'k{cc}_{n}']), [0, 1, 2, 3])
                    sl = nxt
                    nxt = load_w(w_in_d[:, 2048:2560])
                    for t in range(NT):
                        bk = t % 4
                        for kc in range(8):
                            op('pe', I('matmul',
                                psB[:, bk, :], hT[:, kc, t * 128:(t + 1) * 128], wr[sl][:, kc, :],
                                start=(kc == 0), stop=(kc == 7)),
                               r=[f'wr{sl}', HK(kc, t // 4)], w=[f'psB{bk}'])
                        evac_copy(v_sb[:, t, :], psB[:, bk, :], [f'psB{bk}'], [f'v{t}'])
                    op('pool', I('memset', gT[:, :, 0:30], 0.0), w=['gTpad'])
                    sl = nxt
                    nxt = load_w(w_in_d[:, 1536:2048])
                    fm_proj(sl, 4, hrhs, hkeys, 4, 512,
                            lambda cc, n, bk: op('act', I('activation',
                                out=gT[:, cc, 30 + n * 512:30 + (n + 1) * 512], in_=psB[:, bk, :], func=AF.Sigmoid),
                                r=[f'psB{bk}'], w=[f'g{cc}_{n}']), [0, 1, 2, 3])
                    sl = nxt
                    fm_proj(sl, 4, hrhs, hkeys, 4, 512,
                            lambda cc, n, bk: op('dve', I('tensor_tensor',
                                out=gT[:, cc, 30 + n * 512:30 + (n + 1) * 512], in0=psB[:, bk, :],
                                in1=gT[:, cc, 30 + n * 512:30 + (n + 1) * 512], op=ALU.mult),
                                r=[f'psB{bk}', f'g{cc}_{n}'], w=[f'g{cc}_{n}']), [0, 1, 2, 3])

                    for c in range(4):
                        for w_ in range(31):
                            op('dve', I('tensor_scalar',
                                out=dg[:, w_, :], in0=ident[:], scalar1=cwT[:, c, w_:w_ + 1], scalar2=None,
                                op0=ALU.mult), r=['ident', 'cwT'], w=[f'dg{w_}'])
                        for n in range(4):
                            bk = n % 4
                            gkeys = [f'g{c}_{n}'] + ([f'g{c}_{n - 1}'] if n > 0 else ['gTpad'])
                            for w_ in range(31):
                                op('pe', I('matmul',
                                    psB[:, bk, :], dg[:, w_, :], gT[:, c, n * 512 + w_:n * 512 + w_ + 512],
                                    start=(w_ == 0), stop=(w_ == 30)),
                                   r=[f'dg{w_}'] + gkeys, w=[f'psB{bk}'])
                            op('act', I('activation',
                                out=ycv[:, c, n * 512:(n + 1) * 512], in_=psB[:, bk, :], func=AF.Identity,
                                bias=cvb[:, c:c + 1]), r=[f'psB{bk}', 'cvb'], w=[f'y{c}_{n}'])
                    for n in range(4):
                        ns = slice(n * 512, (n + 1) * 512)
                        for c in range(4):
                            op('pe', I('matmul', psB[:, 4, :], meanmat[:], ycv[:, c, ns],
                                                                    start=(c == 0), stop=(c == 3)),
                               r=['meanmat', f'y{c}_{n}'], w=['psB4'])
                        for c in range(4):
                            op('act', I('activation', out=ysq[:, c, :], in_=ycv[:, c, ns],
                                                                         func=AF.Square),
                               r=[f'y{c}_{n}'], w=[f'ysq{c}'])
                        for c in range(4):
                            op('pe', I('matmul', psB[:, 5, :], meanmat[:], ysq[:, c, :],
                                                             start=(c == 0), stop=(c == 3)),
                               r=['meanmat', f'ysq{c}'], w=['psB5'])
                        op('act', I('activation', out=lnw[:, 0, :], in_=psB[:, 4, :], func=AF.Copy),
                           r=['psB4'], w=['lnw0'])
                        op('pool', I('tensor_tensor', out=lnw[:, 1, :], in0=lnw[:, 0, :], in1=lnw[:, 0, :],
                                                             op=ALU.mult), r=['lnw0'], w=['lnw1'])
                        op('dve', I('tensor_tensor', out=lnw[:, 1, :], in0=psB[:, 5, :], in1=lnw[:, 1, :],
                                                            op=ALU.subtract), r=['psB5', 'lnw1'], w=['lnw1'])
                        op('dve', I('tensor_scalar', out=lnw[:, 1, :], in0=lnw[:, 1, :], scalar1=0.0,
                                                            scalar2=None, op0=ALU.max), r=['lnw1'], w=['lnw1'])
                        op('act', I('activation', out=lnw[:, 1, :], in_=lnw[:, 1, :], func=AF.Sqrt, bias=EPS),
                           r=['lnw1'], w=['lnw1'])
                        op('dve', I('reciprocal', out=lnw[:, 2, :], in_=lnw[:, 1, :]), r=['lnw1'], w=['lnw2'])
                        for c in range(4):
                            op('pool', I('tensor_tensor', out=lnw[:, 3, :], in0=ycv[:, c, ns],
                                                                             in1=lnw[:, 0, :], op=ALU.subtract),
                               r=[f'y{c}_{n}', 'lnw0'], w=['lnw3'])
                            op('pool', I('tensor_tensor', out=lnw[:, 3, :], in0=lnw[:, 3, :], in1=lnw[:, 2, :],
                                                                 op=ALU.mult), r=['lnw3', 'lnw2'], w=['lnw3'])
                            op('act', I('activation',
                                out=hT[:, 4 + c, ns], in_=lnw[:, 3, :], func=AF.Silu, scale=cvg[:, c:c + 1],
                                bias=cvbb[:, c:c + 1]), r=['lnw3', 'cvg', 'cvbb'], w=[HK(4 + c, n)])
                    S.barrier()

                with ExitStack() as s1b:
                    spb = [T(s1b, f"spb{i}", [128, 512], F32) for i in range(4)]
                    ab = [T(s1b, f"ab{i}", [128, 512], BF16) for i in range(4)]
                    Rb = [T(s1b, f"Rb{i}", [128, 512], F32) for i in range(2)]
                    osq = [T(s1b, f"osq{i}", [128, 512], F32) for i in range(2)]
                    units = []
                    for j in range(4):
                        for qn in range(4):
                            kcs = list(range(4 * qn + 3, -1, -1))
                            for idx, kc in enumerate(kcs):
                                for hp in range(2):
                                    units.append((j, qn, idx, kc, hp, len(kcs)))
                    nU = len(units)
                    negtri_r = negtriR[:].bitcast(F32R)
                    negones_r = negonesR[:].bitcast(F32R)
                    oqc = [0]

                    def geo(i):
                        j, qn, idx, kc, hp, nk = units[i]
                        return (j, qn, idx, kc, hp, nk, slice(hp * 64, hp * 64 + 64), slice(qn * 512, (qn + 1) * 512),
                                slice(kc * 128, (kc + 1) * 128))

                    def S1(i0_):
                        us = [i0_, i0_ + 1]
                        for i in us:
                            j, qn, idx, kc, hp, nk, P, qs, ks = geo(i)
                            zb = i % 2
                            op('pe', I('matmul', psB[:, zb, :], qk[P, 4 + j, ks], qk[P, j, qs], start=True, stop=True),
                               r=[f'k{j}_{kc // 4}', f'q{j}_{qn}'], w=[f'psB{zb}'])
                        for i in us:
                            j, qn, idx, kc, hp, nk, P, qs, ks = geo(i)
                            zb = i % 2
                            sp_t, spk = spb[i % 4], f'spb{i % 4}'
                            op('act', I('activation', out=sp_t[:].bitcast(F32R), in_=psB[:, zb, :], func=AF.Exp),
                               r=[f'psB{zb}'], w=[spk])
                            op('act', I('activation', out=sp_t[:].bitcast(F32R), in_=sp_t[:], func=AF.Ln, bias=1.0),
                               r=[spk], w=[spk])
                            if kc >= 4 * qn:
                                op('pool', I('tensor_tensor', out=sp_t[:].bitcast(F32R), in0=sp_t[:],
                                             in1=maskf[:, kc - 4 * qn, :], op=ALU.mult), r=[spk, 'maskf'], w=[spk])

                    def S2(i0_):
                        us = [i0_, i0_ + 1]
                        for i in us:
                            j, qn, idx, kc, hp, nk, P, qs, ks = geo(i)
                            eb = 2 + i % 2
                            op('pe', I('matmul', psB[:, eb, :], qk[P, 4 + j, ks], qk[P, j, qs], start=True, stop=False),
                               r=[f'k{j}_{kc // 4}', f'q{j}_{qn}'], w=[f'psB{eb}'])
                        for i in us:
                            j, qn, idx, kc, hp, nk, P, qs, ks = geo(i)
                            eb = 2 + i % 2
                            ek = f'psB{eb}'
                            sp_t, spk = spb[i % 4], f'spb{i % 4}'
                            op('pe', I('matmul', psB[:, eb, :], negtri_r, sp_t[:].bitcast(F32R), start=False,
                                       stop=(idx == 0)), r=['negtriR', spk], w=[ek])
                            if idx > 0:
                                op('pe', I('matmul', psB[:, eb, :], negones_r, Rb[hp][:].bitcast(F32R), start=False,
                                           stop=True), r=['negonesR', f'Rb{hp}'], w=[ek])
                        for i in us:
                            j, qn, idx, kc, hp, nk, P, qs, ks = geo(i)
                            eb = 2 + i % 2
                            ek = f'psB{eb}'
                            sp_t, spk = spb[i % 4], f'spb{i % 4}'
                            ab_t, abk = ab[i % 4], f'ab{i % 4}'
                            op('act', I('activation', out=ab_t[:], in_=psB[:, eb, :], func=AF.Exp), r=[ek], w=[abk])
                            if kc >= 4 * qn:
                                op('dve', I('tensor_tensor', out=ab_t[:], in0=ab_t[:], in1=maskf[:, kc - 4 * qn, :],
                                            op=ALU.mult), r=[abk, 'maskf'], w=[abk])
                            if idx < nk - 1:
                                if idx == 0:
                                    op('pool', I('tensor_copy', out=Rb[hp][:].bitcast(F32R), in_=sp_t[:]), r=[spk],
                                       w=[f'Rb{hp}'])
                                else:
                                    op('pool', I('tensor_tensor', out=Rb[hp][:].bitcast(F32R), in0=Rb[hp][:],
                                                 in1=sp_t[:], op=ALU.add), r=[spk, f'Rb{hp}'], w=[f'Rb{hp}'])

                    def S3(i0_):
                        us = [i0_, i0_ + 1]
                        for i in us:
                            j, qn, idx, kc, hp, nk, P, qs, ks = geo(i)
                            h = 2 * j + hp
                            ab_t, abk = ab[i % 4], f'ab{i % 4}'
                            op('pe', I('matmul', psB[P, 4, :], v_sb[:, kc, h * 64:(h + 1) * 64], ab_t[:],
                                       start=(idx == 0), stop=(idx == nk - 1)), r=[f'v{kc}', abk], w=[f'psB4_{hp}'])
                        j, qn, idx, kc, hp, nk, P, qs, ks = geo(i0_ + 1)
                        if idx == nk - 1:
                            oq_t = osq[oqc[0] & 1]
                            oqk = f'osq{oqc[0] & 1}'
                            oqc[0] += 1
                            op('act', I('activation', out=hT[:, j, qs], in_=psB[:, 4, :], func=AF.Copy,
                                        scale=sbg[:, j:j + 1]), r=['psB4_0', 'psB4_1', 'sbg'], w=[HK(j, qn)])
                            op('act', I('activation', out=oq_t[:], in_=psB[:, 4, :], func=AF.Square),
                               r=['psB4_0', 'psB4_1'], w=[oqk])
                            for tt in range(4):
                                col = (j * 16 + qn * 4 + tt) * 2
                                op('pe', I('matmul', psB[:, 5, col:col + 2], oq_t[:, tt * 128:(tt + 1) * 128], ones2[:],
                                           start=True, stop=True), r=[oqk, 'ones2'], w=['psB5'])

                    nP = nU // 2
                    for p in range(nP + 2):
                        if p < nP:
                            S1(2 * p)
                        if 0 <= p - 1 < nP:
                            S2(2 * (p - 1))
                        if 0 <= p - 2 < nP:
                            S3(2 * (p - 2))
                    ssv = lambda j: psB[:, 5, j * 32:(j + 1) * 32].rearrange("p (t two) -> p t two", two=2)[:, :, 0]
                    op('act', I('activation', out=rsb[:, 0, :], in_=ssv(0), func=AF.Copy), r=['psB5'], w=['rsb'])
                    for j in range(1, 4):
                        op('dve', I('tensor_tensor', out=rsb[:, 0, :], in0=ssv(j), in1=rsb[:, 0, :],
                                                                 op=ALU.add), r=['psB5', 'rsb'], w=['rsb'])
                    op('act', I('activation', out=rsb[:, 1, :], in_=rsb[:, 0, :], func=AF.Sqrt, scale=1.0 / 512,
                                                     bias=EPS), r=['rsb'], w=['rsb'])
                    op('dve', I('reciprocal', out=rsb[:, 2, :], in_=rsb[:, 1, :]), r=['rsb'], w=['rsb'])
                    S.barrier()
                    if stage == 1 and b == 0:
                        op('sp', I('dma_start', out=dbg_mix, in_=hT[:]), dma='dbg0')
                        op('sp', I('dma_start', out=dbg_qk, in_=qk[:]), dma='dbg1')
                        op('sp', I('dma_start', out=dbg_v, in_=v_sb[:]), dma='dbg2')
                        op('sp', I('dma_start', out=dbg_rsb, in_=rsb[:]), dma='dbg3')
                        S.barrier()

            with ExitStack() as s2:
                x_sb = T(s2, "x_sb", [128, NT, DM], F32)
                for t in range(NT):
                    op('sp', I('dma_start', out=x_sb[:, t, :], in_=x_d[b, t * 128:(t + 1) * 128, :]),
                       w=[f'x{t}'], dma=f'x{t}')
                nxt = load_w(w_out_d[:, 0:512])
                for n in range(2):
                    sl = nxt
                    nxt = load_w(w_out_d[:, 512:1024]) if n == 0 else load_w(w_xkv_d[:, 0:512])
                    ns = slice(n * 512, (n + 1) * 512)
                    for t in range(NT):
                        b0, b1 = (t % 2) * 2, (t % 2) * 2 + 1
                        ts_ = slice(t * 128, (t + 1) * 128)
                        for jj in range(4):
                            op('pe', I('matmul',
                                psB[:, b0, :], hT[:, jj, ts_], wr[sl][:, jj, :], start=(jj == 0), stop=(jj == 3)),
                               r=[HK(jj, t // 4), f'wr{sl}'], w=[f'psB{b0}'])
                        for jj in range(4):
                            op('pe', I('matmul',
                                psB[:, b1, :], hT[:, 4 + jj, ts_], wr[sl][:, 4 + jj, :], start=(jj == 0), stop=(jj == 3)),
                               r=[HK(4 + jj, t // 4), f'wr{sl}'], w=[f'psB{b1}'])
                        op('dve', I('tensor_tensor',
                            out=x_sb[:, t, ns], in0=psB[:, b1, :], in1=x_sb[:, t, ns], op=ALU.add),
                           r=[f'psB{b1}', f'x{t}'], w=[f'x{t}'])
                        op('dve', I('scalar_tensor_tensor',
                            out=x_sb[:, t, ns], in0=psB[:, b0, :], scalar=rsb[:, 2, t:t + 1], in1=x_sb[:, t, ns],
                            op0=ALU.mult, op1=ALU.add), r=[f'psB{b0}', 'rsb', f'x{t}'], w=[f'x{t}'])
                if stage == 1:
                    for t in range(NT):
                        ev = op('sp', I('dma_start',
                            out=out_d[b * SEQ + t * 128:b * SEQ + (t + 1) * 128, :], in_=x_sb[:, t, :]),
                            r=[f'x{t}'], dma=f'o{t % 4}')
                    S.barrier()
                    continue

                with ExitStack() as s2f:
                    memin = [T(s2f, f"memin{i}", [128, DM], F32) for i in range(2)]
                    hn2 = [T(s2f, f"hnb{i}", [128, DM], BF16) for i in range(2)]
                    memT = T(s2f, "memT", [128, 8, NMEM], BF16)
                    kxT = T(s2f, "kxT", [128, 8, NMEM], BF16)
                    vx = T(s2f, "vx", [128, 2, DM], BF16)
                    qxT = T(s2f, "qxT", [128, 8, SEQ], BF16)
                    pf = [T(s2f, "pf0", [128, 4, NMEM], F32)] * 2
                    pn = [T(s2f, f"pn{i}", [128, 4, NMEM], BF16) for i in range(2)]
                    pT = [T(s2f, "pT0", [128, 4, 2, 512], BF16)] * 2
                    sm = T(s2f, "sm", [128, 4, 4], F32)
                    for mt in range(2):
                        op('sp', I('dma_start', out=memin[mt][:], in_=mem_d[b, mt * 128:(mt + 1) * 128, :]),
                           w=[f'memin{mt}'], dma=f'memin{mt}')
                        norm_T(memin[mt][:], f'memin{mt}', hn2[mt], f'hnb{mt}', 2,
                               memT[:, :, mt * 128:(mt + 1) * 128], ['memT'], mt, mt)
                    mrhs = lambda kc, n: memT[:, kc, :]
                    mkeys = lambda kc, n: ['memT']
                    for g in range(2):
                        sl = nxt
                        nxt = load_w(w_xkv_d[:, (g + 1) * 512:(g + 2) * 512])
                        fm_proj(sl, 4, mrhs, mkeys, 1, NMEM,
                                lambda cc, n, bk, g=g: evac_copy(kxT[:, g * 4 + cc, :], psB[:, bk, 0:NMEM], [f'psB{bk}'],
                                                                 ['kxT']), [0, 1, 2, 3])
                    for g in range(2):
                        sl = nxt
                        nxt = load_w(w_xkv_d[:, 1536:2048]) if g == 0 else load_w(w_xq_d[:, 0:512])
                        for mt in range(2):
                            bk = mt
                            for kc in range(8):
                                op('pe', I('matmul',
                                    psB[:, bk, :], memT[:, kc, mt * 128:(mt + 1) * 128], wr[sl][:, kc, :],
                                    start=(kc == 0), stop=(kc == 7)), r=['memT', f'wr{sl}'], w=[f'psB{bk}'])
                            evac_copy(vx[:, mt, g * 512:(g + 1) * 512], psB[:, bk, :], [f'psB{bk}'], ['vx'])
                    for t in range(NT):
                        norm_T(x_sb[:, t, :], f'x{t}', hn2[t % 2], f'hnb{t % 2}', 1,
                               hT[:, :, t * 128:(t + 1) * 128], [HK(c, t // 4) for c in range(8)], t, t % 2)
                    for g in range(2):
                        sl = nxt
                        nxt = load_w(w_xq_d[:, 512:1024]) if g == 0 else load_w(w_xo_d[:, 0:512])
                        fm_proj(sl, 4, hrhs, hkeys, 4, 512,
                                lambda cc, n, bk, g=g: evac_copy(qxT[:, g * 4 + cc, n * 512:(n + 1) * 512], psB[:, bk, :],
                                                                 [f'psB{bk}'], [f'qx{g * 4 + cc}_{n}'], scale=1.0 / 16),
                                [0, 1, 2, 3])
                    psS = psB[:, 0:2, :].rearrange("p a (h m) -> p (a h) m", h=2)
                    for n in range(4):
                        pT_t = pT[n % 2]
                        pTk = 'pT0'
                        for tt in range(4):
                            t = n * 4 + tt
                            ts_ = slice(t * 128, (t + 1) * 128)
                            pi = t % 2
                            for hx in range(4):
                                for dc in range(2):
                                    op('pe', I('matmul',
                                        psS[:, hx, :], qxT[:, 2 * hx + dc, ts_], kxT[:, 2 * hx + dc, :],
                                        start=(dc == 0), stop=(dc == 1)),
                                       r=[f'qx{2 * hx + dc}_{n}', 'kxT'], w=[f'psB{hx // 2}'])
                            op('dve', I('tensor_reduce', out=sm[:, 0, :], in_=psS, axis=AX.X, op=ALU.max),
                               r=['psB0', 'psB1'], w=['sm'])
                            op('dve', I('tensor_scalar', out=sm[:, 1, :], in0=sm[:, 0, :], scalar1=-1.0,
                                                                scalar2=None, op0=ALU.mult), r=['sm'], w=['sm'])
                            for hx in range(4):
                                op('act', I('activation',
                                    out=pf[pi][:, hx, :], in_=psS[:, hx, :], func=AF.Exp, bias=sm[:, 1, hx:hx + 1],
                                    accum_out=sm[:, 2, hx:hx + 1]),
                                   r=[f'psB{hx // 2}', 'sm'], w=['pf0', 'sm'])
                            op('dve', I('reciprocal', out=sm[:, 3, :], in_=sm[:, 2, :]), r=['sm'], w=['sm'])
                            for hx in range(4):
                                op('dve', I('tensor_scalar',
                                    out=pn[pi][:, hx, :], in0=pf[pi][:, hx, :], scalar1=sm[:, 3, hx:hx + 1],
                                    scalar2=None, op0=ALU.mult), r=['pf0', 'sm'], w=[f'pn{pi}'])
                            for hx in range(4):
                                for mc in range(2):
                                    op('pe', I('transpose',
                                        out=psA[:, pi, (hx * 2 + mc) * 128:(hx * 2 + mc + 1) * 128],
                                        in_=pn[pi][:, hx, mc * 128:(mc + 1) * 128], identity=ident[:]),
                                       r=[f'pn{pi}', 'ident'], w=[f'psA{pi}'])
                            op('act', I('activation',
                                out=pT_t[:, :, :, tt * 128:(tt + 1) * 128],
                                in_=psA[:, pi, :].rearrange("p (h m c) -> p h m c", h=4, m=2), func=AF.Copy),
                               r=[f'psA{pi}'], w=[pTk])
                        k = 0
                        for hx in range(4):
                            for dc in range(2):
                                bk = 2 + (k % 4)
                                k += 1
                                for mc in range(2):
                                    op('pe', I('matmul',
                                        psB[:, bk, :], vx[:, mc, hx * 256 + dc * 128:hx * 256 + (dc + 1) * 128],
                                        pT_t[:, hx, mc, :], start=(mc == 0), stop=(mc == 1)),
                                       r=['vx', pTk], w=[f'psB{bk}'])
                                evac_copy(hT[:, 2 * hx + dc, n * 512:(n + 1) * 512], psB[:, bk, :], [f'psB{bk}'],
                                          [HK(2 * hx + dc, n)])
                    for n in range(2):
                        sl = nxt
                        nxt = load_w(w_xo_d[:, 512:1024]) if n == 0 else None
                        ns = slice(n * 512, (n + 1) * 512)
                        for t in range(NT):
                            bk = t % 4
                            for cc in range(8):
                                op('pe', I('matmul',
                                    psB[:, bk, :], hT[:, cc, t * 128:(t + 1) * 128], wr[sl][:, cc, :],
                                    start=(cc == 0), stop=(cc == 7)), r=[HK(cc, t // 4), f'wr{sl}'], w=[f'psB{bk}'])
                            op('dve', I('tensor_tensor',
                                out=x_sb[:, t, ns], in0=psB[:, bk, :], in1=x_sb[:, t, ns], op=ALU.add),
                               r=[f'psB{bk}', f'x{t}'], w=[f'x{t}'])
                    S.barrier()
                if stage == 2:
                    for t in range(NT):
                        ev = op('sp', I('dma_start',
                            out=out_d[b * SEQ + t * 128:b * SEQ + (t + 1) * 128, :], in_=x_sb[:, t, :]),
                            r=[f'x{t}'], dma=f'o{t % 4}')
                    S.barrier()
                    continue

                with ExitStack() as s2g:
                    GB = 8
                    gbc = T(s2g, "gbc", [128, DM], F32)
                    h3 = [T(s2g, f"h3_{i}", [128, DM], F32) for i in range(2)]
                    h3b = T(s2g, "h3b", [128, GB, DM], BF16)
                    h3T = [T(s2g, f"h3T{i}", [128, 8, 128], F32) for i in range(2)]
                    LG = T(s2g, "LG", [128, GB, 36], F32)
                    gmax = T(s2g, "gmax", [128, GB], F32)
                    gsh = T(s2g, "gsh", [128, GB, 4], F32)
                    gex = T(s2g, "gex", [128, GB, 4], F32)
                    gsum = T(s2g, "gsum", [128, GB], F32)
                    gw = T(s2g, "gw", [128, GB], F32)
                    gm = T(s2g, "gm", [128, GB, 4], F32)
                    elm = T(s2g, "elm", [128, GB, 4, 8], F32)
                    igr = T(s2g, "igr", [128, GB, 8], F32)
                    ig2 = T(s2g, "ig2", [128, GB, 8], F32)
                    mk1 = T(s2g, "mk1", [128, GB, 8], F32)
                    mk2 = T(s2g, "mk2", [128, GB, 8], F32)
                    m12 = T(s2g, "m12", [128, 4, GB], F32)
                    M1 = T(s2g, "M1", [128, GB, 4, 8], F32)
                    M2 = T(s2g, "M2", [128, GB, 4, 8], F32)
                    S32b = T(s2g, "S32b", [128, GB, 32], BF16)
                    posa = T(s2g, "posa", [128, GB, 32], F32)
                    ova = T(s2g, "ova", [128, GB, 32], F32)
                    slf = T(s2g, "slf", [128, 2, GB], F32)
                    op('sp', I('dma_start', out=gbc[:], in_=ln_ffn_g.partition_broadcast(128)), w=['gbc'],
                       dma='gbc')

                    def G1(t):
                        hi = t % 2
                        tl = t % GB
                        op('act', I('activation', out=junk[:], in_=x_sb[:, t, :], func=AF.Square,
                                    accum_out=stat[:, 0, t:t + 1]), r=[f'x{t}'], w=['junk', 'stat'])
                        rstd_from_ss(stat, t, 1.0 / DM)
                        op('dve', I('scalar_tensor_tensor', out=h3[hi][:], in0=x_sb[:, t, :],
                                    scalar=stat[:, 2, t:t + 1], in1=gbc[:], op0=ALU.mult, op1=ALU.mult),
                           r=[f'x{t}', 'stat', 'gbc'], w=[f'h3_{hi}'])
                        op('pool', I('tensor_copy', out=h3b[:, tl, :], in_=h3[hi][:]), r=[f'h3_{hi}'], w=[f'h3b{tl}'])
                        b4, b5 = (4, 5) if hi == 0 else (2, 3)
                        for kc in range(8):
                            bb = b4 if kc < 4 else b5
                            op('pe', I('transpose', out=psB[:, bb, (kc % 4) * 128:(kc % 4 + 1) * 128],
                                       in_=h3[hi][:, kc * 128:(kc + 1) * 128], identity=identf[:]),
                               r=[f'h3_{hi}', 'identf'], w=[f'psB{bb}'])
                        op('act', I('activation', out=h3T[hi][:, 0:4, :],
                                    in_=psB[:, b4, :].rearrange("p (k c) -> p k c", k=4), func=AF.Copy),
                           r=[f'psB{b4}'], w=[f'h3T{hi}a'])
                        op('dve', I('tensor_copy', out=h3T[hi][:, 4:8, :],
                                    in_=psB[:, b5, :].rearrange("p (k c) -> p k c", k=4)),
                           r=[f'psB{b5}'], w=[f'h3T{hi}b'])
                        for kc in range(8):
                            op('pe', I('matmul', psB[:, hi, 0:36], h3T[hi][:, kc, :], wrt[:, kc, :],
                                       start=(kc == 0), stop=(kc == 7)),
                               r=[f'h3T{hi}a', f'h3T{hi}b', 'wrt'], w=[f'psB{hi}'])
                        op('dve', I('tensor_tensor', out=LG[:, tl, :], in0=psB[:, hi, 0:36], in1=brt[:], op=ALU.add),
                           r=[f'psB{hi}', 'brt'], w=['LG'])
                        op('sp', I('dma_start', out=xres_d[b * SEQ + t * 128:b * SEQ + (t + 1) * 128, :],
                                   in_=x_sb[:, t, :]), r=[f'x{t}'], w=[f'xres{t}'], dma=f'xw{t % 4}')

                    def G23(g):
                        tg0 = b * NT + g * GB
                        V = lambda f, r=(), w=(): op('dve', f, r=['rt2'] + list(r), w=['rt2'] + list(w))
                        GL = LG[:, :, 0:4]
                        EL = LG[:, :, 4:36].rearrange("p t (g e) -> p t g e", g=4)
                        bc3 = lambda ap2, n: ap2[:, :, None].to_broadcast([128, GB, n])
                        V(I('tensor_reduce', out=gmax[:], in_=GL, axis=AX.X, op=ALU.max), r=['LG'])
                        V(I('tensor_tensor', out=gsh[:], in0=GL, in1=bc3(gmax, 4), op=ALU.subtract), r=['LG'])
                        op('act', I('activation', out=gex[:], in_=gsh[:], func=AF.Exp), r=['rt2'], w=['rt2'])
                        V(I('tensor_reduce', out=gsum[:], in_=gex[:], axis=AX.X, op=ALU.add))
                        V(I('reciprocal', out=gw[:], in_=gsum[:]))
                        V(I('tensor_scalar', out=gm[:], in0=gsh[:], scalar1=0.0, scalar2=None, op0=ALU.is_equal))
                        V(I('tensor_tensor', out=elm[:], in0=EL, in1=gm[:, :, :, None].to_broadcast([128, GB, 4, 8]),
                            op=ALU.mult), r=['LG'])
                        V(I('tensor_reduce', out=igr[:], in_=elm[:].rearrange("p t g e -> p t e g"), axis=AX.X,
                            op=ALU.add))
                        V(I('tensor_reduce', out=m12[:, 0, :], in_=igr[:], axis=AX.X, op=ALU.max))
                        V(I('tensor_tensor', out=mk1[:], in0=igr[:], in1=bc3(m12[:, 0, :], 8), op=ALU.is_equal))
                        V(I('scalar_tensor_tensor', out=ig2[:], in0=mk1[:], scalar=-1e30, in1=igr[:], op0=ALU.mult,
                            op1=ALU.add))
                        V(I('tensor_reduce', out=m12[:, 1, :], in_=ig2[:], axis=AX.X, op=ALU.max))
                        V(I('tensor_tensor', out=mk2[:], in0=ig2[:], in1=bc3(m12[:, 1, :], 8), op=ALU.is_equal))
                        V(I('tensor_tensor', out=m12[:, 2, :], in0=m12[:, 1, :], in1=m12[:, 0, :], op=ALU.subtract))
                        op('act', I('activation', out=m12[:, 2, :], in_=m12[:, 2, :], func=AF.Exp), r=['rt2'], w=['rt2'])
                        V(I('tensor_scalar', out=m12[:, 3, :], in0=m12[:, 2, :], scalar1=1.0, scalar2=None, op0=ALU.add))
                        V(I('reciprocal', out=m12[:, 3, :], in_=m12[:, 3, :]))
                        V(I('tensor_tensor', out=wts[:, tg0:tg0 + GB, 0], in0=m12[:, 3, :], in1=gw[:], op=ALU.mult))
                        V(I('tensor_tensor', out=wts[:, tg0:tg0 + GB, 1], in0=m12[:, 2, :], in1=wts[:, tg0:tg0 + GB, 0],
                            op=ALU.mult))
                        gm4 = gm[:, :, :, None].to_broadcast([128, GB, 4, 8])
                        V(I('tensor_tensor', out=M1[:], in0=gm4, in1=mk1[:, :, None, :].to_broadcast([128, GB, 4, 8]),
                            op=ALU.mult))
                        V(I('tensor_tensor', out=M2[:], in0=gm4, in1=mk2[:, :, None, :].to_broadcast([128, GB, 4, 8]),
                            op=ALU.mult))
                        V(I('tensor_tensor', out=elm[:], in0=M1[:], in1=M2[:], op=ALU.add))
                        V(I('tensor_copy', out=S32b[:], in_=elm[:].rearrange("p t g e -> p t (g e)")))
                        for tl in range(GB):
                            pb = tl % 2
                            op('pe', I('matmul', psB[:, pb, 64:96], Lmat[:], S32b[:, tl, :], start=True, stop=True),
                               r=['Lmat', 'rt2'], w=[f'psB{pb}'])
                            op('pe', I('matmul', psB[:, pb, 96:128], onesb[:], S32b[:, tl, :], start=True, stop=True),
                               r=['onesb', 'rt2'], w=[f'psB{pb}'])
                            V(I('tensor_tensor', out=posa[:, tl, :], in0=psB[:, pb, 64:96], in1=base[:], op=ALU.add),
                              r=[f'psB{pb}', 'base'])
                            V(I('tensor_tensor', out=base[:], in0=psB[:, pb, 96:128], in1=base[:], op=ALU.add),
                              r=[f'psB{pb}', 'base'], w=['base'])
                        V(I('tensor_scalar', out=ova[:], in0=posa[:], scalar1=float(CAP), scalar2=1e7, op0=ALU.is_ge,
                            op1=ALU.mult))
                        V(I('tensor_tensor', out=posa[:], in0=posa[:], in1=ova[:], op=ALU.add))
                        V(I('tensor_tensor', out=posa[:], in0=posa[:], in1=ecap[:, None, :].to_broadcast([128, GB, 32]),
                            op=ALU.add))
                        V(I('tensor_tensor', out=ova[:], in0=posa[:], in1=M1[:].rearrange("p t g e -> p t (g e)"),
                            op=ALU.mult))
                        V(I('tensor_reduce', out=slf[:, 0, :], in_=ova[:], axis=AX.X, op=ALU.add))
                        V(I('tensor_tensor', out=ova[:], in0=posa[:], in1=M2[:].rearrange("p t g e -> p t (g e)"),
                            op=ALU.mult))
                        V(I('tensor_reduce', out=slf[:, 1, :], in_=ova[:], axis=AX.X, op=ALU.add))
                        V(I('tensor_copy', out=slots[:, tg0:tg0 + GB, :], in_=slf[:].rearrange("p k t -> p t k")),
                          w=['slotsg'])
                        for tl in range(GB):
                            for k2 in range(2):
                                op('pool', I('indirect_dma_start', out=xd_d[:, :],
                                             out_offset=bass.IndirectOffsetOnAxis(ap=slots[:, tg0 + tl, k2:k2 + 1], axis=0),
                                             in_=h3b[:, tl, :], in_offset=None, bounds_check='REG', oob_is_err=False),
                                   r=['slotsg', f'h3b{tl}'], w=[f'xd{tg0 + tl}_{k2}'], dma=f'sc{tl}_{k2}')

                    for g in range(NT // GB):
                        for tl in range(GB):
                            G1(g * GB + tl)
                        G23(g)
                    S.barrier()

        if stage >= 3:
            with ExitStack() as s3:
                wg = [T(s3, f"wg{i}", [128, 8, 512], BF16) for i in range(2)]
                wu = [T(s3, f"wu{i}", [128, 8, 512], BF16) for i in range(2)]
                wd = [T(s3, f"wd{i}", [128, 4, DM], BF16) for i in range(2)]
                xr = [T(s3, f"xr{i}", [128, DM], BF16) for i in range(3)]
                xT = [T(s3, f"xT{i}", [128, 8, 384], BF16) for i in range(2)]
                sg = [T(s3, f"sg{i}", [128, 384], F32) for i in range(2)]
                h1T = [T(s3, f"h1T{i}", [128, 4, 384], BF16) for i in range(2)]
                ydt = [T(s3, f"ydt{i}", [128, DM], F32) for i in range(2)]

                wst = [T(s3, f"wst{i}", [128, 8, 512], F32) for i in range(3)]

                def load_dma(e_):
                    op('act', I('dma_start', out=wst[0][:], in_=w_gate_d[e_].rearrange("(k p) n -> p k n", p=128)),
                       w=['wst0'], dma='wst0')
                    op('act', I('dma_start', out=wst[1][:], in_=w_up_d[e_].rearrange("(k p) n -> p k n", p=128)),
                       w=['wst1'], dma='wst1')
                    op('act', I('dma_start', out=wst[2][:].rearrange("p (a b) n -> p a (b n)", a=4),
                               in_=w_down_d[e_].rearrange("(k p) n -> p k n", p=128)), w=['wst2'], dma='wst2')

                def cast_gu(e_):
                    i = e_ % 2
                    op('dve', I('tensor_copy', out=wg[i][:], in_=wst[0][:]), r=['wst0'], w=[f'wg{i}'])
                    op('act', I('activation', out=wu[i][:], in_=wst[1][:], func=AF.Copy), r=['wst1'], w=[f'wu{i}'])

                def cast_d(e_):
                    i = e_ % 2
                    wsv = wst[2][:].rearrange("p (a b) n -> p a (b n)", a=4)
                    op('act', I('activation', out=wd[i][:, 0:2, :], in_=wsv[:, 0:2, :], func=AF.Copy),
                       r=['wst2'], w=[f'wd{i}a'])
                    op('dve', I('tensor_copy', out=wd[i][:, 2:4, :], in_=wsv[:, 2:4, :]), r=['wst2'], w=[f'wd{i}b'])

                def load_expert(e_):
                    load_dma(e_)
                    cast_gu(e_)
                    cast_d(e_)

                load_expert(0)
                cnt = 0
                yc = 0
                for e_ in range(NEXP):
                    wi = e_ % 2
                    if e_ + 1 < NEXP:
                        load_dma(e_ + 1)
                    for half in range(2):
                        if half == 1 and e_ + 1 < NEXP:
                            cast_gu(e_ + 1)
                        r0 = e_ * CAP + half * 384
                        hb = cnt % 2
                        cnt += 1
                        for ci in range(3):
                            xi = (cnt * 3 + ci) % 3
                            op('sp', I('dma_start',
                                out=xr[xi][:], in_=xd_d[r0 + ci * 128:r0 + (ci + 1) * 128, :]),
                                r=['xd'], w=[f'xr{xi}'], dma=f'xr{xi}')
                            pa = ci % 2
                            for kc in range(8):
                                op('pe', I('transpose',
                                    out=psA[:, pa, kc * 128:(kc + 1) * 128], in_=xr[xi][:, kc * 128:(kc + 1) * 128],
                                    identity=ident[:]), r=[f'xr{xi}', 'ident'], w=[f'psA{pa}'])
                            evac_copy(xT[hb][:, :, ci * 128:(ci + 1) * 128],
                                      psA[:, pa, :].rearrange("p (k c) -> p k c", k=8), [f'psA{pa}'], [f'xT{hb}'])
                        for dc in range(4):
                            bg, bu = (dc % 2) * 2, (dc % 2) * 2 + 1
                            for kc in range(8):
                                op('pe', I('matmul',
                                    psB[:, bg, 0:384], wg[wi][:, kc, dc * 128:(dc + 1) * 128], xT[hb][:, kc, :],
                                    start=(kc == 0), stop=(kc == 7)), r=[f'wg{wi}', f'xT{hb}'], w=[f'psB{bg}'])
                            for kc in range(8):
                                op('pe', I('matmul',
                                    psB[:, bu, 0:384], wu[wi][:, kc, dc * 128:(dc + 1) * 128], xT[hb][:, kc, :],
                                    start=(kc == 0), stop=(kc == 7)), r=[f'wu{wi}', f'xT{hb}'], w=[f'psB{bu}'])
                            si = dc % 2
                            op('act', I('activation', out=sg[si][:], in_=psB[:, bg, 0:384],
                                                                           func=AF.Silu),
                               r=[f'psB{bg}'], w=[f'sg{si}'])
                            op('dve', I('tensor_tensor',
                                out=h1T[hb][:, dc, :], in0=psB[:, bu, 0:384], in1=sg[si][:], op=ALU.mult),
                               r=[f'psB{bu}', f'sg{si}'], w=[f'h1T{hb}_{dc}'])
                        for ci in range(3):
                            yi = yc % 2
                            yc += 1
                            for nn in range(2):
                                for dc in range(4):
                                    op('pe', I('matmul',
                                        psB[:, 4 + nn, :], h1T[hb][:, dc, ci * 128:(ci + 1) * 128],
                                        wd[wi][:, dc, nn * 512:(nn + 1) * 512], start=(dc == 0), stop=(dc == 3)),
                                       r=[f'h1T{hb}_{dc}', f'wd{wi}a', f'wd{wi}b'], w=[f'psB{4 + nn}'])
                            op('act', I('activation', out=ydt[yi][:, 0:512], in_=psB[:, 4, :], func=AF.Copy),
                               r=['psB4'], w=[f'ydt{yi}a'])
                            op('dve', I('tensor_copy', out=ydt[yi][:, 512:1024], in_=psB[:, 5, :]),
                               r=['psB5'], w=[f'ydt{yi}b'])
                            op('sp', I('dma_start',
                                out=yd_d[r0 + ci * 128:r0 + (ci + 1) * 128, :], in_=ydt[yi][:]),
                                r=[f'ydt{yi}a', f'ydt{yi}b'], w=[f'yd{yc}'], dma=f'yo{yi}')
                    if e_ + 1 < NEXP:
                        cast_d(e_ + 1)
                S.barrier()

            with ExitStack() as s4:
                gbf = T(s4, "gbf", [128, DM], F32)
                y1 = [T(s4, f"y1_{i}", [128, DM], F32) for i in range(2)]
                y2 = [T(s4, f"y2_{i}", [128, DM], F32) for i in range(2)]
                xf = [T(s4, f"xf{i}", [128, DM], F32) for i in range(2)]
                of = [T(s4, f"of{i}", [128, DM], F32) for i in range(2)]
                fst = T(s4, "fst", [128, 3, nseq * NT], F32)
                zt = T(s4, "zt", [128, DM], F32)
                op('pool', I('memset', zt[:], 0.0), w=['zt'])
                op('sp', I('dma_start', out=gbf[:], in_=ln_final_g.partition_broadcast(128)), w=['gbf'], dma='gbf')
                def fetch(tg):
                    i = tg % 2
                    for ybuf, k2, nm in ((y1, 0, 'y1'), (y2, 1, 'y2')):
                        op('act', I('activation', out=ybuf[i][:], in_=zt[:], func=AF.Copy), r=['zt'], w=[f'{nm}_{i}'])
                        op('pool', I('indirect_dma_start', out=ybuf[i][:], out_offset=None, in_=yd_d[:, :],
                                     in_offset=bass.IndirectOffsetOnAxis(ap=slots[:, tg, k2:k2 + 1], axis=0),
                                     bounds_check='REG', oob_is_err=False),
                           r=['yd'], w=[f'{nm}_{i}'], dma=f'{nm}_{i}')
                    op('sp', I('dma_start', out=xf[i][:], in_=xres_d[tg * 128:(tg + 1) * 128, :]),
                       r=['xres'], w=[f'xf{i}'], dma=f'xf{i}')

                def compute(tg):
                    i = tg % 2
                    op('dve', I('scalar_tensor_tensor', out=xf[i][:], in0=y1[i][:], scalar=wts[:, tg, 0:1], in1=xf[i][:],
                                op0=ALU.mult, op1=ALU.add), r=[f'y1_{i}', f'xf{i}'], w=[f'xf{i}'])
                    op('dve', I('scalar_tensor_tensor', out=xf[i][:], in0=y2[i][:], scalar=wts[:, tg, 1:2], in1=xf[i][:],
                                op0=ALU.mult, op1=ALU.add), r=[f'y2_{i}', f'xf{i}'], w=[f'xf{i}'])
                    op('act', I('activation', out=junk[:], in_=xf[i][:], func=AF.Square, accum_out=fst[:, 0, tg:tg + 1]),
                       r=[f'xf{i}'], w=['junk', 'stat'])
                    rstd_from_ss(fst, tg, 1.0 / DM)
                    op('dve', I('scalar_tensor_tensor', out=of[i][:], in0=xf[i][:], scalar=fst[:, 2, tg:tg + 1],
                                in1=gbf[:], op0=ALU.mult, op1=ALU.mult), r=[f'xf{i}', 'stat', 'gbf'], w=[f'of{i}'])
                    op('sp', I('dma_start', out=out_d[tg * 128:(tg + 1) * 128, :], in_=of[i][:]),
                       r=[f'of{i}'], dma=f'of{i}')

                ntile = nseq * NT
                fetch(0)
                for tg in range(ntile):
                    if tg + 1 < ntile:
                        fetch(tg + 1)
                    compute(tg)
                S.barrier()
        S.barrier()
        S.emit()
    return nc


_NC_CACHE = {}


def _prep(inputs, c, nseq=NSEQ):
    f = lambda a: np.ascontiguousarray(np.asarray(a, dtype=np.float32))
    d = {
        "x": f(inputs["x"][c * nseq:(c + 1) * nseq]),
        "mem": f(inputs["mem"][c * nseq:(c + 1) * nseq]),
        "ln_mix_g": f(inputs["ln_mix_g"][0]),
        "w_in": f(inputs["w_in"][0]),
        "sb_out_g": f(inputs["sb_out_g"][0]),
        "conv_w": f(inputs["conv_w"][0]),
        "conv_b": f(inputs["conv_b"][0]),
        "conv_ln_g": f(inputs["conv_ln_g"][0]),
        "conv_ln_b": f(inputs["conv_ln_b"][0]),
        "w_out": f(inputs["w_out"][0]),
        "ln_mem_x_g": f(inputs["ln_mem_x_g"][0]),
        "ln_mem_g": f(inputs["ln_mem_g"][0]),
        "w_xq": f(inputs["w_xq"][0]),
        "w_xkv": f(inputs["w_xkv"][0]),
        "w_xo": f(inputs["w_xo"][0]),
        "ln_ffn_g": f(inputs["ln_ffn_g"][0]),
        "w_rt": f(np.concatenate([np.asarray(inputs["w_group"][0]), np.asarray(inputs["w_er"][0]).reshape(DM, 32)], axis=1)),
        "b_rt": f(np.concatenate([np.asarray(inputs["b_group"][0]), np.asarray(inputs["b_er"][0]).reshape(32)])),
        "w_gate": f(inputs["w_gate"][0]),
        "w_up": f(inputs["w_up"][0]),
        "w_down": f(inputs["w_down"][0]),
        "ln_final_g": f(inputs["ln_final_g"]),
    }
    return d


def kernel(**inputs):
    if 'nc' not in _NC_CACHE:
        _NC_CACHE['nc'] = build()
    nc = _NC_CACHE['nc']
    in_maps = [_prep(inputs, c) for c in range(N_CORES)]
    res = run_bass_kernel_spmd(nc, in_maps, core_ids=list(range(N_CORES)))
    out = np.concatenate([np.asarray(r["out"]).reshape(NSEQ, SEQ, DM) for r in res.results], axis=0)
    return out.astype(np.float32)
```

```python
import numpy as np
import concourse.bass as bass
import concourse.mybir as mybir
from concourse.bass_utils import run_bass_kernel_spmd
from contextlib import ExitStack

F32 = mybir.dt.float32
F32R = mybir.dt.float32r
BF16 = mybir.dt.bfloat16
I32 = mybir.dt.int32
AF = mybir.ActivationFunctionType
ALU = mybir.AluOpType
AX = mybir.AxisListType

ENG = ['pe', 'act', 'dve', 'pool', 'sp']
SAME_ENGINE_SYNC = True

N_CORES = 8
NSEQ = 4
SEQ = 2048
DM = 1024
NT = SEQ // 128
NMEM = 256
NEXP = 32
CAP = 768
NROWS = NEXP * CAP
EPS = 1e-6


def I(name, *args, **kw):
    return (name, args, kw)


class Sched:
    def __init__(self, nc, es):
        self.nc = nc
        self.es = es
        self.ins = {e: [] for e in ENG}
        self.cnt = {e: 0 for e in ENG}
        self.sem = {e: es.enter_context(nc.semaphore('s_' + e)) for e in ENG}
        self.dsem = {}
        self.lastw = {}
        self.readers = {}
        self.waited = {e: {} for e in ENG}

    def _semof(self, key):
        return self.sem[key[1]] if key[0] == 'e' else self.dsem[key[1]][0]

    def op(self, eng, fn, r=(), w=(), dma=None, extra=()):
        deps = {}

        def need(ev):
            if ev is None:
                return
            key, val, src, is_dma = ev
            if (not is_dma) and src == eng and (eng == 'pe' or not SAME_ENGINE_SYNC):
                return
            if deps.get(key, 0) < val:
                deps[key] = val

        for k in r:
            need(self.lastw.get(k))
        for k in w:
            need(self.lastw.get(k))
            for ev in self.readers.get(k, {}).values():
                need(ev)
        for ev in extra:
            need(ev)
        waits = []
        wd = self.waited[eng]
        for key, val in deps.items():
            if wd.get(key, 0) >= val:
                continue
            wd[key] = val
            waits.append((key, val))
        if dma is not None:
            if dma not in self.dsem:
                self.dsem[dma] = [self.es.enter_context(self.nc.semaphore('d_' + dma)), 0]
            self.dsem[dma][1] += 16
            ev = (('d', dma), self.dsem[dma][1], eng, True)
        else:
            self.cnt[eng] += 1
            ev = (('e', eng), self.cnt[eng], eng, False)
        self.ins[eng].append((waits, fn, ev))
        for k in r:
            d = self.readers.setdefault(k, {})
            old = d.get(ev[0])
            if old is None or old[1] < ev[1]:
                d[ev[0]] = ev
        for k in w:
            self.lastw[k] = ev
            self.readers[k] = {}
        return ev

    def barrier(self):
        evs = []
        for e in ENG:
            if self.cnt[e] > 0:
                evs.append((('e', e), self.cnt[e], e, False))
        for slot, (s, c) in self.dsem.items():
            if c > 0:
                evs.append((('d', slot), c, 'sp', True))
        for e in ENG:
            self.op(e, I('nop'), extra=[ev for ev in evs if not (ev[2] == e and not ev[3])])
        self.lastw = {}
        self.readers = {}

    def emit(self):
        nc = self.nc
        with nc.Block() as block:
            def body(name):
                def f(e):
                    bc_reg = None
                    if name == 'pool':
                        bc_reg = e.alloc_register()
                        e.reg_mov(bc_reg, NROWS - 1)
                    for waits, fn, ev in self.ins[name]:
                        for key, val in waits:
                            e.wait_ge(self._semof(key), val)
                        try:
                            kw = fn[2]
                            if kw.get('bounds_check', None) == 'REG':
                                kw = dict(kw, bounds_check=bc_reg)
                            ins = getattr(e, fn[0])(*fn[1], **kw)
                        except Exception:
                            print("EMIT FAIL", name, fn[0], fn[1], fn[2])
                            raise
                        key, val, _, is_dma = ev
                        ins.then_inc(self._semof(key), 16 if is_dma else 1)
                return f
            block.tensor(body('pe'))
            block.scalar(body('act'))
            block.vector(body('dve'))
            block.gpsimd(body('pool'))
            block.sync(body('sp'))


def build(nseq=NSEQ, stage=99):
    nc = bass.Bass('TRN2', target_bir_lowering=False)
    ntok = nseq * SEQ

    def din(name, shape, dt=F32):
        return nc.dram_tensor(name, list(shape), dt, kind="ExternalInput").ap()

    x_d = din("x", [nseq, SEQ, DM])
    mem_d = din("mem", [nseq, NMEM, DM])
    ln_mix_g = din("ln_mix_g", [DM])
    w_in_d = din("w_in", [DM, 2560])
    sb_out_g = din("sb_out_g", [512])
    conv_w_d = din("conv_w", [31, 512])
    conv_b_d = din("conv_b", [512])
    conv_ln_g = din("conv_ln_g", [512])
    conv_ln_b = din("conv_ln_b", [512])
    w_out_d = din("w_out", [DM, DM])
    ln_mem_x_g = din("ln_mem_x_g", [DM])
    ln_mem_g = din("ln_mem_g", [DM])
    w_xq_d = din("w_xq", [DM, DM])
    w_xkv_d = din("w_xkv", [DM, 2 * DM])
    w_xo_d = din("w_xo", [DM, DM])
    ln_ffn_g = din("ln_ffn_g", [DM])
    w_rt_d = din("w_rt", [DM, 36])
    b_rt_d = din("b_rt", [36])
    w_gate_d = din("w_gate", [NEXP, DM, 512])
    w_up_d = din("w_up", [NEXP, DM, 512])
    w_down_d = din("w_down", [NEXP, 512, DM])
    ln_final_g = din("ln_final_g", [DM])
    out_d = nc.dram_tensor("out", [ntok, DM], F32, kind="ExternalOutput").ap()
    if stage == 1:
        dbg_mix = nc.dram_tensor("dbg_mix", [128, 8, SEQ], BF16, kind="ExternalOutput").ap()
        dbg_qk = nc.dram_tensor("dbg_qk", [128, 8, SEQ], BF16, kind="ExternalOutput").ap()
        dbg_v = nc.dram_tensor("dbg_v", [128, NT, 512], BF16, kind="ExternalOutput").ap()
        dbg_rsb = nc.dram_tensor("dbg_rsb", [128, 3, NT], F32, kind="ExternalOutput").ap()
    xd_d = nc.dram_tensor("xd_scr", [NROWS, DM], BF16).ap()
    yd_d = nc.dram_tensor("yd_scr", [NROWS, DM], F32).ap()
    xres_d = nc.dram_tensor("xres_scr", [ntok, DM], F32).ap()

    with ExitStack() as es:
        S = Sched(nc, es)
        op = S.op

        uid = [0]

        def T(scope, name, shape, dt):
            uid[0] += 1
            return scope.enter_context(nc.sbuf_tensor(f"{name}_u{uid[0]}", shape, dt))

        psA = es.enter_context(nc.psum_tensor("psA", [128, 2, 1024], BF16))
        psB = es.enter_context(nc.psum_tensor("psB", [128, 6, 512], F32))

        identf = T(es, "identf", [128, 128], F32)
        ident = T(es, "ident", [128, 128], BF16)
        negtri = T(es, "negtri", [128, 128], F32)
        negones = T(es, "negones", [128, 128], F32)
        negtriR = T(es, "negtriR", [128, 128], F32)
        negonesR = T(es, "negonesR", [128, 128], F32)
        meanmat = T(es, "meanmat", [128, 128], F32)
        ones2 = T(es, "ones2", [128, 2], F32)
        onesb = T(es, "onesb", [128, 128], BF16)
        Lmat = T(es, "Lmat", [128, 128], BF16)
        maskf = T(es, "maskf", [128, 4, 512], BF16)
        ecap = T(es, "ecap", [128, 32], F32)
        ecap_i = T(es, "ecap_i", [128, 32], I32)
        gcols = T(es, "gcols", [128, 3, 8], F32)
        sbg = T(es, "sbg", [128, 4], F32)
        cvb = T(es, "cvb", [128, 4], F32)
        cvg = T(es, "cvg", [128, 4], F32)
        cvbb = T(es, "cvbb", [128, 4], F32)
        cwT = T(es, "cwT", [128, 4, 31], F32)
        hT = T(es, "hT", [128, 8, SEQ], BF16)
        wr = [T(es, f"wr{i}", [128, 8, 512], BF16) for i in range(2)]
        stat = T(es, "stat", [128, 3, NT], F32)
        rsb = T(es, "rsb", [128, 3, NT], F32)
        slots = T(es, "slots", [128, nseq * NT, 2], I32)
        wts = T(es, "wts", [128, nseq * NT, 2], F32)
        base = T(es, "base", [128, 32], F32)
        wrt = T(es, "wrt", [128, 8, 36], F32)
        brt = T(es, "brt", [128, 36], F32)
        junk = T(es, "junk", [128, 1024], BF16)

        def HK(c, n):
            return f"H{c}_{n}"

        def small_col(dst_ap, src_ap, k, key):
            op('sp', I('dma_start', out=dst_ap, in_=src_ap.rearrange("(k p) -> p k", p=128),
                                           allow_slow_non_contiguous=True), w=[key], dma='c_' + key)

        small_col(gcols[:, 0, :], ln_mix_g, 8, 'gc0')
        small_col(gcols[:, 1, :], ln_mem_x_g, 8, 'gc1')
        small_col(gcols[:, 2, :], ln_mem_g, 8, 'gc2')
        small_col(sbg[:], sb_out_g, 4, 'sbg')
        small_col(cvb[:], conv_b_d, 4, 'cvb')
        small_col(cvg[:], conv_ln_g, 4, 'cvg')
        small_col(cvbb[:], conv_ln_b, 4, 'cvbb')
        op('sp', I('dma_start', out=wrt[:], in_=w_rt_d.rearrange("(k p) n -> p k n", p=128)),
           w=['wrt'], dma='c_wrt')
        op('sp', I('dma_start', out=brt[:], in_=b_rt_d.partition_broadcast(128)), w=['brt'], dma='c_brt')

        op('pool', I('memset', identf[:], 0.0), w=['identf'])
        op('pool', I('affine_select', out=identf[:], in_=identf[:], pattern=[[-1, 128]],
                                             compare_op=ALU.not_equal, fill=1.0, base=0, channel_multiplier=1),
           r=['identf'], w=['identf'])
        op('dve', I('tensor_copy', out=ident[:], in_=identf[:]), r=['identf'], w=['ident'])
        op('pool', I('memset', negtri[:], -1.0), w=['negtri'])
        op('pool', I('affine_select', out=negtri[:], in_=negtri[:], pattern=[[-1, 128]],
                                             compare_op=ALU.is_ge, fill=0.0, base=0, channel_multiplier=1),
           r=['negtri'], w=['negtri'])
        op('pool', I('memset', negones[:], -1.0), w=['negones'])
        op('dve', I('tensor_copy', out=negtriR[:].bitcast(F32R), in_=negtri[:]), r=['negtri'], w=['negtriR'])
        op('dve', I('tensor_copy', out=negonesR[:].bitcast(F32R), in_=negones[:]), r=['negones'], w=['negonesR'])
        op('pool', I('memset', meanmat[:], 1.0 / 512), w=['meanmat'])
        op('pool', I('memset', ones2[:], 1.0), w=['ones2'])
        op('pool', I('memset', onesb[:], 1.0), w=['onesb'])
        op('pool', I('memset', Lmat[:], 1.0), w=['Lmat'])
        op('pool', I('affine_select', out=Lmat[:], in_=Lmat[:], pattern=[[1, 128]],
                                             compare_op=ALU.is_gt, fill=0.0, base=0, channel_multiplier=-1),
           r=['Lmat'], w=['Lmat'])
        op('pool', I('memset', maskf[:], 1.0), w=['maskf'])
        for r_ in range(4):
            op('pool', I('affine_select', out=maskf[:, r_, :], in_=maskf[:, r_, :], pattern=[[1, 512]],
                                                        compare_op=ALU.is_gt, fill=0.0, base=-r_ * 128,
                                                        channel_multiplier=-1),
               r=['maskf'], w=['maskf'])
        op('pool', I('iota', ecap_i[:], pattern=[[CAP, 32]], base=0, channel_multiplier=0), w=['ecap_i'])
        op('dve', I('tensor_copy', out=ecap[:], in_=ecap_i[:]), r=['ecap_i'], w=['ecap'])
        op('pool', I('memset', base[:], 0.0), w=['base'])

        with ExitStack() as s0:
            cw_in = T(s0, "cw_in", [31, 512], F32)
            op('sp', I('dma_start', out=cw_in[:], in_=conv_w_d), w=['cw_in'], dma='c_cw')
            for c in range(4):
                op('pe', I('transpose', out=psB[:, 0, c * 32:c * 32 + 31], in_=cw_in[:, c * 128:(c + 1) * 128],
                                                    identity=identf[0:31, 0:31]),
                   r=['cw_in', 'identf'], w=['psB0'])
            op('act', I('activation', out=cwT[:], in_=psB[:, 0, 0:128].rearrange("p (c w) -> p c w", c=4)[:, :, 0:31],
                                             func=AF.Copy), r=['psB0'], w=['cwT'])
            S.barrier()

        wslot = [0]

        def load_w(src2d, nk=8):
            i = wslot[0]
            wslot[0] ^= 1
            op('pool', I('dma_start', out=wr[i][:, 0:nk, :], in_=src2d.rearrange("(k p) n -> p k n", p=128)),
               w=[f'wr{i}'], dma=f'wr{i}')
            return i

        evac_flip = [0]

        def evac_copy(out_ap, in_ap, r, w, scale=None, eng=None):
            if eng is None:
                eng = 'act' if (evac_flip[0] & 1) == 0 else 'dve'
                evac_flip[0] += 1
            if eng == 'act':
                if scale is None:
                    op('act', I('activation', out=out_ap, in_=in_ap, func=AF.Copy), r=r, w=w)
                else:
                    op('act', I('activation', out=out_ap, in_=in_ap, func=AF.Copy, scale=scale), r=r, w=w)
            else:
                if scale is None:
                    op('dve', I('tensor_copy', out=out_ap, in_=in_ap), r=r, w=w)
                else:
                    op('dve', I('tensor_scalar', out=out_ap, in0=in_ap, scalar1=scale, scalar2=None,
                                                        op0=ALU.mult), r=r, w=w)

        def rstd_from_ss(st, col, inv_n):
            op('act', I('activation', out=st[:, 1, col:col + 1], in_=st[:, 0, col:col + 1], func=AF.Sqrt,
                                             scale=inv_n, bias=EPS), r=['stat'], w=['stat'])
            op('dve', I('reciprocal', out=st[:, 2, col:col + 1], in_=st[:, 1, col:col + 1]),
               r=['stat'], w=['stat'])

        def norm_T(src_ap, src_key, hn_t, hn_key, gi, dst3, dst_keys, t, pa):
            op('act', I('activation', out=junk[:], in_=src_ap, func=AF.Square, accum_out=stat[:, 0, t:t + 1]),
               r=[src_key], w=['junk', 'stat'])
            rstd_from_ss(stat, t, 1.0 / DM)
            op('act', I('activation', out=hn_t[:], in_=src_ap, func=AF.Copy, scale=stat[:, 2, t:t + 1]),
               r=[src_key, 'stat'], w=[hn_key])
            for kc in range(8):
                op('pe', I('transpose', out=psA[:, pa, kc * 128:(kc + 1) * 128],
                                                      in_=hn_t[:, kc * 128:(kc + 1) * 128], identity=ident[:]),
                   r=[hn_key, 'ident'], w=[f'psA{pa}'])
            op('dve', I('tensor_tensor', out=dst3, in0=psA[:, pa, :].rearrange("p (k c) -> p k c", k=8),
                                                in1=gcols[:, gi, :, None].to_broadcast([128, 8, 128]), op=ALU.mult),
               r=[f'psA{pa}', f'gc{gi}'], w=dst_keys)

        def fm_proj(slot, ncc, rhs_fn, rhs_keys_fn, nN, N, evac_fn, banks):
            k = 0
            for cc in range(ncc):
                for n in range(nN):
                    bk = banks[k % len(banks)]
                    k += 1
                    for kc in range(8):
                        op('pe', I('matmul',
                            psB[:, bk, 0:N], wr[slot][:, kc, cc * 128:(cc + 1) * 128], rhs_fn(kc, n),
                            start=(kc == 0), stop=(kc == 7)),
                           r=[f'wr{slot}'] + rhs_keys_fn(kc, n), w=[f'psB{bk}'])
                    evac_fn(cc, n, bk)

        dbg = {}

        for b in range(nseq):
            with ExitStack() as s1:
                qk = T(s1, "qk", [128, 8, SEQ], BF16)
                v_sb = T(s1, "v_sb", [128, NT, 512], BF16)
                with ExitStack() as s1a:
                    xin = [T(s1a, f"xin{i}", [128, DM], F32) for i in range(3)]
                    hn = [T(s1a, f"hn{i}", [128, DM], BF16) for i in range(2)]
                    gT = T(s1a, "gT", [128, 4, 30 + SEQ], BF16)
                    ycv = T(s1a, "ycv", [128, 4, SEQ], F32)
                    dg = T(s1a, "dg", [128, 31, 128], BF16)
                    ysq = T(s1a, "ysq", [128, 4, 512], F32)
                    lnw = T(s1a, "lnw", [128, 4, 512], F32)

                    for t in range(NT):
                        xi = xin[t % 3]
                        op('sp', I('dma_start', out=xi[:], in_=x_d[b, t * 128:(t + 1) * 128, :]),
                           w=[f'xin{t % 3}'], dma=f'xin{t % 3}')
                        norm_T(xi[:], f'xin{t % 3}', hn[t % 2], f'hn{t % 2}', 0,
                               hT[:, :, t * 128:(t + 1) * 128], [HK(c, t // 4) for c in range(8)], t, t % 2)

                    hrhs = lambda kc, n: hT[:, kc, n * 512:(n + 1) * 512]
                    hkeys = lambda kc, n: [HK(kc, n)]
                    sl = load_w(w_in_d[:, 0:512])
                    nxt = load_w(w_in_d[:, 512:1024])
                    fm_proj(sl, 4, hrhs, hkeys, 4, 512,
                            lambda cc, n, bk: evac_copy(qk[:, cc, n * 512:(n + 1) * 512], psB[:, bk, :], [f'psB{bk}'],
                                                        [f'q{cc}_{n}'], scale=0.125), [0, 1, 2, 3])
                    sl = nxt
                    nxt = load_w(w_in_d[:, 1024:1536])
                    fm_proj(sl, 4, hrhs, hkeys, 4, 512,
                            lambda cc, n, bk: evac_copy(qk[:, 4 + cc, n * 512:(n + 1) * 512], psB[:, bk, :],
                                                        [f'psB{bk}'], [f'k{cc}_{n}']), [0, 1, 2, 3])
                    sl = nxt
                    nxt = load_w(w_in_d[:, 2048:2560])
                    for t in range(NT):
                        bk = t % 4
                        for kc in range(8):
                            op('pe', I('matmul',
                                psB[:, bk, :], hT[:, kc, t * 128:(t + 1) * 128], wr[sl][:, kc, :],
                                start=(kc == 0), stop=(kc == 7)),
                               r=[f'wr{sl}', HK(kc, t // 4)], w=[f'psB{bk}'])
                        evac_copy(v_sb[:, t, :], psB[:, bk, :], [f'psB{bk}'], [f'v{t}'])
                    op('pool', I('memset', gT[:, :, 0:30], 0.0), w=['gTpad'])
                    sl = nxt
                    nxt = load_w(w_in_d[:, 1536:2048])
                    fm_proj(sl, 4, hrhs, hkeys, 4, 512,
                            lambda cc, n, bk: op('act', I('activation',
                                out=gT[:, cc, 30 + n * 512:30 + (n + 1) * 512], in_=psB[:, bk, :], func=AF.Sigmoid),
                                r=[f'psB{bk}'], w=[f'g{cc}_{n}']), [0, 1, 2, 3])
                    sl = nxt
                    fm_proj(sl, 4, hrhs, hkeys, 4, 512,
                            lambda cc, n, bk: op('dve', I('tensor_tensor',
                                out=gT[:, cc, 30 + n * 512:30 + (n + 1) * 512], in0=psB[:, bk, :],
                                in1=gT[:, cc, 30 + n * 512:30 + (n + 1) * 512], op=ALU.mult),
                                r=[f'psB{bk}', f'g{cc}_{n}'], w=[f'g{cc}_{n}']), [0, 1, 2, 3])

                    for c in range(4):
                        for w_ in range(31):
                            op('dve', I('tensor_scalar',
                                out=dg[:, w_, :], in0=ident[:], scalar1=cwT[:, c, w_:w_ + 1], scalar2=None,
                                op0=ALU.mult), r=['ident', 'cwT'], w=[f'dg{w_}'])
                        for n in range(4):
                            bk = n % 4
                            gkeys = [f'g{c}_{n}'] + ([f'g{c}_{n - 1}'] if n > 0 else ['gTpad'])
                            for w_ in range(31):
                                op('pe', I('matmul',
                                    psB[:, bk, :], dg[:, w_, :], gT[:, c, n * 512 + w_:n * 512 + w_ + 512],
                                    start=(w_ == 0), stop=(w_ == 30)),
                                   r=[f'dg{w_}'] + gkeys, w=[f'psB{bk}'])
                            op('act', I('activation',
                                out=ycv[:, c, n * 512:(n + 1) * 512], in_=psB[:, bk, :], func=AF.Identity,
                                bias=cvb[:, c:c + 1]), r=[f'psB{bk}', 'cvb'], w=[f'y{c}_{n}'])
                    for n in range(4):
                        ns = slice(n * 512, (n + 1) * 512)
                        for c in range(4):
                            op('pe', I('matmul', psB[:, 4, :], meanmat[:], ycv[:, c, ns],
                                                                    start=(c == 0), stop=(c == 3)),
                               r=['meanmat', f'y{c}_{n}'], w=['psB4'])
                        for c in range(4):
                            op('act', I('activation', out=ysq[:, c, :], in_=ycv[:, c, ns],
                                                                         func=AF.Square),
                               r=[f'y{c}_{n}'], w=[f'ysq{c}'])
                        for c in range(4):
                            op('pe', I('matmul', psB[:, 5, :], meanmat[:], ysq[:, c, :],
                                                             start=(c == 0), stop=(c == 3)),
                               r=['meanmat', f'ysq{c}'], w=['psB5'])
                        op('act', I('activation', out=lnw[:, 0, :], in_=psB[:, 4, :], func=AF.Copy),
                           r=['psB4'], w=['lnw0'])
                        op('pool', I('tensor_tensor', out=lnw[:, 1, :], in0=lnw[:, 0, :], in1=lnw[:, 0, :],
                                                             op=ALU.mult), r=['lnw0'], w=['lnw1'])
                        op('dve', I('tensor_tensor', out=lnw[:, 1, :], in0=psB[:, 5, :], in1=lnw[:, 1, :],
                                                            op=ALU.subtract), r=['psB5', 'lnw1'], w=['lnw1'])
                        op('dve', I('tensor_scalar', out=lnw[:, 1, :], in0=lnw[:, 1, :], scalar1=0.0,
                                                            scalar2=None, op0=ALU.max), r=['lnw1'], w=['lnw1'])
                        op('act', I('activation', out=lnw[:, 1, :], in_=lnw[:, 1, :], func=AF.Sqrt, bias=EPS),
                           r=['lnw1'], w=['lnw1'])
                        op('dve', I('reciprocal', out=lnw[:, 2, :], in_=lnw[:, 1, :]), r=['lnw1'], w=['lnw2'])
                        for c in range(4):
                            op('pool', I('tensor_tensor', out=lnw[:, 3, :], in0=ycv[:, c, ns],
                                                                             in1=lnw[:, 0, :], op=ALU.subtract),
                               r=[f'y{c}_{n}', 'lnw0'], w=['lnw3'])
                            op('pool', I('tensor_tensor', out=lnw[:, 3, :], in0=lnw[:, 3, :], in1=lnw[:, 2, :],
                                                                 op=ALU.mult), r=['lnw3', 'lnw2'], w=['lnw3'])
                            op('act', I('activation',
                                out=hT[:, 4 + c, ns], in_=lnw[:, 3, :], func=AF.Silu, scale=cvg[:, c:c + 1],
                                bias=cvbb[:, c:c + 1]), r=['lnw3', 'cvg', 'cvbb'], w=[HK(4 + c, n)])
                    S.barrier()

                with ExitStack() as s1b:
                    spb = [T(s1b, f"spb{i}", [128, 512], F32) for i in range(4)]
                    ab = [T(s1b, f"ab{i}", [128, 512], BF16) for i in range(4)]
                    Rb = [T(s1b, f"Rb{i}", [128, 512], F32) for i in range(2)]
                    osq = [T(s1b, f"osq{i}", [128, 512], F32) for i in range(2)]
                    units = []
                    for j in range(4):
                        for qn in range(4):
                            kcs = list(range(4 * qn + 3, -1, -1))
                            for idx, kc in enumerate(kcs):
                                for hp in range(2):
                                    units.append((j, qn, idx, kc, hp, len(kcs)))
                    nU = len(units)
                    negtri_r = negtriR[:].bitcast(F32R)
                    negones_r = negonesR[:].bitcast(F32R)
                    oqc = [0]

                    def geo(i):
                        j, qn, idx, kc, hp, nk = units[i]
                        return (j, qn, idx, kc, hp, nk, slice(hp * 64, hp * 64 + 64), slice(qn * 512, (qn + 1) * 512),
                                slice(kc * 128, (kc + 1) * 128))

                    def S1(i0_):
                        us = [i0_, i0_ + 1]
                        for i in us:
                            j, qn, idx, kc, hp, nk, P, qs, ks = geo(i)
                            zb = i % 2
                            op('pe', I('matmul', psB[:, zb, :], qk[P, 4 + j, ks], qk[P, j, qs], start=True, stop=True),
                               r=[f'k{j}_{kc // 4}', f'q{j}_{qn}'], w=[f'psB{zb}'])
                        for i in us:
                            j, qn, idx, kc, hp, nk, P, qs, ks = geo(i)
                            zb = i % 2
                            sp_t, spk = spb[i % 4], f'spb{i % 4}'
                            op('act', I('activation', out=sp_t[:].bitcast(F32R), in_=psB[:, zb, :], func=AF.Exp),
                               r=[f'psB{zb}'], w=[spk])
                            op('act', I('activation', out=sp_t[:].bitcast(F32R), in_=sp_t[:], func=AF.Ln, bias=1.0),
                               r=[spk], w=[spk])
                            if kc >= 4 * qn:
                                op('pool', I('tensor_tensor', out=sp_t[:].bitcast(F32R), in0=sp_t[:],
                                             in1=maskf[:, kc - 4 * qn, :], op=ALU.mult), r=[spk, 'maskf'], w=[spk])

                    def S2(i0_):
                        us = [i0_, i0_ + 1]
                        for i in us:
                            j, qn, idx, kc, hp, nk, P, qs, ks = geo(i)
                            eb = 2 + i % 2
                            op('pe', I('matmul', psB[:, eb, :], qk[P, 4 + j, ks], qk[P, j, qs], start=True, stop=False),
                               r=[f'k{j}_{kc // 4}', f'q{j}_{qn}'], w=[f'psB{eb}'])
                        for i in us:
                            j, qn, idx, kc, hp, nk, P, qs, ks = geo(i)
                            eb = 2 + i % 2
                            ek = f'psB{eb}'
                            sp_t, spk = spb[i % 4], f'spb{i % 4}'
                            op('pe', I('matmul', psB[:, eb, :], negtri_r, sp_t[:].bitcast(F32R), start=False,
                                       stop=(idx == 0)), r=['negtriR', spk], w=[ek])
                            if idx > 0:
                                op('pe', I('matmul', psB[:, eb, :], negones_r, Rb[hp][:].bitcast(F32R), start=False,
                                           stop=True), r=['negonesR', f'Rb{hp}'], w=[ek])
                        for i in us:
                            j, qn, idx, kc, hp, nk, P, qs, ks = geo(i)
                            eb = 2 + i % 2
                            ek = f'psB{eb}'
                            sp_t, spk = spb[i % 4], f'spb{i % 4}'
                            ab_t, abk = ab[i % 4], f'ab{i % 4}'
                            op('act', I('activation', out=ab_t[:], in_=psB[:, eb, :], func=AF.Exp), r=[ek], w=[abk])
                            if kc >= 4 * qn:
                                op('dve', I('tensor_tensor', out=ab_t[:], in0=ab_t[:], in1=maskf[:, kc - 4 * qn, :],
                                            op=ALU.mult), r=[abk, 'maskf'], w=[abk])
                            if idx < nk - 1:
                                if idx == 0:
                                    op('pool', I('tensor_copy', out=Rb[hp][:].bitcast(F32R), in_=sp_t[:]), r=[spk],
                                       w=[f'Rb{hp}'])
                                else:
                                    op('pool', I('tensor_tensor', out=Rb[hp][:].bitcast(F32R), in0=Rb[hp][:],
                                                 in1=sp_t[:], op=ALU.add), r=[spk, f'Rb{hp}'], w=[f'Rb{hp}'])

                    def S3(i0_):
                        us = [i0_, i0_ + 1]
                        for i in us:
                            j, qn, idx, kc, hp, nk, P, qs, ks = geo(i)
                            h = 2 * j + hp
                            ab_t, abk = ab[i % 4], f'ab{i % 4}'
                            op('pe', I('matmul', psB[P, 4, :], v_sb[:, kc, h * 64:(h + 1) * 64], ab_t[:],
                                       start=(idx == 0), stop=(idx == nk - 1)), r=[f'v{kc}', abk], w=[f'psB4_{hp}'])
                        j, qn, idx, kc, hp, nk, P, qs, ks = geo(i0_ + 1)
                        if idx == nk - 1:
                            oq_t = osq[oqc[0] & 1]
                            oqk = f'osq{oqc[0] & 1}'
                            oqc[0] += 1
                            op('act', I('activation', out=hT[:, j, qs], in_=psB[:, 4, :], func=AF.Copy,
                                        scale=sbg[:, j:j + 1]), r=['psB4_0', 'psB4_1', 'sbg'], w=[HK(j, qn)])
                            op('act', I('activation', out=oq_t[:], in_=psB[:, 4, :], func=AF.Square),
                               r=['psB4_0', 'psB4_1'], w=[oqk])
                            for tt in range(4):
                                col = (j * 16 + qn * 4 + tt) * 2
                                op('pe', I('matmul', psB[:, 5, col:col + 2], oq_t[:, tt * 128:(tt + 1) * 128], ones2[:],
                                           start=True, stop=True), r=[oqk, 'ones2'], w=['psB5'])

                    nP = nU // 2
                    for p in range(nP + 2):
                        if p < nP:
                            S1(2 * p)
                        if 0 <= p - 1 < nP:
                            S2(2 * (p - 1))
                        if 0 <= p - 2 < nP:
                            S3(2 * (p - 2))
                    ssv = lambda j: psB[:, 5, j * 32:(j + 1) * 32].rearrange("p (t two) -> p t two", two=2)[:, :, 0]
                    op('act', I('activation', out=rsb[:, 0, :], in_=ssv(0), func=AF.Copy), r=['psB5'], w=['rsb'])
                    for j in range(1, 4):
                        op('dve', I('tensor_tensor', out=rsb[:, 0, :], in0=ssv(j), in1=rsb[:, 0, :],
                                                                 op=ALU.add), r=['psB5', 'rsb'], w=['rsb'])
                    op('act', I('activation', out=rsb[:, 1, :], in_=rsb[:, 0, :], func=AF.Sqrt, scale=1.0 / 512,
                                                     bias=EPS), r=['rsb'], w=['rsb'])
                    op('dve', I('reciprocal', out=rsb[:, 2, :], in_=rsb[:, 1, :]), r=['rsb'], w=['rsb'])
                    S.barrier()
                    if stage == 1 and b == 0:
                        op('sp', I('dma_start', out=dbg_mix, in_=hT[:]), dma='dbg0')
                        op('sp', I('dma_start', out=dbg_qk, in_=qk[:]), dma='dbg1')
                        op('sp', I('dma_start', out=dbg_v, in_=v_sb[:]), dma='dbg2')
                        op('sp', I('dma_start', out=dbg_rsb, in_=rsb[:]), dma='dbg3')
                        S.barrier()

            with ExitStack() as s2:
                x_sb = T(s2, "x_sb", [128, NT, DM], F32)
                for t in range(NT):
                    op('sp', I('dma_start', out=x_sb[:, t, :], in_=x_d[b, t * 128:(t + 1) * 128, :]),
                       w=[f'x{t}'], dma=f'x{t}')
                nxt = load_w(w_out_d[:, 0:512])
                for n in range(2):
                    sl = nxt
                    nxt = load_w(w_out_d[:, 512:1024]) if n == 0 else load_w(w_xkv_d[:, 0:512])
                    ns = slice(n * 512, (n + 1) * 512)
                    for t in range(NT):
                        b0, b1 = (t % 2) * 2, (t % 2) * 2 + 1
                        ts_ = slice(t * 128, (t + 1) * 128)
                        for jj in range(4):
                            op('pe', I('matmul',
                                psB[:, b0, :], hT[:, jj, ts_], wr[sl][:, jj, :], start=(jj == 0), stop=(jj == 3)),
                               r=[HK(jj, t // 4), f'wr{sl}'], w=[f'psB{b0}'])
                        for jj in range(4):
                            op('pe', I('matmul',
                                psB[:, b1, :], hT[:, 4 + jj, ts_], wr[sl][:, 4 + jj, :], start=(jj == 0), stop=(jj == 3)),
                               r=[HK(4 + jj, t // 4), f'wr{sl}'], w=[f'psB{b1}'])
                        op('dve', I('tensor_tensor',
                            out=x_sb[:, t, ns], in0=psB[:, b1, :], in1=x_sb[:, t, ns], op=ALU.add),
                           r=[f'psB{b1}', f'x{t}'], w=[f'x{t}'])
                        op('dve', I('scalar_tensor_tensor',
                            out=x_sb[:, t, ns], in0=psB[:, b0, :], scalar=rsb[:, 2, t:t + 1], in1=x_sb[:, t, ns],
                            op0=ALU.mult, op1=ALU.add), r=[f'psB{b0}', 'rsb', f'x{t}'], w=[f'x{t}'])
                if stage == 1:
                    for t in range(NT):
                        ev = op('sp', I('dma_start',
                            out=out_d[b * SEQ + t * 128:b * SEQ + (t + 1) * 128, :], in_=x_sb[:, t, :]),
                            r=[f'x{t}'], dma=f'o{t % 4}')
                    S.barrier()
                    continue

                with ExitStack() as s2f:
                    memin = [T(s2f, f"memin{i}", [128, DM], F32) for i in range(2)]
                    hn2 = [T(s2f, f"hnb{i}", [128, DM], BF16) for i in range(2)]
                    memT = T(s2f, "memT", [128, 8, NMEM], BF16)
                    kxT = T(s2f, "kxT", [128, 8, NMEM], BF16)
                    vx = T(s2f, "vx", [128, 2, DM], BF16)
                    qxT = T(s2f, "qxT", [128, 8, SEQ], BF16)
                    pf = [T(s2f, "pf0", [128, 4, NMEM], F32)] * 2
                    pn = [T(s2f, f"pn{i}", [128, 4, NMEM], BF16) for i in range(2)]
                    pT = [T(s2f, "pT0", [128, 4, 2, 512], BF16)] * 2
                    sm = T(s2f, "sm", [128, 4, 4], F32)
                    for mt in range(2):
                        op('sp', I('dma_start', out=memin[mt][:], in_=mem_d[b, mt * 128:(mt + 1) * 128, :]),
                           w=[f'memin{mt}'], dma=f'memin{mt}')
                        norm_T(memin[mt][:], f'memin{mt}', hn2[mt], f'hnb{mt}', 2,
                               memT[:, :, mt * 128:(mt + 1) * 128], ['memT'], mt, mt)
                    mrhs = lambda kc, n: memT[:, kc, :]
                    mkeys = lambda kc, n: ['memT']
                    for g in range(2):
                        sl = nxt
                        nxt = load_w(w_xkv_d[:, (g + 1) * 512:(g + 2) * 512])
                        fm_proj(sl, 4, mrhs, mkeys, 1, NMEM,
                                lambda cc, n, bk, g=g: evac_copy(kxT[:, g * 4 + cc, :], psB[:, bk, 0:NMEM], [f'psB{bk}'],
                                                                 ['kxT']), [0, 1, 2, 3])
                    for g in range(2):
                        sl = nxt
                        nxt = load_w(w_xkv_d[:, 1536:2048]) if g == 0 else load_w(w_xq_d[:, 0:512])
                        for mt in range(2):
                            bk = mt
                            for kc in range(8):
                                op('pe', I('matmul',
                                    psB[:, bk, :], memT[:, kc, mt * 128:(mt + 1) * 128], wr[sl][:, kc, :],
                                    start=(kc == 0), stop=(kc == 7)), r=['memT', f'wr{sl}'], w=[f'psB{bk}'])
                            evac_copy(vx[:, mt, g * 512:(g + 1) * 512], psB[:, bk, :], [f'psB{bk}'], ['vx'])
                    for t in range(NT):
                        norm_T(x_sb[:, t, :], f'x{t}', hn2[t % 2], f'hnb{t % 2}', 1,
                               hT[:, :, t * 128:(t + 1) * 128], [HK(c, t // 4) for c in range(8)], t, t % 2)
                    for g in range(2):
                        sl = nxt
                        nxt = load_w(w_xq_d[:, 512:1024]) if g == 0 else load_w(w_xo_d[:, 0:512])
                        fm_proj(sl, 4, hrhs, hkeys, 4, 512,
                                lambda cc, n, bk, g=g: evac_copy(qxT[:, g * 4 + cc, n * 512:(n + 1) * 512], psB[:, bk, :],
                                                                 [f'psB{bk}'], [f'qx{g * 4 + cc}_{n}'], scale=1.0 / 16),
                                [0, 1, 2, 3])
                    psSs = [psB[:, 0:2, :].rearrange("p a (h m) -> p (a h) m", h=2),
                            psB[:, 2:4, :].rearrange("p a (h m) -> p (a h) m", h=2)]
                    sm2 = [T(s2f, f"sm2_{i}", [128, 4, 4], F32) for i in range(2)]
                    pvk = [0]

                    def FA(t):
                        pi = t % 2
                        n = t // 4
                        ts_ = slice(t * 128, (t + 1) * 128)
                        psS = psSs[pi]
                        for hx in range(4):
                            for dc in range(2):
                                op('pe', I('matmul', psS[:, hx, :], qxT[:, 2 * hx + dc, ts_], kxT[:, 2 * hx + dc, :],
                                           start=(dc == 0), stop=(dc == 1)),
                                   r=[f'qx{2 * hx + dc}_{n}', 'kxT'], w=[f'psB{2 * pi + hx // 2}'])
                        op('dve', I('tensor_reduce', out=sm2[pi][:, 0, :], in_=psS, axis=AX.X, op=ALU.max),
                           r=[f'psB{2 * pi}', f'psB{2 * pi + 1}'], w=[f'sm{pi}'])
                        op('dve', I('tensor_scalar', out=sm2[pi][:, 1, :], in0=sm2[pi][:, 0, :], scalar1=-1.0,
                                    scalar2=None, op0=ALU.mult), r=[f'sm{pi}'], w=[f'sm{pi}'])

                    def FB(t):
                        pi = t % 2
                        n = t // 4
                        tt = t % 4
                        psS = psSs[pi]
                        smt = sm2[pi]
                        pT_t = pT[n % 2]
                        for hx in range(4):
                            op('act', I('activation', out=pf[pi][:, hx, :], in_=psS[:, hx, :], func=AF.Exp,
                                        bias=smt[:, 1, hx:hx + 1], accum_out=smt[:, 2, hx:hx + 1]),
                               r=[f'psB{2 * pi + hx // 2}', f'sm{pi}'], w=['pf0', f'sm{pi}'])
                        op('dve', I('reciprocal', out=smt[:, 3, :], in_=smt[:, 2, :]), r=[f'sm{pi}'], w=[f'sm{pi}'])
                        for hx in range(4):
                            op('dve', I('tensor_scalar', out=pn[pi][:, hx, :], in0=pf[pi][:, hx, :],
                                        scalar1=smt[:, 3, hx:hx + 1], scalar2=None, op0=ALU.mult),
                               r=['pf0', f'sm{pi}'], w=[f'pn{pi}'])
                        for hx in range(4):
                            for mc in range(2):
                                op('pe', I('transpose', out=psA[:, pi, (hx * 2 + mc) * 128:(hx * 2 + mc + 1) * 128],
                                           in_=pn[pi][:, hx, mc * 128:(mc + 1) * 128], identity=ident[:]),
                                   r=[f'pn{pi}', 'ident'], w=[f'psA{pi}'])
                        op('act', I('activation', out=pT_t[:, :, :, tt * 128:(tt + 1) * 128],
                                    in_=psA[:, pi, :].rearrange("p (h m c) -> p h m c", h=4, m=2), func=AF.Copy),
                           r=[f'psA{pi}'], w=['pT0'])
                        if tt == 3:
                            for hx in range(4):
                                for dc in range(2):
                                    bk = 4 + (pvk[0] % 2)
                                    pvk[0] += 1
                                    for mc in range(2):
                                        op('pe', I('matmul', psB[:, bk, :],
                                                   vx[:, mc, hx * 256 + dc * 128:hx * 256 + (dc + 1) * 128],
                                                   pT_t[:, hx, mc, :], start=(mc == 0), stop=(mc == 1)),
                                           r=['vx', 'pT0'], w=[f'psB{bk}'])
                                    evac_copy(hT[:, 2 * hx + dc, n * 512:(n + 1) * 512], psB[:, bk, :], [f'psB{bk}'],
                                              [HK(2 * hx + dc, n)])

                    FA(0)
                    for t in range(NT):
                        if t + 1 < NT:
                            FA(t + 1)
                        FB(t)
                    for n in range(2):
                        sl = nxt
                        nxt = load_w(w_xo_d[:, 512:1024]) if n == 0 else None
                        ns = slice(n * 512, (n + 1) * 512)
                        for t in range(NT):
                            bk = t % 4
                            for cc in range(8):
                                op('pe', I('matmul',
                                    psB[:, bk, :], hT[:, cc, t * 128:(t + 1) * 128], wr[sl][:, cc, :],
                                    start=(cc == 0), stop=(cc == 7)), r=[HK(cc, t // 4), f'wr{sl}'], w=[f'psB{bk}'])
                            op('dve', I('tensor_tensor',
                                out=x_sb[:, t, ns], in0=psB[:, bk, :], in1=x_sb[:, t, ns], op=ALU.add),
                               r=[f'psB{bk}', f'x{t}'], w=[f'x{t}'])
                    S.barrier()
                if stage == 2:
                    for t in range(NT):
                        ev = op('sp', I('dma_start',
                            out=out_d[b * SEQ + t * 128:b * SEQ + (t + 1) * 128, :], in_=x_sb[:, t, :]),
                            r=[f'x{t}'], dma=f'o{t % 4}')
                    S.barrier()
                    continue

                with ExitStack() as s2g:
                    GB = 8
                    gbc = T(s2g, "gbc", [128, DM], F32)
                    h3 = [T(s2g, f"h3_{i}", [128, DM], F32) for i in range(2)]
                    h3b = T(s2g, "h3b", [128, GB, DM], BF16)
                    h3T = [T(s2g, f"h3T{i}", [128, 8, 128], F32) for i in range(2)]
                    LG = T(s2g, "LG", [128, GB, 36], F32)
                    gmax = T(s2g, "gmax", [128, GB], F32)
                    gsh = T(s2g, "gsh", [128, GB, 4], F32)
                    gex = T(s2g, "gex", [128, GB, 4], F32)
                    gsum = T(s2g, "gsum", [128, GB], F32)
                    gw = T(s2g, "gw", [128, GB], F32)
                    gm = T(s2g, "gm", [128, GB, 4], F32)
                    elm = T(s2g, "elm", [128, GB, 4, 8], F32)
                    igr = T(s2g, "igr", [128, GB, 8], F32)
                    ig2 = T(s2g, "ig2", [128, GB, 8], F32)
                    mk1 = T(s2g, "mk1", [128, GB, 8], F32)
                    mk2 = T(s2g, "mk2", [128, GB, 8], F32)
                    m12 = T(s2g, "m12", [128, 4, GB], F32)
                    M1 = T(s2g, "M1", [128, GB, 4, 8], F32)
                    M2 = T(s2g, "M2", [128, GB, 4, 8], F32)
                    S32b = T(s2g, "S32b", [128, GB, 32], BF16)
                    posa = T(s2g, "posa", [128, GB, 32], F32)
                    ova = T(s2g, "ova", [128, GB, 32], F32)
                    slf = T(s2g, "slf", [128, 2, GB], F32)
                    op('sp', I('dma_start', out=gbc[:], in_=ln_ffn_g.partition_broadcast(128)), w=['gbc'],
                       dma='gbc')

                    def G1(t):
                        hi = t % 2
                        tl = t % GB
                        op('act', I('activation', out=junk[:], in_=x_sb[:, t, :], func=AF.Square,
                                    accum_out=stat[:, 0, t:t + 1]), r=[f'x{t}'], w=['junk', 'stat'])
                        rstd_from_ss(stat, t, 1.0 / DM)
                        op('dve', I('scalar_tensor_tensor', out=h3[hi][:], in0=x_sb[:, t, :],
                                    scalar=stat[:, 2, t:t + 1], in1=gbc[:], op0=ALU.mult, op1=ALU.mult),
                           r=[f'x{t}', 'stat', 'gbc'], w=[f'h3_{hi}'])
                        op('pool', I('tensor_copy', out=h3b[:, tl, :], in_=h3[hi][:]), r=[f'h3_{hi}'], w=[f'h3b{tl}'])
                        b4, b5 = (4, 5) if hi == 0 else (2, 3)
                        for kc in range(8):
                            bb = b4 if kc < 4 else b5
                            op('pe', I('transpose', out=psB[:, bb, (kc % 4) * 128:(kc % 4 + 1) * 128],
                                       in_=h3[hi][:, kc * 128:(kc + 1) * 128], identity=identf[:]),
                               r=[f'h3_{hi}', 'identf'], w=[f'psB{bb}'])
                        op('act', I('activation', out=h3T[hi][:, 0:4, :],
                                    in_=psB[:, b4, :].rearrange("p (k c) -> p k c", k=4), func=AF.Copy),
                           r=[f'psB{b4}'], w=[f'h3T{hi}a'])
                        op('dve', I('tensor_copy', out=h3T[hi][:, 4:8, :],
                                    in_=psB[:, b5, :].rearrange("p (k c) -> p k c", k=4)),
                           r=[f'psB{b5}'], w=[f'h3T{hi}b'])
                        for kc in range(8):
                            op('pe', I('matmul', psB[:, hi, 0:36], h3T[hi][:, kc, :], wrt[:, kc, :],
                                       start=(kc == 0), stop=(kc == 7)),
                               r=[f'h3T{hi}a', f'h3T{hi}b', 'wrt'], w=[f'psB{hi}'])
                        op('dve', I('tensor_tensor', out=LG[:, tl, :], in0=psB[:, hi, 0:36], in1=brt[:], op=ALU.add),
                           r=[f'psB{hi}', 'brt'], w=['LG'])
                        op('sp', I('dma_start', out=xres_d[b * SEQ + t * 128:b * SEQ + (t + 1) * 128, :],
                                   in_=x_sb[:, t, :]), r=[f'x{t}'], w=[f'xres{t}'], dma=f'xw{t % 4}')

                    def G23(g):
                        tg0 = b * NT + g * GB
                        V = lambda f, r=(), w=(): op('dve', f, r=['rt2'] + list(r), w=['rt2'] + list(w))
                        GL = LG[:, :, 0:4]
                        EL = LG[:, :, 4:36].rearrange("p t (g e) -> p t g e", g=4)
                        bc3 = lambda ap2, n: ap2[:, :, None].to_broadcast([128, GB, n])
                        V(I('tensor_reduce', out=gmax[:], in_=GL, axis=AX.X, op=ALU.max), r=['LG'])
                        V(I('tensor_tensor', out=gsh[:], in0=GL, in1=bc3(gmax, 4), op=ALU.subtract), r=['LG'])
                        op('act', I('activation', out=gex[:], in_=gsh[:], func=AF.Exp), r=['rt2'], w=['rt2'])
                        V(I('tensor_reduce', out=gsum[:], in_=gex[:], axis=AX.X, op=ALU.add))
                        V(I('reciprocal', out=gw[:], in_=gsum[:]))
                        V(I('tensor_scalar', out=gm[:], in0=gsh[:], scalar1=0.0, scalar2=None, op0=ALU.is_equal))
                        V(I('tensor_tensor', out=elm[:], in0=EL, in1=gm[:, :, :, None].to_broadcast([128, GB, 4, 8]),
                            op=ALU.mult), r=['LG'])
                        V(I('tensor_reduce', out=igr[:], in_=elm[:].rearrange("p t g e -> p t e g"), axis=AX.X,
                            op=ALU.add))
                        V(I('tensor_reduce', out=m12[:, 0, :], in_=igr[:], axis=AX.X, op=ALU.max))
                        V(I('tensor_tensor', out=mk1[:], in0=igr[:], in1=bc3(m12[:, 0, :], 8), op=ALU.is_equal))
                        V(I('scalar_tensor_tensor', out=ig2[:], in0=mk1[:], scalar=-1e30, in1=igr[:], op0=ALU.mult,
                            op1=ALU.add))
                        V(I('tensor_reduce', out=m12[:, 1, :], in_=ig2[:], axis=AX.X, op=ALU.max))
                        V(I('tensor_tensor', out=mk2[:], in0=ig2[:], in1=bc3(m12[:, 1, :], 8), op=ALU.is_equal))
                        V(I('tensor_tensor', out=m12[:, 2, :], in0=m12[:, 1, :], in1=m12[:, 0, :], op=ALU.subtract))
                        op('act', I('activation', out=m12[:, 2, :], in_=m12[:, 2, :], func=AF.Exp), r=['rt2'], w=['rt2'])
                        V(I('tensor_scalar', out=m12[:, 3, :], in0=m12[:, 2, :], scalar1=1.0, scalar2=None, op0=ALU.add))
                        V(I('reciprocal', out=m12[:, 3, :], in_=m12[:, 3, :]))
                        V(I('tensor_tensor', out=wts[:, tg0:tg0 + GB, 0], in0=m12[:, 3, :], in1=gw[:], op=ALU.mult))
                        V(I('tensor_tensor', out=wts[:, tg0:tg0 + GB, 1], in0=m12[:, 2, :], in1=wts[:, tg0:tg0 + GB, 0],
                            op=ALU.mult))
                        gm4 = gm[:, :, :, None].to_broadcast([128, GB, 4, 8])
                        V(I('tensor_tensor', out=M1[:], in0=gm4, in1=mk1[:, :, None, :].to_broadcast([128, GB, 4, 8]),
                            op=ALU.mult))
                        V(I('tensor_tensor', out=M2[:], in0=gm4, in1=mk2[:, :, None, :].to_broadcast([128, GB, 4, 8]),
                            op=ALU.mult))
                        V(I('tensor_tensor', out=elm[:], in0=M1[:], in1=M2[:], op=ALU.add))
                        V(I('tensor_copy', out=S32b[:], in_=elm[:].rearrange("p t g e -> p t (g e)")))
                        for tl in range(GB):
                            pb = tl % 2
                            op('pe', I('matmul', psB[:, pb, 64:96], Lmat[:], S32b[:, tl, :], start=True, stop=True),
                               r=['Lmat', 'rt2'], w=[f'psB{pb}'])
                            op('pe', I('matmul', psB[:, pb, 96:128], onesb[:], S32b[:, tl, :], start=True, stop=True),
                               r=['onesb', 'rt2'], w=[f'psB{pb}'])
                            V(I('tensor_tensor', out=posa[:, tl, :], in0=psB[:, pb, 64:96], in1=base[:], op=ALU.add),
                              r=[f'psB{pb}', 'base'])
                            V(I('tensor_tensor', out=base[:], in0=psB[:, pb, 96:128], in1=base[:], op=ALU.add),
                              r=[f'psB{pb}', 'base'], w=['base'])
                        V(I('tensor_scalar', out=ova[:], in0=posa[:], scalar1=float(CAP), scalar2=1e7, op0=ALU.is_ge,
                            op1=ALU.mult))
                        V(I('tensor_tensor', out=posa[:], in0=posa[:], in1=ova[:], op=ALU.add))
                        V(I('tensor_tensor', out=posa[:], in0=posa[:], in1=ecap[:, None, :].to_broadcast([128, GB, 32]),
                            op=ALU.add))
                        V(I('tensor_tensor', out=ova[:], in0=posa[:], in1=M1[:].rearrange("p t g e -> p t (g e)"),
                            op=ALU.mult))
                        V(I('tensor_reduce', out=slf[:, 0, :], in_=ova[:], axis=AX.X, op=ALU.add))
                        V(I('tensor_tensor', out=ova[:], in0=posa[:], in1=M2[:].rearrange("p t g e -> p t (g e)"),
                            op=ALU.mult))
                        V(I('tensor_reduce', out=slf[:, 1, :], in_=ova[:], axis=AX.X, op=ALU.add))
                        V(I('tensor_copy', out=slots[:, tg0:tg0 + GB, :], in_=slf[:].rearrange("p k t -> p t k")),
                          w=['slotsg'])
                        for tl in range(GB):
                            for k2 in range(2):
                                op('pool', I('indirect_dma_start', out=xd_d[:, :],
                                             out_offset=bass.IndirectOffsetOnAxis(ap=slots[:, tg0 + tl, k2:k2 + 1], axis=0),
                                             in_=h3b[:, tl, :], in_offset=None, bounds_check='REG', oob_is_err=False),
                                   r=['slotsg', f'h3b{tl}'], w=[f'xd{tg0 + tl}_{k2}'], dma=f'sc{tl}_{k2}')

                    for g in range(NT // GB):
                        for tl in range(GB):
                            G1(g * GB + tl)
                        G23(g)
                    S.barrier()

        if stage >= 3:
            with ExitStack() as s3:
                wg = [T(s3, f"wg{i}", [128, 8, 512], BF16) for i in range(2)]
                wu = [T(s3, f"wu{i}", [128, 8, 512], BF16) for i in range(2)]
                wd = [T(s3, f"wd{i}", [128, 4, DM], BF16) for i in range(2)]
                xr = [T(s3, f"xr{i}", [128, DM], BF16) for i in range(3)]
                xT = [T(s3, f"xT{i}", [128, 8, 384], BF16) for i in range(2)]
                sg = [T(s3, f"sg{i}", [128, 384], F32) for i in range(2)]
                h1T = [T(s3, f"h1T{i}", [128, 4, 384], BF16) for i in range(2)]
                ydt = [T(s3, f"ydt{i}", [128, DM], F32) for i in range(2)]

                wst = [T(s3, f"wst{i}", [128, 8, 512], F32) for i in range(3)]

                def load_dma(e_):
                    op('act', I('dma_start', out=wst[0][:], in_=w_gate_d[e_].rearrange("(k p) n -> p k n", p=128)),
                       w=['wst0'], dma='wst0')
                    op('act', I('dma_start', out=wst[1][:], in_=w_up_d[e_].rearrange("(k p) n -> p k n", p=128)),
                       w=['wst1'], dma='wst1')
                    op('act', I('dma_start', out=wst[2][:].rearrange("p (a b) n -> p a (b n)", a=4),
                               in_=w_down_d[e_].rearrange("(k p) n -> p k n", p=128)), w=['wst2'], dma='wst2')

                def cast_gu(e_):
                    i = e_ % 2
                    op('dve', I('tensor_copy', out=wg[i][:], in_=wst[0][:]), r=['wst0'], w=[f'wg{i}'])
                    op('act', I('activation', out=wu[i][:], in_=wst[1][:], func=AF.Copy), r=['wst1'], w=[f'wu{i}'])

                def cast_d(e_):
                    i = e_ % 2
                    wsv = wst[2][:].rearrange("p (a b) n -> p a (b n)", a=4)
                    op('act', I('activation', out=wd[i][:, 0:2, :], in_=wsv[:, 0:2, :], func=AF.Copy),
                       r=['wst2'], w=[f'wd{i}a'])
                    op('dve', I('tensor_copy', out=wd[i][:, 2:4, :], in_=wsv[:, 2:4, :]), r=['wst2'], w=[f'wd{i}b'])

                def load_expert(e_):
                    load_dma(e_)
                    cast_gu(e_)
                    cast_d(e_)

                load_expert(0)
                cnt = 0
                yc = 0
                for e_ in range(NEXP):
                    wi = e_ % 2
                    if e_ + 1 < NEXP:
                        load_dma(e_ + 1)
                    for half in range(2):
                        if half == 1 and e_ + 1 < NEXP:
                            cast_gu(e_ + 1)
                        r0 = e_ * CAP + half * 384
                        hb = cnt % 2
                        cnt += 1
                        for ci in range(3):
                            xi = (cnt * 3 + ci) % 3
                            op('sp', I('dma_start',
                                out=xr[xi][:], in_=xd_d[r0 + ci * 128:r0 + (ci + 1) * 128, :]),
                                r=['xd'], w=[f'xr{xi}'], dma=f'xr{xi}')
                            pa = ci % 2
                            for kc in range(8):
                                op('pe', I('transpose',
                                    out=psA[:, pa, kc * 128:(kc + 1) * 128], in_=xr[xi][:, kc * 128:(kc + 1) * 128],
                                    identity=ident[:]), r=[f'xr{xi}', 'ident'], w=[f'psA{pa}'])
                            evac_copy(xT[hb][:, :, ci * 128:(ci + 1) * 128],
                                      psA[:, pa, :].rearrange("p (k c) -> p k c", k=8), [f'psA{pa}'], [f'xT{hb}'])
                        for dc in range(4):
                            bg, bu = (dc % 2) * 2, (dc % 2) * 2 + 1
                            for kc in range(8):
                                op('pe', I('matmul',
                                    psB[:, bg, 0:384], wg[wi][:, kc, dc * 128:(dc + 1) * 128], xT[hb][:, kc, :],
                                    start=(kc == 0), stop=(kc == 7)), r=[f'wg{wi}', f'xT{hb}'], w=[f'psB{bg}'])
                            for kc in range(8):
                                op('pe', I('matmul',
                                    psB[:, bu, 0:384], wu[wi][:, kc, dc * 128:(dc + 1) * 128], xT[hb][:, kc, :],
                                    start=(kc == 0), stop=(kc == 7)), r=[f'wu{wi}', f'xT{hb}'], w=[f'psB{bu}'])
                            si = dc % 2
                            op('act', I('activation', out=sg[si][:], in_=psB[:, bg, 0:384],
                                                                           func=AF.Silu),
                               r=[f'psB{bg}'], w=[f'sg{si}'])
                            op('dve', I('tensor_tensor',
                                out=h1T[hb][:, dc, :], in0=psB[:, bu, 0:384], in1=sg[si][:], op=ALU.mult),
                               r=[f'psB{bu}', f'sg{si}'], w=[f'h1T{hb}_{dc}'])
                        for ci in range(3):
                            yi = yc % 2
                            yc += 1
                            for nn in range(2):
                                for dc in range(4):
                                    op('pe', I('matmul',
                                        psB[:, 4 + nn, :], h1T[hb][:, dc, ci * 128:(ci + 1) * 128],
                                        wd[wi][:, dc, nn * 512:(nn + 1) * 512], start=(dc == 0), stop=(dc == 3)),
                                       r=[f'h1T{hb}_{dc}', f'wd{wi}a', f'wd{wi}b'], w=[f'psB{4 + nn}'])
                            op('act', I('activation', out=ydt[yi][:, 0:512], in_=psB[:, 4, :], func=AF.Copy),
                               r=['psB4'], w=[f'ydt{yi}a'])
                            op('dve', I('tensor_copy', out=ydt[yi][:, 512:1024], in_=psB[:, 5, :]),
                               r=['psB5'], w=[f'ydt{yi}b'])
                            op('sp', I('dma_start',
                                out=yd_d[r0 + ci * 128:r0 + (ci + 1) * 128, :], in_=ydt[yi][:]),
                                r=[f'ydt{yi}a', f'ydt{yi}b'], w=[f'yd{yc}'], dma=f'yo{yi}')
                    if e_ + 1 < NEXP:
                        cast_d(e_ + 1)
                S.barrier()

            with ExitStack() as s4:
                gbf = T(s4, "gbf", [128, DM], F32)
                y1 = [T(s4, f"y1_{i}", [128, DM], F32) for i in range(2)]
                y2 = [T(s4, f"y2_{i}", [128, DM], F32) for i in range(2)]
                xf = [T(s4, f"xf{i}", [128, DM], F32) for i in range(2)]
                of = [T(s4, f"of{i}", [128, DM], F32) for i in range(2)]
                fst = T(s4, "fst", [128, 3, nseq * NT], F32)
                zt = T(s4, "zt", [128, DM], F32)
                op('pool', I('memset', zt[:], 0.0), w=['zt'])
                op('sp', I('dma_start', out=gbf[:], in_=ln_final_g.partition_broadcast(128)), w=['gbf'], dma='gbf')
                def fetch(tg):
                    i = tg % 2
                    for ybuf, k2, nm in ((y1, 0, 'y1'), (y2, 1, 'y2')):
                        op('act', I('activation', out=ybuf[i][:], in_=zt[:], func=AF.Copy), r=['zt'], w=[f'{nm}_{i}'])
                        op('pool', I('indirect_dma_start', out=ybuf[i][:], out_offset=None, in_=yd_d[:, :],
                                     in_offset=bass.IndirectOffsetOnAxis(ap=slots[:, tg, k2:k2 + 1], axis=0),
                                     bounds_check='REG', oob_is_err=False),
                           r=['yd'], w=[f'{nm}_{i}'], dma=f'{nm}_{i}')
                    op('sp', I('dma_start', out=xf[i][:], in_=xres_d[tg * 128:(tg + 1) * 128, :]),
                       r=['xres'], w=[f'xf{i}'], dma=f'xf{i}')

                def compute(tg):
                    i = tg % 2
                    op('dve', I('scalar_tensor_tensor', out=xf[i][:], in0=y1[i][:], scalar=wts[:, tg, 0:1], in1=xf[i][:],
                                op0=ALU.mult, op1=ALU.add), r=[f'y1_{i}', f'xf{i}'], w=[f'xf{i}'])
                    op('dve', I('scalar_tensor_tensor', out=xf[i][:], in0=y2[i][:], scalar=wts[:, tg, 1:2], in1=xf[i][:],
                                op0=ALU.mult, op1=ALU.add), r=[f'y2_{i}', f'xf{i}'], w=[f'xf{i}'])
                    op('act', I('activation', out=junk[:], in_=xf[i][:], func=AF.Square, accum_out=fst[:, 0, tg:tg + 1]),
                       r=[f'xf{i}'], w=['junk', 'stat'])
                    rstd_from_ss(fst, tg, 1.0 / DM)
                    op('dve', I('scalar_tensor_tensor', out=of[i][:], in0=xf[i][:], scalar=fst[:, 2, tg:tg + 1],
                                in1=gbf[:], op0=ALU.mult, op1=ALU.mult), r=[f'xf{i}', 'stat', 'gbf'], w=[f'of{i}'])
                    op('sp', I('dma_start', out=out_d[tg * 128:(tg + 1) * 128, :], in_=of[i][:]),
                       r=[f'of{i}'], dma=f'of{i}')

                ntile = nseq * NT
                fetch(0)
                for tg in range(ntile):
                    if tg + 1 < ntile:
                        fetch(tg + 1)
                    compute(tg)
                S.barrier()
        S.barrier()
        S.emit()
    return nc


_NC_CACHE = {}


def _prep(inputs, c, nseq=NSEQ):
    f = lambda a: np.ascontiguousarray(np.asarray(a, dtype=np.float32))
    d = {
        "x": f(inputs["x"][c * nseq:(c + 1) * nseq]),
        "mem": f(inputs["mem"][c * nseq:(c + 1) * nseq]),
        "ln_mix_g": f(inputs["ln_mix_g"][0]),
        "w_in": f(inputs["w_in"][0]),
        "sb_out_g": f(inputs["sb_out_g"][0]),
        "conv_w": f(inputs["conv_w"][0]),
        "conv_b": f(inputs["conv_b"][0]),
        "conv_ln_g": f(inputs["conv_ln_g"][0]),
        "conv_ln_b": f(inputs["conv_ln_b"][0]),
        "w_out": f(inputs["w_out"][0]),
        "ln_mem_x_g": f(inputs["ln_mem_x_g"][0]),
        "ln_mem_g": f(inputs["ln_mem_g"][0]),
        "w_xq": f(inputs["w_xq"][0]),
        "w_xkv": f(inputs["w_xkv"][0]),
        "w_xo": f(inputs["w_xo"][0]),
        "ln_ffn_g": f(inputs["ln_ffn_g"][0]),
        "w_rt": f(np.concatenate([np.asarray(inputs["w_group"][0]), np.asarray(inputs["w_er"][0]).reshape(DM, 32)], axis=1)),
        "b_rt": f(np.concatenate([np.asarray(inputs["b_group"][0]), np.asarray(inputs["b_er"][0]).reshape(32)])),
        "w_gate": f(inputs["w_gate"][0]),
        "w_up": f(inputs["w_up"][0]),
        "w_down": f(inputs["w_down"][0]),
        "ln_final_g": f(inputs["ln_final_g"]),
    }
    return d


def kernel(**inputs):
    if 'nc' not in _NC_CACHE:
        _NC_CACHE['nc'] = build()
    nc = _NC_CACHE['nc']
    in_maps = [_prep(inputs, c) for c in range(N_CORES)]
    res = run_bass_kernel_spmd(nc, in_maps, core_ids=list(range(N_CORES)))
    out = np.concatenate([np.asarray(r["out"]).reshape(NSEQ, SEQ, DM) for r in res.results], axis=0)
    return out.astype(np.float32)
```

```python
import numpy as np
import concourse.bass as bass
import concourse.mybir as mybir
from concourse.bass_utils import run_bass_kernel_spmd
from contextlib import ExitStack

F32 = mybir.dt.float32
F32R = mybir.dt.float32r
BF16 = mybir.dt.bfloat16
I32 = mybir.dt.int32
AF = mybir.ActivationFunctionType
ALU = mybir.AluOpType
AX = mybir.AxisListType

ENG = ['pe', 'act', 'dve', 'pool', 'sp']
SAME_ENGINE_SYNC = True

N_CORES = 8
NSEQ = 4
SEQ = 2048
DM = 1024
NT = SEQ // 128
NMEM = 256
NEXP = 32
CAP = 768
NROWS = NEXP * CAP
EPS = 1e-6


def I(name, *args, **kw):
    return (name, args, kw)


class Sched:
    def __init__(self, nc, es):
        self.nc = nc
        self.es = es
        self.ins = {e: [] for e in ENG}
        self.cnt = {e: 0 for e in ENG}
        self.sem = {e: es.enter_context(nc.semaphore('s_' + e)) for e in ENG}
        self.dsem = {}
        self.lastw = {}
        self.readers = {}
        self.waited = {e: {} for e in ENG}

    def _semof(self, key):
        return self.sem[key[1]] if key[0] == 'e' else self.dsem[key[1]][0]

    def op(self, eng, fn, r=(), w=(), dma=None, extra=()):
        deps = {}

        def need(ev):
            if ev is None:
                return
            key, val, src, is_dma = ev
            if (not is_dma) and src == eng and (eng == 'pe' or not SAME_ENGINE_SYNC):
                return
            if deps.get(key, 0) < val:
                deps[key] = val

        for k in r:
            need(self.lastw.get(k))
        for k in w:
            need(self.lastw.get(k))
            for ev in self.readers.get(k, {}).values():
                need(ev)
        for ev in extra:
            need(ev)
        waits = []
        wd = self.waited[eng]
        for key, val in deps.items():
            if wd.get(key, 0) >= val:
                continue
            wd[key] = val
            waits.append((key, val))
        if dma is not None:
            if dma not in self.dsem:
                self.dsem[dma] = [self.es.enter_context(self.nc.semaphore('d_' + dma)), 0]
            self.dsem[dma][1] += 16
            ev = (('d', dma), self.dsem[dma][1], eng, True)
        else:
            self.cnt[eng] += 1
            ev = (('e', eng), self.cnt[eng], eng, False)
        self.ins[eng].append((waits, fn, ev))
        for k in r:
            d = self.readers.setdefault(k, {})
            old = d.get(ev[0])
            if old is None or old[1] < ev[1]:
                d[ev[0]] = ev
        for k in w:
            self.lastw[k] = ev
            self.readers[k] = {}
        return ev

    def barrier(self):
        evs = []
        for e in ENG:
            if self.cnt[e] > 0:
                evs.append((('e', e), self.cnt[e], e, False))
        for slot, (s, c) in self.dsem.items():
            if c > 0:
                evs.append((('d', slot), c, 'sp', True))
        for e in ENG:
            self.op(e, I('nop'), extra=[ev for ev in evs if not (ev[2] == e and not ev[3])])
        self.lastw = {}
        self.readers = {}

    def emit(self):
        nc = self.nc
        with nc.Block() as block:
            def body(name):
                def f(e):
                    bc_reg = None
                    if name == 'pool':
                        bc_reg = e.alloc_register()
                        e.reg_mov(bc_reg, NROWS - 1)
                    for waits, fn, ev in self.ins[name]:
                        for key, val in waits:
                            e.wait_ge(self._semof(key), val)
                        try:
                            kw = fn[2]
                            if kw.get('bounds_check', None) == 'REG':
                                kw = dict(kw, bounds_check=bc_reg)
                            ins = getattr(e, fn[0])(*fn[1], **kw)
                        except Exception:
                            print("EMIT FAIL", name, fn[0], fn[1], fn[2])
                            raise
                        key, val, _, is_dma = ev
                        ins.then_inc(self._semof(key), 16 if is_dma else 1)
                return f
            block.tensor(body('pe'))
            block.scalar(body('act'))
            block.vector(body('dve'))
            block.gpsimd(body('pool'))
            block.sync(body('sp'))


def build(nseq=NSEQ, stage=99):
    nc = bass.Bass('TRN2', target_bir_lowering=False)
    ntok = nseq * SEQ

    def din(name, shape, dt=F32):
        return nc.dram_tensor(name, list(shape), dt, kind="ExternalInput").ap()

    x_d = din("x", [nseq, SEQ, DM])
    mem_d = din("mem", [nseq, NMEM, DM])
    ln_mix_g = din("ln_mix_g", [DM])
    w_in_d = din("w_in", [DM, 2560])
    sb_out_g = din("sb_out_g", [512])
    conv_w_d = din("conv_w", [31, 512])
    conv_b_d = din("conv_b", [512])
    conv_ln_g = din("conv_ln_g", [512])
    conv_ln_b = din("conv_ln_b", [512])
    w_out_d = din("w_out", [DM, DM])
    ln_mem_x_g = din("ln_mem_x_g", [DM])
    ln_mem_g = din("ln_mem_g", [DM])
    w_xq_d = din("w_xq", [DM, DM])
    w_xkv_d = din("w_xkv", [DM, 2 * DM])
    w_xo_d = din("w_xo", [DM, DM])
    ln_ffn_g = din("ln_ffn_g", [DM])
    w_rt_d = din("w_rt", [DM, 36])
    b_rt_d = din("b_rt", [36])
    w_gate_d = din("w_gate", [NEXP, DM, 512])
    w_up_d = din("w_up", [NEXP, DM, 512])
    w_down_d = din("w_down", [NEXP, 512, DM])
    ln_final_g = din("ln_final_g", [DM])
    out_d = nc.dram_tensor("out", [ntok, DM], F32, kind="ExternalOutput").ap()
    if stage == 1:
        dbg_mix = nc.dram_tensor("dbg_mix", [128, 8, SEQ], BF16, kind="ExternalOutput").ap()
        dbg_qk = nc.dram_tensor("dbg_qk", [128, 8, SEQ], BF16, kind="ExternalOutput").ap()
        dbg_v = nc.dram_tensor("dbg_v", [128, NT, 512], BF16, kind="ExternalOutput").ap()
        dbg_rsb = nc.dram_tensor("dbg_rsb", [128, 3, NT], F32, kind="ExternalOutput").ap()
    xd_d = nc.dram_tensor("xd_scr", [NROWS, DM], BF16).ap()
    yd_d = nc.dram_tensor("yd_scr", [NROWS, DM], F32).ap()
    xres_d = nc.dram_tensor("xres_scr", [ntok, DM], F32).ap()

    with ExitStack() as es:
        S = Sched(nc, es)
        op = S.op

        uid = [0]

        def T(scope, name, shape, dt):
            uid[0] += 1
            return scope.enter_context(nc.sbuf_tensor(f"{name}_u{uid[0]}", shape, dt))

        psA = es.enter_context(nc.psum_tensor("psA", [128, 2, 1024], BF16))
        psB = es.enter_context(nc.psum_tensor("psB", [128, 6, 512], F32))

        identf = T(es, "identf", [128, 128], F32)
        ident = T(es, "ident", [128, 128], BF16)
        negtri = T(es, "negtri", [128, 128], F32)
        negones = T(es, "negones", [128, 128], F32)
        negtriR = T(es, "negtriR", [128, 128], F32)
        negonesR = T(es, "negonesR", [128, 128], F32)
        meanmat = T(es, "meanmat", [128, 128], F32)
        ones2 = T(es, "ones2", [128, 2], F32)
        onesb = T(es, "onesb", [128, 128], BF16)
        Lmat = T(es, "Lmat", [128, 128], BF16)
        maskf = T(es, "maskf", [128, 4, 512], BF16)
        ecap = T(es, "ecap", [128, 32], F32)
        ecap_i = T(es, "ecap_i", [128, 32], I32)
        gcols = T(es, "gcols", [128, 3, 8], F32)
        sbg = T(es, "sbg", [128, 4], F32)
        cvb = T(es, "cvb", [128, 4], F32)
        cvg = T(es, "cvg", [128, 4], F32)
        cvbb = T(es, "cvbb", [128, 4], F32)
        cwT = T(es, "cwT", [128, 4, 31], F32)
        hT = T(es, "hT", [128, 8, SEQ], BF16)
        wr = [T(es, f"wr{i}", [128, 8, 512], BF16) for i in range(2)]
        stat = T(es, "stat", [128, 3, NT], F32)
        rsb = T(es, "rsb", [128, 3, NT], F32)
        slots = T(es, "slots", [128, nseq * NT, 2], I32)
        wts = T(es, "wts", [128, nseq * NT, 2], F32)
        base = T(es, "base", [128, 32], F32)
        wrt = T(es, "wrt", [128, 8, 36], F32)
        brt = T(es, "brt", [128, 36], F32)
        junk = T(es, "junk", [128, 1024], BF16)

        def HK(c, n):
            return f"H{c}_{n}"

        def small_col(dst_ap, src_ap, k, key):
            op('sp', I('dma_start', out=dst_ap, in_=src_ap.rearrange("(k p) -> p k", p=128),
                                           allow_slow_non_contiguous=True), w=[key], dma='c_' + key)

        small_col(gcols[:, 0, :], ln_mix_g, 8, 'gc0')
        small_col(gcols[:, 1, :], ln_mem_x_g, 8, 'gc1')
        small_col(gcols[:, 2, :], ln_mem_g, 8, 'gc2')
        small_col(sbg[:], sb_out_g, 4, 'sbg')
        small_col(cvb[:], conv_b_d, 4, 'cvb')
        small_col(cvg[:], conv_ln_g, 4, 'cvg')
        small_col(cvbb[:], conv_ln_b, 4, 'cvbb')
        op('sp', I('dma_start', out=wrt[:], in_=w_rt_d.rearrange("(k p) n -> p k n", p=128)),
           w=['wrt'], dma='c_wrt')
        op('sp', I('dma_start', out=brt[:], in_=b_rt_d.partition_broadcast(128)), w=['brt'], dma='c_brt')

        op('pool', I('memset', identf[:], 0.0), w=['identf'])
        op('pool', I('affine_select', out=identf[:], in_=identf[:], pattern=[[-1, 128]],
                                             compare_op=ALU.not_equal, fill=1.0, base=0, channel_multiplier=1),
           r=['identf'], w=['identf'])
        op('dve', I('tensor_copy', out=ident[:], in_=identf[:]), r=['identf'], w=['ident'])
        op('pool', I('memset', negtri[:], -1.0), w=['negtri'])
        op('pool', I('affine_select', out=negtri[:], in_=negtri[:], pattern=[[-1, 128]],
                                             compare_op=ALU.is_ge, fill=0.0, base=0, channel_multiplier=1),
           r=['negtri'], w=['negtri'])
        op('pool', I('memset', negones[:], -1.0), w=['negones'])
        op('dve', I('tensor_copy', out=negtriR[:].bitcast(F32R), in_=negtri[:]), r=['negtri'], w=['negtriR'])
        op('dve', I('tensor_copy', out=negonesR[:].bitcast(F32R), in_=negones[:]), r=['negones'], w=['negonesR'])
        op('pool', I('memset', meanmat[:], 1.0 / 512), w=['meanmat'])
        op('pool', I('memset', ones2[:], 1.0), w=['ones2'])
        op('pool', I('memset', onesb[:], 1.0), w=['onesb'])
        op('pool', I('memset', Lmat[:], 1.0), w=['Lmat'])
        op('pool', I('affine_select', out=Lmat[:], in_=Lmat[:], pattern=[[1, 128]],
                                             compare_op=ALU.is_gt, fill=0.0, base=0, channel_multiplier=-1),
           r=['Lmat'], w=['Lmat'])
        op('pool', I('memset', maskf[:], 1.0), w=['maskf'])
        for r_ in range(4):
            op('pool', I('affine_select', out=maskf[:, r_, :], in_=maskf[:, r_, :], pattern=[[1, 512]],
                                                        compare_op=ALU.is_gt, fill=0.0, base=-r_ * 128,
                                                        channel_multiplier=-1),
               r=['maskf'], w=['maskf'])
        op('pool', I('iota', ecap_i[:], pattern=[[CAP, 32]], base=0, channel_multiplier=0), w=['ecap_i'])
        op('dve', I('tensor_copy', out=ecap[:], in_=ecap_i[:]), r=['ecap_i'], w=['ecap'])
        op('pool', I('memset', base[:], 0.0), w=['base'])

        with ExitStack() as s0:
            cw_in = T(s0, "cw_in", [31, 512], F32)
            op('sp', I('dma_start', out=cw_in[:], in_=conv_w_d), w=['cw_in'], dma='c_cw')
            for c in range(4):
                op('pe', I('transpose', out=psB[:, 0, c * 32:c * 32 + 31], in_=cw_in[:, c * 128:(c + 1) * 128],
                                                    identity=identf[0:31, 0:31]),
                   r=['cw_in', 'identf'], w=['psB0'])
            op('act', I('activation', out=cwT[:], in_=psB[:, 0, 0:128].rearrange("p (c w) -> p c w", c=4)[:, :, 0:31],
                                             func=AF.Copy), r=['psB0'], w=['cwT'])
            S.barrier()

        wslot = [0]

        def load_w(src2d, nk=8):
            i = wslot[0]
            wslot[0] ^= 1
            op('pool', I('dma_start', out=wr[i][:, 0:nk, :], in_=src2d.rearrange("(k p) n -> p k n", p=128)),
               w=[f'wr{i}'], dma=f'wr{i}')
            return i

        evac_flip = [0]

        def evac_copy(out_ap, in_ap, r, w, scale=None, eng=None):
            if eng is None:
                eng = 'act' if (evac_flip[0] & 1) == 0 else 'dve'
                evac_flip[0] += 1
            if eng == 'act':
                if scale is None:
                    op('act', I('activation', out=out_ap, in_=in_ap, func=AF.Copy), r=r, w=w)
                else:
                    op('act', I('activation', out=out_ap, in_=in_ap, func=AF.Copy, scale=scale), r=r, w=w)
            else:
                if scale is None:
                    op('dve', I('tensor_copy', out=out_ap, in_=in_ap), r=r, w=w)
                else:
                    op('dve', I('tensor_scalar', out=out_ap, in0=in_ap, scalar1=scale, scalar2=None,
                                                        op0=ALU.mult), r=r, w=w)

        def rstd_from_ss(st, col, inv_n):
            op('act', I('activation', out=st[:, 1, col:col + 1], in_=st[:, 0, col:col + 1], func=AF.Sqrt,
                                             scale=inv_n, bias=EPS), r=['stat'], w=['stat'])
            op('dve', I('reciprocal', out=st[:, 2, col:col + 1], in_=st[:, 1, col:col + 1]),
               r=['stat'], w=['stat'])

        def stats_all(xs, key):
            for t in range(NT):
                op('act', I('activation', out=junk[:], in_=xs[:, t, :], func=AF.Square, accum_out=stat[:, 0, t:t + 1]),
                   r=[f'x{t}'], w=[f'{key}_ss{t}'])
            op('act', I('activation', out=stat[:, 1, :], in_=stat[:, 0, :], func=AF.Sqrt, scale=1.0 / DM, bias=EPS),
               r=[f'{key}_ss{t}' for t in range(NT)], w=[key + '_sd'])
            op('dve', I('reciprocal', out=stat[:, 2, :], in_=stat[:, 1, :]), r=[key + '_sd'], w=[key])

        def norm_T(src_ap, src_key, hn_t, hn_key, gi, dst3, dst_keys, t, pa, pre=None):
            if pre is None:
                op('act', I('activation', out=junk[:], in_=src_ap, func=AF.Square, accum_out=stat[:, 0, t:t + 1]),
                   r=[src_key], w=['stat'])
                rstd_from_ss(stat, t, 1.0 / DM)
                pre = 'stat'
            op('act', I('activation', out=hn_t[:], in_=src_ap, func=AF.Copy, scale=stat[:, 2, t:t + 1]),
               r=[src_key, pre], w=[hn_key])
            for kc in range(8):
                op('pe', I('transpose', out=psA[:, pa, kc * 128:(kc + 1) * 128],
                                                      in_=hn_t[:, kc * 128:(kc + 1) * 128], identity=ident[:]),
                   r=[hn_key, 'ident'], w=[f'psA{pa}'])
            op('dve', I('tensor_tensor', out=dst3, in0=psA[:, pa, :].rearrange("p (k c) -> p k c", k=8),
                                                in1=gcols[:, gi, :, None].to_broadcast([128, 8, 128]), op=ALU.mult),
               r=[f'psA{pa}', f'gc{gi}'], w=dst_keys)

        def fm_proj(slot, ncc, rhs_fn, rhs_keys_fn, nN, N, evac_fn, banks):
            k = 0
            for cc in range(ncc):
                for n in range(nN):
                    bk = banks[k % len(banks)]
                    k += 1
                    for kc in range(8):
                        op('pe', I('matmul',
                            psB[:, bk, 0:N], wr[slot][:, kc, cc * 128:(cc + 1) * 128], rhs_fn(kc, n),
                            start=(kc == 0), stop=(kc == 7)),
                           r=[f'wr{slot}'] + rhs_keys_fn(kc, n), w=[f'psB{bk}'])
                    evac_fn(cc, n, bk)

        dbg = {}

        for b in range(nseq):
            with ExitStack() as s1:
                qk = T(s1, "qk", [128, 8, SEQ], BF16)
                v_sb = T(s1, "v_sb", [128, NT, 512], BF16)
                with ExitStack() as s1a:
                    xin = [T(s1a, f"xin{i}", [128, DM], F32) for i in range(3)]
                    hn = [T(s1a, f"hn{i}", [128, DM], BF16) for i in range(2)]
                    gT = T(s1a, "gT", [128, 4, 30 + SEQ], BF16)
                    ycv = T(s1a, "ycv", [128, 4, SEQ], F32)
                    dg = T(s1a, "dg", [128, 31, 128], BF16)
                    ysq = T(s1a, "ysq", [128, 4, 512], F32)
                    lnw = T(s1a, "lnw", [128, 4, 512], F32)

                    for t in range(NT):
                        xi = xin[t % 3]
                        op('sp', I('dma_start', out=xi[:], in_=x_d[b, t * 128:(t + 1) * 128, :]),
                           w=[f'xin{t % 3}'], dma=f'xin{t % 3}')
                        norm_T(xi[:], f'xin{t % 3}', hn[t % 2], f'hn{t % 2}', 0,
                               hT[:, :, t * 128:(t + 1) * 128], [HK(c, t // 4) for c in range(8)], t, t % 2)

                    hrhs = lambda kc, n: hT[:, kc, n * 512:(n + 1) * 512]
                    hkeys = lambda kc, n: [HK(kc, n)]
                    sl = load_w(w_in_d[:, 0:512])
                    nxt = load_w(w_in_d[:, 512:1024])
                    fm_proj(sl, 4, hrhs, hkeys, 4, 512,
                            lambda cc, n, bk: evac_copy(qk[:, cc, n * 512:(n + 1) * 512], psB[:, bk, :], [f'psB{bk}'],
                                                        [f'q{cc}_{n}'], scale=0.125), [0, 1, 2, 3])
                    sl = nxt
                    nxt = load_w(w_in_d[:, 1024:1536])
                    fm_proj(sl, 4, hrhs, hkeys, 4, 512,
                            lambda cc, n, bk: evac_copy(qk[:, 4 + cc, n * 512:(n + 1) * 512], psB[:, bk, :],
                                                        [f'psB{bk}'], [f'k{cc}_{n}']), [0, 1, 2, 3])
                    sl = nxt
                    nxt = load_w(w_in_d[:, 2048:2560])
                    for t in range(NT):
                        bk = t % 4
                        for kc in range(8):
                            op('pe', I('matmul',
                                psB[:, bk, :], hT[:, kc, t * 128:(t + 1) * 128], wr[sl][:, kc, :],
                                start=(kc == 0), stop=(kc == 7)),
                               r=[f'wr{sl}', HK(kc, t // 4)], w=[f'psB{bk}'])
                        evac_copy(v_sb[:, t, :], psB[:, bk, :], [f'psB{bk}'], [f'v{t}'])
                    op('pool', I('memset', gT[:, :, 0:30], 0.0), w=['gTpad'])
                    sl = nxt
                    nxt = load_w(w_in_d[:, 1536:2048])
                    fm_proj(sl, 4, hrhs, hkeys, 4, 512,
                            lambda cc, n, bk: op('act', I('activation',
                                out=gT[:, cc, 30 + n * 512:30 + (n + 1) * 512], in_=psB[:, bk, :], func=AF.Sigmoid),
                                r=[f'psB{bk}'], w=[f'g{cc}_{n}']), [0, 1, 2, 3])
                    sl = nxt
                    fm_proj(sl, 4, hrhs, hkeys, 4, 512,
                            lambda cc, n, bk: op('dve', I('tensor_tensor',
                                out=gT[:, cc, 30 + n * 512:30 + (n + 1) * 512], in0=psB[:, bk, :],
                                in1=gT[:, cc, 30 + n * 512:30 + (n + 1) * 512], op=ALU.mult),
                                r=[f'psB{bk}', f'g{cc}_{n}'], w=[f'g{cc}_{n}']), [0, 1, 2, 3])

                    for c in range(4):
                        for w_ in range(31):
                            op('dve', I('tensor_scalar',
                                out=dg[:, w_, :], in0=ident[:], scalar1=cwT[:, c, w_:w_ + 1], scalar2=None,
                                op0=ALU.mult), r=['ident', 'cwT'], w=[f'dg{w_}'])
                        for n in range(4):
                            bk = n % 4
                            gkeys = [f'g{c}_{n}'] + ([f'g{c}_{n - 1}'] if n > 0 else ['gTpad'])
                            for w_ in range(31):
                                op('pe', I('matmul',
                                    psB[:, bk, :], dg[:, w_, :], gT[:, c, n * 512 + w_:n * 512 + w_ + 512],
                                    start=(w_ == 0), stop=(w_ == 30)),
                                   r=[f'dg{w_}'] + gkeys, w=[f'psB{bk}'])
                            op('act', I('activation',
                                out=ycv[:, c, n * 512:(n + 1) * 512], in_=psB[:, bk, :], func=AF.Identity,
                                bias=cvb[:, c:c + 1]), r=[f'psB{bk}', 'cvb'], w=[f'y{c}_{n}'])
                    for n in range(4):
                        ns = slice(n * 512, (n + 1) * 512)
                        for c in range(4):
                            op('pe', I('matmul', psB[:, 4, :], meanmat[:], ycv[:, c, ns],
                                                                    start=(c == 0), stop=(c == 3)),
                               r=['meanmat', f'y{c}_{n}'], w=['psB4'])
                        for c in range(4):
                            op('act', I('activation', out=ysq[:, c, :], in_=ycv[:, c, ns],
                                                                         func=AF.Square),
                               r=[f'y{c}_{n}'], w=[f'ysq{c}'])
                        for c in range(4):
                            op('pe', I('matmul', psB[:, 5, :], meanmat[:], ysq[:, c, :],
                                                             start=(c == 0), stop=(c == 3)),
                               r=['meanmat', f'ysq{c}'], w=['psB5'])
                        op('act', I('activation', out=lnw[:, 0, :], in_=psB[:, 4, :], func=AF.Copy),
                           r=['psB4'], w=['lnw0'])
                        op('pool', I('tensor_tensor', out=lnw[:, 1, :], in0=lnw[:, 0, :], in1=lnw[:, 0, :],
                                                             op=ALU.mult), r=['lnw0'], w=['lnw1'])
                        op('dve', I('tensor_tensor', out=lnw[:, 1, :], in0=psB[:, 5, :], in1=lnw[:, 1, :],
                                                            op=ALU.subtract), r=['psB5', 'lnw1'], w=['lnw1'])
                        op('dve', I('tensor_scalar', out=lnw[:, 1, :], in0=lnw[:, 1, :], scalar1=0.0,
                                                            scalar2=None, op0=ALU.max), r=['lnw1'], w=['lnw1'])
                        op('act', I('activation', out=lnw[:, 1, :], in_=lnw[:, 1, :], func=AF.Sqrt, bias=EPS),
                           r=['lnw1'], w=['lnw1'])
                        op('dve', I('reciprocal', out=lnw[:, 2, :], in_=lnw[:, 1, :]), r=['lnw1'], w=['lnw2'])
                        for c in range(4):
                            op('pool', I('tensor_tensor', out=lnw[:, 3, :], in0=ycv[:, c, ns],
                                                                             in1=lnw[:, 0, :], op=ALU.subtract),
                               r=[f'y{c}_{n}', 'lnw0'], w=['lnw3'])
                            op('pool', I('tensor_tensor', out=lnw[:, 3, :], in0=lnw[:, 3, :], in1=lnw[:, 2, :],
                                                                 op=ALU.mult), r=['lnw3', 'lnw2'], w=['lnw3'])
                            op('act', I('activation',
                                out=hT[:, 4 + c, ns], in_=lnw[:, 3, :], func=AF.Silu, scale=cvg[:, c:c + 1],
                                bias=cvbb[:, c:c + 1]), r=['lnw3', 'cvg', 'cvbb'], w=[HK(4 + c, n)])
                    S.barrier()

                with ExitStack() as s1b:
                    spb = [T(s1b, f"spb{i}", [128, 512], F32) for i in range(4)]
                    ab = [T(s1b, f"ab{i}", [128, 512], BF16) for i in range(4)]
                    Rb = [T(s1b, f"Rb{i}", [128, 512], F32) for i in range(2)]
                    osq = [T(s1b, f"osq{i}", [128, 512], F32) for i in range(2)]
                    units = []
                    for j in range(4):
                        for qn in range(4):
                            kcs = list(range(4 * qn + 3, -1, -1))
                            for idx, kc in enumerate(kcs):
                                for hp in range(2):
                                    units.append((j, qn, idx, kc, hp, len(kcs)))
                    nU = len(units)
                    negtri_r = negtriR[:].bitcast(F32R)
                    negones_r = negonesR[:].bitcast(F32R)
                    oqc = [0]

                    def geo(i):
                        j, qn, idx, kc, hp, nk = units[i]
                        return (j, qn, idx, kc, hp, nk, slice(hp * 64, hp * 64 + 64), slice(qn * 512, (qn + 1) * 512),
                                slice(kc * 128, (kc + 1) * 128))

                    def S1(i0_):
                        us = [i0_, i0_ + 1]
                        for i in us:
                            j, qn, idx, kc, hp, nk, P, qs, ks = geo(i)
                            zb = i % 2
                            op('pe', I('matmul', psB[:, zb, :], qk[P, 4 + j, ks], qk[P, j, qs], start=True, stop=True),
                               r=[f'k{j}_{kc // 4}', f'q{j}_{qn}'], w=[f'psB{zb}'])
                        for i in us:
                            j, qn, idx, kc, hp, nk, P, qs, ks = geo(i)
                            zb = i % 2
                            sp_t, spk = spb[i % 4], f'spb{i % 4}'
                            op('act', I('activation', out=sp_t[:].bitcast(F32R), in_=psB[:, zb, :], func=AF.Exp),
                               r=[f'psB{zb}'], w=[spk])
                            op('act', I('activation', out=sp_t[:].bitcast(F32R), in_=sp_t[:], func=AF.Ln, bias=1.0),
                               r=[spk], w=[spk])
                            if kc >= 4 * qn:
                                op('pool', I('tensor_tensor', out=sp_t[:].bitcast(F32R), in0=sp_t[:],
                                             in1=maskf[:, kc - 4 * qn, :], op=ALU.mult), r=[spk, 'maskf'], w=[spk])

                    def S2(i0_):
                        us = [i0_, i0_ + 1]
                        for i in us:
                            j, qn, idx, kc, hp, nk, P, qs, ks = geo(i)
                            eb = 2 + i % 2
                            op('pe', I('matmul', psB[:, eb, :], qk[P, 4 + j, ks], qk[P, j, qs], start=True, stop=False),
                               r=[f'k{j}_{kc // 4}', f'q{j}_{qn}'], w=[f'psB{eb}'])
                        for i in us:
                            j, qn, idx, kc, hp, nk, P, qs, ks = geo(i)
                            eb = 2 + i % 2
                            ek = f'psB{eb}'
                            sp_t, spk = spb[i % 4], f'spb{i % 4}'
                            op('pe', I('matmul', psB[:, eb, :], negtri_r, sp_t[:].bitcast(F32R), start=False,
                                       stop=(idx == 0)), r=['negtriR', spk], w=[ek])
                            if idx > 0:
                                op('pe', I('matmul', psB[:, eb, :], negones_r, Rb[hp][:].bitcast(F32R), start=False,
                                           stop=True), r=['negonesR', f'Rb{hp}'], w=[ek])
                        for i in us:
                            j, qn, idx, kc, hp, nk, P, qs, ks = geo(i)
                            eb = 2 + i % 2
                            ek = f'psB{eb}'
                            sp_t, spk = spb[i % 4], f'spb{i % 4}'
                            ab_t, abk = ab[i % 4], f'ab{i % 4}'
                            op('act', I('activation', out=ab_t[:], in_=psB[:, eb, :], func=AF.Exp), r=[ek], w=[abk])
                            if kc >= 4 * qn:
                                op('dve', I('tensor_tensor', out=ab_t[:], in0=ab_t[:], in1=maskf[:, kc - 4 * qn, :],
                                            op=ALU.mult), r=[abk, 'maskf'], w=[abk])
                            if idx < nk - 1:
                                if idx == 0:
                                    op('pool', I('tensor_copy', out=Rb[hp][:].bitcast(F32R), in_=sp_t[:]), r=[spk],
                                       w=[f'Rb{hp}'])
                                else:
                                    op('pool', I('tensor_tensor', out=Rb[hp][:].bitcast(F32R), in0=Rb[hp][:],
                                                 in1=sp_t[:], op=ALU.add), r=[spk, f'Rb{hp}'], w=[f'Rb{hp}'])

                    def S3(i0_):
                        us = [i0_, i0_ + 1]
                        for i in us:
                            j, qn, idx, kc, hp, nk, P, qs, ks = geo(i)
                            h = 2 * j + hp
                            ab_t, abk = ab[i % 4], f'ab{i % 4}'
                            op('pe', I('matmul', psB[P, 4, :], v_sb[:, kc, h * 64:(h + 1) * 64], ab_t[:],
                                       start=(idx == 0), stop=(idx == nk - 1)), r=[f'v{kc}', abk], w=[f'psB4_{hp}'])
                        j, qn, idx, kc, hp, nk, P, qs, ks = geo(i0_ + 1)
                        if idx == nk - 1:
                            oq_t = osq[oqc[0] & 1]
                            oqk = f'osq{oqc[0] & 1}'
                            oqc[0] += 1
                            op('act', I('activation', out=hT[:, j, qs], in_=psB[:, 4, :], func=AF.Copy,
                                        scale=sbg[:, j:j + 1]), r=['psB4_0', 'psB4_1', 'sbg'], w=[HK(j, qn)])
                            op('act', I('activation', out=oq_t[:], in_=psB[:, 4, :], func=AF.Square),
                               r=['psB4_0', 'psB4_1'], w=[oqk])
                            for tt in range(4):
                                col = (j * 16 + qn * 4 + tt) * 2
                                op('pe', I('matmul', psB[:, 5, col:col + 2], oq_t[:, tt * 128:(tt + 1) * 128], ones2[:],
                                           start=True, stop=True), r=[oqk, 'ones2'], w=['psB5'])

                    nP = nU // 2
                    for p in range(nP + 2):
                        if p < nP:
                            S1(2 * p)
                        if 0 <= p - 1 < nP:
                            S2(2 * (p - 1))
                        if 0 <= p - 2 < nP:
                            S3(2 * (p - 2))
                    ssv = lambda j: psB[:, 5, j * 32:(j + 1) * 32].rearrange("p (t two) -> p t two", two=2)[:, :, 0]
                    op('act', I('activation', out=rsb[:, 0, :], in_=ssv(0), func=AF.Copy), r=['psB5'], w=['rsb'])
                    for j in range(1, 4):
                        op('dve', I('tensor_tensor', out=rsb[:, 0, :], in0=ssv(j), in1=rsb[:, 0, :],
                                                                 op=ALU.add), r=['psB5', 'rsb'], w=['rsb'])
                    op('act', I('activation', out=rsb[:, 1, :], in_=rsb[:, 0, :], func=AF.Sqrt, scale=1.0 / 512,
                                                     bias=EPS), r=['rsb'], w=['rsb'])
                    op('dve', I('reciprocal', out=rsb[:, 2, :], in_=rsb[:, 1, :]), r=['rsb'], w=['rsb'])
                    S.barrier()
                    if stage == 1 and b == 0:
                        op('sp', I('dma_start', out=dbg_mix, in_=hT[:]), dma='dbg0')
                        op('sp', I('dma_start', out=dbg_qk, in_=qk[:]), dma='dbg1')
                        op('sp', I('dma_start', out=dbg_v, in_=v_sb[:]), dma='dbg2')
                        op('sp', I('dma_start', out=dbg_rsb, in_=rsb[:]), dma='dbg3')
                        S.barrier()

            with ExitStack() as s2:
                x_sb = T(s2, "x_sb", [128, NT, DM], F32)
                for t in range(NT):
                    op('sp', I('dma_start', out=x_sb[:, t, :], in_=x_d[b, t * 128:(t + 1) * 128, :]),
                       w=[f'x{t}'], dma=f'x{t}')
                nxt = load_w(w_out_d[:, 0:512])
                for n in range(2):
                    sl = nxt
                    nxt = load_w(w_out_d[:, 512:1024]) if n == 0 else load_w(w_xkv_d[:, 0:512])
                    ns = slice(n * 512, (n + 1) * 512)
                    for t in range(NT):
                        b0, b1 = (t % 2) * 2, (t % 2) * 2 + 1
                        ts_ = slice(t * 128, (t + 1) * 128)
                        for jj in range(4):
                            op('pe', I('matmul',
                                psB[:, b0, :], hT[:, jj, ts_], wr[sl][:, jj, :], start=(jj == 0), stop=(jj == 3)),
                               r=[HK(jj, t // 4), f'wr{sl}'], w=[f'psB{b0}'])
                        for jj in range(4):
                            op('pe', I('matmul',
                                psB[:, b1, :], hT[:, 4 + jj, ts_], wr[sl][:, 4 + jj, :], start=(jj == 0), stop=(jj == 3)),
                               r=[HK(4 + jj, t // 4), f'wr{sl}'], w=[f'psB{b1}'])
                        op('dve', I('tensor_tensor',
                            out=x_sb[:, t, ns], in0=psB[:, b1, :], in1=x_sb[:, t, ns], op=ALU.add),
                           r=[f'psB{b1}', f'x{t}'], w=[f'x{t}'])
                        op('dve', I('scalar_tensor_tensor',
                            out=x_sb[:, t, ns], in0=psB[:, b0, :], scalar=rsb[:, 2, t:t + 1], in1=x_sb[:, t, ns],
                            op0=ALU.mult, op1=ALU.add), r=[f'psB{b0}', 'rsb', f'x{t}'], w=[f'x{t}'])
                if stage == 1:
                    for t in range(NT):
                        ev = op('sp', I('dma_start',
                            out=out_d[b * SEQ + t * 128:b * SEQ + (t + 1) * 128, :], in_=x_sb[:, t, :]),
                            r=[f'x{t}'], dma=f'o{t % 4}')
                    S.barrier()
                    continue

                with ExitStack() as s2f:
                    memin = [T(s2f, f"memin{i}", [128, DM], F32) for i in range(2)]
                    hn2 = [T(s2f, f"hnb{i}", [128, DM], BF16) for i in range(2)]
                    memT = T(s2f, "memT", [128, 8, NMEM], BF16)
                    kxT = T(s2f, "kxT", [128, 8, NMEM], BF16)
                    vx = T(s2f, "vx", [128, 2, DM], BF16)
                    qxT = T(s2f, "qxT", [128, 8, SEQ], BF16)
                    pf = [T(s2f, "pf0", [128, 4, NMEM], F32)] * 2
                    pn = [T(s2f, f"pn{i}", [128, 4, NMEM], BF16) for i in range(2)]
                    pT = [T(s2f, "pT0", [128, 4, 2, 512], BF16)] * 2
                    sm = T(s2f, "sm", [128, 4, 4], F32)
                    for mt in range(2):
                        op('sp', I('dma_start', out=memin[mt][:], in_=mem_d[b, mt * 128:(mt + 1) * 128, :]),
                           w=[f'memin{mt}'], dma=f'memin{mt}')
                        norm_T(memin[mt][:], f'memin{mt}', hn2[mt], f'hnb{mt}', 2,
                               memT[:, :, mt * 128:(mt + 1) * 128], ['memT'], mt, mt)
                    mrhs = lambda kc, n: memT[:, kc, :]
                    mkeys = lambda kc, n: ['memT']
                    for g in range(2):
                        sl = nxt
                        nxt = load_w(w_xkv_d[:, (g + 1) * 512:(g + 2) * 512])
                        fm_proj(sl, 4, mrhs, mkeys, 1, NMEM,
                                lambda cc, n, bk, g=g: evac_copy(kxT[:, g * 4 + cc, :], psB[:, bk, 0:NMEM], [f'psB{bk}'],
                                                                 ['kxT']), [0, 1, 2, 3])
                    for g in range(2):
                        sl = nxt
                        nxt = load_w(w_xkv_d[:, 1536:2048]) if g == 0 else load_w(w_xq_d[:, 0:512])
                        for mt in range(2):
                            bk = mt
                            for kc in range(8):
                                op('pe', I('matmul',
                                    psB[:, bk, :], memT[:, kc, mt * 128:(mt + 1) * 128], wr[sl][:, kc, :],
                                    start=(kc == 0), stop=(kc == 7)), r=['memT', f'wr{sl}'], w=[f'psB{bk}'])
                            evac_copy(vx[:, mt, g * 512:(g + 1) * 512], psB[:, bk, :], [f'psB{bk}'], ['vx'])
                    stats_all(x_sb, 'rstdF')
                    for t in range(NT):
                        norm_T(x_sb[:, t, :], f'x{t}', hn2[t % 2], f'hnb{t % 2}', 1,
                               hT[:, :, t * 128:(t + 1) * 128], [HK(c, t // 4) for c in range(8)], t, t % 2,
                               pre='rstdF')
                    for g in range(2):
                        sl = nxt
                        nxt = load_w(w_xq_d[:, 512:1024]) if g == 0 else load_w(w_xo_d[:, 0:512])
                        fm_proj(sl, 4, hrhs, hkeys, 4, 512,
                                lambda cc, n, bk, g=g: evac_copy(qxT[:, g * 4 + cc, n * 512:(n + 1) * 512], psB[:, bk, :],
                                                                 [f'psB{bk}'], [f'qx{g * 4 + cc}_{n}'], scale=1.0 / 16),
                                [0, 1, 2, 3])
                    psSs = [psB[:, 0:2, :].rearrange("p a (h m) -> p (a h) m", h=2),
                            psB[:, 2:4, :].rearrange("p a (h m) -> p (a h) m", h=2)]
                    sm2 = [T(s2f, f"sm2_{i}", [128, 4, 4], F32) for i in range(2)]
                    pvk = [0]

                    def FA(t):
                        pi = t % 2
                        n = t // 4
                        ts_ = slice(t * 128, (t + 1) * 128)
                        psS = psSs[pi]
                        for hx in range(4):
                            for dc in range(2):
                                op('pe', I('matmul', psS[:, hx, :], qxT[:, 2 * hx + dc, ts_], kxT[:, 2 * hx + dc, :],
                                           start=(dc == 0), stop=(dc == 1)),
                                   r=[f'qx{2 * hx + dc}_{n}', 'kxT'], w=[f'psB{2 * pi + hx // 2}'])
                        op('dve', I('tensor_reduce', out=sm2[pi][:, 0, :], in_=psS, axis=AX.X, op=ALU.max),
                           r=[f'psB{2 * pi}', f'psB{2 * pi + 1}'], w=[f'sm{pi}'])
                        op('dve', I('tensor_scalar', out=sm2[pi][:, 1, :], in0=sm2[pi][:, 0, :], scalar1=-1.0,
                                    scalar2=None, op0=ALU.mult), r=[f'sm{pi}'], w=[f'sm{pi}'])

                    def FB(t):
                        pi = t % 2
                        n = t // 4
                        tt = t % 4
                        psS = psSs[pi]
                        smt = sm2[pi]
                        pT_t = pT[n % 2]
                        for hx in range(4):
                            op('act', I('activation', out=pf[pi][:, hx, :], in_=psS[:, hx, :], func=AF.Exp,
                                        bias=smt[:, 1, hx:hx + 1], accum_out=smt[:, 2, hx:hx + 1]),
                               r=[f'psB{2 * pi + hx // 2}', f'sm{pi}'], w=[f'pf{hx}', f'smacc{pi}_{hx}'])
                        op('dve', I('reciprocal', out=smt[:, 3, :], in_=smt[:, 2, :]),
                           r=[f'smacc{pi}_{hx}' for hx in range(4)], w=[f'smr{pi}'])
                        for hx in range(4):
                            op('dve', I('tensor_scalar', out=pn[pi][:, hx, :], in0=pf[pi][:, hx, :],
                                        scalar1=smt[:, 3, hx:hx + 1], scalar2=None, op0=ALU.mult),
                               r=[f'pf{hx}', f'smr{pi}'], w=[f'pn{pi}_{hx}'])
                        for hx in range(4):
                            for mc in range(2):
                                op('pe', I('transpose', out=psA[:, pi, (hx * 2 + mc) * 128:(hx * 2 + mc + 1) * 128],
                                           in_=pn[pi][:, hx, mc * 128:(mc + 1) * 128], identity=ident[:]),
                                   r=[f'pn{pi}_{hx}', 'ident'], w=[f'psA{pi}'])
                        op('act', I('activation', out=pT_t[:, :, :, tt * 128:(tt + 1) * 128],
                                    in_=psA[:, pi, :].rearrange("p (h m c) -> p h m c", h=4, m=2), func=AF.Copy),
                           r=[f'psA{pi}'], w=['pT0'])
                        if tt == 3:
                            for hx in range(4):
                                for dc in range(2):
                                    bk = 4 + (pvk[0] % 2)
                                    pvk[0] += 1
                                    for mc in range(2):
                                        op('pe', I('matmul', psB[:, bk, :],
                                                   vx[:, mc, hx * 256 + dc * 128:hx * 256 + (dc + 1) * 128],
                                                   pT_t[:, hx, mc, :], start=(mc == 0), stop=(mc == 1)),
                                           r=['vx', 'pT0'], w=[f'psB{bk}'])
                                    evac_copy(hT[:, 2 * hx + dc, n * 512:(n + 1) * 512], psB[:, bk, :], [f'psB{bk}'],
                                              [HK(2 * hx + dc, n)])

                    FA(0)
                    for t in range(NT):
                        if t + 1 < NT:
                            FA(t + 1)
                        FB(t)
                    for n in range(2):
                        sl = nxt
                        nxt = load_w(w_xo_d[:, 512:1024]) if n == 0 else None
                        ns = slice(n * 512, (n + 1) * 512)
                        for t in range(NT):
                            bk = t % 4
                            for cc in range(8):
                                op('pe', I('matmul',
                                    psB[:, bk, :], hT[:, cc, t * 128:(t + 1) * 128], wr[sl][:, cc, :],
                                    start=(cc == 0), stop=(cc == 7)), r=[HK(cc, t // 4), f'wr{sl}'], w=[f'psB{bk}'])
                            op('dve', I('tensor_tensor',
                                out=x_sb[:, t, ns], in0=psB[:, bk, :], in1=x_sb[:, t, ns], op=ALU.add),
                               r=[f'psB{bk}', f'x{t}'], w=[f'x{t}'])
                    S.barrier()
                if stage == 2:
                    for t in range(NT):
                        ev = op('sp', I('dma_start',
                            out=out_d[b * SEQ + t * 128:b * SEQ + (t + 1) * 128, :], in_=x_sb[:, t, :]),
                            r=[f'x{t}'], dma=f'o{t % 4}')
                    S.barrier()
                    continue

                with ExitStack() as s2g:
                    GB = 8
                    gbc = T(s2g, "gbc", [128, DM], F32)
                    h3 = [T(s2g, f"h3_{i}", [128, DM], F32) for i in range(2)]
                    h3b = T(s2g, "h3b", [128, GB, DM], BF16)
                    h3T = [T(s2g, f"h3T{i}", [128, 8, 128], F32) for i in range(2)]
                    LG = T(s2g, "LG", [128, GB, 36], F32)
                    gmax = T(s2g, "gmax", [128, GB], F32)
                    gsh = T(s2g, "gsh", [128, GB, 4], F32)
                    gex = T(s2g, "gex", [128, GB, 4], F32)
                    gsum = T(s2g, "gsum", [128, GB], F32)
                    gw = T(s2g, "gw", [128, GB], F32)
                    gm = T(s2g, "gm", [128, GB, 4], F32)
                    elm = T(s2g, "elm", [128, GB, 4, 8], F32)
                    igr = T(s2g, "igr", [128, GB, 8], F32)
                    ig2 = T(s2g, "ig2", [128, GB, 8], F32)
                    mk1 = T(s2g, "mk1", [128, GB, 8], F32)
                    mk2 = T(s2g, "mk2", [128, GB, 8], F32)
                    m12 = T(s2g, "m12", [128, 4, GB], F32)
                    M1 = T(s2g, "M1", [128, GB, 4, 8], F32)
                    M2 = T(s2g, "M2", [128, GB, 4, 8], F32)
                    S32b = T(s2g, "S32b", [128, GB, 32], BF16)
                    posa = T(s2g, "posa", [128, GB, 32], F32)
                    ova = T(s2g, "ova", [128, GB, 32], F32)
                    slf = T(s2g, "slf", [128, 2, GB], F32)
                    op('sp', I('dma_start', out=gbc[:], in_=ln_ffn_g.partition_broadcast(128)), w=['gbc'],
                       dma='gbc')

                    def G1(t):
                        hi = t % 2
                        tl = t % GB
                        op('dve', I('scalar_tensor_tensor', out=h3[hi][:], in0=x_sb[:, t, :],
                                    scalar=stat[:, 2, t:t + 1], in1=gbc[:], op0=ALU.mult, op1=ALU.mult),
                           r=[f'x{t}', 'rstdG', 'gbc'], w=[f'h3_{hi}'])
                        op('pool', I('tensor_copy', out=h3b[:, tl, :], in_=h3[hi][:]), r=[f'h3_{hi}'], w=[f'h3b{tl}'])
                        b4, b5 = (4, 5) if hi == 0 else (2, 3)
                        for kc in range(8):
                            bb = b4 if kc < 4 else b5
                            op('pe', I('transpose', out=psB[:, bb, (kc % 4) * 128:(kc % 4 + 1) * 128],
                                       in_=h3[hi][:, kc * 128:(kc + 1) * 128], identity=identf[:]),
                               r=[f'h3_{hi}', 'identf'], w=[f'psB{bb}'])
                        op('act', I('activation', out=h3T[hi][:, 0:4, :],
                                    in_=psB[:, b4, :].rearrange("p (k c) -> p k c", k=4), func=AF.Copy),
                           r=[f'psB{b4}'], w=[f'h3T{hi}a'])
                        op('dve', I('tensor_copy', out=h3T[hi][:, 4:8, :],
                                    in_=psB[:, b5, :].rearrange("p (k c) -> p k c", k=4)),
                           r=[f'psB{b5}'], w=[f'h3T{hi}b'])
                        for kc in range(8):
                            op('pe', I('matmul', psB[:, hi, 0:36], h3T[hi][:, kc, :], wrt[:, kc, :],
                                       start=(kc == 0), stop=(kc == 7)),
                               r=[f'h3T{hi}a', f'h3T{hi}b', 'wrt'], w=[f'psB{hi}'])
                        op('dve', I('tensor_tensor', out=LG[:, tl, :], in0=psB[:, hi, 0:36], in1=brt[:], op=ALU.add),
                           r=[f'psB{hi}', 'brt'], w=['LG'])
                        op('sp', I('dma_start', out=xres_d[b * SEQ + t * 128:b * SEQ + (t + 1) * 128, :],
                                   in_=x_sb[:, t, :]), r=[f'x{t}'], w=[f'xres{t}'], dma=f'xw{t % 4}')

                    def G23(g):
                        tg0 = b * NT + g * GB
                        V = lambda f, r=(), w=(): op('dve', f, r=['rt2'] + list(r), w=['rt2'] + list(w))
                        GL = LG[:, :, 0:4]
                        EL = LG[:, :, 4:36].rearrange("p t (g e) -> p t g e", g=4)
                        bc3 = lambda ap2, n: ap2[:, :, None].to_broadcast([128, GB, n])
                        V(I('tensor_reduce', out=gmax[:], in_=GL, axis=AX.X, op=ALU.max), r=['LG'])
                        V(I('tensor_tensor', out=gsh[:], in0=GL, in1=bc3(gmax, 4), op=ALU.subtract), r=['LG'])
                        op('act', I('activation', out=gex[:], in_=gsh[:], func=AF.Exp), r=['rt2'], w=['rt2'])
                        V(I('tensor_reduce', out=gsum[:], in_=gex[:], axis=AX.X, op=ALU.add))
                        V(I('reciprocal', out=gw[:], in_=gsum[:]))
                        V(I('tensor_scalar', out=gm[:], in0=gsh[:], scalar1=0.0, scalar2=None, op0=ALU.is_equal))
                        V(I('tensor_tensor', out=elm[:], in0=EL, in1=gm[:, :, :, None].to_broadcast([128, GB, 4, 8]),
                            op=ALU.mult), r=['LG'])
                        V(I('tensor_reduce', out=igr[:], in_=elm[:].rearrange("p t g e -> p t e g"), axis=AX.X,
                            op=ALU.add))
                        V(I('tensor_reduce', out=m12[:, 0, :], in_=igr[:], axis=AX.X, op=ALU.max))
                        V(I('tensor_tensor', out=mk1[:], in0=igr[:], in1=bc3(m12[:, 0, :], 8), op=ALU.is_equal))
                        V(I('scalar_tensor_tensor', out=ig2[:], in0=mk1[:], scalar=-1e30, in1=igr[:], op0=ALU.mult,
                            op1=ALU.add))
                        V(I('tensor_reduce', out=m12[:, 1, :], in_=ig2[:], axis=AX.X, op=ALU.max))
                        V(I('tensor_tensor', out=mk2[:], in0=ig2[:], in1=bc3(m12[:, 1, :], 8), op=ALU.is_equal))
                        V(I('tensor_tensor', out=m12[:, 2, :], in0=m12[:, 1, :], in1=m12[:, 0, :], op=ALU.subtract))
                        op('act', I('activation', out=m12[:, 2, :], in_=m12[:, 2, :], func=AF.Exp), r=['rt2'], w=['rt2'])
                        V(I('tensor_scalar', out=m12[:, 3, :], in0=m12[:, 2, :], scalar1=1.0, scalar2=None, op0=ALU.add))
                        V(I('reciprocal', out=m12[:, 3, :], in_=m12[:, 3, :]))
                        V(I('tensor_tensor', out=wts[:, tg0:tg0 + GB, 0], in0=m12[:, 3, :], in1=gw[:], op=ALU.mult))
                        V(I('tensor_tensor', out=wts[:, tg0:tg0 + GB, 1], in0=m12[:, 2, :], in1=wts[:, tg0:tg0 + GB, 0],
                            op=ALU.mult))
                        gm4 = gm[:, :, :, None].to_broadcast([128, GB, 4, 8])
                        V(I('tensor_tensor', out=M1[:], in0=gm4, in1=mk1[:, :, None, :].to_broadcast([128, GB, 4, 8]),
                            op=ALU.mult))
                        V(I('tensor_tensor', out=M2[:], in0=gm4, in1=mk2[:, :, None, :].to_broadcast([128, GB, 4, 8]),
                            op=ALU.mult))
                        V(I('tensor_tensor', out=elm[:], in0=M1[:], in1=M2[:], op=ALU.add))
                        V(I('tensor_copy', out=S32b[:], in_=elm[:].rearrange("p t g e -> p t (g e)")))
                        for tl in range(GB):
                            pb = tl % 2
                            op('pe', I('matmul', psB[:, pb, 64:96], Lmat[:], S32b[:, tl, :], start=True, stop=True),
                               r=['Lmat', 'rt2'], w=[f'psB{pb}'])
                            op('pe', I('matmul', psB[:, pb, 96:128], onesb[:], S32b[:, tl, :], start=True, stop=True),
                               r=['onesb', 'rt2'], w=[f'psB{pb}'])
                            V(I('tensor_tensor', out=posa[:, tl, :], in0=psB[:, pb, 64:96], in1=base[:], op=ALU.add),
                              r=[f'psB{pb}', 'base'])
                            V(I('tensor_tensor', out=base[:], in0=psB[:, pb, 96:128], in1=base[:], op=ALU.add),
                              r=[f'psB{pb}', 'base'], w=['base'])
                        V(I('tensor_scalar', out=ova[:], in0=posa[:], scalar1=float(CAP), scalar2=1e7, op0=ALU.is_ge,
                            op1=ALU.mult))
                        V(I('tensor_tensor', out=posa[:], in0=posa[:], in1=ova[:], op=ALU.add))
                        V(I('tensor_tensor', out=posa[:], in0=posa[:], in1=ecap[:, None, :].to_broadcast([128, GB, 32]),
                            op=ALU.add))
                        V(I('tensor_tensor', out=ova[:], in0=posa[:], in1=M1[:].rearrange("p t g e -> p t (g e)"),
                            op=ALU.mult))
                        V(I('tensor_reduce', out=slf[:, 0, :], in_=ova[:], axis=AX.X, op=ALU.add))
                        V(I('tensor_tensor', out=ova[:], in0=posa[:], in1=M2[:].rearrange("p t g e -> p t (g e)"),
                            op=ALU.mult))
                        V(I('tensor_reduce', out=slf[:, 1, :], in_=ova[:], axis=AX.X, op=ALU.add))
                        V(I('tensor_copy', out=slots[:, tg0:tg0 + GB, :], in_=slf[:].rearrange("p k t -> p t k")),
                          w=['slotsg'])
                        for tl in range(GB):
                            for k2 in range(2):
                                op('pool', I('indirect_dma_start', out=xd_d[:, :],
                                             out_offset=bass.IndirectOffsetOnAxis(ap=slots[:, tg0 + tl, k2:k2 + 1], axis=0),
                                             in_=h3b[:, tl, :], in_offset=None, bounds_check='REG', oob_is_err=False),
                                   r=['slotsg', f'h3b{tl}'], w=[f'xd{tg0 + tl}_{k2}'], dma=f'sc{tl}_{k2}')

                    stats_all(x_sb, 'rstdG')
                    for g in range(NT // GB):
                        for tl in range(GB):
                            G1(g * GB + tl)
                        G23(g)
                    S.barrier()

        if stage >= 3:
            with ExitStack() as s3:
                wg = [T(s3, f"wg{i}", [128, 8, 512], BF16) for i in range(2)]
                wu = [T(s3, f"wu{i}", [128, 8, 512], BF16) for i in range(2)]
                wd = [T(s3, f"wd{i}", [128, 4, DM], BF16) for i in range(2)]
                xr = [T(s3, f"xr{i}", [128, DM], BF16) for i in range(3)]
                xT = [T(s3, f"xT{i}", [128, 8, 384], BF16) for i in range(2)]
                sg = [T(s3, f"sg{i}", [128, 384], F32) for i in range(2)]
                h1T = [T(s3, f"h1T{i}", [128, 4, 384], BF16) for i in range(2)]
                ydt = [T(s3, f"ydt{i}", [128, DM], F32) for i in range(2)]

                wst = [T(s3, f"wst{i}", [128, 8, 512], F32) for i in range(3)]

                def load_dma(e_):
                    op('act', I('dma_start', out=wst[0][:], in_=w_gate_d[e_].rearrange("(k p) n -> p k n", p=128)),
                       w=['wst0'], dma='wst0')
                    op('act', I('dma_start', out=wst[1][:], in_=w_up_d[e_].rearrange("(k p) n -> p k n", p=128)),
                       w=['wst1'], dma='wst1')
                    op('act', I('dma_start', out=wst[2][:].rearrange("p (a b) n -> p a (b n)", a=4),
                               in_=w_down_d[e_].rearrange("(k p) n -> p k n", p=128)), w=['wst2'], dma='wst2')

                def cast_gu(e_):
                    i = e_ % 2
                    op('dve', I('tensor_copy', out=wg[i][:], in_=wst[0][:]), r=['wst0'], w=[f'wg{i}'])
                    op('act', I('activation', out=wu[i][:], in_=wst[1][:], func=AF.Copy), r=['wst1'], w=[f'wu{i}'])

                def cast_d(e_):
                    i = e_ % 2
                    wsv = wst[2][:].rearrange("p (a b) n -> p a (b n)", a=4)
                    op('act', I('activation', out=wd[i][:, 0:2, :], in_=wsv[:, 0:2, :], func=AF.Copy),
                       r=['wst2'], w=[f'wd{i}a'])
                    op('dve', I('tensor_copy', out=wd[i][:, 2:4, :], in_=wsv[:, 2:4, :]), r=['wst2'], w=[f'wd{i}b'])

                def load_expert(e_):
                    load_dma(e_)
                    cast_gu(e_)
                    cast_d(e_)

                load_expert(0)
                cnt = 0
                yc = 0
                for e_ in range(NEXP):
                    wi = e_ % 2
                    if e_ + 1 < NEXP:
                        load_dma(e_ + 1)
                    for half in range(2):
                        if half == 1 and e_ + 1 < NEXP:
                            cast_gu(e_ + 1)
                        r0 = e_ * CAP + half * 384
                        hb = cnt % 2
                        cnt += 1
                        for ci in range(3):
                            xi = (cnt * 3 + ci) % 3
                            op('sp', I('dma_start',
                                out=xr[xi][:], in_=xd_d[r0 + ci * 128:r0 + (ci + 1) * 128, :]),
                                r=['xd'], w=[f'xr{xi}'], dma=f'xr{xi}')
                            pa = ci % 2
                            for kc in range(8):
                                op('pe', I('transpose',
                                    out=psA[:, pa, kc * 128:(kc + 1) * 128], in_=xr[xi][:, kc * 128:(kc + 1) * 128],
                                    identity=ident[:]), r=[f'xr{xi}', 'ident'], w=[f'psA{pa}'])
                            evac_copy(xT[hb][:, :, ci * 128:(ci + 1) * 128],
                                      psA[:, pa, :].rearrange("p (k c) -> p k c", k=8), [f'psA{pa}'], [f'xT{hb}'])
                        for dc in range(4):
                            bg, bu = (dc % 2) * 2, (dc % 2) * 2 + 1
                            for kc in range(8):
                                op('pe', I('matmul',
                                    psB[:, bg, 0:384], wg[wi][:, kc, dc * 128:(dc + 1) * 128], xT[hb][:, kc, :],
                                    start=(kc == 0), stop=(kc == 7)), r=[f'wg{wi}', f'xT{hb}'], w=[f'psB{bg}'])
                            for kc in range(8):
                                op('pe', I('matmul',
                                    psB[:, bu, 0:384], wu[wi][:, kc, dc * 128:(dc + 1) * 128], xT[hb][:, kc, :],
                                    start=(kc == 0), stop=(kc == 7)), r=[f'wu{wi}', f'xT{hb}'], w=[f'psB{bu}'])
                            si = dc % 2
                            op('act', I('activation', out=sg[si][:], in_=psB[:, bg, 0:384],
                                                                           func=AF.Silu),
                               r=[f'psB{bg}'], w=[f'sg{si}'])
                            op('dve', I('tensor_tensor',
                                out=h1T[hb][:, dc, :], in0=psB[:, bu, 0:384], in1=sg[si][:], op=ALU.mult),
                               r=[f'psB{bu}', f'sg{si}'], w=[f'h1T{hb}_{dc}'])
                        for ci in range(3):
                            yi = yc % 2
                            yc += 1
                            for nn in range(2):
                                for dc in range(4):
                                    op('pe', I('matmul',
                                        psB[:, 4 + nn, :], h1T[hb][:, dc, ci * 128:(ci + 1) * 128],
                                        wd[wi][:, dc, nn * 512:(nn + 1) * 512], start=(dc == 0), stop=(dc == 3)),
                                       r=[f'h1T{hb}_{dc}', f'wd{wi}a', f'wd{wi}b'], w=[f'psB{4 + nn}'])
                            op('act', I('activation', out=ydt[yi][:, 0:512], in_=psB[:, 4, :], func=AF.Copy),
                               r=['psB4'], w=[f'ydt{yi}a'])
                            op('dve', I('tensor_copy', out=ydt[yi][:, 512:1024], in_=psB[:, 5, :]),
                               r=['psB5'], w=[f'ydt{yi}b'])
                            op('sp', I('dma_start',
                                out=yd_d[r0 + ci * 128:r0 + (ci + 1) * 128, :], in_=ydt[yi][:]),
                                r=[f'ydt{yi}a', f'ydt{yi}b'], w=[f'yd{yc}'], dma=f'yo{yi}')
                    if e_ + 1 < NEXP:
                        cast_d(e_ + 1)
                S.barrier()

            with ExitStack() as s4:
                gbf = T(s4, "gbf", [128, DM], F32)
                y1 = [T(s4, f"y1_{i}", [128, DM], F32) for i in range(2)]
                y2 = [T(s4, f"y2_{i}", [128, DM], F32) for i in range(2)]
                xf = [T(s4, f"xf{i}", [128, DM], F32) for i in range(2)]
                of = [T(s4, f"of{i}", [128, DM], F32) for i in range(2)]
                fst = T(s4, "fst", [128, 3, nseq * NT], F32)
                zt = T(s4, "zt", [128, DM], F32)
                op('pool', I('memset', zt[:], 0.0), w=['zt'])
                op('sp', I('dma_start', out=gbf[:], in_=ln_final_g.partition_broadcast(128)), w=['gbf'], dma='gbf')
                def fetch(tg):
                    i = tg % 2
                    for ybuf, k2, nm in ((y1, 0, 'y1'), (y2, 1, 'y2')):
                        op('act', I('activation', out=ybuf[i][:], in_=zt[:], func=AF.Copy), r=['zt'], w=[f'{nm}_{i}'])
                        op('pool', I('indirect_dma_start', out=ybuf[i][:], out_offset=None, in_=yd_d[:, :],
                                     in_offset=bass.IndirectOffsetOnAxis(ap=slots[:, tg, k2:k2 + 1], axis=0),
                                     bounds_check='REG', oob_is_err=False),
                           r=['yd'], w=[f'{nm}_{i}'], dma=f'{nm}_{i}')
                    op('sp', I('dma_start', out=xf[i][:], in_=xres_d[tg * 128:(tg + 1) * 128, :]),
                       r=['xres'], w=[f'xf{i}'], dma=f'xf{i}')

                def compute(tg):
                    i = tg % 2
                    op('dve', I('scalar_tensor_tensor', out=xf[i][:], in0=y1[i][:], scalar=wts[:, tg, 0:1], in1=xf[i][:],
                                op0=ALU.mult, op1=ALU.add), r=[f'y1_{i}', f'xf{i}'], w=[f'xf{i}'])
                    op('dve', I('scalar_tensor_tensor', out=xf[i][:], in0=y2[i][:], scalar=wts[:, tg, 1:2], in1=xf[i][:],
                                op0=ALU.mult, op1=ALU.add), r=[f'y2_{i}', f'xf{i}'], w=[f'xf{i}'])
                    op('act', I('activation', out=junk[:], in_=xf[i][:], func=AF.Square, accum_out=fst[:, 0, tg:tg + 1]),
                       r=[f'xf{i}'], w=['stat'])
                    rstd_from_ss(fst, tg, 1.0 / DM)
                    op('dve', I('scalar_tensor_tensor', out=of[i][:], in0=xf[i][:], scalar=fst[:, 2, tg:tg + 1],
                                in1=gbf[:], op0=ALU.mult, op1=ALU.mult), r=[f'xf{i}', 'stat', 'gbf'], w=[f'of{i}'])
                    op('sp', I('dma_start', out=out_d[tg * 128:(tg + 1) * 128, :], in_=of[i][:]),
                       r=[f'of{i}'], dma=f'of{i}')

                ntile = nseq * NT
                fetch(0)
                for tg in range(ntile):
                    if tg + 1 < ntile:
                        fetch(tg + 1)
                    compute(tg)
                S.barrier()
        S.barrier()
        S.emit()
    return nc


_NC_CACHE = {}


def _prep(inputs, c, nseq=NSEQ):
    f = lambda a: np.ascontiguousarray(np.asarray(a, dtype=np.float32))
    d = {
        "x": f(inputs["x"][c * nseq:(c + 1) * nseq]),
        "mem": f(inputs["mem"][c * nseq:(c + 1) * nseq]),
        "ln_mix_g": f(inputs["ln_mix_g"][0]),
        "w_in": f(inputs["w_in"][0]),
        "sb_out_g": f(inputs["sb_out_g"][0]),
        "conv_w": f(inputs["conv_w"][0]),
        "conv_b": f(inputs["conv_b"][0]),
        "conv_ln_g": f(inputs["conv_ln_g"][0]),
        "conv_ln_b": f(inputs["conv_ln_b"][0]),
        "w_out": f(inputs["w_out"][0]),
        "ln_mem_x_g": f(inputs["ln_mem_x_g"][0]),
        "ln_mem_g": f(inputs["ln_mem_g"][0]),
        "w_xq": f(inputs["w_xq"][0]),
        "w_xkv": f(inputs["w_xkv"][0]),
        "w_xo": f(inputs["w_xo"][0]),
        "ln_ffn_g": f(inputs["ln_ffn_g"][0]),
        "w_rt": f(np.concatenate([np.asarray(inputs["w_group"][0]), np.asarray(inputs["w_er"][0]).reshape(DM, 32)], axis=1)),
        "b_rt": f(np.concatenate([np.asarray(inputs["b_group"][0]), np.asarray(inputs["b_er"][0]).reshape(32)])),
        "w_gate": f(inputs["w_gate"][0]),
        "w_up": f(inputs["w_up"][0]),
        "w_down": f(inputs["w_down"][0]),
        "ln_final_g": f(inputs["ln_final_g"]),
    }
    return d


def kernel(**inputs):
    if 'nc' not in _NC_CACHE:
        _NC_CACHE['nc'] = build()
    nc = _NC_CACHE['nc']
    in_maps = [_prep(inputs, c) for c in range(N_CORES)]
    res = run_bass_kernel_spmd(nc, in_maps, core_ids=list(range(N_CORES)))
    out = np.concatenate([np.asarray(r["out"]).reshape(NSEQ, SEQ, DM) for r in res.results], axis=0)
    return out.astype(np.float32)
```

```python
import numpy as np
import concourse.bass as bass
import concourse.mybir as mybir
from concourse.bass_utils import run_bass_kernel_spmd
from contextlib import ExitStack

F32 = mybir.dt.float32
F32R = mybir.dt.float32r
BF16 = mybir.dt.bfloat16
I32 = mybir.dt.int32
AF = mybir.ActivationFunctionType
ALU = mybir.AluOpType
AX = mybir.AxisListType

ENG = ['pe', 'act', 'dve', 'pool', 'sp']
SAME_ENGINE_SYNC = True

N_CORES = 8
NSEQ = 4
SEQ = 2048
DM = 1024
NT = SEQ // 128
NMEM = 256
NEXP = 32
CAP = 768
NROWS = NEXP * CAP
EPS = 1e-6


def I(name, *args, **kw):
    return (name, args, kw)


class Sched:
    def __init__(self, nc, es):
        self.nc = nc
        self.es = es
        self.ins = {e: [] for e in ENG}
        self.cnt = {e: 0 for e in ENG}
        self.sem = {e: es.enter_context(nc.semaphore('s_' + e)) for e in ENG}
        self.dsem = {}
        self.lastw = {}
        self.readers = {}
        self.waited = {e: {} for e in ENG}

    def _semof(self, key):
        return self.sem[key[1]] if key[0] == 'e' else self.dsem[key[1]][0]

    def op(self, eng, fn, r=(), w=(), dma=None, extra=()):
        deps = {}

        def need(ev):
            if ev is None:
                return
            key, val, src, is_dma = ev
            if (not is_dma) and src == eng and (eng == 'pe' or not SAME_ENGINE_SYNC):
                return
            if deps.get(key, 0) < val:
                deps[key] = val

        for k in r:
            need(self.lastw.get(k))
        for k in w:
            need(self.lastw.get(k))
            for ev in self.readers.get(k, {}).values():
                need(ev)
        for ev in extra:
            need(ev)
        waits = []
        wd = self.waited[eng]
        for key, val in deps.items():
            if wd.get(key, 0) >= val:
                continue
            wd[key] = val
            waits.append((key, val))
        if dma is not None:
            if dma not in self.dsem:
                self.dsem[dma] = [self.es.enter_context(self.nc.semaphore('d_' + dma)), 0]
            self.dsem[dma][1] += 16
            ev = (('d', dma), self.dsem[dma][1], eng, True)
        else:
            self.cnt[eng] += 1
            ev = (('e', eng), self.cnt[eng], eng, False)
        self.ins[eng].append((waits, fn, ev))
        for k in r:
            d = self.readers.setdefault(k, {})
            old = d.get(ev[0])
            if old is None or old[1] < ev[1]:
                d[ev[0]] = ev
        for k in w:
            self.lastw[k] = ev
            self.readers[k] = {}
        return ev

    def barrier(self):
        evs = []
        for e in ENG:
            if self.cnt[e] > 0:
                evs.append((('e', e), self.cnt[e], e, False))
        for slot, (s, c) in self.dsem.items():
            if c > 0:
                evs.append((('d', slot), c, 'sp', True))
        for e in ENG:
            self.op(e, I('nop'), extra=[ev for ev in evs if not (ev[2] == e and not ev[3])])
        self.lastw = {}
        self.readers = {}

    def emit(self):
        nc = self.nc
        with nc.Block() as block:
            def body(name):
                def f(e):
                    bc_reg = None
                    if name == 'pool':
                        bc_reg = e.alloc_register()
                        e.reg_mov(bc_reg, NROWS - 1)
                    for waits, fn, ev in self.ins[name]:
                        for key, val in waits:
                            e.wait_ge(self._semof(key), val)
                        try:
                            kw = fn[2]
                            if kw.get('bounds_check', None) == 'REG':
                                kw = dict(kw, bounds_check=bc_reg)
                            ins = getattr(e, fn[0])(*fn[1], **kw)
                        except Exception:
                            print("EMIT FAIL", name, fn[0], fn[1], fn[2])
                            raise
                        key, val, _, is_dma = ev
                        ins.then_inc(self._semof(key), 16 if is_dma else 1)
                return f
            block.tensor(body('pe'))
            block.scalar(body('act'))
            block.vector(body('dve'))
            block.gpsimd(body('pool'))
            block.sync(body('sp'))


def build(nseq=NSEQ, stage=99):
    nc = bass.Bass('TRN2', target_bir_lowering=False)
    ntok = nseq * SEQ

    def din(name, shape, dt=F32):
        return nc.dram_tensor(name, list(shape), dt, kind="ExternalInput").ap()

    x_d = din("x", [nseq, SEQ, DM])
    mem_d = din("mem", [nseq, NMEM, DM])
    ln_mix_g = din("ln_mix_g", [DM])
    w_in_d = din("w_in", [DM, 2560])
    sb_out_g = din("sb_out_g", [512])
    conv_w_d = din("conv_w", [31, 512])
    conv_b_d = din("conv_b", [512])
    conv_ln_g = din("conv_ln_g", [512])
    conv_ln_b = din("conv_ln_b", [512])
    w_out_d = din("w_out", [DM, DM])
    ln_mem_x_g = din("ln_mem_x_g", [DM])
    ln_mem_g = din("ln_mem_g", [DM])
    w_xq_d = din("w_xq", [DM, DM])
    w_xkv_d = din("w_xkv", [DM, 2 * DM])
    w_xo_d = din("w_xo", [DM, DM])
    ln_ffn_g = din("ln_ffn_g", [DM])
    w_rt_d = din("w_rt", [DM, 36])
    b_rt_d = din("b_rt", [36])
    w_gate_d = din("w_gate", [NEXP, DM, 512])
    w_up_d = din("w_up", [NEXP, DM, 512])
    w_down_d = din("w_down", [NEXP, 512, DM])
    ln_final_g = din("ln_final_g", [DM])
    out_d = nc.dram_tensor("out", [ntok, DM], F32, kind="ExternalOutput").ap()
    if stage == 1:
        dbg_mix = nc.dram_tensor("dbg_mix", [128, 8, SEQ], BF16, kind="ExternalOutput").ap()
        dbg_qk = nc.dram_tensor("dbg_qk", [128, 8, SEQ], BF16, kind="ExternalOutput").ap()
        dbg_v = nc.dram_tensor("dbg_v", [128, NT, 512], BF16, kind="ExternalOutput").ap()
        dbg_rsb = nc.dram_tensor("dbg_rsb", [128, 3, NT], F32, kind="ExternalOutput").ap()
    xd_d = nc.dram_tensor("xd_scr", [NROWS, DM], BF16).ap()
    yd_d = nc.dram_tensor("yd_scr", [NROWS, DM], F32).ap()
    xres_d = nc.dram_tensor("xres_scr", [ntok, DM], F32).ap()

    with ExitStack() as es:
        S = Sched(nc, es)
        op = S.op

        uid = [0]

        def T(scope, name, shape, dt):
            uid[0] += 1
            return scope.enter_context(nc.sbuf_tensor(f"{name}_u{uid[0]}", shape, dt))

        psA = es.enter_context(nc.psum_tensor("psA", [128, 2, 1024], BF16))
        psB = es.enter_context(nc.psum_tensor("psB", [128, 6, 512], F32))

        identf = T(es, "identf", [128, 128], F32)
        ident = T(es, "ident", [128, 128], BF16)
        negtri = T(es, "negtri", [128, 128], F32)
        negones = T(es, "negones", [128, 128], F32)
        negtriR = T(es, "negtriR", [128, 128], F32)
        negonesR = T(es, "negonesR", [128, 128], F32)
        meanmat = T(es, "meanmat", [128, 128], F32)
        ones2 = T(es, "ones2", [128, 2], F32)
        onesb = T(es, "onesb", [128, 128], BF16)
        Lmat = T(es, "Lmat", [128, 128], BF16)
        maskf = T(es, "maskf", [128, 4, 512], BF16)
        ecap = T(es, "ecap", [128, 32], F32)
        ecap_i = T(es, "ecap_i", [128, 32], I32)
        gcols = T(es, "gcols", [128, 3, 8], F32)
        sbg = T(es, "sbg", [128, 4], F32)
        cvb = T(es, "cvb", [128, 4], F32)
        cvg = T(es, "cvg", [128, 4], F32)
        cvbb = T(es, "cvbb", [128, 4], F32)
        cwT = T(es, "cwT", [128, 4, 31], F32)
        hT = T(es, "hT", [128, 8, SEQ], BF16)
        wr = [T(es, f"wr{i}", [128, 8, 512], BF16) for i in range(2)]
        stat = T(es, "stat", [128, 3, NT], F32)
        rsb = T(es, "rsb", [128, 3, NT], F32)
        slots = T(es, "slots", [128, nseq * NT, 2], I32)
        wts = T(es, "wts", [128, nseq * NT, 2], F32)
        base = T(es, "base", [128, 32], F32)
        wrt = T(es, "wrt", [128, 8, 36], F32)
        brt = T(es, "brt", [128, 36], F32)
        junk = T(es, "junk", [128, 1024], BF16)

        def HK(c, n):
            return f"H{c}_{n}"

        def small_col(dst_ap, src_ap, k, key):
            op('sp', I('dma_start', out=dst_ap, in_=src_ap.rearrange("(k p) -> p k", p=128),
                                           allow_slow_non_contiguous=True), w=[key], dma='c_' + key)

        small_col(gcols[:, 0, :], ln_mix_g, 8, 'gc0')
        small_col(gcols[:, 1, :], ln_mem_x_g, 8, 'gc1')
        small_col(gcols[:, 2, :], ln_mem_g, 8, 'gc2')
        small_col(sbg[:], sb_out_g, 4, 'sbg')
        small_col(cvb[:], conv_b_d, 4, 'cvb')
        small_col(cvg[:], conv_ln_g, 4, 'cvg')
        small_col(cvbb[:], conv_ln_b, 4, 'cvbb')
        op('sp', I('dma_start', out=wrt[:], in_=w_rt_d.rearrange("(k p) n -> p k n", p=128)),
           w=['wrt'], dma='c_wrt')
        op('sp', I('dma_start', out=brt[:], in_=b_rt_d.partition_broadcast(128)), w=['brt'], dma='c_brt')

        op('pool', I('memset', identf[:], 0.0), w=['identf'])
        op('pool', I('affine_select', out=identf[:], in_=identf[:], pattern=[[-1, 128]],
                                             compare_op=ALU.not_equal, fill=1.0, base=0, channel_multiplier=1),
           r=['identf'], w=['identf'])
        op('dve', I('tensor_copy', out=ident[:], in_=identf[:]), r=['identf'], w=['ident'])
        op('pool', I('memset', negtri[:], -1.0), w=['negtri'])
        op('pool', I('affine_select', out=negtri[:], in_=negtri[:], pattern=[[-1, 128]],
                                             compare_op=ALU.is_ge, fill=0.0, base=0, channel_multiplier=1),
           r=['negtri'], w=['negtri'])
        op('pool', I('memset', negones[:], -1.0), w=['negones'])
        op('dve', I('tensor_copy', out=negtriR[:].bitcast(F32R), in_=negtri[:]), r=['negtri'], w=['negtriR'])
        op('dve', I('tensor_copy', out=negonesR[:].bitcast(F32R), in_=negones[:]), r=['negones'], w=['negonesR'])
        op('pool', I('memset', meanmat[:], 1.0 / 512), w=['meanmat'])
        op('pool', I('memset', ones2[:], 1.0), w=['ones2'])
        op('pool', I('memset', onesb[:], 1.0), w=['onesb'])
        op('pool', I('memset', Lmat[:], 1.0), w=['Lmat'])
        op('pool', I('affine_select', out=Lmat[:], in_=Lmat[:], pattern=[[1, 128]],
                                             compare_op=ALU.is_gt, fill=0.0, base=0, channel_multiplier=-1),
           r=['Lmat'], w=['Lmat'])
        op('pool', I('memset', maskf[:], 1.0), w=['maskf'])
        for r_ in range(4):
            op('pool', I('affine_select', out=maskf[:, r_, :], in_=maskf[:, r_, :], pattern=[[1, 512]],
                                                        compare_op=ALU.is_gt, fill=0.0, base=-r_ * 128,
                                                        channel_multiplier=-1),
               r=['maskf'], w=['maskf'])
        op('pool', I('iota', ecap_i[:], pattern=[[CAP, 32]], base=0, channel_multiplier=0), w=['ecap_i'])
        op('dve', I('tensor_copy', out=ecap[:], in_=ecap_i[:]), r=['ecap_i'], w=['ecap'])
        op('pool', I('memset', base[:], 0.0), w=['base'])

        with ExitStack() as s0:
            cw_in = T(s0, "cw_in", [31, 512], F32)
            op('sp', I('dma_start', out=cw_in[:], in_=conv_w_d), w=['cw_in'], dma='c_cw')
            for c in range(4):
                op('pe', I('transpose', out=psB[:, 0, c * 32:c * 32 + 31], in_=cw_in[:, c * 128:(c + 1) * 128],
                                                    identity=identf[0:31, 0:31]),
                   r=['cw_in', 'identf'], w=['psB0'])
            op('act', I('activation', out=cwT[:], in_=psB[:, 0, 0:128].rearrange("p (c w) -> p c w", c=4)[:, :, 0:31],
                                             func=AF.Copy), r=['psB0'], w=['cwT'])
            S.barrier()

        wslot = [0]

        def load_w(src2d, nk=8):
            i = wslot[0]
            wslot[0] ^= 1
            op('pool', I('dma_start', out=wr[i][:, 0:nk, :], in_=src2d.rearrange("(k p) n -> p k n", p=128)),
               w=[f'wr{i}'], dma=f'wr{i}')
            return i

        evac_flip = [0]

        def evac_copy(out_ap, in_ap, r, w, scale=None, eng=None):
            if eng is None:
                eng = 'act' if (evac_flip[0] & 1) == 0 else 'dve'
                evac_flip[0] += 1
            if eng == 'act':
                if scale is None:
                    op('act', I('activation', out=out_ap, in_=in_ap, func=AF.Copy), r=r, w=w)
                else:
                    op('act', I('activation', out=out_ap, in_=in_ap, func=AF.Copy, scale=scale), r=r, w=w)
            else:
                if scale is None:
                    op('dve', I('tensor_copy', out=out_ap, in_=in_ap), r=r, w=w)
                else:
                    op('dve', I('tensor_scalar', out=out_ap, in0=in_ap, scalar1=scale, scalar2=None,
                                                        op0=ALU.mult), r=r, w=w)

        def rstd_from_ss(st, col, inv_n):
            op('act', I('activation', out=st[:, 1, col:col + 1], in_=st[:, 0, col:col + 1], func=AF.Sqrt,
                                             scale=inv_n, bias=EPS), r=['stat'], w=['stat'])
            op('dve', I('reciprocal', out=st[:, 2, col:col + 1], in_=st[:, 1, col:col + 1]),
               r=['stat'], w=['stat'])

        def stats_all(xs, key):
            for t in range(NT):
                op('act', I('activation', out=junk[:], in_=xs[:, t, :], func=AF.Square, accum_out=stat[:, 0, t:t + 1]),
                   r=[f'x{t}'], w=[f'{key}_ss{t}'])
            op('act', I('activation', out=stat[:, 1, :], in_=stat[:, 0, :], func=AF.Sqrt, scale=1.0 / DM, bias=EPS),
               r=[f'{key}_ss{t}' for t in range(NT)], w=[key + '_sd'])
            op('dve', I('reciprocal', out=stat[:, 2, :], in_=stat[:, 1, :]), r=[key + '_sd'], w=[key])

        def norm_T(src_ap, src_key, hn_t, hn_key, gi, dst3, dst_keys, t, pa, pre=None):
            if pre is None:
                op('act', I('activation', out=junk[:], in_=src_ap, func=AF.Square, accum_out=stat[:, 0, t:t + 1]),
                   r=[src_key], w=['stat'])
                rstd_from_ss(stat, t, 1.0 / DM)
                pre = 'stat'
            op('act', I('activation', out=hn_t[:], in_=src_ap, func=AF.Copy, scale=stat[:, 2, t:t + 1]),
               r=[src_key, pre], w=[hn_key])
            for kc in range(8):
                op('pe', I('transpose', out=psA[:, pa, kc * 128:(kc + 1) * 128],
                                                      in_=hn_t[:, kc * 128:(kc + 1) * 128], identity=ident[:]),
                   r=[hn_key, 'ident'], w=[f'psA{pa}'])
            op('dve', I('tensor_tensor', out=dst3, in0=psA[:, pa, :].rearrange("p (k c) -> p k c", k=8),
                                                in1=gcols[:, gi, :, None].to_broadcast([128, 8, 128]), op=ALU.mult),
               r=[f'psA{pa}', f'gc{gi}'], w=dst_keys)

        def fm_proj(slot, ncc, rhs_fn, rhs_keys_fn, nN, N, evac_fn, banks):
            k = 0
            for cc in range(ncc):
                for n in range(nN):
                    bk = banks[k % len(banks)]
                    k += 1
                    for kc in range(8):
                        op('pe', I('matmul',
                            psB[:, bk, 0:N], wr[slot][:, kc, cc * 128:(cc + 1) * 128], rhs_fn(kc, n),
                            start=(kc == 0), stop=(kc == 7)),
                           r=[f'wr{slot}'] + rhs_keys_fn(kc, n), w=[f'psB{bk}'])
                    evac_fn(cc, n, bk)

        dbg = {}

        for b in range(nseq):
            with ExitStack() as s1:
                qk = T(s1, "qk", [128, 8, SEQ], BF16)
                v_sb = T(s1, "v_sb", [128, NT, 512], BF16)
                with ExitStack() as s1a:
                    xin = [T(s1a, f"xin{i}", [128, DM], F32) for i in range(3)]
                    hn = [T(s1a, f"hn{i}", [128, DM], BF16) for i in range(2)]
                    gT = T(s1a, "gT", [128, 4, 30 + SEQ], BF16)
                    ycv = T(s1a, "ycv", [128, 4, SEQ], F32)
                    dg = T(s1a, "dg", [128, 31, 128], BF16)
                    ysq = T(s1a, "ysq", [128, 4, 512], F32)
                    lnw = T(s1a, "lnw", [128, 4, 512], F32)

                    for t in range(NT):
                        xi = xin[t % 3]
                        op('sp', I('dma_start', out=xi[:], in_=x_d[b, t * 128:(t + 1) * 128, :]),
                           w=[f'xin{t % 3}'], dma=f'xin{t % 3}')
                        norm_T(xi[:], f'xin{t % 3}', hn[t % 2], f'hn{t % 2}', 0,
                               hT[:, :, t * 128:(t + 1) * 128], [HK(c, t // 4) for c in range(8)], t, t % 2)

                    hrhs = lambda kc, n: hT[:, kc, n * 512:(n + 1) * 512]
                    hkeys = lambda kc, n: [HK(kc, n)]
                    sl = load_w(w_in_d[:, 0:512])
                    nxt = load_w(w_in_d[:, 512:1024])
                    fm_proj(sl, 4, hrhs, hkeys, 4, 512,
                            lambda cc, n, bk: evac_copy(qk[:, cc, n * 512:(n + 1) * 512], psB[:, bk, :], [f'psB{bk}'],
                                                        [f'q{cc}_{n}'], scale=0.125), [0, 1, 2, 3])
                    sl = nxt
                    nxt = load_w(w_in_d[:, 1024:1536])
                    fm_proj(sl, 4, hrhs, hkeys, 4, 512,
                            lambda cc, n, bk: evac_copy(qk[:, 4 + cc, n * 512:(n + 1) * 512], psB[:, bk, :],
                                                        [f'psB{bk}'], [f'k{cc}_{n}']), [0, 1, 2, 3])
                    sl = nxt
                    nxt = load_w(w_in_d[:, 2048:2560])
                    for t in range(NT):
                        bk = t % 4
                        for kc in range(8):
                            op('pe', I('matmul',
                                psB[:, bk, :], hT[:, kc, t * 128:(t + 1) * 128], wr[sl][:, kc, :],
                                start=(kc == 0), stop=(kc == 7)),
                               r=[f'wr{sl}', HK(kc, t // 4)], w=[f'psB{bk}'])
                        evac_copy(v_sb[:, t, :], psB[:, bk, :], [f'psB{bk}'], [f'v{t}'])
                    op('pool', I('memset', gT[:, :, 0:30], 0.0), w=['gTpad'])
                    sl = nxt
                    nxt = load_w(w_in_d[:, 1536:2048])
                    fm_proj(sl, 4, hrhs, hkeys, 4, 512,
                            lambda cc, n, bk: op('act', I('activation',
                                out=gT[:, cc, 30 + n * 512:30 + (n + 1) * 512], in_=psB[:, bk, :], func=AF.Sigmoid),
                                r=[f'psB{bk}'], w=[f'g{cc}_{n}']), [0, 1, 2, 3])
                    sl = nxt
                    fm_proj(sl, 4, hrhs, hkeys, 4, 512,
                            lambda cc, n, bk: op('dve', I('tensor_tensor',
                                out=gT[:, cc, 30 + n * 512:30 + (n + 1) * 512], in0=psB[:, bk, :],
                                in1=gT[:, cc, 30 + n * 512:30 + (n + 1) * 512], op=ALU.mult),
                                r=[f'psB{bk}', f'g{cc}_{n}'], w=[f'g{cc}_{n}']), [0, 1, 2, 3])

                    for c in range(4):
                        for w_ in range(31):
                            op('dve', I('tensor_scalar',
                                out=dg[:, w_, :], in0=ident[:], scalar1=cwT[:, c, w_:w_ + 1], scalar2=None,
                                op0=ALU.mult), r=['ident', 'cwT'], w=[f'dg{w_}'])
                        for n in range(4):
                            bk = n % 4
                            gkeys = [f'g{c}_{n}'] + ([f'g{c}_{n - 1}'] if n > 0 else ['gTpad'])
                            for w_ in range(31):
                                op('pe', I('matmul',
                                    psB[:, bk, :], dg[:, w_, :], gT[:, c, n * 512 + w_:n * 512 + w_ + 512],
                                    start=(w_ == 0), stop=(w_ == 30)),
                                   r=[f'dg{w_}'] + gkeys, w=[f'psB{bk}'])
                            op('act', I('activation',
                                out=ycv[:, c, n * 512:(n + 1) * 512], in_=psB[:, bk, :], func=AF.Identity,
                                bias=cvb[:, c:c + 1]), r=[f'psB{bk}', 'cvb'], w=[f'y{c}_{n}'])
                    for n in range(4):
                        ns = slice(n * 512, (n + 1) * 512)
                        for c in range(4):
                            op('pe', I('matmul', psB[:, 4, :], meanmat[:], ycv[:, c, ns],
                                                                    start=(c == 0), stop=(c == 3)),
                               r=['meanmat', f'y{c}_{n}'], w=['psB4'])
                        for c in range(4):
                            op('act', I('activation', out=ysq[:, c, :], in_=ycv[:, c, ns],
                                                                         func=AF.Square),
                               r=[f'y{c}_{n}'], w=[f'ysq{c}'])
                        for c in range(4):
                            op('pe', I('matmul', psB[:, 5, :], meanmat[:], ysq[:, c, :],
                                                             start=(c == 0), stop=(c == 3)),
                               r=['meanmat', f'ysq{c}'], w=['psB5'])
                        op('act', I('activation', out=lnw[:, 0, :], in_=psB[:, 4, :], func=AF.Copy),
                           r=['psB4'], w=['lnw0'])
                        op('pool', I('tensor_tensor', out=lnw[:, 1, :], in0=lnw[:, 0, :], in1=lnw[:, 0, :],
                                                             op=ALU.mult), r=['lnw0'], w=['lnw1'])
                        op('dve', I('tensor_tensor', out=lnw[:, 1, :], in0=psB[:, 5, :], in1=lnw[:, 1, :],
                                                            op=ALU.subtract), r=['psB5', 'lnw1'], w=['lnw1'])
                        op('dve', I('tensor_scalar', out=lnw[:, 1, :], in0=lnw[:, 1, :], scalar1=0.0,
                                                            scalar2=None, op0=ALU.max), r=['lnw1'], w=['lnw1'])
                        op('act', I('activation', out=lnw[:, 1, :], in_=lnw[:, 1, :], func=AF.Sqrt, bias=EPS),
                           r=['lnw1'], w=['lnw1'])
                        op('dve', I('reciprocal', out=lnw[:, 2, :], in_=lnw[:, 1, :]), r=['lnw1'], w=['lnw2'])
                        for c in range(4):
                            op('pool', I('tensor_tensor', out=lnw[:, 3, :], in0=ycv[:, c, ns],
                                                                             in1=lnw[:, 0, :], op=ALU.subtract),
                               r=[f'y{c}_{n}', 'lnw0'], w=['lnw3'])
                            op('pool', I('tensor_tensor', out=lnw[:, 3, :], in0=lnw[:, 3, :], in1=lnw[:, 2, :],
                                                                 op=ALU.mult), r=['lnw3', 'lnw2'], w=['lnw3'])
                            op('act', I('activation',
                                out=hT[:, 4 + c, ns], in_=lnw[:, 3, :], func=AF.Silu, scale=cvg[:, c:c + 1],
                                bias=cvbb[:, c:c + 1]), r=['lnw3', 'cvg', 'cvbb'], w=[HK(4 + c, n)])
                    S.barrier()

                with ExitStack() as s1b:
                    spb = [T(s1b, f"spb{i}", [128, 512], F32) for i in range(4)]
                    ab = [T(s1b, f"ab{i}", [128, 512], BF16) for i in range(4)]
                    Rb = [T(s1b, f"Rb{i}", [128, 512], F32) for i in range(2)]
                    osq = [T(s1b, f"osq{i}", [128, 512], F32) for i in range(2)]
                    units = []
                    for j in range(4):
                        for qn in range(4):
                            kcs = list(range(4 * qn + 3, -1, -1))
                            for idx, kc in enumerate(kcs):
                                for hp in range(2):
                                    units.append((j, qn, idx, kc, hp, len(kcs)))
                    nU = len(units)
                    negtri_r = negtriR[:].bitcast(F32R)
                    negones_r = negonesR[:].bitcast(F32R)
                    oqc = [0]

                    def geo(i):
                        j, qn, idx, kc, hp, nk = units[i]
                        return (j, qn, idx, kc, hp, nk, slice(hp * 64, hp * 64 + 64), slice(qn * 512, (qn + 1) * 512),
                                slice(kc * 128, (kc + 1) * 128))

                    def S1(i0_):
                        us = [i0_, i0_ + 1]
                        for i in us:
                            j, qn, idx, kc, hp, nk, P, qs, ks = geo(i)
                            zb = i % 2
                            op('pe', I('matmul', psB[:, zb, :], qk[P, 4 + j, ks], qk[P, j, qs], start=True, stop=True),
                               r=[f'k{j}_{kc // 4}', f'q{j}_{qn}'], w=[f'psB{zb}'])
                        for i in us:
                            j, qn, idx, kc, hp, nk, P, qs, ks = geo(i)
                            zb = i % 2
                            sp_t, spk = spb[i % 4], f'spb{i % 4}'
                            op('act', I('activation', out=sp_t[:].bitcast(F32R), in_=psB[:, zb, :], func=AF.Exp),
                               r=[f'psB{zb}'], w=[spk])
                            op('act', I('activation', out=sp_t[:].bitcast(F32R), in_=sp_t[:], func=AF.Ln, bias=1.0),
                               r=[spk], w=[spk])
                            if kc >= 4 * qn:
                                op('dve', I('tensor_tensor', out=sp_t[:].bitcast(F32R), in0=sp_t[:],
                                            in1=maskf[:, kc - 4 * qn, :], op=ALU.mult), r=[spk, 'maskf'], w=[spk])

                    def S2(i0_):
                        us = [i0_, i0_ + 1]
                        for i in us:
                            j, qn, idx, kc, hp, nk, P, qs, ks = geo(i)
                            eb = 2 + i % 2
                            op('pe', I('matmul', psB[:, eb, :], qk[P, 4 + j, ks], qk[P, j, qs], start=True, stop=False),
                               r=[f'k{j}_{kc // 4}', f'q{j}_{qn}'], w=[f'psB{eb}'])
                        for i in us:
                            j, qn, idx, kc, hp, nk, P, qs, ks = geo(i)
                            eb = 2 + i % 2
                            ek = f'psB{eb}'
                            sp_t, spk = spb[i % 4], f'spb{i % 4}'
                            op('pe', I('matmul', psB[:, eb, :], negtri_r, sp_t[:].bitcast(F32R), start=False,
                                       stop=(idx == 0)), r=['negtriR', spk], w=[ek])
                            if idx > 0:
                                op('pe', I('matmul', psB[:, eb, :], negones_r, Rb[hp][:].bitcast(F32R), start=False,
                                           stop=True), r=['negonesR', f'Rb{hp}'], w=[ek])
                        for i in us:
                            j, qn, idx, kc, hp, nk, P, qs, ks = geo(i)
                            eb = 2 + i % 2
                            ek = f'psB{eb}'
                            sp_t, spk = spb[i % 4], f'spb{i % 4}'
                            ab_t, abk = ab[i % 4], f'ab{i % 4}'
                            op('act', I('activation', out=ab_t[:], in_=psB[:, eb, :], func=AF.Exp), r=[ek], w=[abk])
                            if kc >= 4 * qn:
                                op('dve', I('tensor_tensor', out=ab_t[:], in0=ab_t[:], in1=maskf[:, kc - 4 * qn, :],
                                            op=ALU.mult), r=[abk, 'maskf'], w=[abk])
                            if idx < nk - 1:
                                if idx == 0:
                                    op('pool', I('tensor_copy', out=Rb[hp][:].bitcast(F32R), in_=sp_t[:]), r=[spk],
                                       w=[f'Rb{hp}'])
                                else:
                                    op('pool', I('tensor_tensor', out=Rb[hp][:].bitcast(F32R), in0=Rb[hp][:],
                                                 in1=sp_t[:], op=ALU.add), r=[spk, f'Rb{hp}'], w=[f'Rb{hp}'])

                    def S3(i0_):
                        us = [i0_, i0_ + 1]
                        for i in us:
                            j, qn, idx, kc, hp, nk, P, qs, ks = geo(i)
                            h = 2 * j + hp
                            ab_t, abk = ab[i % 4], f'ab{i % 4}'
                            op('pe', I('matmul', psB[P, 4, :], v_sb[:, kc, h * 64:(h + 1) * 64], ab_t[:],
                                       start=(idx == 0), stop=(idx == nk - 1)), r=[f'v{kc}', abk], w=[f'psB4_{hp}'])
                        j, qn, idx, kc, hp, nk, P, qs, ks = geo(i0_ + 1)
                        if idx == nk - 1:
                            oq_t = osq[oqc[0] & 1]
                            oqk = f'osq{oqc[0] & 1}'
                            oqc[0] += 1
                            op('act', I('activation', out=hT[:, j, qs], in_=psB[:, 4, :], func=AF.Copy,
                                        scale=sbg[:, j:j + 1]), r=['psB4_0', 'psB4_1', 'sbg'], w=[HK(j, qn)])
                            op('act', I('activation', out=oq_t[:], in_=psB[:, 4, :], func=AF.Square),
                               r=['psB4_0', 'psB4_1'], w=[oqk])
                            for tt in range(4):
                                col = (j * 16 + qn * 4 + tt) * 2
                                op('pe', I('matmul', psB[:, 5, col:col + 2], oq_t[:, tt * 128:(tt + 1) * 128], ones2[:],
                                           start=True, stop=True), r=[oqk, 'ones2'], w=['psB5'])

                    nP = nU // 2
                    for p in range(nP + 2):
                        if p < nP:
                            S1(2 * p)
                        if 0 <= p - 1 < nP:
                            S2(2 * (p - 1))
                        if 0 <= p - 2 < nP:
                            S3(2 * (p - 2))
                    ssv = lambda j: psB[:, 5, j * 32:(j + 1) * 32].rearrange("p (t two) -> p t two", two=2)[:, :, 0]
                    op('act', I('activation', out=rsb[:, 0, :], in_=ssv(0), func=AF.Copy), r=['psB5'], w=['rsb'])
                    for j in range(1, 4):
                        op('dve', I('tensor_tensor', out=rsb[:, 0, :], in0=ssv(j), in1=rsb[:, 0, :],
                                                                 op=ALU.add), r=['psB5', 'rsb'], w=['rsb'])
                    op('act', I('activation', out=rsb[:, 1, :], in_=rsb[:, 0, :], func=AF.Sqrt, scale=1.0 / 512,
                                                     bias=EPS), r=['rsb'], w=['rsb'])
                    op('dve', I('reciprocal', out=rsb[:, 2, :], in_=rsb[:, 1, :]), r=['rsb'], w=['rsb'])
                    S.barrier()
                    if stage == 1 and b == 0:
                        op('sp', I('dma_start', out=dbg_mix, in_=hT[:]), dma='dbg0')
                        op('sp', I('dma_start', out=dbg_qk, in_=qk[:]), dma='dbg1')
                        op('sp', I('dma_start', out=dbg_v, in_=v_sb[:]), dma='dbg2')
                        op('sp', I('dma_start', out=dbg_rsb, in_=rsb[:]), dma='dbg3')
                        S.barrier()

            with ExitStack() as s2:
                x_sb = T(s2, "x_sb", [128, NT, DM], F32)
                for t in range(NT):
                    op('sp', I('dma_start', out=x_sb[:, t, :], in_=x_d[b, t * 128:(t + 1) * 128, :]),
                       w=[f'x{t}'], dma=f'x{t}')
                nxt = load_w(w_out_d[:, 0:512])
                for n in range(2):
                    sl = nxt
                    nxt = load_w(w_out_d[:, 512:1024]) if n == 0 else load_w(w_xkv_d[:, 0:512])
                    ns = slice(n * 512, (n + 1) * 512)
                    for t in range(NT):
                        b0, b1 = (t % 2) * 2, (t % 2) * 2 + 1
                        ts_ = slice(t * 128, (t + 1) * 128)
                        for jj in range(4):
                            op('pe', I('matmul',
                                psB[:, b0, :], hT[:, jj, ts_], wr[sl][:, jj, :], start=(jj == 0), stop=(jj == 3)),
                               r=[HK(jj, t // 4), f'wr{sl}'], w=[f'psB{b0}'])
                        for jj in range(4):
                            op('pe', I('matmul',
                                psB[:, b1, :], hT[:, 4 + jj, ts_], wr[sl][:, 4 + jj, :], start=(jj == 0), stop=(jj == 3)),
                               r=[HK(4 + jj, t // 4), f'wr{sl}'], w=[f'psB{b1}'])
                        op('dve', I('tensor_tensor',
                            out=x_sb[:, t, ns], in0=psB[:, b1, :], in1=x_sb[:, t, ns], op=ALU.add),
                           r=[f'psB{b1}', f'x{t}'], w=[f'x{t}'])
                        op('dve', I('scalar_tensor_tensor',
                            out=x_sb[:, t, ns], in0=psB[:, b0, :], scalar=rsb[:, 2, t:t + 1], in1=x_sb[:, t, ns],
                            op0=ALU.mult, op1=ALU.add), r=[f'psB{b0}', 'rsb', f'x{t}'], w=[f'x{t}'])
                if stage == 1:
                    for t in range(NT):
                        ev = op('sp', I('dma_start',
                            out=out_d[b * SEQ + t * 128:b * SEQ + (t + 1) * 128, :], in_=x_sb[:, t, :]),
                            r=[f'x{t}'], dma=f'o{t % 4}')
                    S.barrier()
                    continue

                with ExitStack() as s2f:
                    memin = [T(s2f, f"memin{i}", [128, DM], F32) for i in range(2)]
                    hn2 = [T(s2f, f"hnb{i}", [128, DM], BF16) for i in range(2)]
                    memT = T(s2f, "memT", [128, 8, NMEM], BF16)
                    kxT = T(s2f, "kxT", [128, 8, NMEM], BF16)
                    vx = T(s2f, "vx", [128, 2, DM], BF16)
                    qxT = T(s2f, "qxT", [128, 8, SEQ], BF16)
                    pf = [T(s2f, "pf0", [128, 4, NMEM], F32)] * 2
                    pn = [T(s2f, f"pn{i}", [128, 4, NMEM], BF16) for i in range(2)]
                    pT = [T(s2f, "pT0", [128, 4, 2, 512], BF16)] * 2
                    sm = T(s2f, "sm", [128, 4, 4], F32)
                    for mt in range(2):
                        op('sp', I('dma_start', out=memin[mt][:], in_=mem_d[b, mt * 128:(mt + 1) * 128, :]),
                           w=[f'memin{mt}'], dma=f'memin{mt}')
                        norm_T(memin[mt][:], f'memin{mt}', hn2[mt], f'hnb{mt}', 2,
                               memT[:, :, mt * 128:(mt + 1) * 128], ['memT'], mt, mt)
                    mrhs = lambda kc, n: memT[:, kc, :]
                    mkeys = lambda kc, n: ['memT']
                    for g in range(2):
                        sl = nxt
                        nxt = load_w(w_xkv_d[:, (g + 1) * 512:(g + 2) * 512])
                        fm_proj(sl, 4, mrhs, mkeys, 1, NMEM,
                                lambda cc, n, bk, g=g: evac_copy(kxT[:, g * 4 + cc, :], psB[:, bk, 0:NMEM], [f'psB{bk}'],
                                                                 ['kxT']), [0, 1, 2, 3])
                    for g in range(2):
                        sl = nxt
                        nxt = load_w(w_xkv_d[:, 1536:2048]) if g == 0 else load_w(w_xq_d[:, 0:512])
                        for mt in range(2):
                            bk = mt
                            for kc in range(8):
                                op('pe', I('matmul',
                                    psB[:, bk, :], memT[:, kc, mt * 128:(mt + 1) * 128], wr[sl][:, kc, :],
                                    start=(kc == 0), stop=(kc == 7)), r=['memT', f'wr{sl}'], w=[f'psB{bk}'])
                            evac_copy(vx[:, mt, g * 512:(g + 1) * 512], psB[:, bk, :], [f'psB{bk}'], ['vx'])
                    stats_all(x_sb, 'rstdF')
                    for t in range(NT):
                        norm_T(x_sb[:, t, :], f'x{t}', hn2[t % 2], f'hnb{t % 2}', 1,
                               hT[:, :, t * 128:(t + 1) * 128], [HK(c, t // 4) for c in range(8)], t, t % 2,
                               pre='rstdF')
                    for g in range(2):
                        sl = nxt
                        nxt = load_w(w_xq_d[:, 512:1024]) if g == 0 else load_w(w_xo_d[:, 0:512])
                        fm_proj(sl, 4, hrhs, hkeys, 4, 512,
                                lambda cc, n, bk, g=g: evac_copy(qxT[:, g * 4 + cc, n * 512:(n + 1) * 512], psB[:, bk, :],
                                                                 [f'psB{bk}'], [f'qx{g * 4 + cc}_{n}'], scale=1.0 / 16),
                                [0, 1, 2, 3])
                    psSs = [psB[:, 0:2, :].rearrange("p a (h m) -> p (a h) m", h=2),
                            psB[:, 2:4, :].rearrange("p a (h m) -> p (a h) m", h=2)]
                    sm2 = [T(s2f, f"sm2_{i}", [128, 4, 4], F32) for i in range(2)]
                    pvk = [0]

                    def FA(t):
                        pi = t % 2
                        n = t // 4
                        ts_ = slice(t * 128, (t + 1) * 128)
                        psS = psSs[pi]
                        for hx in range(4):
                            for dc in range(2):
                                op('pe', I('matmul', psS[:, hx, :], qxT[:, 2 * hx + dc, ts_], kxT[:, 2 * hx + dc, :],
                                           start=(dc == 0), stop=(dc == 1)),
                                   r=[f'qx{2 * hx + dc}_{n}', 'kxT'], w=[f'psB{2 * pi + hx // 2}'])
                        op('dve', I('tensor_reduce', out=sm2[pi][:, 0, :], in_=psS, axis=AX.X, op=ALU.max),
                           r=[f'psB{2 * pi}', f'psB{2 * pi + 1}'], w=[f'sm{pi}'])
                        op('dve', I('tensor_scalar', out=sm2[pi][:, 1, :], in0=sm2[pi][:, 0, :], scalar1=-1.0,
                                    scalar2=None, op0=ALU.mult), r=[f'sm{pi}'], w=[f'sm{pi}'])

                    def FB(t):
                        pi = t % 2
                        n = t // 4
                        tt = t % 4
                        psS = psSs[pi]
                        smt = sm2[pi]
                        pT_t = pT[n % 2]
                        for hx in range(4):
                            op('act', I('activation', out=pf[pi][:, hx, :], in_=psS[:, hx, :], func=AF.Exp,
                                        bias=smt[:, 1, hx:hx + 1], accum_out=smt[:, 2, hx:hx + 1]),
                               r=[f'psB{2 * pi + hx // 2}', f'sm{pi}'], w=[f'pf{hx}', f'smacc{pi}_{hx}'])
                        op('dve', I('reciprocal', out=smt[:, 3, :], in_=smt[:, 2, :]),
                           r=[f'smacc{pi}_{hx}' for hx in range(4)], w=[f'smr{pi}'])
                        for hx in range(4):
                            op('dve', I('tensor_scalar', out=pn[pi][:, hx, :], in0=pf[pi][:, hx, :],
                                        scalar1=smt[:, 3, hx:hx + 1], scalar2=None, op0=ALU.mult),
                               r=[f'pf{hx}', f'smr{pi}'], w=[f'pn{pi}_{hx}'])
                        for hx in range(4):
                            for mc in range(2):
                                op('pe', I('transpose', out=psA[:, pi, (hx * 2 + mc) * 128:(hx * 2 + mc + 1) * 128],
                                           in_=pn[pi][:, hx, mc * 128:(mc + 1) * 128], identity=ident[:]),
                                   r=[f'pn{pi}_{hx}', 'ident'], w=[f'psA{pi}'])
                        op('act', I('activation', out=pT_t[:, :, :, tt * 128:(tt + 1) * 128],
                                    in_=psA[:, pi, :].rearrange("p (h m c) -> p h m c", h=4, m=2), func=AF.Copy),
                           r=[f'psA{pi}'], w=['pT0'])
                        if tt == 3:
                            for hx in range(4):
                                for dc in range(2):
                                    bk = 4 + (pvk[0] % 2)
                                    pvk[0] += 1
                                    for mc in range(2):
                                        op('pe', I('matmul', psB[:, bk, :],
                                                   vx[:, mc, hx * 256 + dc * 128:hx * 256 + (dc + 1) * 128],
                                                   pT_t[:, hx, mc, :], start=(mc == 0), stop=(mc == 1)),
                                           r=['vx', 'pT0'], w=[f'psB{bk}'])
                                    evac_copy(hT[:, 2 * hx + dc, n * 512:(n + 1) * 512], psB[:, bk, :], [f'psB{bk}'],
                                              [HK(2 * hx + dc, n)])

                    FA(0)
                    for t in range(NT):
                        if t + 1 < NT:
                            FA(t + 1)
                        FB(t)
                    for n in range(2):
                        sl = nxt
                        nxt = load_w(w_xo_d[:, 512:1024]) if n == 0 else None
                        ns = slice(n * 512, (n + 1) * 512)
                        for t in range(NT):
                            bk = t % 4
                            for cc in range(8):
                                op('pe', I('matmul',
                                    psB[:, bk, :], hT[:, cc, t * 128:(t + 1) * 128], wr[sl][:, cc, :],
                                    start=(cc == 0), stop=(cc == 7)), r=[HK(cc, t // 4), f'wr{sl}'], w=[f'psB{bk}'])
                            op('dve', I('tensor_tensor',
                                out=x_sb[:, t, ns], in0=psB[:, bk, :], in1=x_sb[:, t, ns], op=ALU.add),
                               r=[f'psB{bk}', f'x{t}'], w=[f'x{t}'])
                    S.barrier()
                if stage == 2:
                    for t in range(NT):
                        ev = op('sp', I('dma_start',
                            out=out_d[b * SEQ + t * 128:b * SEQ + (t + 1) * 128, :], in_=x_sb[:, t, :]),
                            r=[f'x{t}'], dma=f'o{t % 4}')
                    S.barrier()
                    continue

                with ExitStack() as s2g:
                    GB = 8
                    gbc = T(s2g, "gbc", [128, DM], F32)
                    h3 = [T(s2g, f"h3_{i}", [128, DM], F32) for i in range(2)]
                    h3b = T(s2g, "h3b", [128, GB, DM], BF16)
                    h3T = [T(s2g, f"h3T{i}", [128, 8, 128], F32) for i in range(2)]
                    LG = T(s2g, "LG", [128, GB, 36], F32)
                    gmax = T(s2g, "gmax", [128, GB], F32)
                    gsh = T(s2g, "gsh", [128, GB, 4], F32)
                    gex = T(s2g, "gex", [128, GB, 4], F32)
                    gsum = T(s2g, "gsum", [128, GB], F32)
                    gw = T(s2g, "gw", [128, GB], F32)
                    gm = T(s2g, "gm", [128, GB, 4], F32)
                    elm = T(s2g, "elm", [128, GB, 4, 8], F32)
                    igr = T(s2g, "igr", [128, GB, 8], F32)
                    ig2 = T(s2g, "ig2", [128, GB, 8], F32)
                    mk1 = T(s2g, "mk1", [128, GB, 8], F32)
                    mk2 = T(s2g, "mk2", [128, GB, 8], F32)
                    m12 = T(s2g, "m12", [128, 4, GB], F32)
                    M1 = T(s2g, "M1", [128, GB, 4, 8], F32)
                    M2 = T(s2g, "M2", [128, GB, 4, 8], F32)
                    S32b = T(s2g, "S32b", [128, GB, 32], BF16)
                    posa = T(s2g, "posa", [128, GB, 32], F32)
                    ova = T(s2g, "ova", [128, GB, 32], F32)
                    slf = T(s2g, "slf", [128, 2, GB], F32)
                    op('sp', I('dma_start', out=gbc[:], in_=ln_ffn_g.partition_broadcast(128)), w=['gbc'],
                       dma='gbc')

                    def G1(t):
                        hi = t % 2
                        tl = t % GB
                        op('dve', I('scalar_tensor_tensor', out=h3[hi][:], in0=x_sb[:, t, :],
                                    scalar=stat[:, 2, t:t + 1], in1=gbc[:], op0=ALU.mult, op1=ALU.mult),
                           r=[f'x{t}', 'rstdG', 'gbc'], w=[f'h3_{hi}'])
                        op('pool', I('tensor_copy', out=h3b[:, tl, :], in_=h3[hi][:]), r=[f'h3_{hi}'], w=[f'h3b{tl}'])
                        b4, b5 = (4, 5) if hi == 0 else (2, 3)
                        for kc in range(8):
                            bb = b4 if kc < 4 else b5
                            op('pe', I('transpose', out=psB[:, bb, (kc % 4) * 128:(kc % 4 + 1) * 128],
                                       in_=h3[hi][:, kc * 128:(kc + 1) * 128], identity=identf[:]),
                               r=[f'h3_{hi}', 'identf'], w=[f'psB{bb}'])
                        op('act', I('activation', out=h3T[hi][:, 0:4, :],
                                    in_=psB[:, b4, :].rearrange("p (k c) -> p k c", k=4), func=AF.Copy),
                           r=[f'psB{b4}'], w=[f'h3T{hi}a'])
                        op('dve', I('tensor_copy', out=h3T[hi][:, 4:8, :],
                                    in_=psB[:, b5, :].rearrange("p (k c) -> p k c", k=4)),
                           r=[f'psB{b5}'], w=[f'h3T{hi}b'])
                        for kc in range(8):
                            op('pe', I('matmul', psB[:, hi, 0:36], h3T[hi][:, kc, :], wrt[:, kc, :],
                                       start=(kc == 0), stop=(kc == 7)),
                               r=[f'h3T{hi}a', f'h3T{hi}b', 'wrt'], w=[f'psB{hi}'])
                        op('dve', I('tensor_tensor', out=LG[:, tl, :], in0=psB[:, hi, 0:36], in1=brt[:], op=ALU.add),
                           r=[f'psB{hi}', 'brt'], w=['LG'])
                        op('sp', I('dma_start', out=xres_d[b * SEQ + t * 128:b * SEQ + (t + 1) * 128, :],
                                   in_=x_sb[:, t, :]), r=[f'x{t}'], w=[f'xres{t}'], dma=f'xw{t % 4}')

                    def G23(g):
                        tg0 = b * NT + g * GB
                        V = lambda f, r=(), w=(): op('dve', f, r=['rt2'] + list(r), w=['rt2'] + list(w))
                        GL = LG[:, :, 0:4]
                        EL = LG[:, :, 4:36].rearrange("p t (g e) -> p t g e", g=4)
                        bc3 = lambda ap2, n: ap2[:, :, None].to_broadcast([128, GB, n])
                        V(I('tensor_reduce', out=gmax[:], in_=GL, axis=AX.X, op=ALU.max), r=['LG'])
                        V(I('tensor_tensor', out=gsh[:], in0=GL, in1=bc3(gmax, 4), op=ALU.subtract), r=['LG'])
                        op('act', I('activation', out=gex[:], in_=gsh[:], func=AF.Exp), r=['rt2'], w=['rt2'])
                        V(I('tensor_reduce', out=gsum[:], in_=gex[:], axis=AX.X, op=ALU.add))
                        V(I('reciprocal', out=gw[:], in_=gsum[:]))
                        V(I('tensor_scalar', out=gm[:], in0=gsh[:], scalar1=0.0, scalar2=None, op0=ALU.is_equal))
                        V(I('tensor_tensor', out=elm[:], in0=EL, in1=gm[:, :, :, None].to_broadcast([128, GB, 4, 8]),
                            op=ALU.mult), r=['LG'])
                        V(I('tensor_reduce', out=igr[:], in_=elm[:].rearrange("p t g e -> p t e g"), axis=AX.X,
                            op=ALU.add))
                        V(I('tensor_reduce', out=m12[:, 0, :], in_=igr[:], axis=AX.X, op=ALU.max))
                        V(I('tensor_tensor', out=mk1[:], in0=igr[:], in1=bc3(m12[:, 0, :], 8), op=ALU.is_equal))
                        V(I('scalar_tensor_tensor', out=ig2[:], in0=mk1[:], scalar=-1e30, in1=igr[:], op0=ALU.mult,
                            op1=ALU.add))
                        V(I('tensor_reduce', out=m12[:, 1, :], in_=ig2[:], axis=AX.X, op=ALU.max))
                        V(I('tensor_tensor', out=mk2[:], in0=ig2[:], in1=bc3(m12[:, 1, :], 8), op=ALU.is_equal))
                        V(I('tensor_tensor', out=m12[:, 2, :], in0=m12[:, 1, :], in1=m12[:, 0, :], op=ALU.subtract))
                        op('act', I('activation', out=m12[:, 2, :], in_=m12[:, 2, :], func=AF.Exp), r=['rt2'], w=['rt2'])
                        V(I('tensor_scalar', out=m12[:, 3, :], in0=m12[:, 2, :], scalar1=1.0, scalar2=None, op0=ALU.add))
                        V(I('reciprocal', out=m12[:, 3, :], in_=m12[:, 3, :]))
                        V(I('tensor_tensor', out=wts[:, tg0:tg0 + GB, 0], in0=m12[:, 3, :], in1=gw[:], op=ALU.mult))
                        V(I('tensor_tensor', out=wts[:, tg0:tg0 + GB, 1], in0=m12[:, 2, :], in1=wts[:, tg0:tg0 + GB, 0],
                            op=ALU.mult))
                        gm4 = gm[:, :, :, None].to_broadcast([128, GB, 4, 8])
                        V(I('tensor_tensor', out=M1[:], in0=gm4, in1=mk1[:, :, None, :].to_broadcast([128, GB, 4, 8]),
                            op=ALU.mult))
                        V(I('tensor_tensor', out=M2[:], in0=gm4, in1=mk2[:, :, None, :].to_broadcast([128, GB, 4, 8]),
                            op=ALU.mult))
                        V(I('tensor_tensor', out=elm[:], in0=M1[:], in1=M2[:], op=ALU.add))
                        V(I('tensor_copy', out=S32b[:], in_=elm[:].rearrange("p t g e -> p t (g e)")))
                        for tl in range(GB):
                            pb = tl % 2
                            op('pe', I('matmul', psB[:, pb, 64:96], Lmat[:], S32b[:, tl, :], start=True, stop=True),
                               r=['Lmat', 'rt2'], w=[f'psB{pb}'])
                            op('pe', I('matmul', psB[:, pb, 96:128], onesb[:], S32b[:, tl, :], start=True, stop=True),
                               r=['onesb', 'rt2'], w=[f'psB{pb}'])
                            V(I('tensor_tensor', out=posa[:, tl, :], in0=psB[:, pb, 64:96], in1=base[:], op=ALU.add),
                              r=[f'psB{pb}', 'base'])
                            V(I('tensor_tensor', out=base[:], in0=psB[:, pb, 96:128], in1=base[:], op=ALU.add),
                              r=[f'psB{pb}', 'base'], w=['base'])
                        V(I('tensor_scalar', out=ova[:], in0=posa[:], scalar1=float(CAP), scalar2=1e7, op0=ALU.is_ge,
                            op1=ALU.mult))
                        V(I('tensor_tensor', out=posa[:], in0=posa[:], in1=ova[:], op=ALU.add))
                        V(I('tensor_tensor', out=posa[:], in0=posa[:], in1=ecap[:, None, :].to_broadcast([128, GB, 32]),
                            op=ALU.add))
                        V(I('tensor_tensor', out=ova[:], in0=posa[:], in1=M1[:].rearrange("p t g e -> p t (g e)"),
                            op=ALU.mult))
                        V(I('tensor_reduce', out=slf[:, 0, :], in_=ova[:], axis=AX.X, op=ALU.add))
                        V(I('tensor_tensor', out=ova[:], in0=posa[:], in1=M2[:].rearrange("p t g e -> p t (g e)"),
                            op=ALU.mult))
                        V(I('tensor_reduce', out=slf[:, 1, :], in_=ova[:], axis=AX.X, op=ALU.add))
                        V(I('tensor_copy', out=slots[:, tg0:tg0 + GB, :], in_=slf[:].rearrange("p k t -> p t k")),
                          w=['slotsg'])
                        for tl in range(GB):
                            for k2 in range(2):
                                op('pool', I('indirect_dma_start', out=xd_d[:, :],
                                             out_offset=bass.IndirectOffsetOnAxis(ap=slots[:, tg0 + tl, k2:k2 + 1], axis=0),
                                             in_=h3b[:, tl, :], in_offset=None, bounds_check='REG', oob_is_err=False),
                                   r=['slotsg', f'h3b{tl}'], w=[f'xd{tg0 + tl}_{k2}'], dma=f'sc{tl}_{k2}')

                    stats_all(x_sb, 'rstdG')
                    for g in range(NT // GB):
                        for tl in range(GB):
                            G1(g * GB + tl)
                        G23(g)
                    S.barrier()

        if stage >= 3:
            with ExitStack() as s3:
                wg = [T(s3, f"wg{i}", [128, 8, 512], BF16) for i in range(2)]
                wu = [T(s3, f"wu{i}", [128, 8, 512], BF16) for i in range(2)]
                wd = [T(s3, f"wd{i}", [128, 4, DM], BF16) for i in range(2)]
                xr = [T(s3, f"xr{i}", [128, DM], BF16) for i in range(3)]
                xT = [T(s3, f"xT{i}", [128, 8, 384], BF16) for i in range(2)]
                sg = [T(s3, f"sg{i}", [128, 384], F32) for i in range(2)]
                h1T = [T(s3, f"h1T{i}", [128, 4, 384], BF16) for i in range(2)]
                ydt = [T(s3, f"ydt{i}", [128, DM], F32) for i in range(2)]

                wst = [T(s3, f"wst{i}", [128, 8, 512], F32) for i in range(3)]

                def load_dma(e_):
                    op('act', I('dma_start', out=wst[0][:], in_=w_gate_d[e_].rearrange("(k p) n -> p k n", p=128)),
                       w=['wst0'], dma='wst0')
                    op('act', I('dma_start', out=wst[1][:], in_=w_up_d[e_].rearrange("(k p) n -> p k n", p=128)),
                       w=['wst1'], dma='wst1')
                    op('act', I('dma_start', out=wst[2][:].rearrange("p (a b) n -> p a (b n)", a=4),
                               in_=w_down_d[e_].rearrange("(k p) n -> p k n", p=128)), w=['wst2'], dma='wst2')

                def cast_gu(e_):
                    i = e_ % 2
                    op('dve', I('tensor_copy', out=wg[i][:], in_=wst[0][:]), r=['wst0'], w=[f'wg{i}'])
                    op('act', I('activation', out=wu[i][:], in_=wst[1][:], func=AF.Copy), r=['wst1'], w=[f'wu{i}'])

                def cast_d(e_):
                    i = e_ % 2
                    wsv = wst[2][:].rearrange("p (a b) n -> p a (b n)", a=4)
                    op('act', I('activation', out=wd[i][:, 0:2, :], in_=wsv[:, 0:2, :], func=AF.Copy),
                       r=['wst2'], w=[f'wd{i}a'])
                    op('dve', I('tensor_copy', out=wd[i][:, 2:4, :], in_=wsv[:, 2:4, :]), r=['wst2'], w=[f'wd{i}b'])

                def load_expert(e_):
                    load_dma(e_)
                    cast_gu(e_)
                    cast_d(e_)

                load_expert(0)
                cnt = 0
                yc = 0
                for e_ in range(NEXP):
                    wi = e_ % 2
                    if e_ + 1 < NEXP:
                        load_dma(e_ + 1)
                    for half in range(2):
                        if half == 1 and e_ + 1 < NEXP:
                            cast_gu(e_ + 1)
                        r0 = e_ * CAP + half * 384
                        hb = cnt % 2
                        cnt += 1
                        for ci in range(3):
                            xi = (cnt * 3 + ci) % 3
                            op('sp', I('dma_start',
                                out=xr[xi][:], in_=xd_d[r0 + ci * 128:r0 + (ci + 1) * 128, :]),
                                r=['xd'], w=[f'xr{xi}'], dma=f'xr{xi}')
                            pa = ci % 2
                            for kc in range(8):
                                op('pe', I('transpose',
                                    out=psA[:, pa, kc * 128:(kc + 1) * 128], in_=xr[xi][:, kc * 128:(kc + 1) * 128],
                                    identity=ident[:]), r=[f'xr{xi}', 'ident'], w=[f'psA{pa}'])
                            evac_copy(xT[hb][:, :, ci * 128:(ci + 1) * 128],
                                      psA[:, pa, :].rearrange("p (k c) -> p k c", k=8), [f'psA{pa}'], [f'xT{hb}'])
                        for dc in range(4):
                            bg, bu = (dc % 2) * 2, (dc % 2) * 2 + 1
                            for kc in range(8):
                                op('pe', I('matmul',
                                    psB[:, bg, 0:384], wg[wi][:, kc, dc * 128:(dc + 1) * 128], xT[hb][:, kc, :],
                                    start=(kc == 0), stop=(kc == 7)), r=[f'wg{wi}', f'xT{hb}'], w=[f'psB{bg}'])
                            for kc in range(8):
                                op('pe', I('matmul',
                                    psB[:, bu, 0:384], wu[wi][:, kc, dc * 128:(dc + 1) * 128], xT[hb][:, kc, :],
                                    start=(kc == 0), stop=(kc == 7)), r=[f'wu{wi}', f'xT{hb}'], w=[f'psB{bu}'])
                            si = dc % 2
                            op('act', I('activation', out=sg[si][:], in_=psB[:, bg, 0:384],
                                                                           func=AF.Silu),
                               r=[f'psB{bg}'], w=[f'sg{si}'])
                            op('dve', I('tensor_tensor',
                                out=h1T[hb][:, dc, :], in0=psB[:, bu, 0:384], in1=sg[si][:], op=ALU.mult),
                               r=[f'psB{bu}', f'sg{si}'], w=[f'h1T{hb}_{dc}'])
                        for ci in range(3):
                            yi = yc % 2
                            yc += 1
                            for nn in range(2):
                                for dc in range(4):
                                    op('pe', I('matmul',
                                        psB[:, 4 + nn, :], h1T[hb][:, dc, ci * 128:(ci + 1) * 128],
                                        wd[wi][:, dc, nn * 512:(nn + 1) * 512], start=(dc == 0), stop=(dc == 3)),
                                       r=[f'h1T{hb}_{dc}', f'wd{wi}a', f'wd{wi}b'], w=[f'psB{4 + nn}'])
                            op('act', I('activation', out=ydt[yi][:, 0:512], in_=psB[:, 4, :], func=AF.Copy),
                               r=['psB4'], w=[f'ydt{yi}a'])
                            op('dve', I('tensor_copy', out=ydt[yi][:, 512:1024], in_=psB[:, 5, :]),
                               r=['psB5'], w=[f'ydt{yi}b'])
                            op('sp', I('dma_start',
                                out=yd_d[r0 + ci * 128:r0 + (ci + 1) * 128, :], in_=ydt[yi][:]),
                                r=[f'ydt{yi}a', f'ydt{yi}b'], w=[f'yd{yc}'], dma=f'yo{yi}')
                    if e_ + 1 < NEXP:
                        cast_d(e_ + 1)
                S.barrier()

            with ExitStack() as s4:
                gbf = T(s4, "gbf", [128, DM], F32)
                y1 = [T(s4, f"y1_{i}", [128, DM], F32) for i in range(2)]
                y2 = [T(s4, f"y2_{i}", [128, DM], F32) for i in range(2)]
                xf = [T(s4, f"xf{i}", [128, DM], F32) for i in range(2)]
                of = [T(s4, f"of{i}", [128, DM], F32) for i in range(2)]
                fst = T(s4, "fst", [128, 3, nseq * NT], F32)
                zt = T(s4, "zt", [128, DM], F32)
                op('pool', I('memset', zt[:], 0.0), w=['zt'])
                op('sp', I('dma_start', out=gbf[:], in_=ln_final_g.partition_broadcast(128)), w=['gbf'], dma='gbf')
                def fetch(tg):
                    i = tg % 2
                    for ybuf, k2, nm in ((y1, 0, 'y1'), (y2, 1, 'y2')):
                        op('act', I('activation', out=ybuf[i][:], in_=zt[:], func=AF.Copy), r=['zt'], w=[f'{nm}_{i}'])
                        op('pool', I('indirect_dma_start', out=ybuf[i][:], out_offset=None, in_=yd_d[:, :],
                                     in_offset=bass.IndirectOffsetOnAxis(ap=slots[:, tg, k2:k2 + 1], axis=0),
                                     bounds_check='REG', oob_is_err=False),
                           r=['yd'], w=[f'{nm}_{i}'], dma=f'{nm}_{i}')
                    op('sp', I('dma_start', out=xf[i][:], in_=xres_d[tg * 128:(tg + 1) * 128, :]),
                       r=['xres'], w=[f'xf{i}'], dma=f'xf{i}')

                def compute(tg):
                    i = tg % 2
                    op('dve', I('scalar_tensor_tensor', out=xf[i][:], in0=y1[i][:], scalar=wts[:, tg, 0:1], in1=xf[i][:],
                                op0=ALU.mult, op1=ALU.add), r=[f'y1_{i}', f'xf{i}'], w=[f'xf{i}'])
                    op('dve', I('scalar_tensor_tensor', out=xf[i][:], in0=y2[i][:], scalar=wts[:, tg, 1:2], in1=xf[i][:],
                                op0=ALU.mult, op1=ALU.add), r=[f'y2_{i}', f'xf{i}'], w=[f'xf{i}'])
                    op('act', I('activation', out=junk[:], in_=xf[i][:], func=AF.Square, accum_out=fst[:, 0, tg:tg + 1]),
                       r=[f'xf{i}'], w=['stat'])
                    rstd_from_ss(fst, tg, 1.0 / DM)
                    op('dve', I('scalar_tensor_tensor', out=of[i][:], in0=xf[i][:], scalar=fst[:, 2, tg:tg + 1],
                                in1=gbf[:], op0=ALU.mult, op1=ALU.mult), r=[f'xf{i}', 'stat', 'gbf'], w=[f'of{i}'])
                    op('sp', I('dma_start', out=out_d[tg * 128:(tg + 1) * 128, :], in_=of[i][:]),
                       r=[f'of{i}'], dma=f'of{i}')

                ntile = nseq * NT
                fetch(0)
                for tg in range(ntile):
                    if tg + 1 < ntile:
                        fetch(tg + 1)
                    compute(tg)
                S.barrier()
        S.barrier()
        S.emit()
    return nc


_NC_CACHE = {}


def _prep(inputs, c, nseq=NSEQ):
    f = lambda a: np.ascontiguousarray(np.asarray(a, dtype=np.float32))
    d = {
        "x": f(inputs["x"][c * nseq:(c + 1) * nseq]),
        "mem": f(inputs["mem"][c * nseq:(c + 1) * nseq]),
        "ln_mix_g": f(inputs["ln_mix_g"][0]),
        "w_in": f(inputs["w_in"][0]),
        "sb_out_g": f(inputs["sb_out_g"][0]),
        "conv_w": f(inputs["conv_w"][0]),
        "conv_b": f(inputs["conv_b"][0]),
        "conv_ln_g": f(inputs["conv_ln_g"][0]),
        "conv_ln_b": f(inputs["conv_ln_b"][0]),
        "w_out": f(inputs["w_out"][0]),
        "ln_mem_x_g": f(inputs["ln_mem_x_g"][0]),
        "ln_mem_g": f(inputs["ln_mem_g"][0]),
        "w_xq": f(inputs["w_xq"][0]),
        "w_xkv": f(inputs["w_xkv"][0]),
        "w_xo": f(inputs["w_xo"][0]),
        "ln_ffn_g": f(inputs["ln_ffn_g"][0]),
        "w_rt": f(np.concatenate([np.asarray(inputs["w_group"][0]), np.asarray(inputs["w_er"][0]).reshape(DM, 32)], axis=1)),
        "b_rt": f(np.concatenate([np.asarray(inputs["b_group"][0]), np.asarray(inputs["b_er"][0]).reshape(32)])),
        "w_gate": f(inputs["w_gate"][0]),
        "w_up": f(inputs["w_up"][0]),
        "w_down": f(inputs["w_down"][0]),
        "ln_final_g": f(inputs["ln_final_g"]),
    }
    return d


def kernel(**inputs):
    if 'nc' not in _NC_CACHE:
        _NC_CACHE['nc'] = build()
    nc = _NC_CACHE['nc']
    in_maps = [_prep(inputs, c) for c in range(N_CORES)]
    res = run_bass_kernel_spmd(nc, in_maps, core_ids=list(range(N_CORES)))
    out = np.concatenate([np.asarray(r["out"]).reshape(NSEQ, SEQ, DM) for r in res.results], axis=0)
    return out.astype(np.float32)
```
